# Optimizing a Trainium2 kernel written in Bass

```python
import math
import jax
import jax.numpy as jnp
from jax import lax
import numpy as np

D_MODEL = 2048
BATCH = 8
SEQ = 2048
DEPTH = 2

GRID_W = 64
CTX_LEN = 256
EPS = 1e-6
F32 = jnp.float32
ROPE_THETA = 10000.0
HEAD_DIM = 128
MIX_W = D_MODEL // 2
Q_BLOCK = 128

A_HEADS = MIX_W // HEAD_DIM
A_KV_HEADS = A_HEADS // 4
WINDOW = 128

B_HEADS = MIX_W // HEAD_DIM
B_DIM = HEAD_DIM // 2
B_VDIM = 2 * B_DIM

C_WIDTH = MIX_W
C_BLOCKS = 8
C_BLOCK = C_WIDTH // C_BLOCKS
CONV_W = 4
CONV_LEFT = 2
LRU_C = 8.0

D_HEADS = MIX_W // HEAD_DIM
D_KDIM = HEAD_DIM
D_VDIM = HEAD_DIM
D_CHUNK = 64

N_BRANCH = 4
N_EXPERTS = 16
N_GROUPS = 4
TOP_K = 2
D_FF = D_MODEL // 4

A_Q = A_HEADS * HEAD_DIM
A_KV = A_KV_HEADS * HEAD_DIM
B_QK = B_HEADS * 2 * B_DIM
B_V = B_HEADS * B_VDIM
D_K = D_HEADS * D_KDIM
D_V = D_HEADS * D_VDIM
SPLITS = (A_Q, A_KV, A_KV, B_QK, B_QK, B_V, C_WIDTH, C_WIDTH, D_K, D_K, D_K, D_V, D_V, N_BRANCH * D_MODEL)
IN_W = A_Q + 2 * A_KV + 2 * B_QK + B_V + 2 * C_WIDTH + 3 * D_K + 2 * D_V + N_BRANCH * D_MODEL

kernel_name = 'hybrid_gated_parallel_mixer_moe_trunk'


def rms_norm(x, g):
    xf = x.astype(F32)
    y = xf * lax.rsqrt(jnp.mean(xf * xf, axis=-1, keepdims=True) + EPS)
    return (y * g.astype(F32)).astype(x.dtype)


def modulate(x, g, shift, scale):
    return rms_norm(x, g) * (1 + scale) + shift


def flip_seq(t):
    return jnp.flip(t, axis=1)


def rope_1d(x, pos):
    half = x.shape[-1] // 2
    inv = ROPE_THETA ** (-jnp.arange(half, dtype=F32) / half)
    ang = pos.astype(F32)[:, None] * inv[None, :]
    bshape = (1, x.shape[1]) + (1,) * (x.ndim - 3) + (half,)
    cos = jnp.cos(ang).reshape(bshape)
    sin = jnp.sin(ang).reshape(bshape)
    xf = x.astype(F32)
    x1, x2 = xf[..., :half], xf[..., half:]
    return jnp.concatenate([x1 * cos - x2 * sin, x2 * cos + x1 * sin], axis=-1).astype(x.dtype)


def rope_2d(x, rows, cols):
    half = x.shape[-1] // 2
    return jnp.concatenate([rope_1d(x[..., :half], rows), rope_1d(x[..., half:], cols)], axis=-1)


def window_attention(qc, kc, vc, ql, kl, vl, sink, ctx_out):
    Bn, L, H, d = ql.shape
    Hk = kl.shape[2]
    G = H // Hk
    Lc = kc.shape[1]
    nb = L // Q_BLOCK
    scale = d ** -0.5
    qb = ql.reshape(Bn, nb, Q_BLOCK, Hk, G, d)

    def band(t):
        tp = jnp.pad(t, ((0, 0), (Q_BLOCK, Q_BLOCK), (0, 0), (0, 0))).reshape(Bn, nb + 2, Q_BLOCK, Hk, d)
        return jnp.concatenate([tp[:, :-2], tp[:, 1:-1], tp[:, 2:]], axis=2)

    kw, vw = band(kl), band(vl)
    qpos = jnp.arange(nb)[:, None] * Q_BLOCK + jnp.arange(Q_BLOCK)[None, :]
    kpos = jnp.arange(nb)[:, None] * Q_BLOCK - Q_BLOCK + jnp.arange(3 * Q_BLOCK)[None, :]
    valid = ((jnp.abs(kpos[:, None, :] - qpos[:, :, None]) <= WINDOW)
             & (kpos[:, None, :] >= 0) & (kpos[:, None, :] < L))
    s_loc = jnp.einsum('bnqhgd,bnkhd->bhgnqk', qb, kw).astype(F32) * scale
    s_loc = jnp.where(valid, s_loc, -jnp.inf)
    s_ctx = jnp.einsum('bnqhgd,bchd->bhgnqc', qb, kc).astype(F32) * scale
    sk = sink.astype(F32).reshape(1, Hk, G, 1, 1, 1)
    s_sink = jnp.broadcast_to(sk, s_ctx.shape[:-1] + (1,))
    p = jax.nn.softmax(jnp.concatenate([s_loc, s_ctx, s_sink], axis=-1), axis=-1)
    k3 = 3 * Q_BLOCK
    p_loc = p[..., :k3].astype(vl.dtype)
    p_ctx = p[..., k3:k3 + Lc].astype(vl.dtype)
    o = jnp.einsum('bhgnqk,bnkhd->bnqhgd', p_loc, vw) + jnp.einsum('bhgnqc,bchd->bnqhgd', p_ctx, vc)
    o_lat = o.reshape(Bn, L, H * d)
    if not ctx_out:
        return None, o_lat
    qcg = qc.reshape(Bn, Lc, Hk, G, d)
    sc = jnp.einsum('bqhgd,bkhd->bhgqk', qcg, kc).astype(F32) * scale
    sc_sink = jnp.broadcast_to(sink.astype(F32).reshape(1, Hk, G, 1, 1), sc.shape[:-1] + (1,))
    pc = jax.nn.softmax(jnp.concatenate([sc, sc_sink], axis=-1), axis=-1)
    oc = jnp.einsum('bhgqk,bkhd->bqhgd', pc[..., :Lc].astype(vc.dtype), vc).reshape(Bn, Lc, H * d)
    return oc, o_lat


def diff_attention(qc, kc, vc, ql, kl, vl, lam, lam_init, subln_g, ctx_out):
    scale = B_DIM ** -0.5

    def attend(q, k, v):
        s = jnp.einsum('bqhcd,bkhcd->bhcqk', q, k).astype(F32) * scale
        p = jax.nn.softmax(s, axis=-1)
        a = (p[:, :, 0] - lam * p[:, :, 1]).astype(v.dtype)
        return jnp.einsum('bhqk,bkhd->bqhd', a, v)

    def post(o):
        y = rms_norm(o, subln_g) * (1.0 - lam_init)
        return y.reshape(o.shape[0], o.shape[1], B_HEADS * B_VDIM)

    k_all = jnp.concatenate([kc, kl], axis=1)
    v_all = jnp.concatenate([vc, vl], axis=1)
    Bn, L = ql.shape[:2]
    nb = L // Q_BLOCK
    qblocks = jnp.moveaxis(ql.reshape((Bn, nb, Q_BLOCK) + ql.shape[2:]), 1, 0)
    o = lax.map(lambda qb: attend(qb, k_all, v_all), qblocks)
    o_lat = jnp.moveaxis(o, 0, 1).reshape(Bn, L, B_HEADS, B_VDIM)
    o_ctx = post(attend(qc, kc, vc)) if ctx_out else None
    return o_ctx, post(o_lat)


def centred_conv(x, w, b):
    L = x.shape[1]
    xp = jnp.pad(x, ((0, 0), (CONV_LEFT, CONV_W - 1 - CONV_LEFT), (0, 0)))
    out = b
    for tap in range(CONV_W):
        out = out + xp[:, tap:tap + L] * w[tap]
    return out


def block_diag(x, w, b):
    Bn, L, _ = x.shape
    y = jnp.einsum('blnc,ncd->blnd', x.reshape(Bn, L, C_BLOCKS, C_BLOCK), w)
    return y.reshape(Bn, L, C_WIDTH) + b


def linear_scan(a, u, h0):
    u = u.at[:, 0].add(a[:, 0] * h0)

    def comb(left, right):
        return (left[0] * right[0], right[0] * left[1] + right[1])

    return lax.associative_scan(comb, (a, u), axis=1)[1]


def rglru_mixer(xc, yc, xl, yl, conv_w, conv_b, w_r, b_r, w_i, b_i, lam, ctx_out):
    Lc = xc.shape[1]
    u = jnp.concatenate([centred_conv(xc, conv_w, conv_b), centred_conv(xl, conv_w, conv_b)], axis=1)
    hc_dirs, hl_dirs = [], []
    for d in range(2):
        r = jax.nn.sigmoid(block_diag(u, w_r[d], b_r[d]).astype(F32))
        i = jax.nn.sigmoid(block_diag(u, w_i[d], b_i[d]).astype(F32))
        log_a = -LRU_C * r * jax.nn.softplus(-lam[d].astype(F32))
        a = jnp.exp(log_a)
        v = jnp.sqrt(-jnp.expm1(2.0 * log_a)) * i * u.astype(F32)
        ac, al, vc, vl = a[:, :Lc], a[:, Lc:], v[:, :Lc], v[:, Lc:]
        if d == 1:
            ac, al, vc, vl = flip_seq(ac), flip_seq(al), flip_seq(vc), flip_seq(vl)
        hc = linear_scan(ac, vc, jnp.zeros_like(vc[:, 0]))
        hl = linear_scan(al, vl, hc[:, -1])
        if d == 1:
            hc, hl = flip_seq(hc), flip_seq(hl)
        hc_dirs.append(hc)
        hl_dirs.append(hl)
    out_l = (hl_dirs[0] + hl_dirs[1]).astype(yl.dtype) * jax.nn.gelu(yl)
    out_c = (hc_dirs[0] + hc_dirs[1]).astype(yc.dtype) * jax.nn.gelu(yc) if ctx_out else None
    return out_c, out_l


def chunk_gla(q, k, v, g, s0):
    Bn, L, H, _ = q.shape
    V = v.shape[-1]
    C = D_CHUNK
    n = L // C

    def chunks(t):
        return t.astype(F32).reshape(Bn, n, C, H, t.shape[-1]).transpose(1, 0, 3, 2, 4)

    q, k, v, g = chunks(q), chunks(k), chunks(v), chunks(g)
    b = jnp.cumsum(g, axis=3)
    causal = jnp.tril(jnp.ones((C, C), dtype=bool))[:, :, None]

    def step(S, xs):
        qn, kn, vn, bn = xs
        rel = bn[:, :, :, None, :] - bn[:, :, None, :, :]
        decay = jnp.exp(jnp.where(causal, rel, -jnp.inf))
        att = jnp.einsum('bhik,bhijk,bhjk->bhij', qn, decay, kn)
        blast = bn[:, :, -1:, :]
        o = jnp.einsum('bhij,bhjv->bhiv', att, vn) + jnp.einsum('bhik,bhkv->bhiv', qn * jnp.exp(bn), S)
        S = jnp.exp(blast[:, :, 0, :, None]) * S + jnp.einsum('bhjk,bhjv->bhkv', kn * jnp.exp(blast - bn), vn)
        return S, o

    S, o = lax.scan(step, s0.astype(F32), (q, k, v, b))
    return o.transpose(1, 0, 3, 2, 4).reshape(Bn, L, H, V), S


def hgrn2_mixer(qc, fc, ic, gc, ql, fl, il, gl, lb, onorm_g, ctx_out):
    Bn = ql.shape[0]
    oc_dirs, ol_dirs = [], []
    for d in range(2):
        lbd = lb[d].astype(F32).reshape(D_HEADS, D_KDIM)

        def gates(z):
            zf = z.astype(F32)
            log_f = jnp.log(lbd + (1.0 - lbd) * jax.nn.sigmoid(zf))
            return log_f, (1.0 - lbd) * jax.nn.sigmoid(-zf)

        gfc, kc = gates(fc[d])
        gfl, kl = gates(fl[d])
        seq_c = (qc, kc, ic, gfc)
        seq_l = (ql, kl, il, gfl)
        if d == 1:
            seq_c = tuple(flip_seq(t) for t in seq_c)
            seq_l = tuple(flip_seq(t) for t in seq_l)
        s0 = jnp.zeros((Bn, D_HEADS, D_KDIM, D_VDIM), F32)
        oc, s_ctx = chunk_gla(seq_c[0], seq_c[1], seq_c[2], seq_c[3], s0)
        ol, _ = chunk_gla(seq_l[0], seq_l[1], seq_l[2], seq_l[3], s_ctx)
        if d == 1:
            oc, ol = flip_seq(oc), flip_seq(ol)
        oc_dirs.append(oc)
        ol_dirs.append(ol)

    def finish(o, g):
        y = rms_norm(o.astype(g.dtype), onorm_g) * jax.nn.silu(g)
        return y.reshape(y.shape[0], y.shape[1], D_V)

    out_l = finish(ol_dirs[0] + ol_dirs[1], gl)
    out_c = finish(oc_dirs[0] + oc_dirs[1], gc) if ctx_out else None
    return out_c, out_l


def moe_ffn(h, w_router, b_router, w_gate, w_up, w_down):
    shape = h.shape
    t = h.reshape(-1, shape[-1])
    n_tok = t.shape[0]
    per = N_EXPERTS // N_GROUPS
    aff = jax.nn.sigmoid(jnp.dot(t, w_router).astype(F32))
    sel = aff + b_router.astype(F32)
    group_score = jnp.sum(lax.top_k(sel.reshape(n_tok, N_GROUPS, per), TOP_K)[0], axis=-1)
    group = jnp.argmax(group_score, axis=-1)
    in_group = (jnp.arange(N_EXPERTS) // per)[None, :] == group[:, None]
    _, idx = lax.top_k(jnp.where(in_group, sel, -jnp.inf), TOP_K)
    w = jnp.take_along_axis(aff, idx, axis=-1)
    w = w / jnp.sum(w, axis=-1, keepdims=True)
    gates = jnp.sum(jax.nn.one_hot(idx, N_EXPERTS, dtype=F32) * w[..., None], axis=1).astype(t.dtype)
    y = jnp.zeros_like(t)
    for e in range(N_EXPERTS):
        hid = jax.nn.silu(jnp.dot(t, w_gate[e])) * jnp.dot(t, w_up[e])
        y = y + gates[:, e:e + 1] * jnp.dot(hid, w_down[e])
    return y.reshape(shape)


def parallel_mixers(hc, hl, rows, cols, lp, layer_idx, ctx_out):
    split_at = np.cumsum(SPLITS)[:-1].tolist()
    (aqc, akc, avc, bqc, bkc, bvc, cxc, cyc, dqc, dffc, dfbc, dic, dgc, gtc) = jnp.split(jnp.dot(hc, lp['w_in']), split_at, axis=-1)
    (aql, akl, avl, bql, bkl, bvl, cxl, cyl, dql, dffl, dfbl, dil, dgl, gtl) = jnp.split(jnp.dot(hl, lp['w_in']), split_at, axis=-1)

    def heads(t, *shape):
        return t.reshape(t.shape[:2] + shape)

    def pos2d(t):
        return rope_2d(t, rows, cols)

    qa_c = rms_norm(heads(aqc, A_HEADS, HEAD_DIM), lp['qn_a'])
    ka_c = rms_norm(heads(akc, A_KV_HEADS, HEAD_DIM), lp['kn_a'])
    qa_l = pos2d(rms_norm(heads(aql, A_HEADS, HEAD_DIM), lp['qn_a']))
    ka_l = pos2d(rms_norm(heads(akl, A_KV_HEADS, HEAD_DIM), lp['kn_a']))
    oa_c, oa_l = window_attention(qa_c, ka_c, heads(avc, A_KV_HEADS, HEAD_DIM),
                                  qa_l, ka_l, heads(avl, A_KV_HEADS, HEAD_DIM), lp['sink_a'], ctx_out)

    qb_c = rms_norm(heads(bqc, B_HEADS, 2, B_DIM), lp['qn_b'])
    kb_c = rms_norm(heads(bkc, B_HEADS, 2, B_DIM), lp['kn_b'])
    qb_l = pos2d(rms_norm(heads(bql, B_HEADS, 2, B_DIM), lp['qn_b']))
    kb_l = pos2d(rms_norm(heads(bkl, B_HEADS, 2, B_DIM), lp['kn_b']))
    lq1, lk1, lq2, lk2 = lp['lam_b'].astype(F32)
    lam_init = 0.8 - 0.6 * math.exp(-0.3 * layer_idx)
    lam = jnp.exp(jnp.sum(lq1 * lk1)) - jnp.exp(jnp.sum(lq2 * lk2)) + lam_init
    ob_c, ob_l = diff_attention(qb_c, kb_c, heads(bvc, B_HEADS, B_VDIM), qb_l, kb_l, heads(bvl, B_HEADS, B_VDIM),
                                lam, lam_init, lp['subln_b'], ctx_out)

    oc_c, oc_l = rglru_mixer(cxc, cyc, cxl, cyl, lp['conv_w'], lp['conv_b'], lp['w_rg'], lp['b_rg'],
                             lp['w_ig'], lp['b_ig'], lp['lru_lambda'], ctx_out)

    od_c, od_l = hgrn2_mixer(heads(dqc, D_HEADS, D_KDIM), (heads(dffc, D_HEADS, D_KDIM), heads(dfbc, D_HEADS, D_KDIM)),
                             heads(dic, D_HEADS, D_VDIM), heads(dgc, D_HEADS, D_VDIM),
                             heads(dql, D_HEADS, D_KDIM), (heads(dffl, D_HEADS, D_KDIM), heads(dfbl, D_HEADS, D_KDIM)),
                             heads(dil, D_HEADS, D_VDIM), heads(dgl, D_HEADS, D_VDIM),
                             lp['lb'], lp['onorm_d'], ctx_out)

    def merge(outs, gate_pre):
        o = jnp.stack(outs, axis=2)
        g = jax.nn.sigmoid(gate_pre.reshape(gate_pre.shape[:2] + (N_BRANCH, D_MODEL)))
        y = jnp.sum(g * jnp.einsum('blnc,ncd->blnd', o, lp['w_branch']), axis=2)
        return jnp.dot(y, lp['w_out'])

    out_l = merge((oa_l, ob_l, oc_l, od_l), gtl)
    out_c = merge((oa_c, ob_c, oc_c, od_c), gtc) if ctx_out else None
    return out_c, out_l


def trunk_layer(xc, xl, c, c_ctx, rows, cols, lp, w_router, b_router, layer_idx, ctx_out):
    mod_l = (jnp.dot(jax.nn.silu(c), lp['w_ada']) + lp['b_ada'])[:, None, :]
    mod_c = (jnp.dot(jax.nn.silu(c_ctx), lp['w_ada']) + lp['b_ada'])[None, None, :]
    sh1l, sc1l, g1l, sh2l, sc2l, g2l = jnp.split(mod_l, 6, axis=-1)
    sh1c, sc1c, g1c, sh2c, sc2c, g2c = jnp.split(mod_c, 6, axis=-1)
    mix_c, mix_l = parallel_mixers(modulate(xc, lp['norm1'], sh1c, sc1c), modulate(xl, lp['norm1'], sh1l, sc1l),
                                   rows, cols, lp, layer_idx, ctx_out)
    xl = xl + g1l * mix_l
    xl = xl + g2l * moe_ffn(modulate(xl, lp['norm2'], sh2l, sc2l), w_router, b_router, lp['w_gate'], lp['w_up'], lp['w_down'])
    if ctx_out:
        xc = xc + g1c * mix_c
        xc = xc + g2c * moe_ffn(modulate(xc, lp['norm2'], sh2c, sc2c), w_router, b_router, lp['w_gate'], lp['w_up'], lp['w_down'])
    return xc, xl


def setup_inputs(seed: int = 0) -> dict:
    key = jax.random.key(seed)
    keys = jax.random.split(key, 40)
    counter = [0]

    def nrm(shape, s):
        k = keys[counter[0]]
        counter[0] += 1
        return jax.random.normal(k, shape, F32) * s

    L = DEPTH
    x = nrm((BATCH, SEQ, D_MODEL), 1.0)
    c = nrm((BATCH, D_MODEL), 1.0)
    ctx = nrm((BATCH, CTX_LEN, D_MODEL), 1.0)
    c_ctx = nrm((D_MODEL,), 1.0)
    w_ada = nrm((L, D_MODEL, 6 * D_MODEL), 0.5 * D_MODEL ** -0.5)
    b_ada = nrm((L, 6 * D_MODEL), 0.02)
    norm1_g = 1.0 + nrm((L, D_MODEL), 0.02)
    norm2_g = 1.0 + nrm((L, D_MODEL), 0.02)
    w_in = nrm((L, D_MODEL, IN_W), D_MODEL ** -0.5)
    qn_a = 1.0 + nrm((L, HEAD_DIM), 0.02)
    kn_a = 1.0 + nrm((L, HEAD_DIM), 0.02)
    sink_a = nrm((L, A_HEADS), 0.5)
    qn_b = 1.0 + nrm((L, B_DIM), 0.02)
    kn_b = 1.0 + nrm((L, B_DIM), 0.02)
    lam_b = nrm((L, 4, B_DIM), 0.1)
    subln_b = 1.0 + nrm((L, B_VDIM), 0.02)
    conv_w = nrm((L, CONV_W, C_WIDTH), CONV_W ** -0.5)
    conv_b = nrm((L, C_WIDTH), 0.02)
    w_rg = nrm((L, 2, C_BLOCKS, C_BLOCK, C_BLOCK), C_BLOCK ** -0.5)
    b_rg = nrm((L, 2, C_WIDTH), 0.02)
    w_ig = nrm((L, 2, C_BLOCKS, C_BLOCK, C_BLOCK), C_BLOCK ** -0.5)
    b_ig = nrm((L, 2, C_WIDTH), 0.02)
    a0 = jax.random.uniform(keys[39], (L, 2, C_WIDTH), F32, 0.9, 0.999)
    lru_lambda = jnp.log(a0) - jnp.log1p(-a0)
    lb_d = nrm((L, 2, D_K), 0.5)
    onorm_d = 1.0 + nrm((L, D_VDIM), 0.02)
    w_branch = nrm((L, N_BRANCH, MIX_W, D_MODEL), MIX_W ** -0.5)
    w_out = nrm((L, D_MODEL, D_MODEL), D_MODEL ** -0.5)
    w_router = nrm((D_MODEL, N_EXPERTS), D_MODEL ** -0.5)
    b_router = nrm((N_EXPERTS,), 0.01)
    w_gate = nrm((L, N_EXPERTS, D_MODEL, D_FF), D_MODEL ** -0.5)
    w_up = nrm((L, N_EXPERTS, D_MODEL, D_FF), D_MODEL ** -0.5)
    w_down = nrm((L, N_EXPERTS, D_FF, D_MODEL), D_FF ** -0.5)
    return {'x': x, 'c': c, 'ctx': ctx, 'c_ctx': c_ctx, 'w_ada': w_ada, 'b_ada': b_ada,
            'norm1_g': norm1_g, 'norm2_g': norm2_g, 'w_in': w_in, 'qn_a': qn_a, 'kn_a': kn_a,
            'sink_a': sink_a, 'qn_b': qn_b, 'kn_b': kn_b, 'lam_b': lam_b, 'subln_b': subln_b,
            'conv_w': conv_w, 'conv_b': conv_b, 'w_rg': w_rg, 'b_rg': b_rg, 'w_ig': w_ig, 'b_ig': b_ig,
            'lru_lambda': lru_lambda, 'lb_d': lb_d, 'onorm_d': onorm_d, 'w_branch': w_branch,
            'w_out': w_out, 'w_router': w_router, 'b_router': b_router, 'w_gate': w_gate,
            'w_up': w_up, 'w_down': w_down}


def reference(x, c, ctx, c_ctx, w_ada, b_ada, norm1_g, norm2_g, w_in, qn_a, kn_a, sink_a, qn_b, kn_b,
              lam_b, subln_b, conv_w, conv_b, w_rg, b_rg, w_ig, b_ig, lru_lambda, lb_d, onorm_d,
              w_branch, w_out, w_router, b_router, w_gate, w_up, w_down):
    n_rows = x.shape[1] // GRID_W
    rows = jnp.repeat(jnp.arange(n_rows), GRID_W)
    cols = jnp.tile(jnp.arange(GRID_W), n_rows)
    lb_w = jax.nn.softmax(lb_d.astype(F32), axis=0)
    lb_all = jnp.cumsum(lb_w, axis=0) - lb_w[0:1]
    xc, xl = ctx, x
    for l in range(DEPTH):
        lp = {'w_ada': w_ada[l], 'b_ada': b_ada[l], 'norm1': norm1_g[l], 'norm2': norm2_g[l],
              'w_in': w_in[l], 'qn_a': qn_a[l], 'kn_a': kn_a[l], 'sink_a': sink_a[l],
              'qn_b': qn_b[l], 'kn_b': kn_b[l], 'lam_b': lam_b[l], 'subln_b': subln_b[l],
              'conv_w': conv_w[l], 'conv_b': conv_b[l], 'w_rg': w_rg[l], 'b_rg': b_rg[l],
              'w_ig': w_ig[l], 'b_ig': b_ig[l], 'lru_lambda': lru_lambda[l], 'lb': lb_all[l],
              'onorm_d': onorm_d[l], 'w_branch': w_branch[l], 'w_out': w_out[l],
              'w_gate': w_gate[l], 'w_up': w_up[l], 'w_down': w_down[l]}
        xc, xl = trunk_layer(xc, xl, c, c_ctx, rows, cols, lp, w_router, b_router, l, l < DEPTH - 1)
    return xl
```

```python
import contextlib
import math
import numpy as np
import concourse.bass as bass
import concourse.mybir as mybir
from concourse.bass_utils import run_bass_kernel_spmd

F32 = mybir.dt.float32
BF16 = mybir.dt.bfloat16
AF = mybir.ActivationFunctionType
ALU = mybir.AluOpType
AX = mybir.AxisListType

ENGS = ("pe", "act", "dve", "pool", "sp")
DMA_RING = 6


class Tok:
    __slots__ = ("lw", "rd", "name")

    def __init__(self, name=""):
        self.lw = None
        self.rd = []
        self.name = name


class Ev:
    __slots__ = ("eng", "kind", "idx", "needed", "sem", "val")

    def __init__(self, eng, kind, idx):
        self.eng = eng
        self.kind = kind
        self.idx = idx
        self.needed = False
        self.sem = None
        self.val = None


class Tile(Tok):
    __slots__ = ("t", "shape", "dtype")

    def __init__(self, t, shape, dtype, name=""):
        super().__init__(name)
        self.t = t
        self.shape = shape
        self.dtype = dtype

    def __getitem__(self, k):
        return self.t[k]


class Prog:
    def __init__(self, nc):
        self.nc = nc
        self.q = {e: [] for e in ENGS}
        self.ndma = {e: 0 for e in ENGS}
        self.dma_evs = {e: [] for e in ENGS}
        self.last_ev = {e: None for e in ENGS}
        self.stack = contextlib.ExitStack()
        self.scopes = []
        self.uid = 0
        self.all_dma_evs = []

    def _name(self, name):
        self.uid += 1
        return f"{name}_{self.uid}"

    def _cur(self):
        return self.scopes[-1] if self.scopes else self.stack

    def sbuf(self, name, shape, dtype=F32):
        t = self._cur().enter_context(self.nc.sbuf_tensor(self._name(name), list(shape), dtype))
        return Tile(t, shape, dtype, name)

    def psum(self, name, shape=(128, 512), dtype=F32):
        t = self._cur().enter_context(self.nc.psum_tensor(self._name(name), list(shape), dtype))
        return Tile(t, shape, dtype, name)

    def dram(self, name, shape, dtype=F32, kind="Internal"):
        t = self.nc.dram_tensor(name, list(shape), dtype, kind=kind)
        return Tile(t, shape, dtype, name)

    @contextlib.contextmanager
    def scope(self):
        self.barrier()
        es = contextlib.ExitStack()
        self.scopes.append(es)
        try:
            yield
        finally:
            self.barrier()
            self.scopes.pop()
            es.close()

    def _emit(self, eng, fn, r, w, kind):
        deps = []
        for t in r:
            if t.lw is not None:
                deps.append(t.lw)
        for t in w:
            if t.lw is not None:
                deps.append(t.lw)
            deps.extend(t.rd)
        if kind == "d":
            i = self.ndma[eng]
            self.ndma[eng] += 1
            ev = Ev(eng, "d", i)
            if i >= DMA_RING:
                deps.append(self.dma_evs[eng][i - DMA_RING])
            self.dma_evs[eng].append(ev)
            self.all_dma_evs.append(ev)
        else:
            ev = Ev(eng, "c", len(self.q[eng]))
        dd = []
        seen = set()
        for d in deps:
            if id(d) in seen:
                continue
            seen.add(id(d))
            if d.kind == "c" and d.eng == eng and eng == "pe":
                continue
            dd.append(d)
            d.needed = True
        self.q[eng].append([dd, fn, ev])
        for t in r:
            t.rd.append(ev)
        for t in w:
            t.lw = ev
            t.rd = []
        if kind == "c":
            self.last_ev[eng] = ev
        return ev

    def op(self, eng, fn, r=(), w=()):
        return self._emit(eng, fn, list(r), list(w), "c")

    def dma(self, eng, out, in_, r=(), w=(), **kw):
        return self._emit(eng, lambda e: e.dma_start(out=out, in_=in_, **kw), list(r), list(w), "d")

    def barrier(self):
        evs = [self.last_ev[e] for e in ENGS if self.last_ev[e] is not None]
        evs += self.all_dma_evs
        self.all_dma_evs = []
        if not evs:
            return
        for e in ENGS:
            deps = []
            for d in evs:
                if d.kind == "c" and d.eng == e:
                    continue
                d.needed = True
                deps.append(d)
            self.q[e].append([deps, None, None])

    def finalize(self):
        nc = self.nc
        st = self.stack
        self.barrier()
        sem_c = {e: st.enter_context(nc.semaphore(f"c_{e}")) for e in ENGS}
        sem_d = {e: [st.enter_context(nc.semaphore(f"d_{e}_{k}")) for k in range(DMA_RING)]
                 for e in ENGS if self.ndma[e] > 0}
        for e in ENGS:
            cnt = 0
            for deps, fn, ev in self.q[e]:
                if ev is None:
                    continue
                if ev.kind == "c":
                    if ev.needed:
                        cnt += 1
                        ev.sem = sem_c[e]
                        ev.val = cnt
                else:
                    ev.sem = sem_d[e][ev.idx % DMA_RING]
                    ev.val = 16 * (ev.idx // DMA_RING + 1)
        getter = {"pe": "tensor", "act": "scalar", "dve": "vector", "pool": "gpsimd", "sp": "sync"}
        stats = [0, 0]
        with nc.Block() as block:
            for e in ENGS:
                items = self.q[e]
                if not items:
                    continue

                def body(eng, items=items):
                    seen = {}
                    for deps, fn, ev in items:
                        for d in deps:
                            key = id(d.sem)
                            if seen.get(key, 0) >= d.val:
                                continue
                            seen[key] = d.val
                            eng.wait_ge(d.sem, d.val)
                            stats[1] += 1
                        if fn is None:
                            continue
                        ins = fn(eng)
                        stats[0] += 1
                        if ev.kind == "d":
                            ins.then_inc(ev.sem, 16)
                        elif ev.needed:
                            ins.then_inc(ev.sem, 1)

                getattr(block, getter[e])(body)
        self.stats = tuple(stats)
        st.close()
        return nc


D = 2048
SEQ = 2048
LC = 256
T = SEQ + LC
NT = T // 128
NCH = D // 128
DEPTH = 2
EPS = 1e-6
IN_W = 19968
NE = 16
DFF = 512
GRID_W = 64
TG = [(0, 512), (512, 512), (1024, 512), (1536, 512), (2048, 256)]


def mk(base, pairs):
    return bass.AP(base.tensor, base.offset, [list(p) for p in pairs])


def bc_last(ap2, n):
    return mk(ap2, [ap2.ap[0], ap2.ap[1], [0, n]])


def bc_mid(ap2, k):
    return mk(ap2, [ap2.ap[0], [0, k], ap2.ap[1]])


def pbc(dt_tile, offset, n, parts=128):
    return bass.AP(dt_tile.t, offset, [[0, parts], [1, n]])


class Ctx:
    pass


def rope_tables():
    pos = np.arange(SEQ)
    rows = (pos // GRID_W).astype(np.float32)
    cols = (pos % GRID_W).astype(np.float32)

    def tab(half):
        inv = (10000.0 ** (-np.arange(half, dtype=np.float32) / half)).astype(np.float32)
        ar = rows[:, None] * inv[None, :]
        ac = cols[:, None] * inv[None, :]
        c = np.concatenate([np.cos(ar), np.cos(ar), np.cos(ac), np.cos(ac)], axis=1)
        s = np.concatenate([-np.sin(ar), np.sin(ar), -np.sin(ac), np.sin(ac)], axis=1)
        return c.astype(np.float32), s.astype(np.float32)

    ca, sa = tab(32)
    cb, sb = tab(16)
    return ca, sa, cb, sb


def build(nlayers=DEPTH, debug=(), only=None, feed=()):
    nc = bass.Bass("TRN2", target_bir_lowering=False)
    P = Prog(nc)
    C = Ctx()
    ext = lambda n, s, d=F32: P.dram(n, s, d, kind="ExternalInput")
    C.x = ext("x", [SEQ, D])
    C.ctx = ext("ctx", [LC, D])
    C.c = ext("c", [1, D])
    C.c_ctx = ext("c_ctx", [1, D])
    C.w_ada = ext("w_ada", [DEPTH, D, 6 * D])
    C.b_ada = ext("b_ada", [DEPTH, 6 * D])
    C.norm1_g = ext("norm1_g", [DEPTH, D])
    C.norm2_g = ext("norm2_g", [DEPTH, D])
    C.w_in = ext("w_in", [DEPTH, D, IN_W])
    C.qn_a = ext("qn_a", [DEPTH, 128])
    C.kn_a = ext("kn_a", [DEPTH, 128])
    C.sink_a = ext("sink_a", [DEPTH, 8])
    C.qn_b = ext("qn_b", [DEPTH, 64])
    C.kn_b = ext("kn_b", [DEPTH, 64])
    C.lam_b = ext("lam_b", [DEPTH, 256])
    C.subln_b = ext("subln_b", [DEPTH, 128])
    C.conv_w = ext("conv_w", [DEPTH, 4, 1024])
    C.conv_b = ext("conv_b", [DEPTH, 1024])
    C.w_rg = ext("w_rg", [DEPTH, 2, 8, 128, 128])
    C.b_rg = ext("b_rg", [DEPTH, 2, 1024])
    C.w_ig = ext("w_ig", [DEPTH, 2, 8, 128, 128])
    C.b_ig = ext("b_ig", [DEPTH, 2, 1024])
    C.lru_lambda = ext("lru_lambda", [DEPTH, 2, 1024])
    C.lb_d = ext("lb_d", [DEPTH, 2, 1024])
    C.onorm_d = ext("onorm_d", [DEPTH, 128])
    C.w_branch = ext("w_branch", [DEPTH, 4, 1024, D])
    C.w_out = ext("w_out", [DEPTH, D, D])
    C.w_router = ext("w_router", [D, NE])
    C.b_router = ext("b_router", [1, NE])
    C.w_gate = ext("w_gate", [DEPTH, NE, D, DFF])
    C.w_up = ext("w_up", [DEPTH, NE, D, DFF])
    C.w_down = ext("w_down", [DEPTH, NE, DFF, D])
    C.ropeA_c = ext("ropeA_c", [SEQ, 128])
    C.ropeA_s = ext("ropeA_s", [SEQ, 128])
    C.ropeB_c = ext("ropeB_c", [SEQ, 64])
    C.ropeB_s = ext("ropeB_s", [SEQ, 64])
    C.out = P.dram("out", [SEQ, D], F32, kind="ExternalOutput")

    def scratch(name, shape, dt):
        kind = "ExternalInput" if name in feed else ("ExternalOutput" if name in debug else "Internal")
        return P.dram(name, shape, dt, kind=kind)

    C.XRES = scratch("XRES", [T, D], F32)
    C.MOD = scratch("MOD", [2, 6 * D], F32)
    C.QAT = scratch("QAT", [8, 128, T], BF16)
    C.KAT = scratch("KAT", [2, 128, T], BF16)
    C.VA = scratch("VA", [T, 256], BF16)
    C.QBT = scratch("QBT", [8, 128, T], BF16)
    C.KBT = scratch("KBT", [8, 128, T], BF16)
    C.VB = scratch("VB", [T, 1024], BF16)
    C.CXT = scratch("CXT", [1024, T], F32)
    C.GCY = scratch("GCY", [1024, T], BF16)
    C.DQT = scratch("DQT", [1024, T], BF16)
    C.ZF = scratch("ZF", [2, 1024, T], F32)
    C.VD = scratch("VD", [T, 1024], BF16)
    C.SGD = scratch("SGD", [T, 1024], BF16)
    C.SGT = scratch("SGT", [4 * D, T], BF16)
    C.OT = scratch("OT", [4, 1024, T], BF16)
    C.YT = scratch("YT", [D, T], BF16)
    C.H2T = scratch("H2T", [D, T], BF16)
    C.GATES = scratch("GATES", [T, NE], F32)
    C.DBG = None
    if "DBGHG" in debug:
        eo = lambda n, sh, dt=F32: P.dram(n, sh, dt, kind="ExternalOutput")
        C.DBG = {"O": eo("dbgO", [64, 36, 128]), "O1": eo("dbgO1", [64, 36, 128]),
                 "qt": eo("dbgqt", [2, 128, T], BF16), "kt": eo("dbgkt", [2, 128, T], BF16),
                 "qh": eo("dbgqh", [2, 128, T], BF16), "kh": eo("dbgkh", [2, 128, T], BF16),
                 "dec": eo("dbgdec", [2, 128, 36]), "Bx": eo("dbgBx", [128, T + 1]), "kk": eo("dbgkk", [128, T])}

    ident_f = P.sbuf("ident_f", [128, 128], F32)
    ident = P.sbuf("ident", [128, 128], BF16)
    ones_f = P.sbuf("ones_f", [128, 128], F32)
    ones = P.sbuf("ones", [128, 128], BF16)
    m_ge = P.sbuf("m_ge", [128, 128], BF16)
    m_le = P.sbuf("m_le", [128, 128], BF16)
    tmpc = P.sbuf("tmpc", [128, 128], F32)
    P.op("pool", lambda e: e.memset(ident_f[:], 0.0), w=[ident_f])
    P.op("pool", lambda e: e.affine_select(out=ident_f[:], in_=ident_f[:], pattern=[[-1, 128]],
                                           compare_op=ALU.not_equal, fill=1.0, base=0, channel_multiplier=1),
         r=[ident_f], w=[ident_f])
    P.op("dve", lambda e: e.tensor_copy(out=ident[:], in_=ident_f[:]), r=[ident_f], w=[ident])
    P.op("pool", lambda e: e.memset(ones_f[:], 1.0), w=[ones_f])
    P.op("dve", lambda e: e.tensor_copy(out=ones[:], in_=ones_f[:]), r=[ones_f], w=[ones])
    P.op("pool", lambda e: e.affine_select(out=tmpc[:], in_=ones_f[:], pattern=[[-1, 128]],
                                           compare_op=ALU.is_ge, fill=0.0, base=0, channel_multiplier=1),
         r=[ones_f], w=[tmpc])
    P.op("dve", lambda e: e.tensor_copy(out=m_ge[:], in_=tmpc[:]), r=[tmpc], w=[m_ge])
    P.op("pool", lambda e: e.affine_select(out=tmpc[:], in_=ones_f[:], pattern=[[1, 128]],
                                           compare_op=ALU.is_ge, fill=0.0, base=0, channel_multiplier=-1),
         r=[ones_f], w=[tmpc])
    P.op("dve", lambda e: e.tensor_copy(out=m_le[:], in_=tmpc[:]), r=[tmpc], w=[m_le])
    C.ident, C.ident_f, C.ones, C.ones_f, C.m_ge, C.m_le = ident, ident_f, ones, ones_f, m_ge, m_le

    P.dma("sp", C.XRES[0:LC, :], C.ctx[:, :])
    for i in range(4):
        P.dma("sp", C.XRES[LC + i * 512:LC + (i + 1) * 512, :], C.x[i * 512:(i + 1) * 512, :])
    P.barrier()

    if only is not None:
        with P.scope():
            LP = layer_consts(P, C, 0)
            if "mod" in only:
                stage_mod(P, C, 0, LP)
            for nm in only:
                if nm != "mod":
                    globals()["stage_" + nm](P, C, 0, LP)
        P.finalize()
        return nc, P
    for l in range(nlayers):
        last = (l == DEPTH - 1)
        with P.scope():
            LP = layer_consts(P, C, l)
            stage_mod(P, C, l, LP)
            with P.scope():
                hT, hT_tok = stage_norm_T(P, C, l, LP)
                stage_proj(P, C, l, LP, hT, hT_tok)
            stage_attn_a(P, C, l, LP)
            stage_attn_b(P, C, l, LP)
            stage_rglru(P, C, l, LP)
            stage_hgrn(P, C, l, LP)
            stage_merge(P, C, l, LP)
            stage_out_norm2(P, C, l, LP)
            stage_moe(P, C, l, LP, last)
    P.finalize()
    return nc, P


def col_load(P, dst_ap, src_tile, off, n, w, eng="sp"):
    k = n // 128
    src = bass.AP(src_tile.t, off, [[1, 128], [128, k]])
    P.dma(eng, dst_ap, src, w=w, allow_slow_non_contiguous=True)


def layer_consts(P, C, l):
    LP = Ctx()
    LP.n1 = P.sbuf("n1", [128, 16])
    LP.n2 = P.sbuf("n2", [128, 16])
    col_load(P, LP.n1[:], C.norm1_g, l * D, D, [LP.n1])
    col_load(P, LP.n2[:], C.norm2_g, l * D, D, [LP.n2])
    LP.qn_a = P.sbuf("qn_a", [128, 128])
    LP.kn_a = P.sbuf("kn_a", [128, 128])
    LP.qn_b = P.sbuf("qn_b", [128, 64])
    LP.kn_b = P.sbuf("kn_b", [128, 64])
    LP.onorm = P.sbuf("onorm", [128, 128])
    P.dma("sp", LP.qn_a[:], pbc(C.qn_a, l * 128, 128), w=[LP.qn_a])
    P.dma("sp", LP.kn_a[:], pbc(C.kn_a, l * 128, 128), w=[LP.kn_a])
    P.dma("sp", LP.qn_b[:], pbc(C.qn_b, l * 64, 64), w=[LP.qn_b])
    P.dma("sp", LP.kn_b[:], pbc(C.kn_b, l * 64, 64), w=[LP.kn_b])
    P.dma("sp", LP.onorm[:], pbc(C.onorm_d, l * 128, 128), w=[LP.onorm])
    LP.esink = P.sbuf("esink", [128, 8])
    P.dma("sp", LP.esink[:], pbc(C.sink_a, l * 8, 8), w=[LP.esink])
    P.op("act", lambda e: e.activation(out=LP.esink[:], in_=LP.esink[:], func=AF.Exp), r=[LP.esink], w=[LP.esink])
    lam_init = 0.8 - 0.6 * math.exp(-0.3 * l)
    LP.lam_init = lam_init
    lb_ = P.sbuf("lamb", [128, 256])
    P.dma("sp", lb_[:], pbc(C.lam_b, l * 256, 256), w=[lb_])
    pr = P.sbuf("lampr", [128, 2, 64])
    lb4 = lb_[:].rearrange("p (a b d) -> p a b d", a=2, b=2)
    P.op("dve", lambda e: e.tensor_tensor(out=pr[:], in0=lb4[:, :, 0, :], in1=lb4[:, :, 1, :], op=ALU.mult),
         r=[lb_], w=[pr])
    s2 = P.sbuf("lams2", [128, 2])
    P.op("dve", lambda e: e.tensor_reduce(out=s2[:], in_=pr[:], axis=AX.X, op=ALU.add), r=[pr], w=[s2])
    P.op("act", lambda e: e.activation(out=s2[:], in_=s2[:], func=AF.Exp), r=[s2], w=[s2])
    LP.nlam = P.sbuf("nlam", [128, 1])
    P.op("dve", lambda e: e.scalar_tensor_tensor(out=LP.nlam[:], in0=s2[:, 1:2], scalar=-lam_init, in1=s2[:, 0:1],
                                                 op0=ALU.add, op1=ALU.subtract), r=[s2], w=[LP.nlam])
    LP.subln = P.sbuf("subln", [128, 1])
    col_load(P, LP.subln[:], C.subln_b, l * 128, 128, [LP.subln])
    P.op("dve", lambda e: e.tensor_scalar(out=LP.subln[:], in0=LP.subln[:], scalar1=(1.0 - lam_init), scalar2=None,
                                          op0=ALU.mult), r=[LP.subln], w=[LP.subln])
    LP.convw = P.sbuf("convw", [128, 4, 8])
    for tap in range(4):
        col_load(P, LP.convw[:, tap, :], C.conv_w, (l * 4 + tap) * 1024, 1024, [LP.convw])
    LP.convb = P.sbuf("convb", [128, 8])
    col_load(P, LP.convb[:], C.conv_b, l * 1024, 1024, [LP.convb])
    LP.brg = P.sbuf("brg", [128, 2, 8])
    LP.big = P.sbuf("big", [128, 2, 8])
    lam = P.sbuf("lrulam", [128, 2, 8])
    for d in range(2):
        col_load(P, LP.brg[:, d, :], C.b_rg, (l * 2 + d) * 1024, 1024, [LP.brg])
        col_load(P, LP.big[:, d, :], C.b_ig, (l * 2 + d) * 1024, 1024, [LP.big])
        col_load(P, lam[:, d, :], C.lru_lambda, (l * 2 + d) * 1024, 1024, [lam])
    P.op("act", lambda e: e.activation(out=lam[:], in_=lam[:], func=AF.Exp, scale=-1.0), r=[lam], w=[lam])
    P.op("act", lambda e: e.activation(out=lam[:], in_=lam[:], func=AF.Ln, bias=1.0, scale=1.0), r=[lam], w=[lam])
    LP.sp8 = P.sbuf("sp8", [128, 2, 8])
    LP.sp16 = P.sbuf("sp16", [128, 2, 8])
    P.op("dve", lambda e: e.tensor_scalar(out=LP.sp8[:], in0=lam[:], scalar1=-8.0, scalar2=None, op0=ALU.mult),
         r=[lam], w=[LP.sp8])
    P.op("dve", lambda e: e.tensor_scalar(out=LP.sp16[:], in0=lam[:], scalar1=-16.0, scalar2=None, op0=ALU.mult),
         r=[lam], w=[LP.sp16])
    LP.lb = P.sbuf("lb", [128, 2, 8])
    LP.oml = P.sbuf("oml", [128, 2, 8])
    LP.noml = P.sbuf("noml", [128, 2, 8])
    if l == 0:
        P.op("pool", lambda e: e.memset(LP.lb[:], 0.0), w=[LP.lb])
    else:
        d0 = P.sbuf("lbd0", [128, 2, 8])
        for d in range(2):
            col_load(P, d0[:, d, :], C.lb_d, (0 * 2 + d) * 1024, 1024, [d0])
            col_load(P, LP.lb[:, d, :], C.lb_d, (1 * 2 + d) * 1024, 1024, [LP.lb])
        P.op("dve", lambda e: e.tensor_tensor(out=LP.lb[:], in0=LP.lb[:], in1=d0[:], op=ALU.subtract),
             r=[LP.lb, d0], w=[LP.lb])
        P.op("act", lambda e: e.activation(out=LP.lb[:], in_=LP.lb[:], func=AF.Sigmoid), r=[LP.lb], w=[LP.lb])
    P.op("dve", lambda e: e.tensor_scalar(out=LP.oml[:], in0=LP.lb[:], scalar1=-1.0, scalar2=1.0, op0=ALU.mult,
                                          op1=ALU.add), r=[LP.lb], w=[LP.oml])
    P.op("dve", lambda e: e.tensor_scalar(out=LP.noml[:], in0=LP.oml[:], scalar1=-1.0, scalar2=None, op0=ALU.mult),
         r=[LP.oml], w=[LP.noml])
    LP.wr = P.sbuf("wr", [128, 16, NE])
    P.dma("sp", LP.wr[:], C.w_router[:, :].rearrange("(c p) e -> p c e", p=128), w=[LP.wr])
    LP.br = P.sbuf("br", [128, NE])
    P.dma("sp", LP.br[:], pbc(C.b_router, 0, NE), w=[LP.br])
    LP.modP = P.sbuf("modP", [128, 2, 6, 16])
    LP.s1 = P.sbuf("s1", [128, 2, 16])
    LP.s2 = P.sbuf("s2", [128, 2, 16])
    return LP


def stage_mod(P, C, l, LP):
    with P.scope():
        cc = P.sbuf("cc", [128, 16, 2])
        col_load(P, cc[:, :, 0], C.c, 0, D, [cc])
        col_load(P, cc[:, :, 1], C.c_ctx, 0, D, [cc])
        scT = P.sbuf("scT", [128, 16, 2], BF16)
        P.op("act", lambda e: e.activation(out=scT[:], in_=cc[:], func=AF.Silu), r=[cc], w=[scT])
        bada = P.sbuf("bada", [2, 6 * D])
        P.dma("sp", bada[:], pbc(C.b_ada, l * 6 * D, 6 * D, parts=2), w=[bada])
        modsb = P.sbuf("modsb", [2, 6 * D])
        wsrc = C.w_ada[l, :, :].rearrange("(c p) n -> p c n", p=128)
        W = [P.sbuf(f"wada{i}", [128, 16, 512], BF16) for i in range(2)]
        ps = [P.psum(f"psmod{i}") for i in range(2)]
        for g in range(24):
            wt = W[g % 2]
            pt = ps[g % 2]
            P.dma("pool", wt[:], wsrc[:, :, g * 512:(g + 1) * 512], w=[wt])
            for c in range(16):
                P.op("pe", lambda e, c=c, wt=wt, pt=pt: e.matmul(pt[0:2, :], lhsT=scT[:, c, :], rhs=wt[:, c, :],
                                                               start=(c == 0), stop=(c == 15)),
                     r=[scT, wt], w=[pt])
            P.op("dve", lambda e, g=g, pt=pt: e.tensor_tensor(out=modsb[:, g * 512:(g + 1) * 512], in0=pt[0:2, :],
                                                              in1=bada[:, g * 512:(g + 1) * 512], op=ALU.add),
                 r=[pt, bada], w=[modsb])
        P.dma("sp", C.MOD[:, :], modsb[:], r=[modsb])
    for r in range(2):
        for k in range(6):
            col_load(P, LP.modP[:, r, k, :], C.MOD, r * 6 * D + k * D, D, [LP.modP])
    for (s, n, k) in ((LP.s1, LP.n1, 1), (LP.s2, LP.n2, 4)):
        for r in range(2):
            P.op("dve", lambda e, s=s, n=n, k=k, r=r: e.scalar_tensor_tensor(
                out=s[:, r, :], in0=LP.modP[:, r, k, :], scalar=1.0, in1=n[:], op0=ALU.add, op1=ALU.mult),
                r=[LP.modP, n], w=[s])


def norm_tile(P, xt, xn_out_dtype_tile, sq, ss, n):
    P.op("act", lambda e: e.activation(out=sq[:], in_=xt[:], func=AF.Square), r=[xt], w=[sq])
    P.op("dve", lambda e: e.tensor_reduce(out=ss[:], in_=sq[:], axis=AX.X, op=ALU.add), r=[sq], w=[ss])
    P.op("act", lambda e: e.activation(out=ss[:], in_=ss[:], func=AF.Sqrt, scale=1.0 / n, bias=EPS), r=[ss], w=[ss])
    P.op("dve", lambda e: e.reciprocal(out=ss[:], in_=ss[:]), r=[ss], w=[ss])
    P.op("dve", lambda e: e.tensor_scalar(out=xn_out_dtype_tile[:], in0=xt[:], scalar1=ss[:, 0:1], scalar2=None,
                                          op0=ALU.mult), r=[xt, ss], w=[xn_out_dtype_tile])


def stage_norm_T(P, C, l, LP):
    hT = P.sbuf("hT", [128, 16, T], BF16)
    hT_tok = [Tok(f"hT{t}") for t in range(NT)]
    with P.scope():
        xt = [P.sbuf(f"xt{i}", [128, D]) for i in range(2)]
        sq = P.sbuf("sq", [128, D])
        xn = [P.sbuf(f"xn{i}", [128, D], BF16) for i in range(2)]
        ss = [P.sbuf(f"ss{i}", [128, 1]) for i in range(2)]
        pst = [P.psum(f"pst{i}", [128, 1024], BF16) for i in range(2)]
        P.dma("sp", xt[0][:], C.XRES[0:128, :], w=[xt[0]])
        for t in range(NT):
            if t + 1 < NT:
                P.dma("sp", xt[(t + 1) % 2][:], C.XRES[(t + 1) * 128:(t + 2) * 128, :], w=[xt[(t + 1) % 2]])
            X, XN, SS = xt[t % 2], xn[t % 2], ss[t % 2]
            norm_tile(P, X, XN, sq, SS, D)
            r = 1 if t < 2 else 0
            for c4 in range(4):
                pt = pst[c4 % 2]
                for j in range(4):
                    c = c4 * 4 + j
                    P.op("pe", lambda e, c=c, j=j, pt=pt, XN=XN: e.transpose(
                        out=pt[:, j * 128:(j + 1) * 128], in_=XN[:, c * 128:(c + 1) * 128], identity=C.ident[:]),
                        r=[XN, C.ident], w=[pt])
                for j in range(4):
                    c = c4 * 4 + j
                    eng = "act" if j % 2 == 0 else "dve"
                    if eng == "act":
                        P.op("act", lambda e, c=c, j=j, pt=pt, t=t, r=r: e.activation(
                            out=hT[:, c, t * 128:(t + 1) * 128], in_=pt[:, j * 128:(j + 1) * 128], func=AF.Identity,
                            scale=LP.s1[:, r, c:c + 1], bias=LP.modP[:, r, 0, c:c + 1]),
                            r=[pt, LP.s1, LP.modP], w=[hT_tok[t]])
                    else:
                        P.op("dve", lambda e, c=c, j=j, pt=pt, t=t, r=r: e.tensor_scalar(
                            out=hT[:, c, t * 128:(t + 1) * 128], in0=pt[:, j * 128:(j + 1) * 128],
                            scalar1=LP.s1[:, r, c:c + 1], scalar2=LP.modP[:, r, 0, c:c + 1], op0=ALU.mult,
                            op1=ALU.add), r=[pt, LP.s1, LP.modP], w=[hT_tok[t]])
    return hT, hT_tok


def proj_groups():
    g = []
    g += [("aq", 0), ("aq", 1), ("akv", 0)]
    g += [("bq", 0), ("bq", 1), ("bk", 0), ("bk", 1), ("bv", 0), ("bv", 1)]
    g += [("cx", 0), ("cx", 1), ("cy", 0), ("cy", 1)]
    g += [("dq", 0), ("dq", 1), ("dff", 0), ("dff", 1), ("dfb", 0), ("dfb", 1)]
    g += [("di", 0), ("di", 1), ("dg", 0), ("dg", 1)]
    g += [("gt", i) for i in range(16)]
    return g


def stage_proj(P, C, l, LP, hT, hT_tok):
    groups = proj_groups()
    assert len(groups) * 512 == IN_W
    with P.scope():
        wsrc = C.w_in[l, :, :].rearrange("(c p) n -> p c n", p=128)
        W = [P.sbuf(f"win{i}", [128, 16, 512], BF16) for i in range(3)]
        ps = [P.psum(f"psproj{i}") for i in range(3)]
        pst = [P.psum(f"pstq{i}", [128, 1024], BF16) for i in range(2)]
        ca = [P.sbuf(f"ca{i}", [128, 128]) for i in range(2)]
        sa = [P.sbuf(f"sa{i}", [128, 128]) for i in range(2)]
        cb = [P.sbuf(f"cb{i}", [128, 64]) for i in range(2)]
        sb = [P.sbuf(f"sb{i}", [128, 64]) for i in range(2)]
        sq = P.sbuf("qsq", [128, 512])
        ss = P.sbuf("qss", [128, 8])
        qn = P.sbuf("qn", [128, 512])
        t1 = P.sbuf("qt1", [128, 512])
        t2 = P.sbuf("qt2", [128, 512])
        qb = [P.sbuf(f"qb{i}", [128, 512], BF16) for i in range(2)]
        qT = [P.sbuf(f"qT{i}", [128, 512], BF16) for i in range(2)]
        ob = [P.sbuf(f"ob{i}", [128, 512], BF16) for i in range(3)]
        of = [P.sbuf(f"of{i}", [128, 512], F32) for i in range(2)]
        gl = [P.sbuf(f"gl{i}", [128, 512], F32) for i in range(2)]
        cnt = {"ps": 0, "ob": 0, "of": 0, "qb": 0, "pst": 0, "qT": 0, "rope": 0}

        def nxt(key, lst):
            i = cnt[key]
            cnt[key] += 1
            return lst[i % len(lst)]

        def qk_epi(pt, t, ncomp, dim, gain, cosT, sinT, dst, dst_h0, nheads_out):
            w = ncomp * dim
            P.op("act", lambda e: e.activation(out=sq[:, :w], in_=pt[:, :w], func=AF.Square), r=[pt], w=[sq])
            P.op("dve", lambda e: e.tensor_reduce(out=ss[:, :ncomp], in_=sq[:, :w].rearrange("p (h d) -> p h d", d=dim),
                                                  axis=AX.X, op=ALU.add), r=[sq], w=[ss])
            P.op("act", lambda e: e.activation(out=ss[:, :ncomp], in_=ss[:, :ncomp], func=AF.Sqrt, scale=1.0 / dim,
                                               bias=EPS), r=[ss], w=[ss])
            P.op("dve", lambda e: e.reciprocal(out=ss[:, :ncomp], in_=ss[:, :ncomp]), r=[ss], w=[ss])
            v3 = lambda tl: tl[:, :w].rearrange("p (h d) -> p h d", d=dim)
            P.op("dve", lambda e: e.tensor_tensor(out=v3(qn), in0=v3(pt), in1=bc_last(ss[:, :ncomp], dim),
                                                  op=ALU.mult), r=[pt, ss], w=[qn])
            QB = nxt("qb", qb)
            if t >= 2:
                P.op("pool", lambda e: e.tensor_tensor(out=v3(qn), in0=v3(qn), in1=bc_mid(gain[:, :dim], ncomp),
                                                       op=ALU.mult), r=[qn, gain], w=[qn])
                hd = dim // 4
                ng = w // (2 * hd)
                v4 = lambda tl: tl[:, :w].rearrange("p (g s i) -> p g s i", s=2, i=hd)
                def tb(tab, s):
                    b = tab[:, :].rearrange("p (g s i) -> p g s i", s=2, i=hd)[:, :, s, :]
                    return mk(b, [b.ap[0], [0, ncomp], b.ap[1], b.ap[2]])
                q5 = lambda tl, s: tl[:, :w].rearrange("p (h g s i) -> p h g s i", h=ncomp, s=2, i=hd)[:, :, :, s, :]
                P.op("dve", lambda e: e.tensor_tensor(out=v3(t1), in0=v3(qn), in1=bc_mid(cosT[:, :dim], ncomp),
                                                      op=ALU.mult), r=[qn, cosT], w=[t1])
                for s in range(2):
                    P.op("pool", lambda e, s=s: e.tensor_tensor(out=q5(t2, s), in0=q5(qn, 1 - s), in1=tb(sinT, s),
                                                                op=ALU.mult), r=[qn, sinT], w=[t2])
                P.op("dve", lambda e: e.tensor_tensor(out=QB[:, :w], in0=t1[:, :w], in1=t2[:, :w], op=ALU.add),
                     r=[t1, t2], w=[QB])
            else:
                P.op("pool", lambda e: e.tensor_tensor(out=v3(QB), in0=v3(qn), in1=bc_mid(gain[:, :dim], ncomp),
                                                       op=ALU.mult), r=[qn, gain], w=[QB])
            nblk = w // 128
            PT = nxt("pst", pst)
            for j in range(nblk):
                P.op("pe", lambda e, j=j: e.transpose(out=PT[:, j * 128:(j + 1) * 128],
                                                      in_=QB[:, j * 128:(j + 1) * 128], identity=C.ident[:]),
                     r=[QB, C.ident], w=[PT])
            QT = nxt("qT", qT)
            P.op("act", lambda e: e.activation(out=QT[:, :w], in_=PT[:, :w], func=AF.Copy), r=[PT], w=[QT])
            dsta = bass.AP(dst.t, dst_h0 * 128 * T + t * 128, [[T, 128], [128 * T, nblk], [1, 128]])
            P.dma("sp", dsta, QT[:, :w].rearrange("p (h q) -> p h q", q=128), r=[QT])

        for gi, (kind, idx) in enumerate(groups):
            wt = W[gi % 3]
            P.dma("pool", wt[:], wsrc[:, :, gi * 512:(gi + 1) * 512], w=[wt])
            tokmajor = kind in ("aq", "akv", "bq", "bk", "bv", "di", "dg")
            if tokmajor:
                for t in range(NT):
                    pt = nxt("ps", ps)
                    for c in range(16):
                        P.op("pe", lambda e, c=c, t=t, pt=pt, wt=wt: e.matmul(
                            pt[:], lhsT=hT[:, c, t * 128:(t + 1) * 128], rhs=wt[:, c, :], start=(c == 0),
                            stop=(c == 15)), r=[hT_tok[t], wt], w=[pt])
                    lat = t >= 2
                    if lat and kind in ("aq", "akv", "bq", "bk"):
                        k = cnt["rope"] % 2
                        cnt["rope"] += 1
                        r0 = (t - 2) * 128
                        if kind in ("aq", "akv"):
                            P.dma("sp", ca[k][:], C.ropeA_c[r0:r0 + 128, :], w=[ca[k]])
                            P.dma("sp", sa[k][:], C.ropeA_s[r0:r0 + 128, :], w=[sa[k]])
                            cT, sT = ca[k], sa[k]
                        else:
                            P.dma("sp", cb[k][:], C.ropeB_c[r0:r0 + 128, :], w=[cb[k]])
                            P.dma("sp", sb[k][:], C.ropeB_s[r0:r0 + 128, :], w=[sb[k]])
                            cT, sT = cb[k], sb[k]
                    else:
                        cT = sT = None
                    if kind == "aq":
                        qk_epi(pt, t, 4, 128, LP.qn_a, cT, sT, C.QAT, idx * 4, 4)
                    elif kind == "akv":
                        qk_epi(pt, t, 2, 128, LP.kn_a, cT, sT, C.KAT, 0, 2)
                        O = nxt("ob", ob)
                        P.op("act", lambda e, O=O, pt=pt: e.activation(out=O[:, :256], in_=pt[:, 256:512],
                                                                       func=AF.Copy), r=[pt], w=[O])
                        P.dma("sp", C.VA[t * 128:(t + 1) * 128, :], O[:, :256], r=[O])
                    elif kind == "bq":
                        qk_epi(pt, t, 8, 64, LP.qn_b, cT, sT, C.QBT, idx * 4, 4)
                    elif kind == "bk":
                        qk_epi(pt, t, 8, 64, LP.kn_b, cT, sT, C.KBT, idx * 4, 4)
                    else:
                        O = nxt("ob", ob)
                        dst = {"bv": C.VB, "di": C.VD, "dg": C.SGD}[kind]
                        fn = AF.Silu if kind == "dg" else AF.Copy
                        P.op("act", lambda e, O=O, pt=pt, fn=fn: e.activation(out=O[:], in_=pt[:], func=fn),
                             r=[pt], w=[O])
                        P.dma("sp", dst[t * 128:(t + 1) * 128, idx * 512:(idx + 1) * 512], O[:], r=[O])
            else:
                for cbk in range(4):
                    for (t0, tn) in TG:
                        pt = nxt("ps", ps)
                        toks = [hT_tok[t] for t in range(t0 // 128, (t0 + tn) // 128)]
                        for c in range(16):
                            P.op("pe", lambda e, c=c, pt=pt, wt=wt, cbk=cbk, t0=t0, tn=tn: e.matmul(
                                pt[:, :tn], lhsT=wt[:, c, cbk * 128:(cbk + 1) * 128], rhs=hT[:, c, t0:t0 + tn],
                                start=(c == 0), stop=(c == 15)), r=toks + [wt], w=[pt])
                        row = idx * 512 + cbk * 128
                        if kind in ("cx", "dff", "dfb"):
                            O = nxt("of", of)
                            P.op("act", lambda e, O=O, pt=pt, tn=tn: e.activation(out=O[:, :tn], in_=pt[:, :tn],
                                                                                  func=AF.Copy), r=[pt], w=[O])
                            if kind == "cx":
                                dst = C.CXT[row:row + 128, t0:t0 + tn]
                            else:
                                dst = C.ZF[0 if kind == "dff" else 1, row:row + 128, t0:t0 + tn]
                            P.dma("sp", dst, O[:, :tn], r=[O])
                        elif kind == "cy":
                            G = nxt("of", gl)
                            O = nxt("ob", ob)
                            P.op("act", lambda e, G=G, pt=pt, tn=tn: e.activation(out=G[:, :tn], in_=pt[:, :tn],
                                                                                  func=AF.Square), r=[pt], w=[G])
                            P.op("dve", lambda e, G=G, tn=tn: e.tensor_scalar(
                                out=G[:, :tn], in0=G[:, :tn], scalar1=0.044715 * 1.5957691216, scalar2=1.5957691216,
                                op0=ALU.mult, op1=ALU.add), r=[G], w=[G])
                            P.op("dve", lambda e, G=G, pt=pt, tn=tn: e.tensor_tensor(
                                out=G[:, :tn], in0=G[:, :tn], in1=pt[:, :tn], op=ALU.mult), r=[G, pt], w=[G])
                            P.op("act", lambda e, G=G, tn=tn: e.activation(out=G[:, :tn], in_=G[:, :tn],
                                                                           func=AF.Sigmoid), r=[G], w=[G])
                            P.op("dve", lambda e, G=G, O=O, pt=pt, tn=tn: e.tensor_tensor(
                                out=O[:, :tn], in0=G[:, :tn], in1=pt[:, :tn], op=ALU.mult), r=[G, pt], w=[O])
                            P.dma("sp", C.GCY[row:row + 128, t0:t0 + tn], O[:, :tn], r=[O])
                        else:
                            O = nxt("ob", ob)
                            fn = AF.Sigmoid if kind == "gt" else AF.Copy
                            P.op("act", lambda e, O=O, pt=pt, tn=tn, fn=fn: e.activation(
                                out=O[:, :tn], in_=pt[:, :tn], func=fn), r=[pt], w=[O])
                            dst = (C.SGT if kind == "gt" else C.DQT)[row:row + 128, t0:t0 + tn]
                            P.dma("sp", dst, O[:, :tn], r=[O])


def stage_attn_a(P, C, l, LP):
    scale = 128 ** -0.5
    with P.scope():
        QA = P.sbuf("QA", [128, 8, T], BF16)
        KA = P.sbuf("KA", [128, 2, T], BF16)
        VAs = P.sbuf("VAs", [128, NT, 256], BF16)
        OA = P.sbuf("OA", [128, 8, T], BF16)
        P.dma("sp", QA[:], C.QAT[:, :, :].rearrange("h d t -> d h t"), w=[QA])
        P.dma("sp", KA[:], C.KAT[:, :, :].rearrange("h d t -> d h t"), w=[KA])
        P.dma("sp", VAs[:], C.VA[:, :].rearrange("(n p) v -> p n v", p=128), w=[VAs])
        pss = [P.psum(f"pss{i}") for i in range(2)]
        pso = [P.psum(f"pso{i}") for i in range(2)]
        psz = [P.psum(f"psz{i}") for i in range(2)]
        pT = [P.sbuf(f"pT{i}", [128, 512], BF16) for i in range(3)]
        zs = P.sbuf("zs", [128, 512])
        ot = P.sbuf("ot", [128, 512])
        OA_tok = Tok("OA")
        it = 0
        ip = 0
        for qb in range(NT):
            if qb < 2:
                kbs = [(0, None), (1, None)]
            else:
                n = qb - 2
                kbs = [(0, None), (1, None)]
                if n >= 1:
                    kbs.append((qb - 1, C.m_ge))
                kbs.append((qb, None))
                if n <= 14:
                    kbs.append((qb + 1, C.m_le))
            for hk in range(2):
                po, pz = pso[it % 2], psz[it % 2]
                it += 1
                rhs_q = QA[:, 4 * hk:4 * hk + 4, qb * 128:(qb + 1) * 128]
                for ki, (kb, mask) in enumerate(kbs):
                    psx = pss[ip % 2]
                    pt_ = pT[ip % 3]
                    ip += 1
                    P.op("pe", lambda e, psx=psx, kb=kb, hk=hk, rhs_q=rhs_q: e.matmul(
                        psx[:].rearrange("p (h q) -> p h q", h=4), lhsT=KA[:, hk, kb * 128:(kb + 1) * 128], rhs=rhs_q,
                        start=True, stop=True), r=[KA, QA], w=[psx])
                    P.op("act", lambda e, psx=psx, pt_=pt_: e.activation(out=pt_[:], in_=psx[:], func=AF.Exp,
                                                                         scale=scale), r=[psx], w=[pt_])
                    if mask is not None:
                        P.op("pool", lambda e, pt_=pt_, mask=mask: e.tensor_tensor(
                            out=pt_[:].rearrange("p (h q) -> p h q", h=4),
                            in0=pt_[:].rearrange("p (h q) -> p h q", h=4), in1=bc_mid(mask[:, :], 4), op=ALU.mult),
                            r=[pt_, mask], w=[pt_])
                    st, sp_ = (ki == 0), (ki == len(kbs) - 1)
                    P.op("pe", lambda e, po=po, kb=kb, hk=hk, pt_=pt_, st=st, sp_=sp_: e.matmul(
                        po[:], lhsT=VAs[:, kb, hk * 128:(hk + 1) * 128], rhs=pt_[:], start=st, stop=sp_),
                        r=[VAs, pt_], w=[po])
                    P.op("pe", lambda e, pz=pz, pt_=pt_, st=st, sp_=sp_: e.matmul(
                        pz[:], lhsT=C.ones[:], rhs=pt_[:], start=st, stop=sp_), r=[C.ones, pt_], w=[pz])
                P.op("dve", lambda e, pz=pz, hk=hk: e.tensor_tensor(
                    out=zs[:].rearrange("p (h q) -> p h q", h=4), in0=pz[:].rearrange("p (h q) -> p h q", h=4),
                    in1=bc_last(LP.esink[:, 4 * hk:4 * hk + 4], 128), op=ALU.add), r=[pz, LP.esink], w=[zs])
                P.op("dve", lambda e: e.reciprocal(out=zs[:], in_=zs[:]), r=[zs], w=[zs])
                P.op("dve", lambda e, po=po, hk=hk, qb=qb: e.tensor_tensor(
                    out=OA[:, 4 * hk:4 * hk + 4, qb * 128:(qb + 1) * 128],
                    in0=po[:].rearrange("p (h q) -> p h q", h=4), in1=zs[:].rearrange("p (h q) -> p h q", h=4),
                    op=ALU.mult), r=[po, zs], w=[OA_tok])
        P.dma("sp", C.OT[0, :, :].rearrange("(h d) t -> d h t", d=128), OA[:], r=[OA_tok])


def stage_attn_b(P, C, l, LP):
    scale = 64 ** -0.5
    with P.scope():
        QB_ = [P.sbuf(f"QBh{i}", [128, T], BF16) for i in range(2)]
        KB_ = [P.sbuf(f"KBh{i}", [128, T], BF16) for i in range(2)]
        VB_ = [P.sbuf(f"VBh{i}", [128, NT, 128], BF16) for i in range(2)]
        OB_ = [P.sbuf(f"OBh{i}", [128, T], BF16) for i in range(2)]
        pss = [P.psum(f"bpss{i}") for i in range(2)]
        pso = [P.psum(f"bpso{i}") for i in range(2)]
        psz = [P.psum(f"bpsz{i}") for i in range(2)]
        psn = P.psum("bpsn")
        pT = [P.sbuf(f"bpT{i}", [128, 512], BF16) for i in range(3)]
        r1 = P.sbuf("br1", [128, 512])
        r2 = P.sbuf("br2", [128, 512])
        o1 = P.sbuf("bo1", [128, 512])
        o2 = P.sbuf("bo2", [128, 512])
        osq = P.sbuf("bosq", [128, 512], BF16)
        ip = 0

        def load(h):
            k = h % 2
            P.dma("sp", QB_[k][:], C.QBT[h, :, :], w=[QB_[k]])
            P.dma("sp", KB_[k][:], C.KBT[h, :, :], w=[KB_[k]])
            P.dma("sp", VB_[k][:], C.VB[:, h * 128:(h + 1) * 128].rearrange("(n p) v -> p n v", p=128), w=[VB_[k]])

        load(0)
        for h in range(8):
            if h + 1 < 8:
                load(h + 1)
            Q, K, V, O = QB_[h % 2], KB_[h % 2], VB_[h % 2], OB_[h % 2]
            for (q0, qn_, nkb) in [(0, 256, 2)] + [(LC + i * 512, 512, NT) for i in range(4)]:
                for c in range(2):
                    po, pz = pso[c], psz[c]
                    for kb in range(nkb):
                        psx = pss[ip % 2]
                        pt_ = pT[ip % 3]
                        ip += 1
                        P.op("pe", lambda e, psx=psx, K=K, Q=Q, c=c, kb=kb, q0=q0, qn_=qn_: e.matmul(
                            psx[:, :qn_], lhsT=K[c * 64:(c + 1) * 64, kb * 128:(kb + 1) * 128],
                            rhs=Q[c * 64:(c + 1) * 64, q0:q0 + qn_], start=True, stop=True), r=[K, Q], w=[psx])
                        P.op("act", lambda e, psx=psx, pt_=pt_, qn_=qn_: e.activation(
                            out=pt_[:, :qn_], in_=psx[:, :qn_], func=AF.Exp, scale=scale), r=[psx], w=[pt_])
                        st, sp_ = (kb == 0), (kb == nkb - 1)
                        P.op("pe", lambda e, po=po, V=V, kb=kb, pt_=pt_, st=st, sp_=sp_, qn_=qn_: e.matmul(
                            po[:, :qn_], lhsT=V[:, kb, :], rhs=pt_[:, :qn_], start=st, stop=sp_), r=[V, pt_], w=[po])
                        P.op("pe", lambda e, pz=pz, pt_=pt_, st=st, sp_=sp_, qn_=qn_: e.matmul(
                            pz[:, :qn_], lhsT=C.ones[:], rhs=pt_[:, :qn_], start=st, stop=sp_),
                            r=[C.ones, pt_], w=[pz])
                P.op("dve", lambda e, qn_=qn_: e.reciprocal(out=r1[:, :qn_], in_=psz[0][:, :qn_]), r=[psz[0]], w=[r1])
                P.op("dve", lambda e, qn_=qn_: e.reciprocal(out=r2[:, :qn_], in_=psz[1][:, :qn_]), r=[psz[1]], w=[r2])
                P.op("dve", lambda e, qn_=qn_: e.tensor_tensor(out=o1[:, :qn_], in0=pso[0][:, :qn_], in1=r1[:, :qn_],
                                                               op=ALU.mult), r=[pso[0], r1], w=[o1])
                P.op("dve", lambda e, qn_=qn_: e.tensor_tensor(out=o2[:, :qn_], in0=pso[1][:, :qn_], in1=r2[:, :qn_],
                                                               op=ALU.mult), r=[pso[1], r2], w=[o2])
                P.op("dve", lambda e, qn_=qn_: e.scalar_tensor_tensor(
                    out=o1[:, :qn_], in0=o2[:, :qn_], scalar=LP.nlam[:, 0:1], in1=o1[:, :qn_], op0=ALU.mult,
                    op1=ALU.add), r=[o1, o2, LP.nlam], w=[o1])
                P.op("act", lambda e, qn_=qn_: e.activation(out=osq[:, :qn_], in_=o1[:, :qn_], func=AF.Square),
                     r=[o1], w=[osq])
                P.op("pe", lambda e, qn_=qn_: e.matmul(psn[:, :qn_], lhsT=C.ones[:], rhs=osq[:, :qn_], start=True,
                                                       stop=True), r=[C.ones, osq], w=[psn])
                P.op("act", lambda e, qn_=qn_: e.activation(out=r1[:, :qn_], in_=psn[:, :qn_], func=AF.Sqrt,
                                                            scale=1.0 / 128, bias=EPS), r=[psn], w=[r1])
                P.op("dve", lambda e, qn_=qn_: e.reciprocal(out=r1[:, :qn_], in_=r1[:, :qn_]), r=[r1], w=[r1])
                P.op("dve", lambda e, qn_=qn_, q0=q0, O=O: e.scalar_tensor_tensor(
                    out=O[:, q0:q0 + qn_], in0=o1[:, :qn_], scalar=LP.subln[:, 0:1], in1=r1[:, :qn_], op0=ALU.mult,
                    op1=ALU.mult), r=[o1, r1, LP.subln], w=[O])
            P.dma("sp", C.OT[1, h * 128:(h + 1) * 128, :], O[:], r=[O])


def rev(ap2):
    (ps, pn), (s, n) = ap2.ap
    return bass.AP(ap2.tensor, ap2.offset + s * (n - 1), [[ps, pn], [-s, n]])


def stage_rglru(P, C, l, LP):
    with P.scope():
        wr = P.sbuf("wrg", [128, 16, 128], BF16)
        wi = P.sbuf("wig", [128, 16, 128], BF16)
        P.dma("pool", wr[:], C.w_rg[l, :, :, :, :].rearrange("d b i o -> i (d b) o"), w=[wr])
        P.dma("pool", wi[:], C.w_ig[l, :, :, :, :].rearrange("d b i o -> i (d b) o"), w=[wi])
        x = P.sbuf("cx", [128, T])
        u = P.sbuf("cu", [128, T])
        ub = P.sbuf("cub", [128, T], BF16)
        rr = P.sbuf("crr", [128, T])
        ii = P.sbuf("cii", [128, T])
        aa = P.sbuf("caa", [128, T])
        vv = P.sbuf("cvv", [128, T])
        hh = [P.sbuf(f"chh{d}", [128, T]) for d in range(2)]
        gy = P.sbuf("cgy", [128, T], BF16)
        oo = P.sbuf("coo", [128, T], BF16)
        ps = [P.psum(f"cps{i}") for i in range(4)]
        ip = 0
        for ch in range(8):
            P.dma("sp", x[:], C.CXT[ch * 128:(ch + 1) * 128, :], w=[x])
            P.dma("sp", gy[:], C.GCY[ch * 128:(ch + 1) * 128, :], w=[gy])
            P.op("dve", lambda e, ch=ch: e.tensor_scalar(out=u[:], in0=x[:], scalar1=LP.convw[:, 2, ch:ch + 1],
                                                         scalar2=LP.convb[:, ch:ch + 1], op0=ALU.mult, op1=ALU.add),
                 r=[x, LP.convw, LP.convb], w=[u])
            for (s0, s1) in ((0, LC), (LC, T)):
                for tap in (0, 1, 3):
                    off = tap - 2
                    lo = s0 + max(0, -off)
                    hi = s1 - max(0, off)
                    P.op("dve", lambda e, ch=ch, tap=tap, lo=lo, hi=hi, off=off: e.scalar_tensor_tensor(
                        out=u[:, lo:hi], in0=x[:, lo + off:hi + off], scalar=LP.convw[:, tap, ch:ch + 1],
                        in1=u[:, lo:hi], op0=ALU.mult, op1=ALU.add), r=[x, u, LP.convw], w=[u])
            P.op("act", lambda e: e.activation(out=ub[:], in_=u[:], func=AF.Copy), r=[u], w=[ub])
            for d in range(2):
                for (t0, tn) in TG:
                    pr_, pi_ = ps[ip % 4], ps[(ip + 1) % 4]
                    ip += 2
                    P.op("pe", lambda e, pr_=pr_, d=d, ch=ch, t0=t0, tn=tn: e.matmul(
                        pr_[:, :tn], lhsT=wr[:, d * 8 + ch, :], rhs=ub[:, t0:t0 + tn], start=True, stop=True),
                        r=[wr, ub], w=[pr_])
                    P.op("pe", lambda e, pi_=pi_, d=d, ch=ch, t0=t0, tn=tn: e.matmul(
                        pi_[:, :tn], lhsT=wi[:, d * 8 + ch, :], rhs=ub[:, t0:t0 + tn], start=True, stop=True),
                        r=[wi, ub], w=[pi_])
                    P.op("act", lambda e, pr_=pr_, d=d, ch=ch, t0=t0, tn=tn: e.activation(
                        out=rr[:, t0:t0 + tn], in_=pr_[:, :tn], func=AF.Sigmoid, bias=LP.brg[:, d, ch:ch + 1]),
                        r=[pr_, LP.brg], w=[rr])
                    P.op("act", lambda e, pi_=pi_, d=d, ch=ch, t0=t0, tn=tn: e.activation(
                        out=ii[:, t0:t0 + tn], in_=pi_[:, :tn], func=AF.Sigmoid, bias=LP.big[:, d, ch:ch + 1]),
                        r=[pi_, LP.big], w=[ii])
                P.op("act", lambda e, d=d, ch=ch: e.activation(out=aa[:], in_=rr[:], func=AF.Exp,
                                                               scale=LP.sp8[:, d, ch:ch + 1]), r=[rr, LP.sp8], w=[aa])
                P.op("act", lambda e, d=d, ch=ch: e.activation(out=vv[:], in_=rr[:], func=AF.Exp,
                                                               scale=LP.sp16[:, d, ch:ch + 1]), r=[rr, LP.sp16], w=[vv])
                P.op("act", lambda e: e.activation(out=vv[:], in_=vv[:], func=AF.Sqrt, scale=-1.0, bias=1.0),
                     r=[vv], w=[vv])
                P.op("dve", lambda e: e.tensor_tensor(out=vv[:], in0=vv[:], in1=ii[:], op=ALU.mult), r=[vv, ii], w=[vv])
                P.op("dve", lambda e: e.tensor_tensor(out=vv[:], in0=vv[:], in1=u[:], op=ALU.mult), r=[vv, u], w=[vv])
                H = hh[d]
                if d == 0:
                    P.op("dve", lambda e, H=H: e.tensor_tensor_scan(out=H[:], data0=aa[:], data1=vv[:], initial=0.0,
                                                                    op0=ALU.mult, op1=ALU.add), r=[aa, vv], w=[H])
                else:
                    P.op("dve", lambda e, H=H: e.tensor_tensor_scan(
                        out=rev(H[:, 0:LC]), data0=rev(aa[:, 0:LC]), data1=rev(vv[:, 0:LC]), initial=0.0,
                        op0=ALU.mult, op1=ALU.add), r=[aa, vv], w=[H])
                    P.op("dve", lambda e, H=H: e.tensor_tensor_scan(
                        out=rev(H[:, LC:T]), data0=rev(aa[:, LC:T]), data1=rev(vv[:, LC:T]), initial=H[:, 0:1],
                        op0=ALU.mult, op1=ALU.add), r=[aa, vv, H], w=[H])
            P.op("dve", lambda e: e.tensor_tensor(out=hh[0][:], in0=hh[0][:], in1=hh[1][:], op=ALU.add),
                 r=[hh[0], hh[1]], w=[hh[0]])
            P.op("dve", lambda e: e.tensor_tensor(out=oo[:], in0=hh[0][:], in1=gy[:], op=ALU.mult),
                 r=[hh[0], gy], w=[oo])
            P.dma("sp", C.OT[2, ch * 128:(ch + 1) * 128, :], oo[:], r=[oo])


def stage_hgrn(P, C, l, LP):
    NCK = T // 64
    with P.scope():
        o1 = C.ones_f[:, 0:1]
        ones_bc = mk(o1, [o1.ap[0], [0, T]])
        mf = P.sbuf("mf", [64, 64], BF16)
        mb = P.sbuf("mb", [64, 64], BF16)
        P.op("dve", lambda e: e.tensor_copy(out=mf[:], in_=C.m_le[0:64, 0:64]), r=[C.m_le], w=[mf])
        P.op("dve", lambda e: e.tensor_copy(out=mb[:], in_=C.m_ge[0:64, 0:64]), r=[C.m_ge], w=[mb])
        z = P.sbuf("dz", [128, T])
        sg = P.sbuf("dsg", [128, T])
        kk = P.sbuf("dkk", [128, T])
        Bi = P.sbuf("dBi", [128, T])
        Be_ = P.sbuf("dBe", [128, T])
        E = P.sbuf("dE", [128, T])
        E2 = z
        qT = P.sbuf("dqT", [128, T], BF16)
        vt = P.sbuf("dvt", [64, NCK, 128], BF16)
        sgd = P.sbuf("dsgd", [64, NCK, 128], BF16)
        qt_ = [P.sbuf(f"dqt{d}", [128, T], BF16) for d in range(2)]
        kt_ = [P.sbuf(f"dkt{d}", [128, T], BF16) for d in range(2)]
        qh_ = [P.sbuf(f"dqh{d}", [128, T], BF16) for d in range(2)]
        kh_ = [P.sbuf(f"dkh{d}", [128, T], BF16) for d in range(2)]
        kx_ = [P.sbuf(f"dkx{d}", [128, T], BF16) for d in range(2)]
        dec = [P.sbuf(f"ddec{d}", [128, NCK]) for d in range(2)]
        Sf = [P.sbuf(f"dSf{d}", [128, 128]) for d in range(2)]
        Sb = [P.sbuf(f"dSb{d}", [128, 128], BF16) for d in range(2)]
        Od = [P.sbuf(f"dO{d}", [64, NCK, 128]) for d in range(2)]
        Od_tok = [[Tok(f"Od{d}_{c}") for c in range(NCK)] for d in range(2)]
        att = [[P.sbuf(f"datt{d}{i}", [64, 64], BF16) for i in range(2)] for d in range(2)]
        khs = [[P.sbuf(f"dkhs{d}{i}", [64, 128], BF16) for i in range(2)] for d in range(2)]
        ps_att = [P.psum(f"dpsatt{d}") for d in range(2)]
        for d in range(2):
            P.op("dve", lambda e, d=d: e.memset(ps_att[d][:], 0.0), w=[ps_att[d]])
        ps_kh = [P.psum(f"dpskh{d}", [128, 1024], BF16) for d in range(2)]
        ps_o = [P.psum(f"dpso{d}") for d in range(2)]
        ps_s = [P.psum(f"dpss{d}") for d in range(2)]
        osq = P.sbuf("dosq", [64, NCK, 128])
        oss = P.sbuf("doss", [64, NCK])
        oy = P.sbuf("doy", [64, NCK, 128], BF16)
        odT = P.sbuf("dodT", [128, T], BF16)

        c3 = lambda tl, off=0: tl[:, off:off + T].rearrange("p (c i) -> p c i", i=64)
        for h in range(8):
            P.dma("sp", qT[:], C.DQT[h * 128:(h + 1) * 128, :], w=[qT])
            P.dma("sp", vt[:], C.VD[:, h * 128:(h + 1) * 128].rearrange("(c p) v -> p c v", p=64), w=[vt])
            P.dma("sp", sgd[:], C.SGD[:, h * 128:(h + 1) * 128].rearrange("(c p) v -> p c v", p=64), w=[sgd])
            def gate_math(d, h):
                P.dma("sp", z[:], C.ZF[d, h * 128:(h + 1) * 128, :], w=[z])
                P.op("act", lambda e: e.activation(out=sg[:], in_=z[:], func=AF.Sigmoid), r=[z], w=[sg])
                P.op("dve", lambda e, d=d, h=h: e.tensor_scalar(
                    out=kk[:], in0=sg[:], scalar1=LP.noml[:, d, h:h + 1], scalar2=LP.oml[:, d, h:h + 1], op0=ALU.mult,
                    op1=ALU.add), r=[sg, LP.noml, LP.oml], w=[kk])
                P.op("act", lambda e, d=d, h=h: e.activation(out=sg[:], in_=sg[:], func=AF.Ln,
                                                             scale=LP.oml[:, d, h:h + 1], bias=LP.lb[:, d, h:h + 1]),
                     r=[sg, LP.oml, LP.lb], w=[sg])
                P.op("dve", lambda e: e.tensor_tensor_scan(out=Bi[:], data0=ones_bc, data1=sg[:],
                                                           initial=0.0, op0=ALU.mult, op1=ALU.add),
                     r=[C.ones_f, sg], w=[Bi])
                P.op("dve", lambda e: e.tensor_tensor(out=Be_[:], in0=Bi[:], in1=sg[:], op=ALU.subtract),
                     r=[Bi, sg], w=[Be_])
                Bx = Bi
                Bsrc = Bi if d == 0 else Be_
                Bv = c3(Bsrc)
                Bs1 = c3(Be_)[:, :, 0:1]
                Be = c3(Bi)[:, :, 63:64]
                c32 = lambda tl, off=0: tl[:, off:off + T].rearrange("p (c i) -> p c i", i=32)
                Bv32 = c32(Bsrc)
                Bref = c32(Bsrc)[:, :, 16:17]
                bcl = lambda a: mk(a, [a.ap[0], a.ap[1], [0, 64]])
                bcl32 = lambda a: mk(a, [a.ap[0], a.ap[1], [0, 32]])
                if d == 0:
                    P.op("dve", lambda e: e.tensor_tensor(out=c32(E), in0=Bv32, in1=bcl32(Bref), op=ALU.subtract),
                         r=[Bi, Be_], w=[E])
                else:
                    P.op("dve", lambda e: e.tensor_tensor(out=c32(E), in0=bcl32(Bref), in1=Bv32, op=ALU.subtract),
                         r=[Bi, Be_], w=[E])
                P.op("act", lambda e: e.activation(out=E[:], in_=E[:], func=AF.Exp), r=[E], w=[E])
                P.op("dve", lambda e, d=d: e.tensor_tensor(out=qt_[d][:], in0=qT[:], in1=E[:], op=ALU.mult),
                     r=[qT, E], w=[qt_[d]])
                P.op("dve", lambda e: e.reciprocal(out=E[:], in_=E[:]), r=[E], w=[E])
                P.op("dve", lambda e, d=d: e.tensor_tensor(out=kt_[d][:], in0=kk[:], in1=E[:], op=ALU.mult),
                     r=[kk, E], w=[kt_[d]])
                if d == 0:
                    P.op("dve", lambda e: e.tensor_tensor(out=c3(E), in0=Bv, in1=bcl(Bs1), op=ALU.subtract),
                         r=[Bi, Be_], w=[E])
                    P.op("dve", lambda e: e.tensor_tensor(out=c3(E2), in0=bcl(Be), in1=Bv, op=ALU.subtract),
                         r=[Bi, Be_], w=[E2])
                else:
                    P.op("dve", lambda e: e.tensor_tensor(out=c3(E), in0=bcl(Be), in1=Bv, op=ALU.subtract),
                         r=[Bi, Be_], w=[E])
                    P.op("dve", lambda e: e.tensor_tensor(out=c3(E2), in0=Bv, in1=bcl(Bs1), op=ALU.subtract),
                         r=[Bi, Be_], w=[E2])
                P.op("act", lambda e: e.activation(out=E[:], in_=E[:], func=AF.Exp), r=[E], w=[E])
                P.op("act", lambda e: e.activation(out=E2[:], in_=E2[:], func=AF.Exp), r=[E2], w=[E2])
                P.op("dve", lambda e, d=d: e.tensor_tensor(out=qh_[d][:], in0=qT[:], in1=E[:], op=ALU.mult),
                     r=[qT, E], w=[qh_[d]])
                P.op("dve", lambda e: e.reciprocal(out=E[:], in_=E[:]), r=[E], w=[E])
                P.op("dve", lambda e, d=d: e.tensor_tensor(out=kx_[d][:], in0=kk[:], in1=E[:], op=ALU.mult),
                     r=[kk, E], w=[kx_[d]])
                P.op("dve", lambda e, d=d: e.tensor_tensor(out=kh_[d][:], in0=kk[:], in1=E2[:], op=ALU.mult),
                     r=[kk, E2], w=[kh_[d]])
                P.op("dve", lambda e, d=d: e.tensor_tensor(out=dec[d][:].rearrange("p (c o) -> p c o", o=1), in0=Be,
                                                           in1=Bs1, op=ALU.subtract), r=[Bi, Be_], w=[dec[d]])
                P.op("act", lambda e, d=d: e.activation(out=dec[d][:], in_=dec[d][:], func=AF.Exp),
                     r=[dec[d]], w=[dec[d]])
                P.op("pool", lambda e, d=d: e.memset(Sf[d][:], 0.0), w=[Sf[d]])
                P.op("pool", lambda e, d=d: e.memset(Sb[d][:], 0.0), w=[Sb[d]])

            for d in range(2):
                gate_math(d, h)
            order = [list(range(NCK)), [3, 2, 1, 0] + list(range(NCK - 1, 3, -1))]
            for step in range(NCK):
                for d in range(2):
                    c = order[d][step]
                    sl = slice(c * 64, (c + 1) * 64)
                    A = att[d][step % 2]
                    KH = khs[d][step % 2]
                    mask = mf if d == 0 else mb
                    c0 = c * 64
                    for hb in range(2):
                        P.op("pe", lambda e, d=d, c0=c0, hb=hb: e.matmul(
                            ps_att[d][hb * 32:(hb + 1) * 32, hb * 32:(hb + 1) * 32],
                            lhsT=kt_[d][:, c0 + hb * 32:c0 + (hb + 1) * 32],
                            rhs=qt_[d][:, c0 + hb * 32:c0 + (hb + 1) * 32], start=True, stop=True),
                            r=[kt_[d], qt_[d]], w=[ps_att[d]])
                    jh, ih = (0, 1) if d == 0 else (1, 0)
                    P.op("pe", lambda e, d=d, c0=c0, jh=jh, ih=ih: e.matmul(
                        ps_att[d][jh * 32:(jh + 1) * 32, ih * 32:(ih + 1) * 32],
                        lhsT=kx_[d][:, c0 + jh * 32:c0 + (jh + 1) * 32],
                        rhs=qh_[d][:, c0 + ih * 32:c0 + (ih + 1) * 32], start=True, stop=True),
                        r=[kx_[d], qh_[d]], w=[ps_att[d]])
                    P.op("dve", lambda e, d=d, A=A, mask=mask: e.tensor_tensor(
                        out=A[:], in0=ps_att[d][0:64, 0:64], in1=mask[:], op=ALU.mult), r=[ps_att[d], mask], w=[A])
                    P.op("pe", lambda e, d=d, sl=sl: e.transpose(out=ps_kh[d][0:64, 0:128], in_=kh_[d][:, sl],
                                                                 identity=C.ident[:]),
                         r=[kh_[d], C.ident], w=[ps_kh[d]])
                    P.op("act", lambda e, d=d, KH=KH: e.activation(out=KH[:], in_=ps_kh[d][0:64, 0:128],
                                                                   func=AF.Copy), r=[ps_kh[d]], w=[KH])
                    P.op("pe", lambda e, d=d, A=A, c=c: e.matmul(ps_o[d][0:64, 0:128], lhsT=A[:], rhs=vt[:, c, :],
                                                                 start=True, stop=False), r=[A, vt], w=[ps_o[d]])
                    P.op("pe", lambda e, d=d, sl=sl: e.matmul(ps_o[d][0:64, 0:128], lhsT=qh_[d][:, sl], rhs=Sb[d][:],
                                                              start=False, stop=True),
                         r=[qh_[d], Sb[d]], w=[ps_o[d]])
                    P.op("act", lambda e, d=d, c=c: e.activation(out=Od[d][:, c, :], in_=ps_o[d][0:64, 0:128],
                                                                 func=AF.Copy), r=[ps_o[d]], w=[Od_tok[d][c]])
                    P.op("pe", lambda e, d=d, KH=KH, c=c: e.matmul(ps_s[d][:, 0:128], lhsT=KH[:], rhs=vt[:, c, :],
                                                                   start=True, stop=True), r=[KH, vt], w=[ps_s[d]])
                    P.op("dve", lambda e, d=d, c=c: e.scalar_tensor_tensor(
                        out=Sf[d][:], in0=Sf[d][:], scalar=dec[d][:, c:c + 1], in1=ps_s[d][:, 0:128], op0=ALU.mult,
                        op1=ALU.add), r=[Sf[d], dec[d], ps_s[d]], w=[Sf[d]])
                    P.op("act", lambda e, d=d: e.activation(out=Sb[d][:], in_=Sf[d][:], func=AF.Copy),
                         r=[Sf[d]], w=[Sb[d]])
            allO = [tk for d in range(2) for tk in Od_tok[d]]
            P.op("dve", lambda e: e.tensor_tensor(out=Od[0][:], in0=Od[0][:], in1=Od[1][:], op=ALU.add),
                 r=allO, w=[Od[0]])
            P.op("act", lambda e: e.activation(out=osq[:], in_=Od[0][:], func=AF.Square), r=[Od[0]], w=[osq])
            P.op("dve", lambda e: e.tensor_reduce(out=oss[:], in_=osq[:], axis=AX.X, op=ALU.add), r=[osq], w=[oss])
            P.op("act", lambda e: e.activation(out=oss[:], in_=oss[:], func=AF.Sqrt, scale=1.0 / 128, bias=EPS),
                 r=[oss], w=[oss])
            P.op("dve", lambda e: e.reciprocal(out=oss[:], in_=oss[:]), r=[oss], w=[oss])
            P.op("dve", lambda e: e.tensor_tensor(out=osq[:], in0=Od[0][:], in1=bc_last(oss[:, :], 128), op=ALU.mult),
                 r=[Od[0], oss], w=[osq])
            P.op("pool", lambda e: e.tensor_tensor(out=osq[:], in0=osq[:], in1=bc_mid(LP.onorm[0:64, :], NCK),
                                                   op=ALU.mult), r=[osq, LP.onorm], w=[osq])
            P.op("dve", lambda e: e.tensor_tensor(out=oy[:], in0=osq[:], in1=sgd[:], op=ALU.mult),
                 r=[osq, sgd], w=[oy])
            for c8 in range(0, NCK, 8):
                n8 = min(8, NCK - c8)
                pt = ps_kh[(c8 // 8) % 2]
                for j in range(n8):
                    P.op("pe", lambda e, pt=pt, j=j, c8=c8: e.transpose(
                        out=pt[:, j * 64:(j + 1) * 64], in_=oy[:, c8 + j, :], identity=C.ident[0:64, 0:64]),
                        r=[oy, C.ident], w=[pt])
                P.op("act", lambda e, pt=pt, c8=c8, n8=n8: e.activation(
                    out=odT[:, c8 * 64:(c8 + n8) * 64], in_=pt[:, 0:n8 * 64], func=AF.Copy), r=[pt], w=[odT])
            P.dma("sp", C.OT[3, h * 128:(h + 1) * 128, :], odT[:], r=[odT])
            if h == 7 and getattr(C, "DBG", None) is not None:
                P.dma("sp", C.DBG["O"][:, :, :], Od[0][:], r=[Od[0]])
                P.dma("sp", C.DBG["O1"][:, :, :], Od[1][:], r=[Od[1]])
                for d in range(2):
                    P.dma("sp", C.DBG["qt"][d, :, :], qt_[d][:], r=[qt_[d]])
                    P.dma("sp", C.DBG["kt"][d, :, :], kt_[d][:], r=[kt_[d]])
                    P.dma("sp", C.DBG["qh"][d, :, :], qh_[d][:], r=[qh_[d]])
                    P.dma("sp", C.DBG["kh"][d, :, :], kh_[d][:], r=[kh_[d]])
                    P.dma("sp", C.DBG["dec"][d, :, :], dec[d][:], r=[dec[d]])
                P.dma("sp", C.DBG["kk"][:, :], kk[:], r=[kk])


def stage_merge(P, C, l, LP):
    SG = 1152
    SUB = [(0, 384), (384, 384), (768, 384)]
    with P.scope():
        oT = P.sbuf("moT", [128, 4, 8, SG], BF16)
        yT = P.sbuf("myT", [128, SG], BF16)
        sgt = [P.sbuf(f"msgt{i}", [128, 4, SG], BF16) for i in range(2)]
        wb = [P.sbuf(f"mwb{i}", [128, 4, 8, 128], BF16) for i in range(2)]
        acc = [P.sbuf(f"macc{i}", [128, 384]) for i in range(2)]
        tmp = [P.sbuf(f"mtmp{i}", [128, 384]) for i in range(2)]
        ps = [P.psum(f"mps{i}") for i in range(4)]
        ip = 0
        ia = 0
        for sgi in range(2):
            g0 = sgi * SG
            for n in range(4):
                P.dma("sp", oT[:, n, :, :], C.OT[n, :, g0:g0 + SG].rearrange("(c p) t -> p c t", p=128), w=[oT])
            for dmb in range(16):
                k = dmb % 2
                for n in range(4):
                    P.dma("pool", wb[k][:, n, :, :],
                          C.w_branch[l, n, :, dmb * 128:(dmb + 1) * 128].rearrange("(c p) o -> p c o", p=128),
                          w=[wb[k]])
                P.dma("sp", sgt[k][:], bass.AP(C.SGT.t, (dmb * 128) * T + g0, [[T, 128], [D * T, 4], [1, SG]]),
                      w=[sgt[k]])
                for (s0, sn) in SUB:
                    A = acc[ia % 2]
                    TM = tmp[ia % 2]
                    ia += 1
                    for n in range(4):
                        pt = ps[ip % 4]
                        ip += 1
                        for c in range(8):
                            P.op("pe", lambda e, pt=pt, k=k, n=n, c=c, s0=s0, sn=sn: e.matmul(
                                pt[:, :sn], lhsT=wb[k][:, n, c, :], rhs=oT[:, n, c, s0:s0 + sn], start=(c == 0),
                                stop=(c == 7)), r=[wb[k], oT], w=[pt])
                        if n == 0:
                            P.op("dve", lambda e, pt=pt, k=k, A=A, s0=s0, sn=sn: e.tensor_tensor(
                                out=A[:, :sn], in0=pt[:, :sn], in1=sgt[k][:, 0, s0:s0 + sn], op=ALU.mult),
                                r=[pt, sgt[k]], w=[A])
                        else:
                            P.op("dve", lambda e, pt=pt, k=k, n=n, TM=TM, s0=s0, sn=sn: e.tensor_tensor(
                                out=TM[:, :sn], in0=pt[:, :sn], in1=sgt[k][:, n, s0:s0 + sn], op=ALU.mult),
                                r=[pt, sgt[k]], w=[TM])
                            if n < 3:
                                P.op("pool", lambda e, A=A, TM=TM, sn=sn: e.tensor_tensor(
                                    out=A[:, :sn], in0=A[:, :sn], in1=TM[:, :sn], op=ALU.add), r=[A, TM], w=[A])
                            else:
                                P.op("pool", lambda e, A=A, TM=TM, s0=s0, sn=sn: e.tensor_tensor(
                                    out=yT[:, s0:s0 + sn], in0=A[:, :sn], in1=TM[:, :sn], op=ALU.add),
                                    r=[A, TM], w=[yT])
                P.dma("sp", C.YT[dmb * 128:(dmb + 1) * 128, g0:g0 + SG], yT[:], r=[yT])


def stage_out_norm2(P, C, l, LP):
    with P.scope():
        yT = P.sbuf("oyT", [128, 16, T], BF16)
        P.dma("sp", yT[:], C.YT[:, :].rearrange("(c p) t -> p c t", p=128), w=[yT])
        wo = P.sbuf("owo", [128, 16, D], BF16)
        for c4 in range(4):
            P.dma("pool", wo[:, c4 * 4:(c4 + 1) * 4, :],
                  C.w_out[l, c4 * 512:(c4 + 1) * 512, :].rearrange("(c p) o -> p c o", p=128), w=[wo])
        g1 = P.sbuf("og1", [128, D])
        P.dma("sp", g1[:], pbc(C.MOD, 1 * 6 * D + 2 * D, D), w=[g1])
        xt = [P.sbuf(f"oxt{i}", [128, D]) for i in range(2)]
        tmp = [P.sbuf(f"otmp{i}", [128, 512]) for i in range(2)]
        sq = P.sbuf("osq", [128, D])
        xn = P.sbuf("oxn", [128, D])
        ss = P.sbuf("oss", [128, 1])
        hf = P.sbuf("ohf", [128, 16, 128])
        hb = [P.sbuf(f"ohb{i}", [128, 16, 128], BF16) for i in range(2)]
        ps = [P.psum(f"ops{i}") for i in range(4)]
        pst = [P.psum(f"opst{i}") for i in range(2)]
        psr = P.psum("opsr")
        aff = P.sbuf("raff", [128, NE])
        sel = P.sbuf("rsel", [128, NE])
        r4 = [P.sbuf(f"r4_{i}", [128, 4]) for i in range(8)]
        msk = P.sbuf("rmsk", [128, NE])
        gm = P.sbuf("rgm", [128, 1])
        gt_ = [P.sbuf(f"rgt{i}", [128, NE]) for i in range(2)]
        P.dma("sp", xt[0][:], C.XRES[0:128, :], w=[xt[0]])
        for t in range(NT):
            if t + 1 < NT:
                P.dma("sp", xt[(t + 1) % 2][:], C.XRES[(t + 1) * 128:(t + 2) * 128, :], w=[xt[(t + 1) % 2]])
            X = xt[t % 2]
            r = 1 if t < 2 else 0
            if t == 2:
                P.dma("sp", g1[:], pbc(C.MOD, 0 * 6 * D + 2 * D, D), w=[g1])
            for cg in range(4):
                pt = ps[cg]
                for c in range(16):
                    P.op("pe", lambda e, pt=pt, c=c, t=t, cg=cg: e.matmul(
                        pt[:], lhsT=yT[:, c, t * 128:(t + 1) * 128], rhs=wo[:, c, cg * 512:(cg + 1) * 512],
                        start=(c == 0), stop=(c == 15)), r=[yT, wo], w=[pt])
                TM = tmp[cg % 2]
                P.op("dve", lambda e, pt=pt, cg=cg, TM=TM: e.tensor_tensor(
                    out=TM[:], in0=pt[:], in1=g1[:, cg * 512:(cg + 1) * 512],
                    op=ALU.mult), r=[pt, g1], w=[TM])
                P.op("pool", lambda e, X=X, TM=TM, cg=cg: e.tensor_tensor(
                    out=X[:, cg * 512:(cg + 1) * 512], in0=X[:, cg * 512:(cg + 1) * 512], in1=TM[:], op=ALU.add),
                    r=[X, TM], w=[X])
            P.dma("sp", C.XRES[t * 128:(t + 1) * 128, :], X[:], r=[X])
            norm_tile(P, X, xn, sq, ss, D)
            HB = hb[t % 2]
            for c4 in range(4):
                pt = pst[c4 % 2]
                for j in range(4):
                    c = c4 * 4 + j
                    P.op("pe", lambda e, c=c, j=j, pt=pt: e.transpose(
                        out=pt[:, j * 128:(j + 1) * 128], in_=xn[:, c * 128:(c + 1) * 128], identity=C.ident_f[:]),
                        r=[xn, C.ident_f], w=[pt])
                for j in range(4):
                    c = c4 * 4 + j
                    if j % 2 == 0:
                        P.op("act", lambda e, c=c, j=j, pt=pt, r=r: e.activation(
                            out=hf[:, c, :], in_=pt[:, j * 128:(j + 1) * 128], func=AF.Identity,
                            scale=LP.s2[:, r, c:c + 1], bias=LP.modP[:, r, 3, c:c + 1]),
                            r=[pt, LP.s2, LP.modP], w=[hf])
                    else:
                        P.op("dve", lambda e, c=c, j=j, pt=pt, r=r: e.tensor_scalar(
                            out=hf[:, c, :], in0=pt[:, j * 128:(j + 1) * 128], scalar1=LP.s2[:, r, c:c + 1],
                            scalar2=LP.modP[:, r, 3, c:c + 1], op0=ALU.mult, op1=ALU.add),
                            r=[pt, LP.s2, LP.modP], w=[hf])
            P.op("act", lambda e, HB=HB: e.activation(out=HB[:], in_=hf[:], func=AF.Copy), r=[hf], w=[HB])
            P.dma("sp", C.H2T[:, t * 128:(t + 1) * 128].rearrange("(c p) t -> p c t", p=128), HB[:], r=[HB])
            for c in range(16):
                P.op("pe", lambda e, c=c: e.matmul(psr[:, 0:NE], lhsT=hf[:, c, :], rhs=LP.wr[:, c, :], start=(c == 0),
                                                   stop=(c == 15)), r=[hf, LP.wr], w=[psr])
            P.op("act", lambda e: e.activation(out=aff[:], in_=psr[:, 0:NE], func=AF.Sigmoid), r=[psr], w=[aff])
            P.op("dve", lambda e: e.tensor_tensor(out=sel[:], in0=aff[:], in1=LP.br[:], op=ALU.add),
                 r=[aff, LP.br], w=[sel])
            s3 = sel[:].rearrange("p (g k) -> p g k", k=4)
            hi1, lo1, hi2, lo2, top1, sec, gs, ing = r4
            tt = lambda o, a, b, op, rr, ww: P.op("dve", lambda e: e.tensor_tensor(out=o, in0=a, in1=b, op=op),
                                                  r=rr, w=ww)
            tt(hi1[:], s3[:, :, 0], s3[:, :, 1], ALU.max, [sel], [hi1])
            tt(lo1[:], s3[:, :, 0], s3[:, :, 1], ALU.min, [sel], [lo1])
            tt(hi2[:], s3[:, :, 2], s3[:, :, 3], ALU.max, [sel], [hi2])
            tt(lo2[:], s3[:, :, 2], s3[:, :, 3], ALU.min, [sel], [lo2])
            tt(top1[:], hi1[:], hi2[:], ALU.max, [hi1, hi2], [top1])
            tt(sec[:], hi1[:], hi2[:], ALU.min, [hi1, hi2], [sec])
            tt(lo1[:], lo1[:], lo2[:], ALU.max, [lo1, lo2], [lo1])
            tt(sec[:], sec[:], lo1[:], ALU.max, [sec, lo1], [sec])
            tt(gs[:], top1[:], sec[:], ALU.add, [top1, sec], [gs])
            P.op("dve", lambda e: e.tensor_reduce(out=gm[:], in_=gs[:], axis=AX.X, op=ALU.max), r=[gs], w=[gm])
            P.op("dve", lambda e: e.tensor_scalar(out=ing[:], in0=gs[:], scalar1=gm[:, 0:1], scalar2=None,
                                                  op0=ALU.is_equal), r=[gs, gm], w=[ing])
            m3 = msk[:].rearrange("p (g k) -> p g k", k=4)
            P.op("dve", lambda e: e.tensor_tensor(out=m3, in0=s3, in1=bc_last(sec[:, :], 4), op=ALU.is_ge),
                 r=[sel, sec], w=[msk])
            P.op("dve", lambda e: e.tensor_tensor(out=m3, in0=m3, in1=bc_last(ing[:, :], 4), op=ALU.mult),
                 r=[msk, ing], w=[msk])
            P.op("dve", lambda e: e.tensor_tensor(out=msk[:], in0=msk[:], in1=aff[:], op=ALU.mult),
                 r=[msk, aff], w=[msk])
            P.op("dve", lambda e: e.tensor_reduce(out=gm[:], in_=msk[:], axis=AX.X, op=ALU.add), r=[msk], w=[gm])
            P.op("dve", lambda e: e.reciprocal(out=gm[:], in_=gm[:]), r=[gm], w=[gm])
            G = gt_[t % 2]
            P.op("dve", lambda e, G=G: e.tensor_scalar(out=G[:], in0=msk[:], scalar1=gm[:, 0:1], scalar2=None,
                                                       op0=ALU.mult), r=[msk, gm], w=[G])
            P.dma("sp", C.GATES[t * 128:(t + 1) * 128, :], G[:], r=[G])


def stage_moe(P, C, l, LP, last=False):
    with P.scope():
        g2 = [P.sbuf(f"eg2{r}", [128, D]) for r in range(2)]
        for r in range(2):
            P.dma("sp", g2[r][:], pbc(C.MOD, r * 6 * D + 5 * D, D), w=[g2[r]])
        hT = P.sbuf("ehT", [128, 16, 512], BF16)
        gts = P.sbuf("egts", [128, 4, NE])
        yacc = P.sbuf("eyacc", [128, 4, D])
        yacc_tok = [Tok(f"yacc{i}") for i in range(4)]
        wg = [P.sbuf(f"ewg{i}", [128, 16, DFF], BF16) for i in range(2)]
        wu = [P.sbuf(f"ewu{i}", [128, 16, DFF], BF16) for i in range(2)]
        wd = [P.sbuf(f"ewd{i}", [128, 4, D], BF16) for i in range(2)]
        sg = [P.sbuf(f"esg{i}", [128, DFF]) for i in range(2)]
        hid = [P.sbuf(f"ehid{i}", [128, DFF], BF16) for i in range(2)]
        hidT = [P.sbuf(f"ehidT{i}", [128, DFF], BF16) for i in range(2)]
        xt = P.sbuf("ext", [128, D])
        psg = [P.psum(f"epsg{i}") for i in range(2)]
        psu = [P.psum(f"epsu{i}") for i in range(2)]
        pst = P.psum("epst", [128, 1024], BF16)
        psy = [P.psum(f"epsy{i}") for i in range(2)]
        it = 0
        iy = 0
        iw = 0
        for (t0, tn) in TG:
            nt = tn // 128
            P.dma("sp", hT[:, :, :tn], C.H2T[:, t0:t0 + tn].rearrange("(c p) t -> p c t", p=128), w=[hT])
            P.dma("sp", gts[:, :nt, :], C.GATES[t0:t0 + tn, :].rearrange("(n p) e -> p n e", p=128), w=[gts])
            for ex in range(NE):
                k = iw % 2
                iw += 1
                P.dma("pool", wg[k][:], C.w_gate[l, ex, :, :].rearrange("(c p) f -> p c f", p=128), w=[wg[k]])
                P.dma("pool", wu[k][:], C.w_up[l, ex, :, :].rearrange("(c p) f -> p c f", p=128), w=[wu[k]])
                P.dma("pool", wd[k][:], C.w_down[l, ex, :, :].rearrange("(c p) o -> p c o", p=128), w=[wd[k]])
                for ti in range(nt):
                    pg, pu = psg[it % 2], psu[it % 2]
                    SG_, H, HT = sg[it % 2], hid[it % 2], hidT[it % 2]
                    it += 1
                    for c in range(16):
                        P.op("pe", lambda e, pg=pg, c=c, ti=ti, k=k: e.matmul(
                            pg[:], lhsT=hT[:, c, ti * 128:(ti + 1) * 128], rhs=wg[k][:, c, :], start=(c == 0),
                            stop=(c == 15)), r=[hT, wg[k]], w=[pg])
                    for c in range(16):
                        P.op("pe", lambda e, pu=pu, c=c, ti=ti, k=k: e.matmul(
                            pu[:], lhsT=hT[:, c, ti * 128:(ti + 1) * 128], rhs=wu[k][:, c, :], start=(c == 0),
                            stop=(c == 15)), r=[hT, wu[k]], w=[pu])
                    P.op("act", lambda e, pg=pg, SG_=SG_: e.activation(out=SG_[:], in_=pg[:], func=AF.Silu),
                         r=[pg], w=[SG_])
                    P.op("dve", lambda e, SG_=SG_, pu=pu, H=H, ti=ti, ex=ex: e.scalar_tensor_tensor(
                        out=H[:], in0=SG_[:], scalar=gts[:, ti, ex:ex + 1], in1=pu[:], op0=ALU.mult, op1=ALU.mult),
                        r=[SG_, pu, gts], w=[H])
                    for j in range(4):
                        P.op("pe", lambda e, j=j, H=H: e.transpose(out=pst[:, j * 128:(j + 1) * 128],
                                                                   in_=H[:, j * 128:(j + 1) * 128],
                                                                   identity=C.ident[:]), r=[H, C.ident], w=[pst])
                    P.op("act", lambda e, HT=HT: e.activation(out=HT[:], in_=pst[:, 0:512], func=AF.Copy),
                         r=[pst], w=[HT])
                    for cg in range(4):
                        py = psy[iy % 2]
                        iy += 1
                        for f in range(4):
                            P.op("pe", lambda e, py=py, f=f, cg=cg, HT=HT, k=k: e.matmul(
                                py[:], lhsT=HT[:, f * 128:(f + 1) * 128], rhs=wd[k][:, f, cg * 512:(cg + 1) * 512],
                                start=(f == 0), stop=(f == 3)), r=[HT, wd[k]], w=[py])
                        eng = "dve" if cg % 2 == 0 else "pool"
                        if ex == 0:
                            P.op("act", lambda e, py=py, ti=ti, cg=cg: e.activation(
                                out=yacc[:, ti, cg * 512:(cg + 1) * 512], in_=py[:], func=AF.Copy),
                                r=[py], w=[yacc_tok[ti]])
                        else:
                            P.op("dve", lambda e, py=py, ti=ti, cg=cg: e.tensor_tensor(
                                out=yacc[:, ti, cg * 512:(cg + 1) * 512], in0=yacc[:, ti, cg * 512:(cg + 1) * 512],
                                in1=py[:], op=ALU.add), r=[py, yacc_tok[ti]], w=[yacc_tok[ti]])
            for ti in range(nt):
                tt_ = t0 // 128 + ti
                r = 1 if tt_ < 2 else 0
                if last and tt_ < 2:
                    continue
                P.dma("sp", xt[:], C.XRES[tt_ * 128:(tt_ + 1) * 128, :], w=[xt])
                P.op("dve", lambda e, ti=ti, r=r: e.tensor_tensor(out=yacc[:, ti, :], in0=yacc[:, ti, :],
                                                                  in1=g2[r][:], op=ALU.mult),
                     r=[yacc_tok[ti], g2[r]], w=[yacc_tok[ti]])
                P.op("pool", lambda e, ti=ti: e.tensor_tensor(out=xt[:], in0=xt[:], in1=yacc[:, ti, :], op=ALU.add),
                     r=[xt, yacc_tok[ti]], w=[xt])
                if last:
                    P.dma("sp", C.out[(tt_ - 2) * 128:(tt_ - 1) * 128, :], xt[:], r=[xt])
                else:
                    P.dma("sp", C.XRES[tt_ * 128:(tt_ + 1) * 128, :], xt[:], r=[xt])


_CACHE = {}


def make_in_maps(inputs, ncores=8):
    ca, sa, cb, sb = rope_tables()
    f = lambda a: np.ascontiguousarray(np.asarray(a, dtype=np.float32))
    shared = {
        "c_ctx": f(inputs["c_ctx"]).reshape(1, D),
        "w_ada": f(inputs["w_ada"]), "b_ada": f(inputs["b_ada"]),
        "norm1_g": f(inputs["norm1_g"]), "norm2_g": f(inputs["norm2_g"]),
        "w_in": f(inputs["w_in"]), "qn_a": f(inputs["qn_a"]), "kn_a": f(inputs["kn_a"]),
        "sink_a": f(inputs["sink_a"]), "qn_b": f(inputs["qn_b"]), "kn_b": f(inputs["kn_b"]),
        "lam_b": f(inputs["lam_b"]).reshape(DEPTH, 256), "subln_b": f(inputs["subln_b"]),
        "conv_w": f(inputs["conv_w"]), "conv_b": f(inputs["conv_b"]),
        "w_rg": f(inputs["w_rg"]), "b_rg": f(inputs["b_rg"]), "w_ig": f(inputs["w_ig"]), "b_ig": f(inputs["b_ig"]),
        "lru_lambda": f(inputs["lru_lambda"]), "lb_d": f(inputs["lb_d"]), "onorm_d": f(inputs["onorm_d"]),
        "w_branch": f(inputs["w_branch"]), "w_out": f(inputs["w_out"]),
        "w_router": f(inputs["w_router"]), "b_router": f(inputs["b_router"]).reshape(1, NE),
        "w_gate": f(inputs["w_gate"]), "w_up": f(inputs["w_up"]), "w_down": f(inputs["w_down"]),
        "ropeA_c": ca, "ropeA_s": sa, "ropeB_c": cb, "ropeB_s": sb,
    }
    x = f(inputs["x"])
    ctx = f(inputs["ctx"])
    c = f(inputs["c"])
    maps = []
    for b in range(ncores):
        m = dict(shared)
        m["x"] = x[b]
        m["ctx"] = ctx[b]
        m["c"] = c[b].reshape(1, D)
        maps.append(m)
    return maps


def kernel(**inputs):
    if "nc" not in _CACHE:
        _CACHE["nc"] = build()[0]
    nc = _CACHE["nc"]
    maps = make_in_maps(inputs, 8)
    res = run_bass_kernel_spmd(nc, maps, core_ids=list(range(8)))
    return np.stack([np.asarray(r["out"], dtype=np.float32) for r in res.results], axis=0)
```

```python
import contextlib
import math
import numpy as np
import concourse.bass as bass
import concourse.mybir as mybir
from concourse.bass_utils import run_bass_kernel_spmd

F32 = mybir.dt.float32
BF16 = mybir.dt.bfloat16
AF = mybir.ActivationFunctionType
ALU = mybir.AluOpType
AX = mybir.AxisListType

ENGS = ("pe", "act", "dve", "pool", "sp")
DMA_RING = 6


class Tok:
    __slots__ = ("lw", "rd", "name")

    def __init__(self, name=""):
        self.lw = None
        self.rd = []
        self.name = name


class Ev:
    __slots__ = ("eng", "kind", "idx", "needed", "sem", "val")

    def __init__(self, eng, kind, idx):
        self.eng = eng
        self.kind = kind
        self.idx = idx
        self.needed = False
        self.sem = None
        self.val = None


class Tile(Tok):
    __slots__ = ("t", "shape", "dtype")

    def __init__(self, t, shape, dtype, name=""):
        super().__init__(name)
        self.t = t
        self.shape = shape
        self.dtype = dtype

    def __getitem__(self, k):
        return self.t[k]


class Prog:
    def __init__(self, nc):
        self.nc = nc
        self.q = {e: [] for e in ENGS}
        self.ndma = {e: 0 for e in ENGS}
        self.dma_evs = {e: [] for e in ENGS}
        self.last_ev = {e: None for e in ENGS}
        self.stack = contextlib.ExitStack()
        self.scopes = []
        self.uid = 0
        self.all_dma_evs = []

    def _name(self, name):
        self.uid += 1
        return f"{name}_{self.uid}"

    def _cur(self):
        return self.scopes[-1] if self.scopes else self.stack

    def sbuf(self, name, shape, dtype=F32):
        t = self._cur().enter_context(self.nc.sbuf_tensor(self._name(name), list(shape), dtype))
        return Tile(t, shape, dtype, name)

    def psum(self, name, shape=(128, 512), dtype=F32):
        t = self._cur().enter_context(self.nc.psum_tensor(self._name(name), list(shape), dtype))
        return Tile(t, shape, dtype, name)

    def dram(self, name, shape, dtype=F32, kind="Internal"):
        t = self.nc.dram_tensor(name, list(shape), dtype, kind=kind)
        return Tile(t, shape, dtype, name)

    @contextlib.contextmanager
    def scope(self):
        self.barrier()
        es = contextlib.ExitStack()
        self.scopes.append(es)
        try:
            yield
        finally:
            self.barrier()
            self.scopes.pop()
            es.close()

    def _emit(self, eng, fn, r, w, kind):
        deps = []
        for t in r:
            if t.lw is not None:
                deps.append(t.lw)
        for t in w:
            if t.lw is not None:
                deps.append(t.lw)
            deps.extend(t.rd)
        if kind == "d":
            i = self.ndma[eng]
            self.ndma[eng] += 1
            ev = Ev(eng, "d", i)
            if i >= DMA_RING:
                deps.append(self.dma_evs[eng][i - DMA_RING])
            self.dma_evs[eng].append(ev)
            self.all_dma_evs.append(ev)
        else:
            ev = Ev(eng, "c", len(self.q[eng]))
        dd = []
        seen = set()
        for d in deps:
            if id(d) in seen:
                continue
            seen.add(id(d))
            if d.kind == "c" and d.eng == eng and eng == "pe":
                continue
            dd.append(d)
            d.needed = True
        self.q[eng].append([dd, fn, ev])
        for t in r:
            t.rd.append(ev)
        for t in w:
            t.lw = ev
            t.rd = []
        if kind == "c":
            self.last_ev[eng] = ev
        return ev

    def op(self, eng, fn, r=(), w=()):
        return self._emit(eng, fn, list(r), list(w), "c")

    def dma(self, eng, out, in_, r=(), w=(), **kw):
        return self._emit(eng, lambda e: e.dma_start(out=out, in_=in_, **kw), list(r), list(w), "d")

    def barrier(self):
        evs = [self.last_ev[e] for e in ENGS if self.last_ev[e] is not None]
        evs += self.all_dma_evs
        self.all_dma_evs = []
        if not evs:
            return
        for e in ENGS:
            deps = []
            for d in evs:
                if d.kind == "c" and d.eng == e:
                    continue
                d.needed = True
                deps.append(d)
            self.q[e].append([deps, None, None])

    def finalize(self):
        nc = self.nc
        st = self.stack
        self.barrier()
        sem_c = {e: st.enter_context(nc.semaphore(f"c_{e}")) for e in ENGS}
        sem_d = {e: [st.enter_context(nc.semaphore(f"d_{e}_{k}")) for k in range(DMA_RING)]
                 for e in ENGS if self.ndma[e] > 0}
        for e in ENGS:
            cnt = 0
            for deps, fn, ev in self.q[e]:
                if ev is None:
                    continue
                if ev.kind == "c":
                    if ev.needed:
                        cnt += 1
                        ev.sem = sem_c[e]
                        ev.val = cnt
                else:
                    ev.sem = sem_d[e][ev.idx % DMA_RING]
                    ev.val = 16 * (ev.idx // DMA_RING + 1)
        getter = {"pe": "tensor", "act": "scalar", "dve": "vector", "pool": "gpsimd", "sp": "sync"}
        stats = [0, 0]
        with nc.Block() as block:
            for e in ENGS:
                items = self.q[e]
                if not items:
                    continue

                def body(eng, items=items):
                    seen = {}
                    for deps, fn, ev in items:
                        for d in deps:
                            key = id(d.sem)
                            if seen.get(key, 0) >= d.val:
                                continue
                            seen[key] = d.val
                            eng.wait_ge(d.sem, d.val)
                            stats[1] += 1
                        if fn is None:
                            continue
                        ins = fn(eng)
                        stats[0] += 1
                        if ev.kind == "d":
                            ins.then_inc(ev.sem, 16)
                        elif ev.needed:
                            ins.then_inc(ev.sem, 1)

                getattr(block, getter[e])(body)
        self.stats = tuple(stats)
        st.close()
        return nc


D = 2048
SEQ = 2048
LC = 256
T = SEQ + LC
NT = T // 128
NCH = D // 128
DEPTH = 2
EPS = 1e-6
IN_W = 19968
NE = 16
DFF = 512
GRID_W = 64
TG = [(0, 512), (512, 512), (1024, 512), (1536, 512), (2048, 256)]


def mk(base, pairs):
    return bass.AP(base.tensor, base.offset, [list(p) for p in pairs])


def bc_last(ap2, n):
    return mk(ap2, [ap2.ap[0], ap2.ap[1], [0, n]])


def bc_mid(ap2, k):
    return mk(ap2, [ap2.ap[0], [0, k], ap2.ap[1]])


def pbc(dt_tile, offset, n, parts=128):
    return bass.AP(dt_tile.t, offset, [[0, parts], [1, n]])


class Ctx:
    pass


def rope_tables():
    pos = np.arange(SEQ)
    rows = (pos // GRID_W).astype(np.float32)
    cols = (pos % GRID_W).astype(np.float32)

    def tab(half):
        inv = (10000.0 ** (-np.arange(half, dtype=np.float32) / half)).astype(np.float32)
        ar = rows[:, None] * inv[None, :]
        ac = cols[:, None] * inv[None, :]
        c = np.concatenate([np.cos(ar), np.cos(ar), np.cos(ac), np.cos(ac)], axis=1)
        s = np.concatenate([-np.sin(ar), np.sin(ar), -np.sin(ac), np.sin(ac)], axis=1)
        return c.astype(np.float32), s.astype(np.float32)

    ca, sa = tab(32)
    cb, sb = tab(16)
    return ca, sa, cb, sb


def build(nlayers=DEPTH, debug=(), only=None, feed=()):
    nc = bass.Bass("TRN2", target_bir_lowering=False)
    P = Prog(nc)
    C = Ctx()
    ext = lambda n, s, d=F32: P.dram(n, s, d, kind="ExternalInput")
    C.x = ext("x", [SEQ, D])
    C.ctx = ext("ctx", [LC, D])
    C.c = ext("c", [1, D])
    C.c_ctx = ext("c_ctx", [1, D])
    C.w_ada = ext("w_ada", [DEPTH, D, 6 * D])
    C.b_ada = ext("b_ada", [DEPTH, 6 * D])
    C.norm1_g = ext("norm1_g", [DEPTH, D])
    C.norm2_g = ext("norm2_g", [DEPTH, D])
    C.w_in = ext("w_in", [DEPTH, D, IN_W])
    C.qn_a = ext("qn_a", [DEPTH, 128])
    C.kn_a = ext("kn_a", [DEPTH, 128])
    C.sink_a = ext("sink_a", [DEPTH, 8])
    C.qn_b = ext("qn_b", [DEPTH, 64])
    C.kn_b = ext("kn_b", [DEPTH, 64])
    C.lam_b = ext("lam_b", [DEPTH, 256])
    C.subln_b = ext("subln_b", [DEPTH, 128])
    C.conv_w = ext("conv_w", [DEPTH, 4, 1024])
    C.conv_b = ext("conv_b", [DEPTH, 1024])
    C.w_rg = ext("w_rg", [DEPTH, 2, 8, 128, 128])
    C.b_rg = ext("b_rg", [DEPTH, 2, 1024])
    C.w_ig = ext("w_ig", [DEPTH, 2, 8, 128, 128])
    C.b_ig = ext("b_ig", [DEPTH, 2, 1024])
    C.lru_lambda = ext("lru_lambda", [DEPTH, 2, 1024])
    C.lb_d = ext("lb_d", [DEPTH, 2, 1024])
    C.onorm_d = ext("onorm_d", [DEPTH, 128])
    C.w_branch = ext("w_branch", [DEPTH, 4, 1024, D])
    C.w_out = ext("w_out", [DEPTH, D, D])
    C.w_router = ext("w_router", [D, NE])
    C.b_router = ext("b_router", [1, NE])
    C.w_gate = ext("w_gate", [DEPTH, NE, D, DFF])
    C.w_up = ext("w_up", [DEPTH, NE, D, DFF])
    C.w_down = ext("w_down", [DEPTH, NE, DFF, D])
    C.ropeA_c = ext("ropeA_c", [SEQ, 128])
    C.ropeA_s = ext("ropeA_s", [SEQ, 128])
    C.ropeB_c = ext("ropeB_c", [SEQ, 64])
    C.ropeB_s = ext("ropeB_s", [SEQ, 64])
    C.out = P.dram("out", [SEQ, D], F32, kind="ExternalOutput")

    def scratch(name, shape, dt):
        kind = "ExternalInput" if name in feed else ("ExternalOutput" if name in debug else "Internal")
        return P.dram(name, shape, dt, kind=kind)

    C.XRES = scratch("XRES", [T, D], F32)
    C.MOD = scratch("MOD", [2, 6 * D], F32)
    C.QAT = scratch("QAT", [8, 128, T], BF16)
    C.KAT = scratch("KAT", [2, 128, T], BF16)
    C.VA = scratch("VA", [T, 256], BF16)
    C.QBT = scratch("QBT", [8, 128, T], BF16)
    C.KBT = scratch("KBT", [8, 128, T], BF16)
    C.VB = scratch("VB", [T, 1024], BF16)
    C.CXT = scratch("CXT", [1024, T], F32)
    C.GCY = scratch("GCY", [1024, T], BF16)
    C.DQT = scratch("DQT", [1024, T], BF16)
    C.ZF = scratch("ZF", [2, 1024, T], F32)
    C.VD = scratch("VD", [T, 1024], BF16)
    C.SGD = scratch("SGD", [T, 1024], BF16)
    C.SGT = scratch("SGT", [4 * D, T], BF16)
    C.OT = scratch("OT", [4, 1024, T], BF16)
    C.YT = scratch("YT", [D, T], BF16)
    C.H2T = scratch("H2T", [D, T], BF16)
    C.GATES = scratch("GATES", [T, NE], F32)
    C.DBG = None
    if "DBGHG" in debug:
        eo = lambda n, sh, dt=F32: P.dram(n, sh, dt, kind="ExternalOutput")
        C.DBG = {"O": eo("dbgO", [64, 36, 128]), "O1": eo("dbgO1", [64, 36, 128]),
                 "qt": eo("dbgqt", [2, 128, T], BF16), "kt": eo("dbgkt", [2, 128, T], BF16),
                 "qh": eo("dbgqh", [2, 128, T], BF16), "kh": eo("dbgkh", [2, 128, T], BF16),
                 "dec": eo("dbgdec", [2, 128, 36]), "Bx": eo("dbgBx", [128, T + 1]), "kk": eo("dbgkk", [128, T])}

    ident_f = P.sbuf("ident_f", [128, 128], F32)
    ident = P.sbuf("ident", [128, 128], BF16)
    ones_f = P.sbuf("ones_f", [128, 128], F32)
    ones = P.sbuf("ones", [128, 128], BF16)
    m_ge = P.sbuf("m_ge", [128, 128], BF16)
    m_le = P.sbuf("m_le", [128, 128], BF16)
    tmpc = P.sbuf("tmpc", [128, 128], F32)
    P.op("pool", lambda e: e.memset(ident_f[:], 0.0), w=[ident_f])
    P.op("pool", lambda e: e.affine_select(out=ident_f[:], in_=ident_f[:], pattern=[[-1, 128]],
                                           compare_op=ALU.not_equal, fill=1.0, base=0, channel_multiplier=1),
         r=[ident_f], w=[ident_f])
    P.op("dve", lambda e: e.tensor_copy(out=ident[:], in_=ident_f[:]), r=[ident_f], w=[ident])
    P.op("pool", lambda e: e.memset(ones_f[:], 1.0), w=[ones_f])
    P.op("dve", lambda e: e.tensor_copy(out=ones[:], in_=ones_f[:]), r=[ones_f], w=[ones])
    P.op("pool", lambda e: e.affine_select(out=tmpc[:], in_=ones_f[:], pattern=[[-1, 128]],
                                           compare_op=ALU.is_ge, fill=0.0, base=0, channel_multiplier=1),
         r=[ones_f], w=[tmpc])
    P.op("dve", lambda e: e.tensor_copy(out=m_ge[:], in_=tmpc[:]), r=[tmpc], w=[m_ge])
    P.op("pool", lambda e: e.affine_select(out=tmpc[:], in_=ones_f[:], pattern=[[1, 128]],
                                           compare_op=ALU.is_ge, fill=0.0, base=0, channel_multiplier=-1),
         r=[ones_f], w=[tmpc])
    P.op("dve", lambda e: e.tensor_copy(out=m_le[:], in_=tmpc[:]), r=[tmpc], w=[m_le])
    C.ident, C.ident_f, C.ones, C.ones_f, C.m_ge, C.m_le = ident, ident_f, ones, ones_f, m_ge, m_le

    P.dma("sp", C.XRES[0:LC, :], C.ctx[:, :])
    for i in range(4):
        P.dma("sp", C.XRES[LC + i * 512:LC + (i + 1) * 512, :], C.x[i * 512:(i + 1) * 512, :])
    P.barrier()

    if only is not None:
        with P.scope():
            LP = layer_consts(P, C, 0)
            if "mod" in only:
                stage_mod(P, C, 0, LP)
            for nm in only:
                if nm == "np":
                    with P.scope():
                        hT, hT_tok = stage_norm_T(P, C, 0, LP)
                        stage_proj(P, C, 0, LP, hT, hT_tok)
                elif nm != "mod":
                    globals()["stage_" + nm](P, C, 0, LP)
        P.finalize()
        return nc, P
    for l in range(nlayers):
        last = (l == DEPTH - 1)
        with P.scope():
            LP = layer_consts(P, C, l)
            stage_mod(P, C, l, LP)
            with P.scope():
                hT, hT_tok = stage_norm_T(P, C, l, LP)
                stage_proj(P, C, l, LP, hT, hT_tok)
            stage_attn_a(P, C, l, LP)
            stage_attn_b(P, C, l, LP)
            stage_rglru(P, C, l, LP)
            stage_hgrn(P, C, l, LP)
            stage_merge(P, C, l, LP)
            stage_out_norm2(P, C, l, LP)
            stage_moe(P, C, l, LP, last)
    P.finalize()
    return nc, P


def col_load(P, dst_ap, src_tile, off, n, w, eng="sp"):
    k = n // 128
    src = bass.AP(src_tile.t, off, [[1, 128], [128, k]])
    P.dma(eng, dst_ap, src, w=w, allow_slow_non_contiguous=True)


def layer_consts(P, C, l):
    LP = Ctx()
    LP.n1 = P.sbuf("n1", [128, 16])
    LP.n2 = P.sbuf("n2", [128, 16])
    col_load(P, LP.n1[:], C.norm1_g, l * D, D, [LP.n1])
    col_load(P, LP.n2[:], C.norm2_g, l * D, D, [LP.n2])
    LP.qn_a = P.sbuf("qn_a", [128, 128])
    LP.kn_a = P.sbuf("kn_a", [128, 128])
    LP.qn_b = P.sbuf("qn_b", [128, 64])
    LP.kn_b = P.sbuf("kn_b", [128, 64])
    LP.onorm = P.sbuf("onorm", [128, 128])
    P.dma("sp", LP.qn_a[:], pbc(C.qn_a, l * 128, 128), w=[LP.qn_a])
    P.dma("sp", LP.kn_a[:], pbc(C.kn_a, l * 128, 128), w=[LP.kn_a])
    P.dma("sp", LP.qn_b[:], pbc(C.qn_b, l * 64, 64), w=[LP.qn_b])
    P.dma("sp", LP.kn_b[:], pbc(C.kn_b, l * 64, 64), w=[LP.kn_b])
    P.dma("sp", LP.onorm[:], pbc(C.onorm_d, l * 128, 128), w=[LP.onorm])
    LP.esink = P.sbuf("esink", [128, 8])
    P.dma("sp", LP.esink[:], pbc(C.sink_a, l * 8, 8), w=[LP.esink])
    P.op("act", lambda e: e.activation(out=LP.esink[:], in_=LP.esink[:], func=AF.Exp), r=[LP.esink], w=[LP.esink])
    lam_init = 0.8 - 0.6 * math.exp(-0.3 * l)
    LP.lam_init = lam_init
    lb_ = P.sbuf("lamb", [128, 256])
    P.dma("sp", lb_[:], pbc(C.lam_b, l * 256, 256), w=[lb_])
    pr = P.sbuf("lampr", [128, 2, 64])
    lb4 = lb_[:].rearrange("p (a b d) -> p a b d", a=2, b=2)
    P.op("dve", lambda e: e.tensor_tensor(out=pr[:], in0=lb4[:, :, 0, :], in1=lb4[:, :, 1, :], op=ALU.mult),
         r=[lb_], w=[pr])
    s2 = P.sbuf("lams2", [128, 2])
    P.op("dve", lambda e: e.tensor_reduce(out=s2[:], in_=pr[:], axis=AX.X, op=ALU.add), r=[pr], w=[s2])
    P.op("act", lambda e: e.activation(out=s2[:], in_=s2[:], func=AF.Exp), r=[s2], w=[s2])
    LP.nlam = P.sbuf("nlam", [128, 1])
    P.op("dve", lambda e: e.scalar_tensor_tensor(out=LP.nlam[:], in0=s2[:, 1:2], scalar=-lam_init, in1=s2[:, 0:1],
                                                 op0=ALU.add, op1=ALU.subtract), r=[s2], w=[LP.nlam])
    LP.subln = P.sbuf("subln", [128, 1])
    col_load(P, LP.subln[:], C.subln_b, l * 128, 128, [LP.subln])
    P.op("dve", lambda e: e.tensor_scalar(out=LP.subln[:], in0=LP.subln[:], scalar1=(1.0 - lam_init), scalar2=None,
                                          op0=ALU.mult), r=[LP.subln], w=[LP.subln])
    LP.convw = P.sbuf("convw", [128, 4, 8])
    for tap in range(4):
        col_load(P, LP.convw[:, tap, :], C.conv_w, (l * 4 + tap) * 1024, 1024, [LP.convw])
    LP.convb = P.sbuf("convb", [128, 8])
    col_load(P, LP.convb[:], C.conv_b, l * 1024, 1024, [LP.convb])
    LP.brg = P.sbuf("brg", [128, 2, 8])
    LP.big = P.sbuf("big", [128, 2, 8])
    lam = P.sbuf("lrulam", [128, 2, 8])
    for d in range(2):
        col_load(P, LP.brg[:, d, :], C.b_rg, (l * 2 + d) * 1024, 1024, [LP.brg])
        col_load(P, LP.big[:, d, :], C.b_ig, (l * 2 + d) * 1024, 1024, [LP.big])
        col_load(P, lam[:, d, :], C.lru_lambda, (l * 2 + d) * 1024, 1024, [lam])
    P.op("act", lambda e: e.activation(out=lam[:], in_=lam[:], func=AF.Exp, scale=-1.0), r=[lam], w=[lam])
    P.op("act", lambda e: e.activation(out=lam[:], in_=lam[:], func=AF.Ln, bias=1.0, scale=1.0), r=[lam], w=[lam])
    LP.sp8 = P.sbuf("sp8", [128, 2, 8])
    LP.sp16 = P.sbuf("sp16", [128, 2, 8])
    P.op("dve", lambda e: e.tensor_scalar(out=LP.sp8[:], in0=lam[:], scalar1=-8.0, scalar2=None, op0=ALU.mult),
         r=[lam], w=[LP.sp8])
    P.op("dve", lambda e: e.tensor_scalar(out=LP.sp16[:], in0=lam[:], scalar1=-16.0, scalar2=None, op0=ALU.mult),
         r=[lam], w=[LP.sp16])
    LP.lb = P.sbuf("lb", [128, 2, 8])
    LP.oml = P.sbuf("oml", [128, 2, 8])
    LP.noml = P.sbuf("noml", [128, 2, 8])
    if l == 0:
        P.op("pool", lambda e: e.memset(LP.lb[:], 0.0), w=[LP.lb])
    else:
        d0 = P.sbuf("lbd0", [128, 2, 8])
        for d in range(2):
            col_load(P, d0[:, d, :], C.lb_d, (0 * 2 + d) * 1024, 1024, [d0])
            col_load(P, LP.lb[:, d, :], C.lb_d, (1 * 2 + d) * 1024, 1024, [LP.lb])
        P.op("dve", lambda e: e.tensor_tensor(out=LP.lb[:], in0=LP.lb[:], in1=d0[:], op=ALU.subtract),
             r=[LP.lb, d0], w=[LP.lb])
        P.op("act", lambda e: e.activation(out=LP.lb[:], in_=LP.lb[:], func=AF.Sigmoid), r=[LP.lb], w=[LP.lb])
    P.op("dve", lambda e: e.tensor_scalar(out=LP.oml[:], in0=LP.lb[:], scalar1=-1.0, scalar2=1.0, op0=ALU.mult,
                                          op1=ALU.add), r=[LP.lb], w=[LP.oml])
    P.op("dve", lambda e: e.tensor_scalar(out=LP.noml[:], in0=LP.oml[:], scalar1=-1.0, scalar2=None, op0=ALU.mult),
         r=[LP.oml], w=[LP.noml])
    LP.wr = P.sbuf("wr", [128, 16, NE])
    P.dma("sp", LP.wr[:], C.w_router[:, :].rearrange("(c p) e -> p c e", p=128), w=[LP.wr])
    LP.br = P.sbuf("br", [128, NE])
    P.dma("sp", LP.br[:], pbc(C.b_router, 0, NE), w=[LP.br])
    LP.modP = P.sbuf("modP", [128, 2, 6, 16])
    LP.s1 = P.sbuf("s1", [128, 2, 16])
    LP.s2 = P.sbuf("s2", [128, 2, 16])
    return LP


def stage_mod(P, C, l, LP):
    with P.scope():
        cc = P.sbuf("cc", [128, 16, 2])
        col_load(P, cc[:, :, 0], C.c, 0, D, [cc])
        col_load(P, cc[:, :, 1], C.c_ctx, 0, D, [cc])
        scT = P.sbuf("scT", [128, 16, 2], BF16)
        P.op("act", lambda e: e.activation(out=scT[:], in_=cc[:], func=AF.Silu), r=[cc], w=[scT])
        bada = P.sbuf("bada", [2, 6 * D])
        P.dma("sp", bada[:], pbc(C.b_ada, l * 6 * D, 6 * D, parts=2), w=[bada])
        modsb = P.sbuf("modsb", [2, 6 * D])
        wsrc = C.w_ada[l, :, :].rearrange("(c p) n -> p c n", p=128)
        W = [P.sbuf(f"wada{i}", [128, 16, 512], BF16) for i in range(2)]
        ps = [P.psum(f"psmod{i}") for i in range(2)]
        for g in range(24):
            wt = W[g % 2]
            pt = ps[g % 2]
            P.dma("pool", wt[:], wsrc[:, :, g * 512:(g + 1) * 512], w=[wt])
            for c in range(16):
                P.op("pe", lambda e, c=c, wt=wt, pt=pt: e.matmul(pt[0:2, :], lhsT=scT[:, c, :], rhs=wt[:, c, :],
                                                               start=(c == 0), stop=(c == 15)),
                     r=[scT, wt], w=[pt])
            P.op("dve", lambda e, g=g, pt=pt: e.tensor_tensor(out=modsb[:, g * 512:(g + 1) * 512], in0=pt[0:2, :],
                                                              in1=bada[:, g * 512:(g + 1) * 512], op=ALU.add),
                 r=[pt, bada], w=[modsb])
        P.dma("sp", C.MOD[:, :], modsb[:], r=[modsb])
    for r in range(2):
        for k in range(6):
            col_load(P, LP.modP[:, r, k, :], C.MOD, r * 6 * D + k * D, D, [LP.modP])
    for (s, n, k) in ((LP.s1, LP.n1, 1), (LP.s2, LP.n2, 4)):
        for r in range(2):
            P.op("dve", lambda e, s=s, n=n, k=k, r=r: e.scalar_tensor_tensor(
                out=s[:, r, :], in0=LP.modP[:, r, k, :], scalar=1.0, in1=n[:], op0=ALU.add, op1=ALU.mult),
                r=[LP.modP, n], w=[s])


def norm_tile(P, xt, xn_out_dtype_tile, sq, ss, n):
    P.op("act", lambda e: e.activation(out=sq[:], in_=xt[:], func=AF.Square), r=[xt], w=[sq])
    P.op("dve", lambda e: e.tensor_reduce(out=ss[:], in_=sq[:], axis=AX.X, op=ALU.add), r=[sq], w=[ss])
    P.op("act", lambda e: e.activation(out=ss[:], in_=ss[:], func=AF.Sqrt, scale=1.0 / n, bias=EPS), r=[ss], w=[ss])
    P.op("dve", lambda e: e.reciprocal(out=ss[:], in_=ss[:]), r=[ss], w=[ss])
    P.op("dve", lambda e: e.tensor_scalar(out=xn_out_dtype_tile[:], in0=xt[:], scalar1=ss[:, 0:1], scalar2=None,
                                          op0=ALU.mult), r=[xt, ss], w=[xn_out_dtype_tile])


def stage_norm_T(P, C, l, LP):
    hT = P.sbuf("hT", [128, 16, T], BF16)
    hT_tok = [Tok(f"hT{t}") for t in range(NT)]
    with P.scope():
        xt = [P.sbuf(f"xt{i}", [128, D]) for i in range(2)]
        sq = P.sbuf("sq", [128, D])
        xn = [P.sbuf(f"xn{i}", [128, D], BF16) for i in range(2)]
        ss = [P.sbuf(f"ss{i}", [128, 1]) for i in range(2)]
        pst = [P.psum(f"pst{i}", [128, 1024], BF16) for i in range(2)]
        P.dma("sp", xt[0][:], C.XRES[0:128, :], w=[xt[0]])
        for t in range(NT):
            if t + 1 < NT:
                P.dma("sp", xt[(t + 1) % 2][:], C.XRES[(t + 1) * 128:(t + 2) * 128, :], w=[xt[(t + 1) % 2]])
            X, XN, SS = xt[t % 2], xn[t % 2], ss[t % 2]
            norm_tile(P, X, XN, sq, SS, D)
            r = 1 if t < 2 else 0
            for c4 in range(4):
                pt = pst[c4 % 2]
                for j in range(4):
                    c = c4 * 4 + j
                    P.op("pe", lambda e, c=c, j=j, pt=pt, XN=XN: e.transpose(
                        out=pt[:, j * 128:(j + 1) * 128], in_=XN[:, c * 128:(c + 1) * 128], identity=C.ident[:]),
                        r=[XN, C.ident], w=[pt])
                for j in range(4):
                    c = c4 * 4 + j
                    eng = "act" if j % 2 == 0 else "dve"
                    if eng == "act":
                        P.op("act", lambda e, c=c, j=j, pt=pt, t=t, r=r: e.activation(
                            out=hT[:, c, t * 128:(t + 1) * 128], in_=pt[:, j * 128:(j + 1) * 128], func=AF.Identity,
                            scale=LP.s1[:, r, c:c + 1], bias=LP.modP[:, r, 0, c:c + 1]),
                            r=[pt, LP.s1, LP.modP], w=[hT_tok[t]])
                    else:
                        P.op("dve", lambda e, c=c, j=j, pt=pt, t=t, r=r: e.tensor_scalar(
                            out=hT[:, c, t * 128:(t + 1) * 128], in0=pt[:, j * 128:(j + 1) * 128],
                            scalar1=LP.s1[:, r, c:c + 1], scalar2=LP.modP[:, r, 0, c:c + 1], op0=ALU.mult,
                            op1=ALU.add), r=[pt, LP.s1, LP.modP], w=[hT_tok[t]])
    return hT, hT_tok


def proj_groups():
    g = []
    g += [("aq", 0), ("aq", 1), ("akv", 0)]
    g += [("bq", 0), ("bq", 1), ("bk", 0), ("bk", 1), ("bv", 0), ("bv", 1)]
    g += [("cx", 0), ("cx", 1), ("cy", 0), ("cy", 1)]
    g += [("dq", 0), ("dq", 1), ("dff", 0), ("dff", 1), ("dfb", 0), ("dfb", 1)]
    g += [("di", 0), ("di", 1), ("dg", 0), ("dg", 1)]
    g += [("gt", i) for i in range(16)]
    return g


def stage_proj(P, C, l, LP, hT, hT_tok):
    groups = proj_groups()
    assert len(groups) * 512 == IN_W
    with P.scope():
        wsrc = C.w_in[l, :, :].rearrange("(c p) n -> p c n", p=128)
        W = [P.sbuf(f"win{i}", [128, 16, 512], BF16) for i in range(3)]
        ps = [P.psum(f"psproj{i}") for i in range(5)]
        pst = [P.psum(f"pstq{i}", [128, 1024], BF16) for i in range(2)]
        ca = [P.sbuf(f"ca{i}", [128, 128]) for i in range(2)]
        sa = [P.sbuf(f"sa{i}", [128, 128]) for i in range(2)]
        cb = [P.sbuf(f"cb{i}", [128, 64]) for i in range(2)]
        sb = [P.sbuf(f"sb{i}", [128, 64]) for i in range(2)]
        sq = P.sbuf("qsq", [128, 512])
        ss = P.sbuf("qss", [128, 8])
        qn = P.sbuf("qn", [128, 512])
        t1 = P.sbuf("qt1", [128, 512])
        t2 = P.sbuf("qt2", [128, 512])
        qb = [P.sbuf(f"qb{i}", [128, 512], BF16) for i in range(2)]
        qT = [P.sbuf(f"qT{i}", [128, 512], BF16) for i in range(2)]
        ob = [P.sbuf(f"ob{i}", [128, 512], BF16) for i in range(3)]
        of = [P.sbuf(f"of{i}", [128, 512], F32) for i in range(2)]
        gl = [P.sbuf(f"gl{i}", [128, 512], F32) for i in range(2)]
        cnt = {"ps": 0, "ob": 0, "of": 0, "qb": 0, "pst": 0, "qT": 0, "rope": 0}

        def nxt(key, lst):
            i = cnt[key]
            cnt[key] += 1
            return lst[i % len(lst)]

        def qk_epi(pt, t, ncomp, dim, gain, cosT, sinT, dst, dst_h0, nheads_out):
            w = ncomp * dim
            P.op("act", lambda e: e.activation(out=sq[:, :w], in_=pt[:, :w], func=AF.Square), r=[pt], w=[sq])
            P.op("dve", lambda e: e.tensor_reduce(out=ss[:, :ncomp], in_=sq[:, :w].rearrange("p (h d) -> p h d", d=dim),
                                                  axis=AX.X, op=ALU.add), r=[sq], w=[ss])
            P.op("act", lambda e: e.activation(out=ss[:, :ncomp], in_=ss[:, :ncomp], func=AF.Sqrt, scale=1.0 / dim,
                                               bias=EPS), r=[ss], w=[ss])
            P.op("dve", lambda e: e.reciprocal(out=ss[:, :ncomp], in_=ss[:, :ncomp]), r=[ss], w=[ss])
            v3 = lambda tl: tl[:, :w].rearrange("p (h d) -> p h d", d=dim)
            P.op("dve", lambda e: e.tensor_tensor(out=v3(qn), in0=v3(pt), in1=bc_last(ss[:, :ncomp], dim),
                                                  op=ALU.mult), r=[pt, ss], w=[qn])
            QB = nxt("qb", qb)
            if t >= 2:
                P.op("pool", lambda e: e.tensor_tensor(out=v3(qn), in0=v3(qn), in1=bc_mid(gain[:, :dim], ncomp),
                                                       op=ALU.mult), r=[qn, gain], w=[qn])
                hd = dim // 4
                ng = w // (2 * hd)
                v4 = lambda tl: tl[:, :w].rearrange("p (g s i) -> p g s i", s=2, i=hd)
                def tb(tab, s):
                    b = tab[:, :].rearrange("p (g s i) -> p g s i", s=2, i=hd)[:, :, s, :]
                    return mk(b, [b.ap[0], [0, ncomp], b.ap[1], b.ap[2]])
                q5 = lambda tl, s: tl[:, :w].rearrange("p (h g s i) -> p h g s i", h=ncomp, s=2, i=hd)[:, :, :, s, :]
                P.op("dve", lambda e: e.tensor_tensor(out=v3(t1), in0=v3(qn), in1=bc_mid(cosT[:, :dim], ncomp),
                                                      op=ALU.mult), r=[qn, cosT], w=[t1])
                for s in range(2):
                    P.op("pool", lambda e, s=s: e.tensor_tensor(out=q5(t2, s), in0=q5(qn, 1 - s), in1=tb(sinT, s),
                                                                op=ALU.mult), r=[qn, sinT], w=[t2])
                P.op("dve", lambda e: e.tensor_tensor(out=QB[:, :w], in0=t1[:, :w], in1=t2[:, :w], op=ALU.add),
                     r=[t1, t2], w=[QB])
            else:
                P.op("pool", lambda e: e.tensor_tensor(out=v3(QB), in0=v3(qn), in1=bc_mid(gain[:, :dim], ncomp),
                                                       op=ALU.mult), r=[qn, gain], w=[QB])
            nblk = w // 128
            PT = nxt("pst", pst)
            for j in range(nblk):
                P.op("pe", lambda e, j=j: e.transpose(out=PT[:, j * 128:(j + 1) * 128],
                                                      in_=QB[:, j * 128:(j + 1) * 128], identity=C.ident[:]),
                     r=[QB, C.ident], w=[PT])
            QT = nxt("qT", qT)
            P.op("act", lambda e: e.activation(out=QT[:, :w], in_=PT[:, :w], func=AF.Copy), r=[PT], w=[QT])
            dsta = bass.AP(dst.t, dst_h0 * 128 * T + t * 128, [[T, 128], [128 * T, nblk], [1, 128]])
            P.dma("sp", dsta, QT[:, :w].rearrange("p (h q) -> p h q", q=128), r=[QT])

        for gi, (kind, idx) in enumerate(groups):
            wt = W[gi % 3]
            P.dma("pool", wt[:], wsrc[:, :, gi * 512:(gi + 1) * 512], w=[wt])
            tokmajor = kind in ("aq", "akv", "bq", "bk", "bv", "di", "dg")
            if tokmajor:
                for t in range(NT):
                    pt = nxt("ps", ps)
                    for c in range(16):
                        P.op("pe", lambda e, c=c, t=t, pt=pt, wt=wt: e.matmul(
                            pt[:], lhsT=hT[:, c, t * 128:(t + 1) * 128], rhs=wt[:, c, :], start=(c == 0),
                            stop=(c == 15)), r=[hT_tok[t], wt], w=[pt])
                    lat = t >= 2
                    if lat and kind in ("aq", "akv", "bq", "bk"):
                        k = cnt["rope"] % 2
                        cnt["rope"] += 1
                        r0 = (t - 2) * 128
                        if kind in ("aq", "akv"):
                            P.dma("sp", ca[k][:], C.ropeA_c[r0:r0 + 128, :], w=[ca[k]])
                            P.dma("sp", sa[k][:], C.ropeA_s[r0:r0 + 128, :], w=[sa[k]])
                            cT, sT = ca[k], sa[k]
                        else:
                            P.dma("sp", cb[k][:], C.ropeB_c[r0:r0 + 128, :], w=[cb[k]])
                            P.dma("sp", sb[k][:], C.ropeB_s[r0:r0 + 128, :], w=[sb[k]])
                            cT, sT = cb[k], sb[k]
                    else:
                        cT = sT = None
                    if kind == "aq":
                        qk_epi(pt, t, 4, 128, LP.qn_a, cT, sT, C.QAT, idx * 4, 4)
                    elif kind == "akv":
                        qk_epi(pt, t, 2, 128, LP.kn_a, cT, sT, C.KAT, 0, 2)
                        O = nxt("ob", ob)
                        P.op("act", lambda e, O=O, pt=pt: e.activation(out=O[:, :256], in_=pt[:, 256:512],
                                                                       func=AF.Copy), r=[pt], w=[O])
                        P.dma("sp", C.VA[t * 128:(t + 1) * 128, :], O[:, :256], r=[O])
                    elif kind == "bq":
                        qk_epi(pt, t, 8, 64, LP.qn_b, cT, sT, C.QBT, idx * 4, 4)
                    elif kind == "bk":
                        qk_epi(pt, t, 8, 64, LP.kn_b, cT, sT, C.KBT, idx * 4, 4)
                    else:
                        O = nxt("ob", ob)
                        dst = {"bv": C.VB, "di": C.VD, "dg": C.SGD}[kind]
                        fn = AF.Silu if kind == "dg" else AF.Copy
                        P.op("act", lambda e, O=O, pt=pt, fn=fn: e.activation(out=O[:], in_=pt[:], func=fn),
                             r=[pt], w=[O])
                        P.dma("sp", dst[t * 128:(t + 1) * 128, idx * 512:(idx + 1) * 512], O[:], r=[O])
            else:
                for cbk in range(4):
                    for (t0, tn) in TG:
                        pt = nxt("ps", ps)
                        toks = [hT_tok[t] for t in range(t0 // 128, (t0 + tn) // 128)]
                        for c in range(16):
                            P.op("pe", lambda e, c=c, pt=pt, wt=wt, cbk=cbk, t0=t0, tn=tn: e.matmul(
                                pt[:, :tn], lhsT=wt[:, c, cbk * 128:(cbk + 1) * 128], rhs=hT[:, c, t0:t0 + tn],
                                start=(c == 0), stop=(c == 15)), r=toks + [wt], w=[pt])
                        row = idx * 512 + cbk * 128
                        if kind in ("cx", "dff", "dfb"):
                            O = nxt("of", of)
                            P.op("act", lambda e, O=O, pt=pt, tn=tn: e.activation(out=O[:, :tn], in_=pt[:, :tn],
                                                                                  func=AF.Copy), r=[pt], w=[O])
                            if kind == "cx":
                                dst = C.CXT[row:row + 128, t0:t0 + tn]
                            else:
                                dst = C.ZF[0 if kind == "dff" else 1, row:row + 128, t0:t0 + tn]
                            P.dma("sp", dst, O[:, :tn], r=[O])
                        elif kind == "cy":
                            G = nxt("of", gl)
                            O = nxt("ob", ob)
                            P.op("act", lambda e, G=G, pt=pt, tn=tn: e.activation(out=G[:, :tn], in_=pt[:, :tn],
                                                                                  func=AF.Square), r=[pt], w=[G])
                            P.op("dve", lambda e, G=G, tn=tn: e.tensor_scalar(
                                out=G[:, :tn], in0=G[:, :tn], scalar1=0.044715 * 1.5957691216, scalar2=1.5957691216,
                                op0=ALU.mult, op1=ALU.add), r=[G], w=[G])
                            P.op("dve", lambda e, G=G, pt=pt, tn=tn: e.tensor_tensor(
                                out=G[:, :tn], in0=G[:, :tn], in1=pt[:, :tn], op=ALU.mult), r=[G, pt], w=[G])
                            P.op("act", lambda e, G=G, tn=tn: e.activation(out=G[:, :tn], in_=G[:, :tn],
                                                                           func=AF.Sigmoid), r=[G], w=[G])
                            P.op("dve", lambda e, G=G, O=O, pt=pt, tn=tn: e.tensor_tensor(
                                out=O[:, :tn], in0=G[:, :tn], in1=pt[:, :tn], op=ALU.mult), r=[G, pt], w=[O])
                            P.dma("sp", C.GCY[row:row + 128, t0:t0 + tn], O[:, :tn], r=[O])
                        else:
                            O = nxt("ob", ob)
                            fn = AF.Sigmoid if kind == "gt" else AF.Copy
                            P.op("act", lambda e, O=O, pt=pt, tn=tn, fn=fn: e.activation(
                                out=O[:, :tn], in_=pt[:, :tn], func=fn), r=[pt], w=[O])
                            dst = (C.SGT if kind == "gt" else C.DQT)[row:row + 128, t0:t0 + tn]
                            P.dma("sp", dst, O[:, :tn], r=[O])


def stage_attn_a(P, C, l, LP):
    scale = 128 ** -0.5
    with P.scope():
        QA = P.sbuf("QA", [128, 8, T], BF16)
        KA = P.sbuf("KA", [128, 2, T], BF16)
        VAs = P.sbuf("VAs", [128, NT, 256], BF16)
        OA = P.sbuf("OA", [128, 8, T], BF16)
        P.dma("sp", QA[:], C.QAT[:, :, :].rearrange("h d t -> d h t"), w=[QA])
        P.dma("sp", KA[:], C.KAT[:, :, :].rearrange("h d t -> d h t"), w=[KA])
        P.dma("sp", VAs[:], C.VA[:, :].rearrange("(n p) v -> p n v", p=128), w=[VAs])
        pss = [P.psum(f"pss{i}") for i in range(2)]
        pso = [P.psum(f"pso{i}") for i in range(2)]
        psz = [P.psum(f"psz{i}") for i in range(2)]
        pT = [P.sbuf(f"pT{i}", [128, 512], BF16) for i in range(3)]
        zs = P.sbuf("zs", [128, 512])
        ot = P.sbuf("ot", [128, 512])
        OA_tok = Tok("OA")
        steps = []
        it = 0
        for qb in range(NT):
            if qb < 2:
                kbs = [(0, None), (1, None)]
            else:
                n = qb - 2
                kbs = [(0, None), (1, None)]
                if n >= 1:
                    kbs.append((qb - 1, C.m_ge))
                kbs.append((qb, None))
                if n <= 14:
                    kbs.append((qb + 1, C.m_le))
            for hk in range(2):
                po, pz = pso[it % 2], psz[it % 2]
                it += 1
                for ki, (kb, mask) in enumerate(kbs):
                    steps.append(dict(qb=qb, hk=hk, kb=kb, mask=mask, po=po, pz=pz, first=(ki == 0),
                                      last=(ki == len(kbs) - 1)))
        for i, st in enumerate(steps):
            st["psx"] = pss[i % 2]
            st["pt"] = pT[i % 3]

        def emit_s(st):
            psx, kb, hk, qb = st["psx"], st["kb"], st["hk"], st["qb"]
            rhs_q = QA[:, 4 * hk:4 * hk + 4, qb * 128:(qb + 1) * 128]
            P.op("pe", lambda e: e.matmul(psx[:].rearrange("p (h q) -> p h q", h=4),
                                          lhsT=KA[:, hk, kb * 128:(kb + 1) * 128], rhs=rhs_q, start=True, stop=True),
                 r=[KA, QA], w=[psx])

        def emit_rest(st):
            psx, pt_, kb, hk, qb, mask, po, pz = (st["psx"], st["pt"], st["kb"], st["hk"], st["qb"], st["mask"],
                                                  st["po"], st["pz"])
            f, l_ = st["first"], st["last"]
            P.op("act", lambda e: e.activation(out=pt_[:], in_=psx[:], func=AF.Exp, scale=scale), r=[psx], w=[pt_])
            if mask is not None:
                P.op("pool", lambda e: e.tensor_tensor(
                    out=pt_[:].rearrange("p (h q) -> p h q", h=4), in0=pt_[:].rearrange("p (h q) -> p h q", h=4),
                    in1=bc_mid(mask[:, :], 4), op=ALU.mult), r=[pt_, mask], w=[pt_])
            P.op("pe", lambda e: e.matmul(po[:], lhsT=VAs[:, kb, hk * 128:(hk + 1) * 128], rhs=pt_[:], start=f,
                                          stop=l_), r=[VAs, pt_], w=[po])
            P.op("pe", lambda e: e.matmul(pz[:], lhsT=C.ones[:], rhs=pt_[:], start=f, stop=l_),
                 r=[C.ones, pt_], w=[pz])
            if l_:
                P.op("dve", lambda e: e.tensor_tensor(
                    out=zs[:].rearrange("p (h q) -> p h q", h=4), in0=pz[:].rearrange("p (h q) -> p h q", h=4),
                    in1=bc_last(LP.esink[:, 4 * hk:4 * hk + 4], 128), op=ALU.add), r=[pz, LP.esink], w=[zs])
                P.op("dve", lambda e: e.reciprocal(out=zs[:], in_=zs[:]), r=[zs], w=[zs])
                P.op("dve", lambda e: e.tensor_tensor(
                    out=OA[:, 4 * hk:4 * hk + 4, qb * 128:(qb + 1) * 128],
                    in0=po[:].rearrange("p (h q) -> p h q", h=4), in1=zs[:].rearrange("p (h q) -> p h q", h=4),
                    op=ALU.mult), r=[po, zs], w=[OA_tok])

        emit_s(steps[0])
        for i, st in enumerate(steps):
            if i + 1 < len(steps):
                emit_s(steps[i + 1])
            emit_rest(st)
        P.dma("sp", C.OT[0, :, :].rearrange("(h d) t -> d h t", d=128), OA[:], r=[OA_tok])


def stage_attn_b(P, C, l, LP):
    scale = 64 ** -0.5
    with P.scope():
        QB_ = [P.sbuf(f"QBh{i}", [128, T], BF16) for i in range(2)]
        KB_ = [P.sbuf(f"KBh{i}", [128, T], BF16) for i in range(2)]
        VB_ = [P.sbuf(f"VBh{i}", [128, NT, 128], BF16) for i in range(2)]
        OB_ = [P.sbuf(f"OBh{i}", [128, T], BF16) for i in range(2)]
        pss = [P.psum(f"bpss{i}") for i in range(2)]
        pso = [P.psum(f"bpso{i}") for i in range(2)]
        psz = [P.psum(f"bpsz{i}") for i in range(2)]
        psn = P.psum("bpsn")
        pT = [P.sbuf(f"bpT{i}", [128, 512], BF16) for i in range(3)]
        r1 = P.sbuf("br1", [128, 512])
        r2 = P.sbuf("br2", [128, 512])
        o1 = P.sbuf("bo1", [128, 512])
        o2 = P.sbuf("bo2", [128, 512])
        osq = P.sbuf("bosq", [128, 512], BF16)
        ip = 0

        def load(h):
            k = h % 2
            P.dma("sp", QB_[k][:], C.QBT[h, :, :], w=[QB_[k]])
            P.dma("sp", KB_[k][:], C.KBT[h, :, :], w=[KB_[k]])
            P.dma("sp", VB_[k][:], C.VB[:, h * 128:(h + 1) * 128].rearrange("(n p) v -> p n v", p=128), w=[VB_[k]])

        load(0)
        for h in range(8):
            if h + 1 < 8:
                load(h + 1)
            Q, K, V, O = QB_[h % 2], KB_[h % 2], VB_[h % 2], OB_[h % 2]
            steps = []
            for (q0, qn_, nkb) in [(0, 256, 2)] + [(LC + i * 512, 512, NT) for i in range(4)]:
                for c in range(2):
                    for kb in range(nkb):
                        steps.append(dict(q0=q0, qn=qn_, c=c, kb=kb, first=(kb == 0), last=(kb == nkb - 1),
                                          fin=(c == 1 and kb == nkb - 1)))
            for st in steps:
                st["psx"] = pss[ip % 2]
                st["pt"] = pT[ip % 3]
                ip += 1

            def emit_s(st, K=K, Q=Q):
                psx, c, kb, q0, qn_ = st["psx"], st["c"], st["kb"], st["q0"], st["qn"]
                P.op("pe", lambda e: e.matmul(psx[:, :qn_], lhsT=K[c * 64:(c + 1) * 64, kb * 128:(kb + 1) * 128],
                                              rhs=Q[c * 64:(c + 1) * 64, q0:q0 + qn_], start=True, stop=True),
                     r=[K, Q], w=[psx])

            def emit_rest(st, V=V, O=O):
                psx, pt_, c, kb, q0, qn_ = st["psx"], st["pt"], st["c"], st["kb"], st["q0"], st["qn"]
                f, l_ = st["first"], st["last"]
                po, pz = pso[c], psz[c]
                P.op("act", lambda e: e.activation(out=pt_[:, :qn_], in_=psx[:, :qn_], func=AF.Exp, scale=scale),
                     r=[psx], w=[pt_])
                P.op("pe", lambda e: e.matmul(po[:, :qn_], lhsT=V[:, kb, :], rhs=pt_[:, :qn_], start=f, stop=l_),
                     r=[V, pt_], w=[po])
                P.op("pe", lambda e: e.matmul(pz[:, :qn_], lhsT=C.ones[:], rhs=pt_[:, :qn_], start=f, stop=l_),
                     r=[C.ones, pt_], w=[pz])
                if not st["fin"]:
                    return
                P.op("dve", lambda e: e.reciprocal(out=r1[:, :qn_], in_=psz[0][:, :qn_]), r=[psz[0]], w=[r1])
                P.op("dve", lambda e: e.reciprocal(out=r2[:, :qn_], in_=psz[1][:, :qn_]), r=[psz[1]], w=[r2])
                P.op("dve", lambda e: e.tensor_tensor(out=o1[:, :qn_], in0=pso[0][:, :qn_], in1=r1[:, :qn_],
                                                      op=ALU.mult), r=[pso[0], r1], w=[o1])
                P.op("dve", lambda e: e.tensor_tensor(out=o2[:, :qn_], in0=pso[1][:, :qn_], in1=r2[:, :qn_],
                                                      op=ALU.mult), r=[pso[1], r2], w=[o2])
                P.op("dve", lambda e: e.scalar_tensor_tensor(
                    out=o1[:, :qn_], in0=o2[:, :qn_], scalar=LP.nlam[:, 0:1], in1=o1[:, :qn_], op0=ALU.mult,
                    op1=ALU.add), r=[o1, o2, LP.nlam], w=[o1])
                P.op("act", lambda e: e.activation(out=osq[:, :qn_], in_=o1[:, :qn_], func=AF.Square),
                     r=[o1], w=[osq])
                P.op("pe", lambda e: e.matmul(psn[:, :qn_], lhsT=C.ones[:], rhs=osq[:, :qn_], start=True, stop=True),
                     r=[C.ones, osq], w=[psn])
                P.op("act", lambda e: e.activation(out=r1[:, :qn_], in_=psn[:, :qn_], func=AF.Sqrt,
                                                   scale=1.0 / 128, bias=EPS), r=[psn], w=[r1])
                P.op("dve", lambda e: e.reciprocal(out=r1[:, :qn_], in_=r1[:, :qn_]), r=[r1], w=[r1])
                P.op("dve", lambda e: e.scalar_tensor_tensor(
                    out=O[:, q0:q0 + qn_], in0=o1[:, :qn_], scalar=LP.subln[:, 0:1], in1=r1[:, :qn_], op0=ALU.mult,
                    op1=ALU.mult), r=[o1, r1, LP.subln], w=[O])

            emit_s(steps[0])
            for i, st in enumerate(steps):
                if i + 1 < len(steps):
                    emit_s(steps[i + 1])
                emit_rest(st)
            P.dma("sp", C.OT[1, h * 128:(h + 1) * 128, :], O[:], r=[O])


def rev(ap2):
    (ps, pn), (s, n) = ap2.ap
    return bass.AP(ap2.tensor, ap2.offset + s * (n - 1), [[ps, pn], [-s, n]])


def stage_rglru(P, C, l, LP):
    with P.scope():
        wr = P.sbuf("wrg", [128, 16, 128], BF16)
        wi = P.sbuf("wig", [128, 16, 128], BF16)
        P.dma("pool", wr[:], C.w_rg[l, :, :, :, :].rearrange("d b i o -> i (d b) o"), w=[wr])
        P.dma("pool", wi[:], C.w_ig[l, :, :, :, :].rearrange("d b i o -> i (d b) o"), w=[wi])
        x = P.sbuf("cx", [128, T])
        u = P.sbuf("cu", [128, T])
        ub = P.sbuf("cub", [128, T], BF16)
        rr = P.sbuf("crr", [128, T])
        ii = P.sbuf("cii", [128, T])
        aa = P.sbuf("caa", [128, T])
        vv = P.sbuf("cvv", [128, T])
        hh = [P.sbuf(f"chh{d}", [128, T]) for d in range(2)]
        gy = P.sbuf("cgy", [128, T], BF16)
        oo = P.sbuf("coo", [128, T], BF16)
        ps = [P.psum(f"cps{i}") for i in range(4)]
        ip = 0
        for ch in range(8):
            P.dma("sp", x[:], C.CXT[ch * 128:(ch + 1) * 128, :], w=[x])
            P.dma("sp", gy[:], C.GCY[ch * 128:(ch + 1) * 128, :], w=[gy])
            P.op("dve", lambda e, ch=ch: e.tensor_scalar(out=u[:], in0=x[:], scalar1=LP.convw[:, 2, ch:ch + 1],
                                                         scalar2=LP.convb[:, ch:ch + 1], op0=ALU.mult, op1=ALU.add),
                 r=[x, LP.convw, LP.convb], w=[u])
            for (s0, s1) in ((0, LC), (LC, T)):
                for tap in (0, 1, 3):
                    off = tap - 2
                    lo = s0 + max(0, -off)
                    hi = s1 - max(0, off)
                    P.op("dve", lambda e, ch=ch, tap=tap, lo=lo, hi=hi, off=off: e.scalar_tensor_tensor(
                        out=u[:, lo:hi], in0=x[:, lo + off:hi + off], scalar=LP.convw[:, tap, ch:ch + 1],
                        in1=u[:, lo:hi], op0=ALU.mult, op1=ALU.add), r=[x, u, LP.convw], w=[u])
            P.op("act", lambda e: e.activation(out=ub[:], in_=u[:], func=AF.Copy), r=[u], w=[ub])
            for d in range(2):
                for (t0, tn) in TG:
                    pr_, pi_ = ps[ip % 4], ps[(ip + 1) % 4]
                    ip += 2
                    P.op("pe", lambda e, pr_=pr_, d=d, ch=ch, t0=t0, tn=tn: e.matmul(
                        pr_[:, :tn], lhsT=wr[:, d * 8 + ch, :], rhs=ub[:, t0:t0 + tn], start=True, stop=True),
                        r=[wr, ub], w=[pr_])
                    P.op("pe", lambda e, pi_=pi_, d=d, ch=ch, t0=t0, tn=tn: e.matmul(
                        pi_[:, :tn], lhsT=wi[:, d * 8 + ch, :], rhs=ub[:, t0:t0 + tn], start=True, stop=True),
                        r=[wi, ub], w=[pi_])
                    P.op("act", lambda e, pr_=pr_, d=d, ch=ch, t0=t0, tn=tn: e.activation(
                        out=rr[:, t0:t0 + tn], in_=pr_[:, :tn], func=AF.Sigmoid, bias=LP.brg[:, d, ch:ch + 1]),
                        r=[pr_, LP.brg], w=[rr])
                    P.op("act", lambda e, pi_=pi_, d=d, ch=ch, t0=t0, tn=tn: e.activation(
                        out=ii[:, t0:t0 + tn], in_=pi_[:, :tn], func=AF.Sigmoid, bias=LP.big[:, d, ch:ch + 1]),
                        r=[pi_, LP.big], w=[ii])
                P.op("act", lambda e, d=d, ch=ch: e.activation(out=aa[:], in_=rr[:], func=AF.Exp,
                                                               scale=LP.sp8[:, d, ch:ch + 1]), r=[rr, LP.sp8], w=[aa])
                P.op("act", lambda e, d=d, ch=ch: e.activation(out=vv[:], in_=rr[:], func=AF.Exp,
                                                               scale=LP.sp16[:, d, ch:ch + 1]), r=[rr, LP.sp16], w=[vv])
                P.op("act", lambda e: e.activation(out=vv[:], in_=vv[:], func=AF.Sqrt, scale=-1.0, bias=1.0),
                     r=[vv], w=[vv])
                P.op("dve", lambda e: e.tensor_tensor(out=vv[:], in0=vv[:], in1=ii[:], op=ALU.mult), r=[vv, ii], w=[vv])
                P.op("dve", lambda e: e.tensor_tensor(out=vv[:], in0=vv[:], in1=u[:], op=ALU.mult), r=[vv, u], w=[vv])
                H = hh[d]
                if d == 0:
                    P.op("dve", lambda e, H=H: e.tensor_tensor_scan(out=H[:], data0=aa[:], data1=vv[:], initial=0.0,
                                                                    op0=ALU.mult, op1=ALU.add), r=[aa, vv], w=[H])
                else:
                    P.op("dve", lambda e, H=H: e.tensor_tensor_scan(
                        out=rev(H[:, 0:LC]), data0=rev(aa[:, 0:LC]), data1=rev(vv[:, 0:LC]), initial=0.0,
                        op0=ALU.mult, op1=ALU.add), r=[aa, vv], w=[H])
                    P.op("dve", lambda e, H=H: e.tensor_tensor_scan(
                        out=rev(H[:, LC:T]), data0=rev(aa[:, LC:T]), data1=rev(vv[:, LC:T]), initial=H[:, 0:1],
                        op0=ALU.mult, op1=ALU.add), r=[aa, vv, H], w=[H])
            P.op("dve", lambda e: e.tensor_tensor(out=hh[0][:], in0=hh[0][:], in1=hh[1][:], op=ALU.add),
                 r=[hh[0], hh[1]], w=[hh[0]])
            P.op("dve", lambda e: e.tensor_tensor(out=oo[:], in0=hh[0][:], in1=gy[:], op=ALU.mult),
                 r=[hh[0], gy], w=[oo])
            P.dma("sp", C.OT[2, ch * 128:(ch + 1) * 128, :], oo[:], r=[oo])


def stage_hgrn(P, C, l, LP):
    NCK = T // 64
    with P.scope():
        o1 = C.ones_f[:, 0:1]
        ones_bc = mk(o1, [o1.ap[0], [0, T]])
        mf = P.sbuf("mf", [64, 64], BF16)
        mb = P.sbuf("mb", [64, 64], BF16)
        P.op("dve", lambda e: e.tensor_copy(out=mf[:], in_=C.m_le[0:64, 0:64]), r=[C.m_le], w=[mf])
        P.op("dve", lambda e: e.tensor_copy(out=mb[:], in_=C.m_ge[0:64, 0:64]), r=[C.m_ge], w=[mb])
        z = P.sbuf("dz", [128, T])
        sg = P.sbuf("dsg", [128, T])
        kk = P.sbuf("dkk", [128, T])
        Bi = P.sbuf("dBi", [128, T])
        Be_ = P.sbuf("dBe", [128, T])
        E = P.sbuf("dE", [128, T])
        E2 = z
        qT = P.sbuf("dqT", [128, T], BF16)
        vt = P.sbuf("dvt", [64, NCK, 128], BF16)
        sgd = P.sbuf("dsgd", [64, NCK, 128], BF16)
        qt_ = [P.sbuf(f"dqt{d}", [128, T], BF16) for d in range(2)]
        kt_ = [P.sbuf(f"dkt{d}", [128, T], BF16) for d in range(2)]
        qh_ = [P.sbuf(f"dqh{d}", [128, T], BF16) for d in range(2)]
        kh_ = [P.sbuf(f"dkh{d}", [128, T], BF16) for d in range(2)]
        kx_ = [P.sbuf(f"dkx{d}", [128, T], BF16) for d in range(2)]
        dec = [P.sbuf(f"ddec{d}", [128, NCK]) for d in range(2)]
        Sf = [P.sbuf(f"dSf{d}", [128, 128]) for d in range(2)]
        Sb = [P.sbuf(f"dSb{d}", [128, 128], BF16) for d in range(2)]
        Od = [P.sbuf(f"dO{d}", [64, NCK, 128]) for d in range(2)]
        Od_tok = [[Tok(f"Od{d}_{c}") for c in range(NCK)] for d in range(2)]
        att = [[P.sbuf(f"datt{d}{i}", [64, 64], BF16) for i in range(2)] for d in range(2)]
        khs = [[P.sbuf(f"dkhs{d}{i}", [64, 128], BF16) for i in range(2)] for d in range(2)]
        ps_att = [P.psum(f"dpsatt{d}") for d in range(2)]
        for d in range(2):
            P.op("dve", lambda e, d=d: e.memset(ps_att[d][:], 0.0), w=[ps_att[d]])
        ps_kh = [P.psum(f"dpskh{d}", [128, 1024], BF16) for d in range(2)]
        ps_o = [P.psum(f"dpso{d}") for d in range(2)]
        ps_s = [P.psum(f"dpss{d}") for d in range(2)]
        osq = P.sbuf("dosq", [64, NCK, 128])
        oss = P.sbuf("doss", [64, NCK])
        oy = P.sbuf("doy", [64, NCK, 128], BF16)
        odT = P.sbuf("dodT", [128, T], BF16)

        c3 = lambda tl, off=0: tl[:, off:off + T].rearrange("p (c i) -> p c i", i=64)
        for h in range(8):
            P.dma("sp", qT[:], C.DQT[h * 128:(h + 1) * 128, :], w=[qT])
            P.dma("sp", vt[:], C.VD[:, h * 128:(h + 1) * 128].rearrange("(c p) v -> p c v", p=64), w=[vt])
            P.dma("sp", sgd[:], C.SGD[:, h * 128:(h + 1) * 128].rearrange("(c p) v -> p c v", p=64), w=[sgd])
            def gate_math(d, h):
                P.dma("sp", z[:], C.ZF[d, h * 128:(h + 1) * 128, :], w=[z])
                P.op("act", lambda e: e.activation(out=sg[:], in_=z[:], func=AF.Sigmoid), r=[z], w=[sg])
                P.op("dve", lambda e, d=d, h=h: e.tensor_scalar(
                    out=kk[:], in0=sg[:], scalar1=LP.noml[:, d, h:h + 1], scalar2=LP.oml[:, d, h:h + 1], op0=ALU.mult,
                    op1=ALU.add), r=[sg, LP.noml, LP.oml], w=[kk])
                P.op("act", lambda e, d=d, h=h: e.activation(out=sg[:], in_=sg[:], func=AF.Ln,
                                                             scale=LP.oml[:, d, h:h + 1], bias=LP.lb[:, d, h:h + 1]),
                     r=[sg, LP.oml, LP.lb], w=[sg])
                P.op("dve", lambda e: e.tensor_tensor_scan(out=Bi[:], data0=ones_bc, data1=sg[:],
                                                           initial=0.0, op0=ALU.mult, op1=ALU.add),
                     r=[C.ones_f, sg], w=[Bi])
                P.op("dve", lambda e: e.tensor_tensor(out=Be_[:], in0=Bi[:], in1=sg[:], op=ALU.subtract),
                     r=[Bi, sg], w=[Be_])
                Bx = Bi
                Bsrc = Bi if d == 0 else Be_
                Bv = c3(Bsrc)
                Bs1 = c3(Be_)[:, :, 0:1]
                Be = c3(Bi)[:, :, 63:64]
                c32 = lambda tl, off=0: tl[:, off:off + T].rearrange("p (c i) -> p c i", i=32)
                Bv32 = c32(Bsrc)
                Bref = c32(Bsrc)[:, :, 16:17]
                bcl = lambda a: mk(a, [a.ap[0], a.ap[1], [0, 64]])
                bcl32 = lambda a: mk(a, [a.ap[0], a.ap[1], [0, 32]])
                if d == 0:
                    P.op("dve", lambda e: e.tensor_tensor(out=c32(E), in0=Bv32, in1=bcl32(Bref), op=ALU.subtract),
                         r=[Bi, Be_], w=[E])
                else:
                    P.op("dve", lambda e: e.tensor_tensor(out=c32(E), in0=bcl32(Bref), in1=Bv32, op=ALU.subtract),
                         r=[Bi, Be_], w=[E])
                P.op("act", lambda e: e.activation(out=E[:], in_=E[:], func=AF.Exp), r=[E], w=[E])
                P.op("dve", lambda e, d=d: e.tensor_tensor(out=qt_[d][:], in0=qT[:], in1=E[:], op=ALU.mult),
                     r=[qT, E], w=[qt_[d]])
                P.op("dve", lambda e: e.reciprocal(out=E[:], in_=E[:]), r=[E], w=[E])
                P.op("dve", lambda e, d=d: e.tensor_tensor(out=kt_[d][:], in0=kk[:], in1=E[:], op=ALU.mult),
                     r=[kk, E], w=[kt_[d]])
                if d == 0:
                    P.op("dve", lambda e: e.tensor_tensor(out=c3(E), in0=Bv, in1=bcl(Bs1), op=ALU.subtract),
                         r=[Bi, Be_], w=[E])
                    P.op("dve", lambda e: e.tensor_tensor(out=c3(E2), in0=bcl(Be), in1=Bv, op=ALU.subtract),
                         r=[Bi, Be_], w=[E2])
                else:
                    P.op("dve", lambda e: e.tensor_tensor(out=c3(E), in0=bcl(Be), in1=Bv, op=ALU.subtract),
                         r=[Bi, Be_], w=[E])
                    P.op("dve", lambda e: e.tensor_tensor(out=c3(E2), in0=Bv, in1=bcl(Bs1), op=ALU.subtract),
                         r=[Bi, Be_], w=[E2])
                P.op("act", lambda e: e.activation(out=E[:], in_=E[:], func=AF.Exp), r=[E], w=[E])
                P.op("act", lambda e: e.activation(out=E2[:], in_=E2[:], func=AF.Exp), r=[E2], w=[E2])
                P.op("dve", lambda e, d=d: e.tensor_tensor(out=qh_[d][:], in0=qT[:], in1=E[:], op=ALU.mult),
                     r=[qT, E], w=[qh_[d]])
                P.op("dve", lambda e: e.reciprocal(out=E[:], in_=E[:]), r=[E], w=[E])
                P.op("dve", lambda e, d=d: e.tensor_tensor(out=kx_[d][:], in0=kk[:], in1=E[:], op=ALU.mult),
                     r=[kk, E], w=[kx_[d]])
                P.op("dve", lambda e, d=d: e.tensor_tensor(out=kh_[d][:], in0=kk[:], in1=E2[:], op=ALU.mult),
                     r=[kk, E2], w=[kh_[d]])
                P.op("dve", lambda e, d=d: e.tensor_tensor(out=dec[d][:].rearrange("p (c o) -> p c o", o=1), in0=Be,
                                                           in1=Bs1, op=ALU.subtract), r=[Bi, Be_], w=[dec[d]])
                P.op("act", lambda e, d=d: e.activation(out=dec[d][:], in_=dec[d][:], func=AF.Exp),
                     r=[dec[d]], w=[dec[d]])
                P.op("pool", lambda e, d=d: e.memset(Sf[d][:], 0.0), w=[Sf[d]])
                P.op("pool", lambda e, d=d: e.memset(Sb[d][:], 0.0), w=[Sb[d]])

            for d in range(2):
                gate_math(d, h)
            order = [list(range(NCK)), [3, 2, 1, 0] + list(range(NCK - 1, 3, -1))]
            for step in range(NCK):
                for d in range(2):
                    c = order[d][step]
                    sl = slice(c * 64, (c + 1) * 64)
                    A = att[d][step % 2]
                    KH = khs[d][step % 2]
                    mask = mf if d == 0 else mb
                    c0 = c * 64
                    for hb in range(2):
                        P.op("pe", lambda e, d=d, c0=c0, hb=hb: e.matmul(
                            ps_att[d][hb * 32:(hb + 1) * 32, hb * 32:(hb + 1) * 32],
                            lhsT=kt_[d][:, c0 + hb * 32:c0 + (hb + 1) * 32],
                            rhs=qt_[d][:, c0 + hb * 32:c0 + (hb + 1) * 32], start=True, stop=True),
                            r=[kt_[d], qt_[d]], w=[ps_att[d]])
                    jh, ih = (0, 1) if d == 0 else (1, 0)
                    P.op("pe", lambda e, d=d, c0=c0, jh=jh, ih=ih: e.matmul(
                        ps_att[d][jh * 32:(jh + 1) * 32, ih * 32:(ih + 1) * 32],
                        lhsT=kx_[d][:, c0 + jh * 32:c0 + (jh + 1) * 32],
                        rhs=qh_[d][:, c0 + ih * 32:c0 + (ih + 1) * 32], start=True, stop=True),
                        r=[kx_[d], qh_[d]], w=[ps_att[d]])
                    P.op("dve", lambda e, d=d, A=A, mask=mask: e.tensor_tensor(
                        out=A[:], in0=ps_att[d][0:64, 0:64], in1=mask[:], op=ALU.mult), r=[ps_att[d], mask], w=[A])
                    P.op("pe", lambda e, d=d, sl=sl: e.transpose(out=ps_kh[d][0:64, 0:128], in_=kh_[d][:, sl],
                                                                 identity=C.ident[:]),
                         r=[kh_[d], C.ident], w=[ps_kh[d]])
                    P.op("act", lambda e, d=d, KH=KH: e.activation(out=KH[:], in_=ps_kh[d][0:64, 0:128],
                                                                   func=AF.Copy), r=[ps_kh[d]], w=[KH])
                    P.op("pe", lambda e, d=d, A=A, c=c: e.matmul(ps_o[d][0:64, 0:128], lhsT=A[:], rhs=vt[:, c, :],
                                                                 start=True, stop=False), r=[A, vt], w=[ps_o[d]])
                    P.op("pe", lambda e, d=d, sl=sl: e.matmul(ps_o[d][0:64, 0:128], lhsT=qh_[d][:, sl], rhs=Sb[d][:],
                                                              start=False, stop=True),
                         r=[qh_[d], Sb[d]], w=[ps_o[d]])
                    P.op("act", lambda e, d=d, c=c: e.activation(out=Od[d][:, c, :], in_=ps_o[d][0:64, 0:128],
                                                                 func=AF.Copy), r=[ps_o[d]], w=[Od_tok[d][c]])
                    P.op("pe", lambda e, d=d, KH=KH, c=c: e.matmul(ps_s[d][:, 0:128], lhsT=KH[:], rhs=vt[:, c, :],
                                                                   start=True, stop=True), r=[KH, vt], w=[ps_s[d]])
                    P.op("dve", lambda e, d=d, c=c: e.scalar_tensor_tensor(
                        out=Sf[d][:], in0=Sf[d][:], scalar=dec[d][:, c:c + 1], in1=ps_s[d][:, 0:128], op0=ALU.mult,
                        op1=ALU.add), r=[Sf[d], dec[d], ps_s[d]], w=[Sf[d]])
                    P.op("act", lambda e, d=d: e.activation(out=Sb[d][:], in_=Sf[d][:], func=AF.Copy),
                         r=[Sf[d]], w=[Sb[d]])
            allO = [tk for d in range(2) for tk in Od_tok[d]]
            P.op("dve", lambda e: e.tensor_tensor(out=Od[0][:], in0=Od[0][:], in1=Od[1][:], op=ALU.add),
                 r=allO, w=[Od[0]])
            P.op("act", lambda e: e.activation(out=osq[:], in_=Od[0][:], func=AF.Square), r=[Od[0]], w=[osq])
            P.op("dve", lambda e: e.tensor_reduce(out=oss[:], in_=osq[:], axis=AX.X, op=ALU.add), r=[osq], w=[oss])
            P.op("act", lambda e: e.activation(out=oss[:], in_=oss[:], func=AF.Sqrt, scale=1.0 / 128, bias=EPS),
                 r=[oss], w=[oss])
            P.op("dve", lambda e: e.reciprocal(out=oss[:], in_=oss[:]), r=[oss], w=[oss])
            P.op("dve", lambda e: e.tensor_tensor(out=osq[:], in0=Od[0][:], in1=bc_last(oss[:, :], 128), op=ALU.mult),
                 r=[Od[0], oss], w=[osq])
            P.op("pool", lambda e: e.tensor_tensor(out=osq[:], in0=osq[:], in1=bc_mid(LP.onorm[0:64, :], NCK),
                                                   op=ALU.mult), r=[osq, LP.onorm], w=[osq])
            P.op("dve", lambda e: e.tensor_tensor(out=oy[:], in0=osq[:], in1=sgd[:], op=ALU.mult),
                 r=[osq, sgd], w=[oy])
            for c8 in range(0, NCK, 8):
                n8 = min(8, NCK - c8)
                pt = ps_kh[(c8 // 8) % 2]
                for j in range(n8):
                    P.op("pe", lambda e, pt=pt, j=j, c8=c8: e.transpose(
                        out=pt[:, j * 64:(j + 1) * 64], in_=oy[:, c8 + j, :], identity=C.ident[0:64, 0:64]),
                        r=[oy, C.ident], w=[pt])
                P.op("act", lambda e, pt=pt, c8=c8, n8=n8: e.activation(
                    out=odT[:, c8 * 64:(c8 + n8) * 64], in_=pt[:, 0:n8 * 64], func=AF.Copy), r=[pt], w=[odT])
            P.dma("sp", C.OT[3, h * 128:(h + 1) * 128, :], odT[:], r=[odT])
            if h == 7 and getattr(C, "DBG", None) is not None:
                P.dma("sp", C.DBG["O"][:, :, :], Od[0][:], r=[Od[0]])
                P.dma("sp", C.DBG["O1"][:, :, :], Od[1][:], r=[Od[1]])
                for d in range(2):
                    P.dma("sp", C.DBG["qt"][d, :, :], qt_[d][:], r=[qt_[d]])
                    P.dma("sp", C.DBG["kt"][d, :, :], kt_[d][:], r=[kt_[d]])
                    P.dma("sp", C.DBG["qh"][d, :, :], qh_[d][:], r=[qh_[d]])
                    P.dma("sp", C.DBG["kh"][d, :, :], kh_[d][:], r=[kh_[d]])
                    P.dma("sp", C.DBG["dec"][d, :, :], dec[d][:], r=[dec[d]])
                P.dma("sp", C.DBG["kk"][:, :], kk[:], r=[kk])


def stage_merge(P, C, l, LP):
    SG = 1152
    SUB = [(0, 384), (384, 384), (768, 384)]
    with P.scope():
        oT = P.sbuf("moT", [128, 4, 8, SG], BF16)
        yT = P.sbuf("myT", [128, SG], BF16)
        sgt = [P.sbuf(f"msgt{i}", [128, 4, SG], BF16) for i in range(2)]
        wb = [P.sbuf(f"mwb{i}", [128, 4, 8, 128], BF16) for i in range(2)]
        acc = [P.sbuf(f"macc{i}", [128, 384]) for i in range(2)]
        tmp = [P.sbuf(f"mtmp{i}", [128, 384]) for i in range(2)]
        ps = [P.psum(f"mps{i}") for i in range(4)]
        ip = 0
        ia = 0
        for sgi in range(2):
            g0 = sgi * SG
            for n in range(4):
                P.dma("sp", oT[:, n, :, :], C.OT[n, :, g0:g0 + SG].rearrange("(c p) t -> p c t", p=128), w=[oT])
            for dmb in range(16):
                k = dmb % 2
                for n in range(4):
                    P.dma("pool", wb[k][:, n, :, :],
                          C.w_branch[l, n, :, dmb * 128:(dmb + 1) * 128].rearrange("(c p) o -> p c o", p=128),
                          w=[wb[k]])
                P.dma("sp", sgt[k][:], bass.AP(C.SGT.t, (dmb * 128) * T + g0, [[T, 128], [D * T, 4], [1, SG]]),
                      w=[sgt[k]])
                for (s0, sn) in SUB:
                    A = acc[ia % 2]
                    TM = tmp[ia % 2]
                    ia += 1
                    for n in range(4):
                        pt = ps[ip % 4]
                        ip += 1
                        for c in range(8):
                            P.op("pe", lambda e, pt=pt, k=k, n=n, c=c, s0=s0, sn=sn: e.matmul(
                                pt[:, :sn], lhsT=wb[k][:, n, c, :], rhs=oT[:, n, c, s0:s0 + sn], start=(c == 0),
                                stop=(c == 7)), r=[wb[k], oT], w=[pt])
                        if n == 0:
                            P.op("dve", lambda e, pt=pt, k=k, A=A, s0=s0, sn=sn: e.tensor_tensor(
                                out=A[:, :sn], in0=pt[:, :sn], in1=sgt[k][:, 0, s0:s0 + sn], op=ALU.mult),
                                r=[pt, sgt[k]], w=[A])
                        else:
                            P.op("dve", lambda e, pt=pt, k=k, n=n, TM=TM, s0=s0, sn=sn: e.tensor_tensor(
                                out=TM[:, :sn], in0=pt[:, :sn], in1=sgt[k][:, n, s0:s0 + sn], op=ALU.mult),
                                r=[pt, sgt[k]], w=[TM])
                            if n < 3:
                                P.op("pool", lambda e, A=A, TM=TM, sn=sn: e.tensor_tensor(
                                    out=A[:, :sn], in0=A[:, :sn], in1=TM[:, :sn], op=ALU.add), r=[A, TM], w=[A])
                            else:
                                P.op("pool", lambda e, A=A, TM=TM, s0=s0, sn=sn: e.tensor_tensor(
                                    out=yT[:, s0:s0 + sn], in0=A[:, :sn], in1=TM[:, :sn], op=ALU.add),
                                    r=[A, TM], w=[yT])
                P.dma("sp", C.YT[dmb * 128:(dmb + 1) * 128, g0:g0 + SG], yT[:], r=[yT])


def stage_out_norm2(P, C, l, LP):
    with P.scope():
        yT = P.sbuf("oyT", [128, 16, T], BF16)
        P.dma("sp", yT[:], C.YT[:, :].rearrange("(c p) t -> p c t", p=128), w=[yT])
        wo = P.sbuf("owo", [128, 16, D], BF16)
        for c4 in range(4):
            P.dma("pool", wo[:, c4 * 4:(c4 + 1) * 4, :],
                  C.w_out[l, c4 * 512:(c4 + 1) * 512, :].rearrange("(c p) o -> p c o", p=128), w=[wo])
        g1 = P.sbuf("og1", [128, D])
        P.dma("sp", g1[:], pbc(C.MOD, 1 * 6 * D + 2 * D, D), w=[g1])
        xt = [P.sbuf(f"oxt{i}", [128, D]) for i in range(2)]
        tmp = [P.sbuf(f"otmp{i}", [128, 512]) for i in range(2)]
        sq = P.sbuf("osq", [128, D])
        xn = P.sbuf("oxn", [128, D])
        ss = P.sbuf("oss", [128, 1])
        hf = P.sbuf("ohf", [128, 16, 128])
        hb = [P.sbuf(f"ohb{i}", [128, 16, 128], BF16) for i in range(2)]
        ps = [P.psum(f"ops{i}") for i in range(4)]
        pst = [P.psum(f"opst{i}") for i in range(2)]
        psr = P.psum("opsr")
        aff = P.sbuf("raff", [128, NE])
        sel = P.sbuf("rsel", [128, NE])
        r4 = [P.sbuf(f"r4_{i}", [128, 4]) for i in range(8)]
        msk = P.sbuf("rmsk", [128, NE])
        gm = P.sbuf("rgm", [128, 1])
        gt_ = [P.sbuf(f"rgt{i}", [128, NE]) for i in range(2)]
        P.dma("sp", xt[0][:], C.XRES[0:128, :], w=[xt[0]])
        for t in range(NT):
            if t + 1 < NT:
                P.dma("sp", xt[(t + 1) % 2][:], C.XRES[(t + 1) * 128:(t + 2) * 128, :], w=[xt[(t + 1) % 2]])
            X = xt[t % 2]
            r = 1 if t < 2 else 0
            if t == 2:
                P.dma("sp", g1[:], pbc(C.MOD, 0 * 6 * D + 2 * D, D), w=[g1])
            for cg in range(4):
                pt = ps[cg]
                for c in range(16):
                    P.op("pe", lambda e, pt=pt, c=c, t=t, cg=cg: e.matmul(
                        pt[:], lhsT=yT[:, c, t * 128:(t + 1) * 128], rhs=wo[:, c, cg * 512:(cg + 1) * 512],
                        start=(c == 0), stop=(c == 15)), r=[yT, wo], w=[pt])
                TM = tmp[cg % 2]
                P.op("dve", lambda e, pt=pt, cg=cg, TM=TM: e.tensor_tensor(
                    out=TM[:], in0=pt[:], in1=g1[:, cg * 512:(cg + 1) * 512],
                    op=ALU.mult), r=[pt, g1], w=[TM])
                P.op("pool", lambda e, X=X, TM=TM, cg=cg: e.tensor_tensor(
                    out=X[:, cg * 512:(cg + 1) * 512], in0=X[:, cg * 512:(cg + 1) * 512], in1=TM[:], op=ALU.add),
                    r=[X, TM], w=[X])
            P.dma("sp", C.XRES[t * 128:(t + 1) * 128, :], X[:], r=[X])
            norm_tile(P, X, xn, sq, ss, D)
            HB = hb[t % 2]
            for c4 in range(4):
                pt = pst[c4 % 2]
                for j in range(4):
                    c = c4 * 4 + j
                    P.op("pe", lambda e, c=c, j=j, pt=pt: e.transpose(
                        out=pt[:, j * 128:(j + 1) * 128], in_=xn[:, c * 128:(c + 1) * 128], identity=C.ident_f[:]),
                        r=[xn, C.ident_f], w=[pt])
                for j in range(4):
                    c = c4 * 4 + j
                    if j % 2 == 0:
                        P.op("act", lambda e, c=c, j=j, pt=pt, r=r: e.activation(
                            out=hf[:, c, :], in_=pt[:, j * 128:(j + 1) * 128], func=AF.Identity,
                            scale=LP.s2[:, r, c:c + 1], bias=LP.modP[:, r, 3, c:c + 1]),
                            r=[pt, LP.s2, LP.modP], w=[hf])
                    else:
                        P.op("dve", lambda e, c=c, j=j, pt=pt, r=r: e.tensor_scalar(
                            out=hf[:, c, :], in0=pt[:, j * 128:(j + 1) * 128], scalar1=LP.s2[:, r, c:c + 1],
                            scalar2=LP.modP[:, r, 3, c:c + 1], op0=ALU.mult, op1=ALU.add),
                            r=[pt, LP.s2, LP.modP], w=[hf])
            P.op("act", lambda e, HB=HB: e.activation(out=HB[:], in_=hf[:], func=AF.Copy), r=[hf], w=[HB])
            P.dma("sp", C.H2T[:, t * 128:(t + 1) * 128].rearrange("(c p) t -> p c t", p=128), HB[:], r=[HB])
            for c in range(16):
                P.op("pe", lambda e, c=c: e.matmul(psr[:, 0:NE], lhsT=hf[:, c, :], rhs=LP.wr[:, c, :], start=(c == 0),
                                                   stop=(c == 15)), r=[hf, LP.wr], w=[psr])
            P.op("act", lambda e: e.activation(out=aff[:], in_=psr[:, 0:NE], func=AF.Sigmoid), r=[psr], w=[aff])
            P.op("dve", lambda e: e.tensor_tensor(out=sel[:], in0=aff[:], in1=LP.br[:], op=ALU.add),
                 r=[aff, LP.br], w=[sel])
            s3 = sel[:].rearrange("p (g k) -> p g k", k=4)
            hi1, lo1, hi2, lo2, top1, sec, gs, ing = r4
            tt = lambda o, a, b, op, rr, ww: P.op("dve", lambda e: e.tensor_tensor(out=o, in0=a, in1=b, op=op),
                                                  r=rr, w=ww)
            tt(hi1[:], s3[:, :, 0], s3[:, :, 1], ALU.max, [sel], [hi1])
            tt(lo1[:], s3[:, :, 0], s3[:, :, 1], ALU.min, [sel], [lo1])
            tt(hi2[:], s3[:, :, 2], s3[:, :, 3], ALU.max, [sel], [hi2])
            tt(lo2[:], s3[:, :, 2], s3[:, :, 3], ALU.min, [sel], [lo2])
            tt(top1[:], hi1[:], hi2[:], ALU.max, [hi1, hi2], [top1])
            tt(sec[:], hi1[:], hi2[:], ALU.min, [hi1, hi2], [sec])
            tt(lo1[:], lo1[:], lo2[:], ALU.max, [lo1, lo2], [lo1])
            tt(sec[:], sec[:], lo1[:], ALU.max, [sec, lo1], [sec])
            tt(gs[:], top1[:], sec[:], ALU.add, [top1, sec], [gs])
            P.op("dve", lambda e: e.tensor_reduce(out=gm[:], in_=gs[:], axis=AX.X, op=ALU.max), r=[gs], w=[gm])
            P.op("dve", lambda e: e.tensor_scalar(out=ing[:], in0=gs[:], scalar1=gm[:, 0:1], scalar2=None,
                                                  op0=ALU.is_equal), r=[gs, gm], w=[ing])
            m3 = msk[:].rearrange("p (g k) -> p g k", k=4)
            P.op("dve", lambda e: e.tensor_tensor(out=m3, in0=s3, in1=bc_last(sec[:, :], 4), op=ALU.is_ge),
                 r=[sel, sec], w=[msk])
            P.op("dve", lambda e: e.tensor_tensor(out=m3, in0=m3, in1=bc_last(ing[:, :], 4), op=ALU.mult),
                 r=[msk, ing], w=[msk])
            P.op("dve", lambda e: e.tensor_tensor(out=msk[:], in0=msk[:], in1=aff[:], op=ALU.mult),
                 r=[msk, aff], w=[msk])
            P.op("dve", lambda e: e.tensor_reduce(out=gm[:], in_=msk[:], axis=AX.X, op=ALU.add), r=[msk], w=[gm])
            P.op("dve", lambda e: e.reciprocal(out=gm[:], in_=gm[:]), r=[gm], w=[gm])
            G = gt_[t % 2]
            P.op("dve", lambda e, G=G: e.tensor_scalar(out=G[:], in0=msk[:], scalar1=gm[:, 0:1], scalar2=None,
                                                       op0=ALU.mult), r=[msk, gm], w=[G])
            P.dma("sp", C.GATES[t * 128:(t + 1) * 128, :], G[:], r=[G])


def stage_moe(P, C, l, LP, last=False):
    GT = 768
    with P.scope():
        g2 = P.sbuf("eg2", [128, D])
        hT = P.sbuf("ehT", [128, 16, GT], BF16)
        gts = P.sbuf("egts", [128, 6, NE])
        yacc = P.sbuf("eyacc", [128, 6, D])
        yacc_tok = [Tok(f"yacc{i}") for i in range(6)]
        wg = [P.sbuf(f"ewg{i}", [128, 16, DFF], BF16) for i in range(2)]
        wu = [P.sbuf(f"ewu{i}", [128, 16, DFF], BF16) for i in range(2)]
        wd = [P.sbuf(f"ewd{i}", [128, 4, D], BF16) for i in range(2)]
        sg = [P.sbuf(f"esg{i}", [128, DFF]) for i in range(2)]
        hid = [P.sbuf(f"ehid{i}", [128, DFF], BF16) for i in range(2)]
        hidT = [P.sbuf(f"ehidT{i}", [128, DFF], BF16) for i in range(2)]
        xt = P.sbuf("ext", [128, D])
        psg = [P.psum(f"epsg{i}") for i in range(2)]
        psu = [P.psum(f"epsu{i}") for i in range(2)]
        pst = P.psum("epst", [128, 1024], BF16)
        psy = [P.psum(f"epsy{i}") for i in range(2)]
        cnt = {"it": 0, "iy": 0}

        def issue_w(ex):
            k = ex % 2
            P.dma("pool", wg[k][:], C.w_gate[l, ex, :, :].rearrange("(c p) f -> p c f", p=128), w=[wg[k]])
            P.dma("pool", wu[k][:], C.w_up[l, ex, :, :].rearrange("(c p) f -> p c f", p=128), w=[wu[k]])
            P.dma("pool", wd[k][:], C.w_down[l, ex, :, :].rearrange("(c p) o -> p c o", p=128), w=[wd[k]])

        def emit_gu(st):
            ex, ti, pg, pu = st["ex"], st["ti"], st["pg"], st["pu"]
            k = ex % 2
            for c in range(16):
                P.op("pe", lambda e, c=c: e.matmul(pg[:], lhsT=hT[:, c, ti * 128:(ti + 1) * 128], rhs=wg[k][:, c, :],
                                                   start=(c == 0), stop=(c == 15)), r=[hT, wg[k]], w=[pg])
            for c in range(16):
                P.op("pe", lambda e, c=c: e.matmul(pu[:], lhsT=hT[:, c, ti * 128:(ti + 1) * 128], rhs=wu[k][:, c, :],
                                                   start=(c == 0), stop=(c == 15)), r=[hT, wu[k]], w=[pu])

        def emit_rest(st):
            ex, ti, pg, pu, SG_, H, HT = st["ex"], st["ti"], st["pg"], st["pu"], st["sg"], st["hid"], st["hidT"]
            k = ex % 2
            P.op("act", lambda e: e.activation(out=SG_[:], in_=pg[:], func=AF.Silu), r=[pg], w=[SG_])
            P.op("dve", lambda e: e.scalar_tensor_tensor(out=H[:], in0=SG_[:], scalar=gts[:, ti, ex:ex + 1], in1=pu[:],
                                                         op0=ALU.mult, op1=ALU.mult), r=[SG_, pu, gts], w=[H])
            for j in range(4):
                P.op("pe", lambda e, j=j: e.transpose(out=pst[:, j * 128:(j + 1) * 128],
                                                      in_=H[:, j * 128:(j + 1) * 128], identity=C.ident[:]),
                     r=[H, C.ident], w=[pst])
            P.op("act", lambda e: e.activation(out=HT[:], in_=pst[:, 0:512], func=AF.Copy), r=[pst], w=[HT])
            for cg in range(4):
                py = psy[cnt["iy"] % 2]
                cnt["iy"] += 1
                for f in range(4):
                    P.op("pe", lambda e, f=f, cg=cg, py=py: e.matmul(
                        py[:], lhsT=HT[:, f * 128:(f + 1) * 128], rhs=wd[k][:, f, cg * 512:(cg + 1) * 512],
                        start=(f == 0), stop=(f == 3)), r=[HT, wd[k]], w=[py])
                if st["firstex"]:
                    P.op("act", lambda e, py=py, cg=cg: e.activation(
                        out=yacc[:, ti, cg * 512:(cg + 1) * 512], in_=py[:], func=AF.Copy),
                        r=[py], w=[yacc_tok[ti]])
                else:
                    P.op("dve", lambda e, py=py, cg=cg: e.tensor_tensor(
                        out=yacc[:, ti, cg * 512:(cg + 1) * 512], in0=yacc[:, ti, cg * 512:(cg + 1) * 512],
                        in1=py[:], op=ALU.add), r=[py, yacc_tok[ti]], w=[yacc_tok[ti]])

        cur_g2 = None
        for t0 in range(0, T, GT):
            nt = GT // 128
            tiles = [ti for ti in range(nt) if not (last and (t0 // 128 + ti) < 2)]
            P.dma("sp", hT[:], C.H2T[:, t0:t0 + GT].rearrange("(c p) t -> p c t", p=128), w=[hT])
            P.dma("sp", gts[:], C.GATES[t0:t0 + GT, :].rearrange("(n p) e -> p n e", p=128), w=[gts])
            issue_w(0)
            steps = []
            for ex in range(NE):
                for ti in tiles:
                    i = cnt["it"]
                    cnt["it"] += 1
                    steps.append(dict(ex=ex, ti=ti, pg=psg[i % 2], pu=psu[i % 2], sg=sg[i % 2], hid=hid[i % 2],
                                      hidT=hidT[i % 2], firstex=(ex == 0), wfirst=(ti == tiles[0])))
            emit_gu(steps[0])
            for i, st in enumerate(steps):
                if st["wfirst"] and st["ex"] + 1 < NE:
                    issue_w(st["ex"] + 1)
                if i + 1 < len(steps):
                    emit_gu(steps[i + 1])
                emit_rest(st)
            for ti in tiles:
                tt_ = t0 // 128 + ti
                r = 1 if tt_ < 2 else 0
                if cur_g2 != r:
                    P.dma("sp", g2[:], pbc(C.MOD, r * 6 * D + 5 * D, D), w=[g2])
                    cur_g2 = r
                P.dma("sp", xt[:], C.XRES[tt_ * 128:(tt_ + 1) * 128, :], w=[xt])
                P.op("dve", lambda e, ti=ti: e.tensor_tensor(out=yacc[:, ti, :], in0=yacc[:, ti, :], in1=g2[:],
                                                             op=ALU.mult), r=[yacc_tok[ti], g2], w=[yacc_tok[ti]])
                P.op("pool", lambda e, ti=ti: e.tensor_tensor(out=xt[:], in0=xt[:], in1=yacc[:, ti, :], op=ALU.add),
                     r=[xt, yacc_tok[ti]], w=[xt])
                if last:
                    P.dma("sp", C.out[(tt_ - 2) * 128:(tt_ - 1) * 128, :], xt[:], r=[xt])
                else:
                    P.dma("sp", C.XRES[tt_ * 128:(tt_ + 1) * 128, :], xt[:], r=[xt])


_CACHE = {}


def make_in_maps(inputs, ncores=8):
    ca, sa, cb, sb = rope_tables()
    f = lambda a: np.ascontiguousarray(np.asarray(a, dtype=np.float32))
    shared = {
        "c_ctx": f(inputs["c_ctx"]).reshape(1, D),
        "w_ada": f(inputs["w_ada"]), "b_ada": f(inputs["b_ada"]),
        "norm1_g": f(inputs["norm1_g"]), "norm2_g": f(inputs["norm2_g"]),
        "w_in": f(inputs["w_in"]), "qn_a": f(inputs["qn_a"]), "kn_a": f(inputs["kn_a"]),
        "sink_a": f(inputs["sink_a"]), "qn_b": f(inputs["qn_b"]), "kn_b": f(inputs["kn_b"]),
        "lam_b": f(inputs["lam_b"]).reshape(DEPTH, 256), "subln_b": f(inputs["subln_b"]),
        "conv_w": f(inputs["conv_w"]), "conv_b": f(inputs["conv_b"]),
        "w_rg": f(inputs["w_rg"]), "b_rg": f(inputs["b_rg"]), "w_ig": f(inputs["w_ig"]), "b_ig": f(inputs["b_ig"]),
        "lru_lambda": f(inputs["lru_lambda"]), "lb_d": f(inputs["lb_d"]), "onorm_d": f(inputs["onorm_d"]),
        "w_branch": f(inputs["w_branch"]), "w_out": f(inputs["w_out"]),
        "w_router": f(inputs["w_router"]), "b_router": f(inputs["b_router"]).reshape(1, NE),
        "w_gate": f(inputs["w_gate"]), "w_up": f(inputs["w_up"]), "w_down": f(inputs["w_down"]),
        "ropeA_c": ca, "ropeA_s": sa, "ropeB_c": cb, "ropeB_s": sb,
    }
    x = f(inputs["x"])
    ctx = f(inputs["ctx"])
    c = f(inputs["c"])
    maps = []
    for b in range(ncores):
        m = dict(shared)
        m["x"] = x[b]
        m["ctx"] = ctx[b]
        m["c"] = c[b].reshape(1, D)
        maps.append(m)
    return maps


def kernel(**inputs):
    if "nc" not in _CACHE:
        _CACHE["nc"] = build()[0]
    nc = _CACHE["nc"]
    maps = make_in_maps(inputs, 8)
    res = run_bass_kernel_spmd(nc, maps, core_ids=list(range(8)))
    return np.stack([np.asarray(r["out"], dtype=np.float32) for r in res.results], axis=0)
```

```python
import contextlib
import math
import numpy as np
import concourse.bass as bass
import concourse.mybir as mybir
from concourse.bass_utils import run_bass_kernel_spmd

F32 = mybir.dt.float32
BF16 = mybir.dt.bfloat16
AF = mybir.ActivationFunctionType
ALU = mybir.AluOpType
AX = mybir.AxisListType

ENGS = ("pe", "act", "dve", "pool", "sp")
DMA_RING = 6


class Tok:
    __slots__ = ("lw", "rd", "name")

    def __init__(self, name=""):
        self.lw = None
        self.rd = []
        self.name = name


class Ev:
    __slots__ = ("eng", "kind", "idx", "needed", "sem", "val")

    def __init__(self, eng, kind, idx):
        self.eng = eng
        self.kind = kind
        self.idx = idx
        self.needed = False
        self.sem = None
        self.val = None


class Tile(Tok):
    __slots__ = ("t", "shape", "dtype")

    def __init__(self, t, shape, dtype, name=""):
        super().__init__(name)
        self.t = t
        self.shape = shape
        self.dtype = dtype

    def __getitem__(self, k):
        return self.t[k]


class Prog:
    def __init__(self, nc):
        self.nc = nc
        self.q = {e: [] for e in ENGS}
        self.ndma = {e: 0 for e in ENGS}
        self.dma_evs = {e: [] for e in ENGS}
        self.last_ev = {e: None for e in ENGS}
        self.stack = contextlib.ExitStack()
        self.scopes = []
        self.uid = 0
        self.all_dma_evs = []

    def _name(self, name):
        self.uid += 1
        return f"{name}_{self.uid}"

    def _cur(self):
        return self.scopes[-1] if self.scopes else self.stack

    def sbuf(self, name, shape, dtype=F32):
        t = self._cur().enter_context(self.nc.sbuf_tensor(self._name(name), list(shape), dtype))
        return Tile(t, shape, dtype, name)

    def psum(self, name, shape=(128, 512), dtype=F32):
        t = self._cur().enter_context(self.nc.psum_tensor(self._name(name), list(shape), dtype))
        return Tile(t, shape, dtype, name)

    def dram(self, name, shape, dtype=F32, kind="Internal"):
        t = self.nc.dram_tensor(name, list(shape), dtype, kind=kind)
        return Tile(t, shape, dtype, name)

    @contextlib.contextmanager
    def scope(self):
        self.barrier()
        es = contextlib.ExitStack()
        self.scopes.append(es)
        try:
            yield
        finally:
            self.barrier()
            self.scopes.pop()
            es.close()

    def _emit(self, eng, fn, r, w, kind):
        deps = []
        for t in r:
            if t.lw is not None:
                deps.append(t.lw)
        for t in w:
            if t.lw is not None:
                deps.append(t.lw)
            deps.extend(t.rd)
        if kind == "d":
            i = self.ndma[eng]
            self.ndma[eng] += 1
            ev = Ev(eng, "d", i)
            if i >= DMA_RING:
                deps.append(self.dma_evs[eng][i - DMA_RING])
            self.dma_evs[eng].append(ev)
            self.all_dma_evs.append(ev)
        else:
            ev = Ev(eng, "c", len(self.q[eng]))
        dd = []
        seen = set()
        for d in deps:
            if id(d) in seen:
                continue
            seen.add(id(d))
            if d.kind == "c" and d.eng == eng and eng == "pe":
                continue
            dd.append(d)
            d.needed = True
        self.q[eng].append([dd, fn, ev])
        for t in r:
            t.rd.append(ev)
        for t in w:
            t.lw = ev
            t.rd = []
        if kind == "c":
            self.last_ev[eng] = ev
        return ev

    def op(self, eng, fn, r=(), w=()):
        return self._emit(eng, fn, list(r), list(w), "c")

    def dma(self, eng, out, in_, r=(), w=(), **kw):
        return self._emit(eng, lambda e: e.dma_start(out=out, in_=in_, **kw), list(r), list(w), "d")

    def barrier(self):
        evs = [self.last_ev[e] for e in ENGS if self.last_ev[e] is not None]
        evs += self.all_dma_evs
        self.all_dma_evs = []
        if not evs:
            return
        for e in ENGS:
            deps = []
            for d in evs:
                if d.kind == "c" and d.eng == e:
                    continue
                d.needed = True
                deps.append(d)
            self.q[e].append([deps, None, None])

    def finalize(self):
        nc = self.nc
        st = self.stack
        self.barrier()
        sem_c = {e: st.enter_context(nc.semaphore(f"c_{e}")) for e in ENGS}
        sem_d = {e: [st.enter_context(nc.semaphore(f"d_{e}_{k}")) for k in range(DMA_RING)]
                 for e in ENGS if self.ndma[e] > 0}
        for e in ENGS:
            cnt = 0
            for deps, fn, ev in self.q[e]:
                if ev is None:
                    continue
                if ev.kind == "c":
                    if ev.needed:
                        cnt += 1
                        ev.sem = sem_c[e]
                        ev.val = cnt
                else:
                    ev.sem = sem_d[e][ev.idx % DMA_RING]
                    ev.val = 16 * (ev.idx // DMA_RING + 1)
        getter = {"pe": "tensor", "act": "scalar", "dve": "vector", "pool": "gpsimd", "sp": "sync"}
        stats = [0, 0]
        with nc.Block() as block:
            for e in ENGS:
                items = self.q[e]
                if not items:
                    continue

                def body(eng, items=items):
                    seen = {}
                    for deps, fn, ev in items:
                        for d in deps:
                            key = id(d.sem)
                            if seen.get(key, 0) >= d.val:
                                continue
                            seen[key] = d.val
                            eng.wait_ge(d.sem, d.val)
                            stats[1] += 1
                        if fn is None:
                            continue
                        ins = fn(eng)
                        stats[0] += 1
                        if ev.kind == "d":
                            ins.then_inc(ev.sem, 16)
                        elif ev.needed:
                            ins.then_inc(ev.sem, 1)

                getattr(block, getter[e])(body)
        self.stats = tuple(stats)
        st.close()
        return nc


D = 2048
SEQ = 2048
LC = 256
T = SEQ + LC
NT = T // 128
NCH = D // 128
DEPTH = 2
EPS = 1e-6
IN_W = 19968
NE = 16
DFF = 512
GRID_W = 64
TG = [(0, 512), (512, 512), (1024, 512), (1536, 512), (2048, 256)]


def mk(base, pairs):
    return bass.AP(base.tensor, base.offset, [list(p) for p in pairs])


def bc_last(ap2, n):
    return mk(ap2, [ap2.ap[0], ap2.ap[1], [0, n]])


def bc_mid(ap2, k):
    return mk(ap2, [ap2.ap[0], [0, k], ap2.ap[1]])


def pbc(dt_tile, offset, n, parts=128):
    return bass.AP(dt_tile.t, offset, [[0, parts], [1, n]])


class Ctx:
    pass


def rope_tables():
    pos = np.arange(SEQ)
    rows = (pos // GRID_W).astype(np.float32)
    cols = (pos % GRID_W).astype(np.float32)

    def tab(half):
        inv = (10000.0 ** (-np.arange(half, dtype=np.float32) / half)).astype(np.float32)
        ar = rows[:, None] * inv[None, :]
        ac = cols[:, None] * inv[None, :]
        c = np.concatenate([np.cos(ar), np.cos(ar), np.cos(ac), np.cos(ac)], axis=1)
        s = np.concatenate([-np.sin(ar), np.sin(ar), -np.sin(ac), np.sin(ac)], axis=1)
        return c.astype(np.float32), s.astype(np.float32)

    ca, sa = tab(32)
    cb, sb = tab(16)
    return ca, sa, cb, sb


def build(nlayers=DEPTH, debug=(), only=None, feed=()):
    nc = bass.Bass("TRN2", target_bir_lowering=False)
    P = Prog(nc)
    C = Ctx()
    ext = lambda n, s, d=F32: P.dram(n, s, d, kind="ExternalInput")
    C.x = ext("x", [SEQ, D])
    C.ctx = ext("ctx", [LC, D])
    C.c = ext("c", [1, D])
    C.c_ctx = ext("c_ctx", [1, D])
    C.w_ada = ext("w_ada", [DEPTH, D, 6 * D])
    C.b_ada = ext("b_ada", [DEPTH, 6 * D])
    C.norm1_g = ext("norm1_g", [DEPTH, D])
    C.norm2_g = ext("norm2_g", [DEPTH, D])
    C.w_in = ext("w_in", [DEPTH, D, IN_W])
    C.qn_a = ext("qn_a", [DEPTH, 128])
    C.kn_a = ext("kn_a", [DEPTH, 128])
    C.sink_a = ext("sink_a", [DEPTH, 8])
    C.qn_b = ext("qn_b", [DEPTH, 64])
    C.kn_b = ext("kn_b", [DEPTH, 64])
    C.lam_b = ext("lam_b", [DEPTH, 256])
    C.subln_b = ext("subln_b", [DEPTH, 128])
    C.conv_w = ext("conv_w", [DEPTH, 4, 1024])
    C.conv_b = ext("conv_b", [DEPTH, 1024])
    C.w_rg = ext("w_rg", [DEPTH, 2, 8, 128, 128])
    C.b_rg = ext("b_rg", [DEPTH, 2, 1024])
    C.w_ig = ext("w_ig", [DEPTH, 2, 8, 128, 128])
    C.b_ig = ext("b_ig", [DEPTH, 2, 1024])
    C.lru_lambda = ext("lru_lambda", [DEPTH, 2, 1024])
    C.lb_d = ext("lb_d", [DEPTH, 2, 1024])
    C.onorm_d = ext("onorm_d", [DEPTH, 128])
    C.w_branch = ext("w_branch", [DEPTH, 4, 1024, D])
    C.w_out = ext("w_out", [DEPTH, D, D])
    C.w_router = ext("w_router", [D, NE])
    C.b_router = ext("b_router", [1, NE])
    C.w_gate = ext("w_gate", [DEPTH, NE, D, DFF])
    C.w_up = ext("w_up", [DEPTH, NE, D, DFF])
    C.w_down = ext("w_down", [DEPTH, NE, DFF, D])
    C.ropeA_c = ext("ropeA_c", [SEQ, 128])
    C.ropeA_s = ext("ropeA_s", [SEQ, 128])
    C.ropeB_c = ext("ropeB_c", [SEQ, 64])
    C.ropeB_s = ext("ropeB_s", [SEQ, 64])
    C.out = P.dram("out", [SEQ, D], F32, kind="ExternalOutput")

    def scratch(name, shape, dt):
        kind = "ExternalInput" if name in feed else ("ExternalOutput" if name in debug else "Internal")
        return P.dram(name, shape, dt, kind=kind)

    C.XRES = scratch("XRES", [T, D], F32)
    C.MOD = scratch("MOD", [2, 6 * D], F32)
    C.QAT = scratch("QAT", [8, 128, T], BF16)
    C.KAT = scratch("KAT", [2, 128, T], BF16)
    C.VA = scratch("VA", [T, 256], BF16)
    C.QBT = scratch("QBT", [8, 128, T], BF16)
    C.KBT = scratch("KBT", [8, 128, T], BF16)
    C.VB = scratch("VB", [T, 1024], BF16)
    C.CXT = scratch("CXT", [1024, T], F32)
    C.GCY = scratch("GCY", [1024, T], BF16)
    C.DQT = scratch("DQT", [1024, T], BF16)
    C.ZF = scratch("ZF", [2, 1024, T], F32)
    C.VD = scratch("VD", [T, 1024], BF16)
    C.SGD = scratch("SGD", [T, 1024], BF16)
    C.SGT = scratch("SGT", [4 * D, T], BF16)
    C.OT = scratch("OT", [4, 1024, T], BF16)
    C.YT = scratch("YT", [D, T], BF16)
    C.H2T = scratch("H2T", [D, T], BF16)
    C.GATES = scratch("GATES", [T, NE], F32)
    C.DBG = None
    if "DBGHG" in debug:
        eo = lambda n, sh, dt=F32: P.dram(n, sh, dt, kind="ExternalOutput")
        C.DBG = {"O": eo("dbgO", [64, 36, 128]), "O1": eo("dbgO1", [64, 36, 128]),
                 "qt": eo("dbgqt", [2, 128, T], BF16), "kt": eo("dbgkt", [2, 128, T], BF16),
                 "qh": eo("dbgqh", [2, 128, T], BF16), "kh": eo("dbgkh", [2, 128, T], BF16),
                 "dec": eo("dbgdec", [2, 128, 36]), "Bx": eo("dbgBx", [128, T + 1]), "kk": eo("dbgkk", [128, T])}

    ident_f = P.sbuf("ident_f", [128, 128], F32)
    ident = P.sbuf("ident", [128, 128], BF16)
    ones_f = P.sbuf("ones_f", [128, 128], F32)
    ones = P.sbuf("ones", [128, 128], BF16)
    m_ge = P.sbuf("m_ge", [128, 128], BF16)
    m_le = P.sbuf("m_le", [128, 128], BF16)
    tmpc = P.sbuf("tmpc", [128, 128], F32)
    P.op("pool", lambda e: e.memset(ident_f[:], 0.0), w=[ident_f])
    P.op("pool", lambda e: e.affine_select(out=ident_f[:], in_=ident_f[:], pattern=[[-1, 128]],
                                           compare_op=ALU.not_equal, fill=1.0, base=0, channel_multiplier=1),
         r=[ident_f], w=[ident_f])
    P.op("dve", lambda e: e.tensor_copy(out=ident[:], in_=ident_f[:]), r=[ident_f], w=[ident])
    P.op("pool", lambda e: e.memset(ones_f[:], 1.0), w=[ones_f])
    P.op("dve", lambda e: e.tensor_copy(out=ones[:], in_=ones_f[:]), r=[ones_f], w=[ones])
    P.op("pool", lambda e: e.affine_select(out=tmpc[:], in_=ones_f[:], pattern=[[-1, 128]],
                                           compare_op=ALU.is_ge, fill=0.0, base=0, channel_multiplier=1),
         r=[ones_f], w=[tmpc])
    P.op("dve", lambda e: e.tensor_copy(out=m_ge[:], in_=tmpc[:]), r=[tmpc], w=[m_ge])
    P.op("pool", lambda e: e.affine_select(out=tmpc[:], in_=ones_f[:], pattern=[[1, 128]],
                                           compare_op=ALU.is_ge, fill=0.0, base=0, channel_multiplier=-1),
         r=[ones_f], w=[tmpc])
    P.op("dve", lambda e: e.tensor_copy(out=m_le[:], in_=tmpc[:]), r=[tmpc], w=[m_le])
    C.ident, C.ident_f, C.ones, C.ones_f, C.m_ge, C.m_le = ident, ident_f, ones, ones_f, m_ge, m_le

    P.dma("sp", C.XRES[0:LC, :], C.ctx[:, :])
    for i in range(4):
        P.dma("sp", C.XRES[LC + i * 512:LC + (i + 1) * 512, :], C.x[i * 512:(i + 1) * 512, :])
    P.barrier()

    if only is not None:
        with P.scope():
            LP = layer_consts(P, C, 0)
            if "mod" in only:
                stage_mod(P, C, 0, LP)
            for nm in only:
                if nm == "np":
                    with P.scope():
                        hT, hT_tok = stage_norm_T(P, C, 0, LP)
                        stage_proj(P, C, 0, LP, hT, hT_tok)
                elif nm != "mod":
                    globals()["stage_" + nm](P, C, 0, LP)
        P.finalize()
        return nc, P
    for l in range(nlayers):
        last = (l == DEPTH - 1)
        with P.scope():
            LP = layer_consts(P, C, l)
            stage_mod(P, C, l, LP)
            with P.scope():
                hT, hT_tok = stage_norm_T(P, C, l, LP)
                stage_proj(P, C, l, LP, hT, hT_tok)
            stage_attn_a(P, C, l, LP)
            stage_attn_b(P, C, l, LP)
            stage_rglru(P, C, l, LP)
            stage_hgrn(P, C, l, LP)
            stage_merge(P, C, l, LP)
            stage_out_norm2(P, C, l, LP)
            stage_moe(P, C, l, LP, last)
    P.finalize()
    return nc, P


def col_load(P, dst_ap, src_tile, off, n, w, eng="sp"):
    k = n // 128
    src = bass.AP(src_tile.t, off, [[1, 128], [128, k]])
    P.dma(eng, dst_ap, src, w=w, allow_slow_non_contiguous=True)


def layer_consts(P, C, l):
    LP = Ctx()
    LP.n1 = P.sbuf("n1", [128, 16])
    LP.n2 = P.sbuf("n2", [128, 16])
    col_load(P, LP.n1[:], C.norm1_g, l * D, D, [LP.n1])
    col_load(P, LP.n2[:], C.norm2_g, l * D, D, [LP.n2])
    LP.qn_a = P.sbuf("qn_a", [128, 128])
    LP.kn_a = P.sbuf("kn_a", [128, 128])
    LP.qn_b = P.sbuf("qn_b", [128, 64])
    LP.kn_b = P.sbuf("kn_b", [128, 64])
    LP.onorm = P.sbuf("onorm", [128, 128])
    P.dma("sp", LP.qn_a[:], pbc(C.qn_a, l * 128, 128), w=[LP.qn_a])
    P.dma("sp", LP.kn_a[:], pbc(C.kn_a, l * 128, 128), w=[LP.kn_a])
    P.dma("sp", LP.qn_b[:], pbc(C.qn_b, l * 64, 64), w=[LP.qn_b])
    P.dma("sp", LP.kn_b[:], pbc(C.kn_b, l * 64, 64), w=[LP.kn_b])
    P.dma("sp", LP.onorm[:], pbc(C.onorm_d, l * 128, 128), w=[LP.onorm])
    LP.esink = P.sbuf("esink", [128, 8])
    P.dma("sp", LP.esink[:], pbc(C.sink_a, l * 8, 8), w=[LP.esink])
    P.op("act", lambda e: e.activation(out=LP.esink[:], in_=LP.esink[:], func=AF.Exp), r=[LP.esink], w=[LP.esink])
    lam_init = 0.8 - 0.6 * math.exp(-0.3 * l)
    LP.lam_init = lam_init
    lb_ = P.sbuf("lamb", [128, 256])
    P.dma("sp", lb_[:], pbc(C.lam_b, l * 256, 256), w=[lb_])
    pr = P.sbuf("lampr", [128, 2, 64])
    lb4 = lb_[:].rearrange("p (a b d) -> p a b d", a=2, b=2)
    P.op("dve", lambda e: e.tensor_tensor(out=pr[:], in0=lb4[:, :, 0, :], in1=lb4[:, :, 1, :], op=ALU.mult),
         r=[lb_], w=[pr])
    s2 = P.sbuf("lams2", [128, 2])
    P.op("dve", lambda e: e.tensor_reduce(out=s2[:], in_=pr[:], axis=AX.X, op=ALU.add), r=[pr], w=[s2])
    P.op("act", lambda e: e.activation(out=s2[:], in_=s2[:], func=AF.Exp), r=[s2], w=[s2])
    LP.nlam = P.sbuf("nlam", [128, 1])
    P.op("dve", lambda e: e.scalar_tensor_tensor(out=LP.nlam[:], in0=s2[:, 1:2], scalar=-lam_init, in1=s2[:, 0:1],
                                                 op0=ALU.add, op1=ALU.subtract), r=[s2], w=[LP.nlam])
    LP.subln = P.sbuf("subln", [128, 1])
    col_load(P, LP.subln[:], C.subln_b, l * 128, 128, [LP.subln])
    P.op("dve", lambda e: e.tensor_scalar(out=LP.subln[:], in0=LP.subln[:], scalar1=(1.0 - lam_init), scalar2=None,
                                          op0=ALU.mult), r=[LP.subln], w=[LP.subln])
    LP.convw = P.sbuf("convw", [128, 4, 8])
    for tap in range(4):
        col_load(P, LP.convw[:, tap, :], C.conv_w, (l * 4 + tap) * 1024, 1024, [LP.convw])
    LP.convb = P.sbuf("convb", [128, 8])
    col_load(P, LP.convb[:], C.conv_b, l * 1024, 1024, [LP.convb])
    LP.brg = P.sbuf("brg", [128, 2, 8])
    LP.big = P.sbuf("big", [128, 2, 8])
    lam = P.sbuf("lrulam", [128, 2, 8])
    for d in range(2):
        col_load(P, LP.brg[:, d, :], C.b_rg, (l * 2 + d) * 1024, 1024, [LP.brg])
        col_load(P, LP.big[:, d, :], C.b_ig, (l * 2 + d) * 1024, 1024, [LP.big])
        col_load(P, lam[:, d, :], C.lru_lambda, (l * 2 + d) * 1024, 1024, [lam])
    P.op("act", lambda e: e.activation(out=lam[:], in_=lam[:], func=AF.Exp, scale=-1.0), r=[lam], w=[lam])
    P.op("act", lambda e: e.activation(out=lam[:], in_=lam[:], func=AF.Ln, bias=1.0, scale=1.0), r=[lam], w=[lam])
    LP.sp8 = P.sbuf("sp8", [128, 2, 8])
    LP.sp16 = P.sbuf("sp16", [128, 2, 8])
    P.op("dve", lambda e: e.tensor_scalar(out=LP.sp8[:], in0=lam[:], scalar1=-8.0, scalar2=None, op0=ALU.mult),
         r=[lam], w=[LP.sp8])
    P.op("dve", lambda e: e.tensor_scalar(out=LP.sp16[:], in0=lam[:], scalar1=-16.0, scalar2=None, op0=ALU.mult),
         r=[lam], w=[LP.sp16])
    LP.lb = P.sbuf("lb", [128, 2, 8])
    LP.oml = P.sbuf("oml", [128, 2, 8])
    LP.noml = P.sbuf("noml", [128, 2, 8])
    if l == 0:
        P.op("pool", lambda e: e.memset(LP.lb[:], 0.0), w=[LP.lb])
    else:
        d0 = P.sbuf("lbd0", [128, 2, 8])
        for d in range(2):
            col_load(P, d0[:, d, :], C.lb_d, (0 * 2 + d) * 1024, 1024, [d0])
            col_load(P, LP.lb[:, d, :], C.lb_d, (1 * 2 + d) * 1024, 1024, [LP.lb])
        P.op("dve", lambda e: e.tensor_tensor(out=LP.lb[:], in0=LP.lb[:], in1=d0[:], op=ALU.subtract),
             r=[LP.lb, d0], w=[LP.lb])
        P.op("act", lambda e: e.activation(out=LP.lb[:], in_=LP.lb[:], func=AF.Sigmoid), r=[LP.lb], w=[LP.lb])
    P.op("dve", lambda e: e.tensor_scalar(out=LP.oml[:], in0=LP.lb[:], scalar1=-1.0, scalar2=1.0, op0=ALU.mult,
                                          op1=ALU.add), r=[LP.lb], w=[LP.oml])
    P.op("dve", lambda e: e.tensor_scalar(out=LP.noml[:], in0=LP.oml[:], scalar1=-1.0, scalar2=None, op0=ALU.mult),
         r=[LP.oml], w=[LP.noml])
    LP.wr = P.sbuf("wr", [128, 16, NE])
    P.dma("sp", LP.wr[:], C.w_router[:, :].rearrange("(c p) e -> p c e", p=128), w=[LP.wr])
    LP.br = P.sbuf("br", [128, NE])
    P.dma("sp", LP.br[:], pbc(C.b_router, 0, NE), w=[LP.br])
    LP.modP = P.sbuf("modP", [128, 2, 6, 16])
    LP.s1 = P.sbuf("s1", [128, 2, 16])
    LP.s2 = P.sbuf("s2", [128, 2, 16])
    return LP


def stage_mod(P, C, l, LP):
    with P.scope():
        cc = P.sbuf("cc", [128, 16, 2])
        col_load(P, cc[:, :, 0], C.c, 0, D, [cc])
        col_load(P, cc[:, :, 1], C.c_ctx, 0, D, [cc])
        scT = P.sbuf("scT", [128, 16, 2], BF16)
        P.op("act", lambda e: e.activation(out=scT[:], in_=cc[:], func=AF.Silu), r=[cc], w=[scT])
        bada = P.sbuf("bada", [2, 6 * D])
        P.dma("sp", bada[:], pbc(C.b_ada, l * 6 * D, 6 * D, parts=2), w=[bada])
        modsb = P.sbuf("modsb", [2, 6 * D])
        wsrc = C.w_ada[l, :, :].rearrange("(c p) n -> p c n", p=128)
        W = [P.sbuf(f"wada{i}", [128, 16, 512], BF16) for i in range(2)]
        ps = [P.psum(f"psmod{i}") for i in range(2)]
        for g in range(24):
            wt = W[g % 2]
            pt = ps[g % 2]
            P.dma("pool", wt[:], wsrc[:, :, g * 512:(g + 1) * 512], w=[wt])
            for c in range(16):
                P.op("pe", lambda e, c=c, wt=wt, pt=pt: e.matmul(pt[0:2, :], lhsT=scT[:, c, :], rhs=wt[:, c, :],
                                                               start=(c == 0), stop=(c == 15)),
                     r=[scT, wt], w=[pt])
            P.op("dve", lambda e, g=g, pt=pt: e.tensor_tensor(out=modsb[:, g * 512:(g + 1) * 512], in0=pt[0:2, :],
                                                              in1=bada[:, g * 512:(g + 1) * 512], op=ALU.add),
                 r=[pt, bada], w=[modsb])
        P.dma("sp", C.MOD[:, :], modsb[:], r=[modsb])
    for r in range(2):
        for k in range(6):
            col_load(P, LP.modP[:, r, k, :], C.MOD, r * 6 * D + k * D, D, [LP.modP])
    for (s, n, k) in ((LP.s1, LP.n1, 1), (LP.s2, LP.n2, 4)):
        for r in range(2):
            P.op("dve", lambda e, s=s, n=n, k=k, r=r: e.scalar_tensor_tensor(
                out=s[:, r, :], in0=LP.modP[:, r, k, :], scalar=1.0, in1=n[:], op0=ALU.add, op1=ALU.mult),
                r=[LP.modP, n], w=[s])


def norm_tile(P, xt, xn_out_dtype_tile, sq, ss, n):
    P.op("act", lambda e: e.activation(out=sq[:], in_=xt[:], func=AF.Square), r=[xt], w=[sq])
    P.op("dve", lambda e: e.tensor_reduce(out=ss[:], in_=sq[:], axis=AX.X, op=ALU.add), r=[sq], w=[ss])
    P.op("act", lambda e: e.activation(out=ss[:], in_=ss[:], func=AF.Sqrt, scale=1.0 / n, bias=EPS), r=[ss], w=[ss])
    P.op("dve", lambda e: e.reciprocal(out=ss[:], in_=ss[:]), r=[ss], w=[ss])
    P.op("dve", lambda e: e.tensor_scalar(out=xn_out_dtype_tile[:], in0=xt[:], scalar1=ss[:, 0:1], scalar2=None,
                                          op0=ALU.mult), r=[xt, ss], w=[xn_out_dtype_tile])


def stage_norm_T(P, C, l, LP):
    hT = P.sbuf("hT", [128, 16, T], BF16)
    hT_tok = [Tok(f"hT{t}") for t in range(NT)]
    with P.scope():
        xt = [P.sbuf(f"xt{i}", [128, D]) for i in range(2)]
        sq = P.sbuf("sq", [128, D])
        xn = [P.sbuf(f"xn{i}", [128, D], BF16) for i in range(2)]
        ss = [P.sbuf(f"ss{i}", [128, 1]) for i in range(2)]
        pst = [P.psum(f"pst{i}", [128, 1024], BF16) for i in range(2)]
        P.dma("sp", xt[0][:], C.XRES[0:128, :], w=[xt[0]])
        for t in range(NT):
            if t + 1 < NT:
                P.dma("sp", xt[(t + 1) % 2][:], C.XRES[(t + 1) * 128:(t + 2) * 128, :], w=[xt[(t + 1) % 2]])
            X, XN, SS = xt[t % 2], xn[t % 2], ss[t % 2]
            norm_tile(P, X, XN, sq, SS, D)
            r = 1 if t < 2 else 0
            for c4 in range(4):
                pt = pst[c4 % 2]
                for j in range(4):
                    c = c4 * 4 + j
                    P.op("pe", lambda e, c=c, j=j, pt=pt, XN=XN: e.transpose(
                        out=pt[:, j * 128:(j + 1) * 128], in_=XN[:, c * 128:(c + 1) * 128], identity=C.ident[:]),
                        r=[XN, C.ident], w=[pt])
                for j in range(4):
                    c = c4 * 4 + j
                    eng = "act" if j % 2 == 0 else "dve"
                    if eng == "act":
                        P.op("act", lambda e, c=c, j=j, pt=pt, t=t, r=r: e.activation(
                            out=hT[:, c, t * 128:(t + 1) * 128], in_=pt[:, j * 128:(j + 1) * 128], func=AF.Identity,
                            scale=LP.s1[:, r, c:c + 1], bias=LP.modP[:, r, 0, c:c + 1]),
                            r=[pt, LP.s1, LP.modP], w=[hT_tok[t]])
                    else:
                        P.op("dve", lambda e, c=c, j=j, pt=pt, t=t, r=r: e.tensor_scalar(
                            out=hT[:, c, t * 128:(t + 1) * 128], in0=pt[:, j * 128:(j + 1) * 128],
                            scalar1=LP.s1[:, r, c:c + 1], scalar2=LP.modP[:, r, 0, c:c + 1], op0=ALU.mult,
                            op1=ALU.add), r=[pt, LP.s1, LP.modP], w=[hT_tok[t]])
    return hT, hT_tok


def proj_groups():
    g = []
    g += [("aq", 0), ("aq", 1), ("akv", 0)]
    g += [("bq", 0), ("bq", 1), ("bk", 0), ("bk", 1), ("bv", 0), ("bv", 1)]
    g += [("cx", 0), ("cx", 1), ("cy", 0), ("cy", 1)]
    g += [("dq", 0), ("dq", 1), ("dff", 0), ("dff", 1), ("dfb", 0), ("dfb", 1)]
    g += [("di", 0), ("di", 1), ("dg", 0), ("dg", 1)]
    g += [("gt", i) for i in range(16)]
    return g


def stage_proj(P, C, l, LP, hT, hT_tok):
    groups = proj_groups()
    assert len(groups) * 512 == IN_W
    with P.scope():
        wsrc = C.w_in[l, :, :].rearrange("(c p) n -> p c n", p=128)
        W = [P.sbuf(f"win{i}", [128, 16, 512], BF16) for i in range(3)]
        ps = [P.psum(f"psproj{i}") for i in range(5)]
        pst = [P.psum(f"pstq{i}", [128, 1024], BF16) for i in range(2)]
        ca = [P.sbuf(f"ca{i}", [128, 128]) for i in range(2)]
        sa = [P.sbuf(f"sa{i}", [128, 128]) for i in range(2)]
        cb = [P.sbuf(f"cb{i}", [128, 64]) for i in range(2)]
        sb = [P.sbuf(f"sb{i}", [128, 64]) for i in range(2)]
        sq = P.sbuf("qsq", [128, 512])
        ss = P.sbuf("qss", [128, 8])
        qn = P.sbuf("qn", [128, 512])
        t1 = P.sbuf("qt1", [128, 512])
        t2 = P.sbuf("qt2", [128, 512])
        qb = [P.sbuf(f"qb{i}", [128, 512], BF16) for i in range(4)]
        qT = [P.sbuf(f"qT{i}", [128, 512], BF16) for i in range(2)]
        ob = [P.sbuf(f"ob{i}", [128, 512], BF16) for i in range(3)]
        of = [P.sbuf(f"of{i}", [128, 512], F32) for i in range(2)]
        gl = [P.sbuf(f"gl{i}", [128, 512], F32) for i in range(2)]
        cnt = {"ps": 0, "ob": 0, "of": 0, "qb": 0, "pst": 0, "qT": 0, "rope": 0}

        def nxt(key, lst):
            i = cnt[key]
            cnt[key] += 1
            return lst[i % len(lst)]

        pending = []

        def flush_pending(keep):
            while len(pending) > keep:
                pending.pop(0)()

        def qk_epi(pt, t, ncomp, dim, gain, cosT, sinT, dst, dst_h0, nheads_out):
            w = ncomp * dim
            P.op("act", lambda e: e.activation(out=sq[:, :w], in_=pt[:, :w], func=AF.Square), r=[pt], w=[sq])
            P.op("dve", lambda e: e.tensor_reduce(out=ss[:, :ncomp], in_=sq[:, :w].rearrange("p (h d) -> p h d", d=dim),
                                                  axis=AX.X, op=ALU.add), r=[sq], w=[ss])
            P.op("act", lambda e: e.activation(out=ss[:, :ncomp], in_=ss[:, :ncomp], func=AF.Sqrt, scale=1.0 / dim,
                                               bias=EPS), r=[ss], w=[ss])
            P.op("dve", lambda e: e.reciprocal(out=ss[:, :ncomp], in_=ss[:, :ncomp]), r=[ss], w=[ss])
            v3 = lambda tl: tl[:, :w].rearrange("p (h d) -> p h d", d=dim)
            P.op("dve", lambda e: e.tensor_tensor(out=v3(qn), in0=v3(pt), in1=bc_last(ss[:, :ncomp], dim),
                                                  op=ALU.mult), r=[pt, ss], w=[qn])
            QB = nxt("qb", qb)
            if t >= 2:
                P.op("pool", lambda e: e.tensor_tensor(out=v3(qn), in0=v3(qn), in1=bc_mid(gain[:, :dim], ncomp),
                                                       op=ALU.mult), r=[qn, gain], w=[qn])
                hd = dim // 4
                ng = w // (2 * hd)
                v4 = lambda tl: tl[:, :w].rearrange("p (g s i) -> p g s i", s=2, i=hd)
                def tb(tab, s):
                    b = tab[:, :].rearrange("p (g s i) -> p g s i", s=2, i=hd)[:, :, s, :]
                    return mk(b, [b.ap[0], [0, ncomp], b.ap[1], b.ap[2]])
                q5 = lambda tl, s: tl[:, :w].rearrange("p (h g s i) -> p h g s i", h=ncomp, s=2, i=hd)[:, :, :, s, :]
                P.op("dve", lambda e: e.tensor_tensor(out=v3(t1), in0=v3(qn), in1=bc_mid(cosT[:, :dim], ncomp),
                                                      op=ALU.mult), r=[qn, cosT], w=[t1])
                for s in range(2):
                    P.op("pool", lambda e, s=s: e.tensor_tensor(out=q5(t2, s), in0=q5(qn, 1 - s), in1=tb(sinT, s),
                                                                op=ALU.mult), r=[qn, sinT], w=[t2])
                P.op("dve", lambda e: e.tensor_tensor(out=QB[:, :w], in0=t1[:, :w], in1=t2[:, :w], op=ALU.add),
                     r=[t1, t2], w=[QB])
            else:
                P.op("pool", lambda e: e.tensor_tensor(out=v3(QB), in0=v3(qn), in1=bc_mid(gain[:, :dim], ncomp),
                                                       op=ALU.mult), r=[qn, gain], w=[QB])
            nblk = w // 128

            def part_b():
                PT = nxt("pst", pst)
                for j in range(nblk):
                    P.op("pe", lambda e, j=j: e.transpose(out=PT[:, j * 128:(j + 1) * 128],
                                                          in_=QB[:, j * 128:(j + 1) * 128], identity=C.ident[:]),
                         r=[QB, C.ident], w=[PT])
                QT = nxt("qT", qT)
                P.op("act", lambda e: e.activation(out=QT[:, :w], in_=PT[:, :w], func=AF.Copy), r=[PT], w=[QT])
                dsta = bass.AP(dst.t, dst_h0 * 128 * T + t * 128, [[T, 128], [128 * T, nblk], [1, 128]])
                P.dma("sp", dsta, QT[:, :w].rearrange("p (h q) -> p h q", q=128), r=[QT])

            pending.append(part_b)

        for gi, (kind, idx) in enumerate(groups):
            wt = W[gi % 3]
            P.dma("pool", wt[:], wsrc[:, :, gi * 512:(gi + 1) * 512], w=[wt])
            tokmajor = kind in ("aq", "akv", "bq", "bk", "bv", "di", "dg")
            if tokmajor:
                for t in range(NT):
                    pt = nxt("ps", ps)
                    for c in range(16):
                        P.op("pe", lambda e, c=c, t=t, pt=pt, wt=wt: e.matmul(
                            pt[:], lhsT=hT[:, c, t * 128:(t + 1) * 128], rhs=wt[:, c, :], start=(c == 0),
                            stop=(c == 15)), r=[hT_tok[t], wt], w=[pt])
                    flush_pending(1)
                    lat = t >= 2
                    if lat and kind in ("aq", "akv", "bq", "bk"):
                        k = cnt["rope"] % 2
                        cnt["rope"] += 1
                        r0 = (t - 2) * 128
                        if kind in ("aq", "akv"):
                            P.dma("sp", ca[k][:], C.ropeA_c[r0:r0 + 128, :], w=[ca[k]])
                            P.dma("sp", sa[k][:], C.ropeA_s[r0:r0 + 128, :], w=[sa[k]])
                            cT, sT = ca[k], sa[k]
                        else:
                            P.dma("sp", cb[k][:], C.ropeB_c[r0:r0 + 128, :], w=[cb[k]])
                            P.dma("sp", sb[k][:], C.ropeB_s[r0:r0 + 128, :], w=[sb[k]])
                            cT, sT = cb[k], sb[k]
                    else:
                        cT = sT = None
                    if kind == "aq":
                        qk_epi(pt, t, 4, 128, LP.qn_a, cT, sT, C.QAT, idx * 4, 4)
                    elif kind == "akv":
                        qk_epi(pt, t, 2, 128, LP.kn_a, cT, sT, C.KAT, 0, 2)
                        O = nxt("ob", ob)
                        P.op("act", lambda e, O=O, pt=pt: e.activation(out=O[:, :256], in_=pt[:, 256:512],
                                                                       func=AF.Copy), r=[pt], w=[O])
                        P.dma("sp", C.VA[t * 128:(t + 1) * 128, :], O[:, :256], r=[O])
                    elif kind == "bq":
                        qk_epi(pt, t, 8, 64, LP.qn_b, cT, sT, C.QBT, idx * 4, 4)
                    elif kind == "bk":
                        qk_epi(pt, t, 8, 64, LP.kn_b, cT, sT, C.KBT, idx * 4, 4)
                    else:
                        O = nxt("ob", ob)
                        dst = {"bv": C.VB, "di": C.VD, "dg": C.SGD}[kind]
                        fn = AF.Silu if kind == "dg" else AF.Copy
                        P.op("act", lambda e, O=O, pt=pt, fn=fn: e.activation(out=O[:], in_=pt[:], func=fn),
                             r=[pt], w=[O])
                        P.dma("sp", dst[t * 128:(t + 1) * 128, idx * 512:(idx + 1) * 512], O[:], r=[O])
            else:
                flush_pending(0)
                for cbk in range(4):
                    for (t0, tn) in TG:
                        pt = nxt("ps", ps)
                        toks = [hT_tok[t] for t in range(t0 // 128, (t0 + tn) // 128)]
                        for c in range(16):
                            P.op("pe", lambda e, c=c, pt=pt, wt=wt, cbk=cbk, t0=t0, tn=tn: e.matmul(
                                pt[:, :tn], lhsT=wt[:, c, cbk * 128:(cbk + 1) * 128], rhs=hT[:, c, t0:t0 + tn],
                                start=(c == 0), stop=(c == 15)), r=toks + [wt], w=[pt])
                        row = idx * 512 + cbk * 128
                        if kind in ("cx", "dff", "dfb"):
                            O = nxt("of", of)
                            P.op("act", lambda e, O=O, pt=pt, tn=tn: e.activation(out=O[:, :tn], in_=pt[:, :tn],
                                                                                  func=AF.Copy), r=[pt], w=[O])
                            if kind == "cx":
                                dst = C.CXT[row:row + 128, t0:t0 + tn]
                            else:
                                dst = C.ZF[0 if kind == "dff" else 1, row:row + 128, t0:t0 + tn]
                            P.dma("sp", dst, O[:, :tn], r=[O])
                        elif kind == "cy":
                            G = nxt("of", gl)
                            O = nxt("ob", ob)
                            P.op("act", lambda e, G=G, pt=pt, tn=tn: e.activation(out=G[:, :tn], in_=pt[:, :tn],
                                                                                  func=AF.Square), r=[pt], w=[G])
                            P.op("dve", lambda e, G=G, tn=tn: e.tensor_scalar(
                                out=G[:, :tn], in0=G[:, :tn], scalar1=0.044715 * 1.5957691216, scalar2=1.5957691216,
                                op0=ALU.mult, op1=ALU.add), r=[G], w=[G])
                            P.op("dve", lambda e, G=G, pt=pt, tn=tn: e.tensor_tensor(
                                out=G[:, :tn], in0=G[:, :tn], in1=pt[:, :tn], op=ALU.mult), r=[G, pt], w=[G])
                            P.op("act", lambda e, G=G, tn=tn: e.activation(out=G[:, :tn], in_=G[:, :tn],
                                                                           func=AF.Sigmoid), r=[G], w=[G])
                            P.op("dve", lambda e, G=G, O=O, pt=pt, tn=tn: e.tensor_tensor(
                                out=O[:, :tn], in0=G[:, :tn], in1=pt[:, :tn], op=ALU.mult), r=[G, pt], w=[O])
                            P.dma("sp", C.GCY[row:row + 128, t0:t0 + tn], O[:, :tn], r=[O])
                        else:
                            O = nxt("ob", ob)
                            fn = AF.Sigmoid if kind == "gt" else AF.Copy
                            P.op("act", lambda e, O=O, pt=pt, tn=tn, fn=fn: e.activation(
                                out=O[:, :tn], in_=pt[:, :tn], func=fn), r=[pt], w=[O])
                            dst = (C.SGT if kind == "gt" else C.DQT)[row:row + 128, t0:t0 + tn]
                            P.dma("sp", dst, O[:, :tn], r=[O])


def stage_attn_a(P, C, l, LP):
    scale = 128 ** -0.5
    with P.scope():
        QA = P.sbuf("QA", [128, 8, T], BF16)
        KA = P.sbuf("KA", [128, 2, T], BF16)
        VAs = P.sbuf("VAs", [128, NT, 256], BF16)
        OA = P.sbuf("OA", [128, 8, T], BF16)
        P.dma("sp", QA[:], C.QAT[:, :, :].rearrange("h d t -> d h t"), w=[QA])
        P.dma("sp", KA[:], C.KAT[:, :, :].rearrange("h d t -> d h t"), w=[KA])
        P.dma("sp", VAs[:], C.VA[:, :].rearrange("(n p) v -> p n v", p=128), w=[VAs])
        pss = [P.psum(f"pss{i}") for i in range(2)]
        pso = [P.psum(f"pso{i}") for i in range(2)]
        psz = [P.psum(f"psz{i}") for i in range(2)]
        pT = [P.sbuf(f"pT{i}", [128, 512], BF16) for i in range(3)]
        zs = P.sbuf("zs", [128, 512])
        ot = P.sbuf("ot", [128, 512])
        OA_tok = Tok("OA")
        steps = []
        it = 0
        for qb in range(NT):
            if qb < 2:
                kbs = [(0, None), (1, None)]
            else:
                n = qb - 2
                kbs = [(0, None), (1, None)]
                if n >= 1:
                    kbs.append((qb - 1, C.m_ge))
                kbs.append((qb, None))
                if n <= 14:
                    kbs.append((qb + 1, C.m_le))
            for hk in range(2):
                po, pz = pso[it % 2], psz[it % 2]
                it += 1
                for ki, (kb, mask) in enumerate(kbs):
                    steps.append(dict(qb=qb, hk=hk, kb=kb, mask=mask, po=po, pz=pz, first=(ki == 0),
                                      last=(ki == len(kbs) - 1)))
        for i, st in enumerate(steps):
            st["psx"] = pss[i % 2]
            st["pt"] = pT[i % 3]

        def emit_s(st):
            psx, kb, hk, qb = st["psx"], st["kb"], st["hk"], st["qb"]
            rhs_q = QA[:, 4 * hk:4 * hk + 4, qb * 128:(qb + 1) * 128]
            P.op("pe", lambda e: e.matmul(psx[:].rearrange("p (h q) -> p h q", h=4),
                                          lhsT=KA[:, hk, kb * 128:(kb + 1) * 128], rhs=rhs_q, start=True, stop=True),
                 r=[KA, QA], w=[psx])

        def emit_rest(st):
            psx, pt_, kb, hk, qb, mask, po, pz = (st["psx"], st["pt"], st["kb"], st["hk"], st["qb"], st["mask"],
                                                  st["po"], st["pz"])
            f, l_ = st["first"], st["last"]
            P.op("act", lambda e: e.activation(out=pt_[:], in_=psx[:], func=AF.Exp, scale=scale), r=[psx], w=[pt_])
            if mask is not None:
                P.op("pool", lambda e: e.tensor_tensor(
                    out=pt_[:].rearrange("p (h q) -> p h q", h=4), in0=pt_[:].rearrange("p (h q) -> p h q", h=4),
                    in1=bc_mid(mask[:, :], 4), op=ALU.mult), r=[pt_, mask], w=[pt_])
            P.op("pe", lambda e: e.matmul(po[:], lhsT=VAs[:, kb, hk * 128:(hk + 1) * 128], rhs=pt_[:], start=f,
                                          stop=l_), r=[VAs, pt_], w=[po])
            P.op("pe", lambda e: e.matmul(pz[:], lhsT=C.ones[:], rhs=pt_[:], start=f, stop=l_),
                 r=[C.ones, pt_], w=[pz])
            if l_:
                P.op("dve", lambda e: e.tensor_tensor(
                    out=zs[:].rearrange("p (h q) -> p h q", h=4), in0=pz[:].rearrange("p (h q) -> p h q", h=4),
                    in1=bc_last(LP.esink[:, 4 * hk:4 * hk + 4], 128), op=ALU.add), r=[pz, LP.esink], w=[zs])
                P.op("dve", lambda e: e.reciprocal(out=zs[:], in_=zs[:]), r=[zs], w=[zs])
                P.op("dve", lambda e: e.tensor_tensor(
                    out=OA[:, 4 * hk:4 * hk + 4, qb * 128:(qb + 1) * 128],
                    in0=po[:].rearrange("p (h q) -> p h q", h=4), in1=zs[:].rearrange("p (h q) -> p h q", h=4),
                    op=ALU.mult), r=[po, zs], w=[OA_tok])

        emit_s(steps[0])
        for i, st in enumerate(steps):
            if i + 1 < len(steps):
                emit_s(steps[i + 1])
            emit_rest(st)
        P.dma("sp", C.OT[0, :, :].rearrange("(h d) t -> d h t", d=128), OA[:], r=[OA_tok])


def stage_attn_b(P, C, l, LP):
    scale = 64 ** -0.5
    with P.scope():
        QB_ = [P.sbuf(f"QBh{i}", [128, T], BF16) for i in range(2)]
        KB_ = [P.sbuf(f"KBh{i}", [128, T], BF16) for i in range(2)]
        VB_ = [P.sbuf(f"VBh{i}", [128, NT, 128], BF16) for i in range(2)]
        OB_ = [P.sbuf(f"OBh{i}", [128, T], BF16) for i in range(2)]
        pss = [P.psum(f"bpss{i}") for i in range(2)]
        pso = [P.psum(f"bpso{i}") for i in range(2)]
        psz = [P.psum(f"bpsz{i}") for i in range(2)]
        psn = P.psum("bpsn")
        pT = [P.sbuf(f"bpT{i}", [128, 512], BF16) for i in range(3)]
        r1 = P.sbuf("br1", [128, 512])
        r2 = P.sbuf("br2", [128, 512])
        o1 = P.sbuf("bo1", [128, 512])
        o2 = P.sbuf("bo2", [128, 512])
        osq = P.sbuf("bosq", [128, 512], BF16)
        ip = 0

        def load(h):
            k = h % 2
            P.dma("sp", QB_[k][:], C.QBT[h, :, :], w=[QB_[k]])
            P.dma("sp", KB_[k][:], C.KBT[h, :, :], w=[KB_[k]])
            P.dma("sp", VB_[k][:], C.VB[:, h * 128:(h + 1) * 128].rearrange("(n p) v -> p n v", p=128), w=[VB_[k]])

        load(0)
        for h in range(8):
            if h + 1 < 8:
                load(h + 1)
            Q, K, V, O = QB_[h % 2], KB_[h % 2], VB_[h % 2], OB_[h % 2]
            steps = []
            for (q0, qn_, nkb) in [(0, 256, 2)] + [(LC + i * 512, 512, NT) for i in range(4)]:
                for c in range(2):
                    for kb in range(nkb):
                        steps.append(dict(q0=q0, qn=qn_, c=c, kb=kb, first=(kb == 0), last=(kb == nkb - 1),
                                          fin=(c == 1 and kb == nkb - 1)))
            for st in steps:
                st["psx"] = pss[ip % 2]
                st["pt"] = pT[ip % 3]
                ip += 1

            def emit_s(st, K=K, Q=Q):
                psx, c, kb, q0, qn_ = st["psx"], st["c"], st["kb"], st["q0"], st["qn"]
                P.op("pe", lambda e: e.matmul(psx[:, :qn_], lhsT=K[c * 64:(c + 1) * 64, kb * 128:(kb + 1) * 128],
                                              rhs=Q[c * 64:(c + 1) * 64, q0:q0 + qn_], start=True, stop=True),
                     r=[K, Q], w=[psx])

            def emit_rest(st, V=V, O=O):
                psx, pt_, c, kb, q0, qn_ = st["psx"], st["pt"], st["c"], st["kb"], st["q0"], st["qn"]
                f, l_ = st["first"], st["last"]
                po, pz = pso[c], psz[c]
                P.op("act", lambda e: e.activation(out=pt_[:, :qn_], in_=psx[:, :qn_], func=AF.Exp, scale=scale),
                     r=[psx], w=[pt_])
                P.op("pe", lambda e: e.matmul(po[:, :qn_], lhsT=V[:, kb, :], rhs=pt_[:, :qn_], start=f, stop=l_),
                     r=[V, pt_], w=[po])
                P.op("pe", lambda e: e.matmul(pz[:, :qn_], lhsT=C.ones[:], rhs=pt_[:, :qn_], start=f, stop=l_),
                     r=[C.ones, pt_], w=[pz])
                if not st["fin"]:
                    return
                P.op("dve", lambda e: e.reciprocal(out=r1[:, :qn_], in_=psz[0][:, :qn_]), r=[psz[0]], w=[r1])
                P.op("dve", lambda e: e.reciprocal(out=r2[:, :qn_], in_=psz[1][:, :qn_]), r=[psz[1]], w=[r2])
                P.op("dve", lambda e: e.tensor_tensor(out=o1[:, :qn_], in0=pso[0][:, :qn_], in1=r1[:, :qn_],
                                                      op=ALU.mult), r=[pso[0], r1], w=[o1])
                P.op("dve", lambda e: e.tensor_tensor(out=o2[:, :qn_], in0=pso[1][:, :qn_], in1=r2[:, :qn_],
                                                      op=ALU.mult), r=[pso[1], r2], w=[o2])
                P.op("dve", lambda e: e.scalar_tensor_tensor(
                    out=o1[:, :qn_], in0=o2[:, :qn_], scalar=LP.nlam[:, 0:1], in1=o1[:, :qn_], op0=ALU.mult,
                    op1=ALU.add), r=[o1, o2, LP.nlam], w=[o1])
                P.op("act", lambda e: e.activation(out=osq[:, :qn_], in_=o1[:, :qn_], func=AF.Square),
                     r=[o1], w=[osq])
                P.op("pe", lambda e: e.matmul(psn[:, :qn_], lhsT=C.ones[:], rhs=osq[:, :qn_], start=True, stop=True),
                     r=[C.ones, osq], w=[psn])
                P.op("act", lambda e: e.activation(out=r1[:, :qn_], in_=psn[:, :qn_], func=AF.Sqrt,
                                                   scale=1.0 / 128, bias=EPS), r=[psn], w=[r1])
                P.op("dve", lambda e: e.reciprocal(out=r1[:, :qn_], in_=r1[:, :qn_]), r=[r1], w=[r1])
                P.op("dve", lambda e: e.scalar_tensor_tensor(
                    out=O[:, q0:q0 + qn_], in0=o1[:, :qn_], scalar=LP.subln[:, 0:1], in1=r1[:, :qn_], op0=ALU.mult,
                    op1=ALU.mult), r=[o1, r1, LP.subln], w=[O])

            emit_s(steps[0])
            for i, st in enumerate(steps):
                if i + 1 < len(steps):
                    emit_s(steps[i + 1])
                emit_rest(st)
            P.dma("sp", C.OT[1, h * 128:(h + 1) * 128, :], O[:], r=[O])


def rev(ap2):
    (ps, pn), (s, n) = ap2.ap
    return bass.AP(ap2.tensor, ap2.offset + s * (n - 1), [[ps, pn], [-s, n]])


def stage_rglru(P, C, l, LP):
    with P.scope():
        wr = P.sbuf("wrg", [128, 16, 128], BF16)
        wi = P.sbuf("wig", [128, 16, 128], BF16)
        P.dma("pool", wr[:], C.w_rg[l, :, :, :, :].rearrange("d b i o -> i (d b) o"), w=[wr])
        P.dma("pool", wi[:], C.w_ig[l, :, :, :, :].rearrange("d b i o -> i (d b) o"), w=[wi])
        x = P.sbuf("cx", [128, T])
        u = P.sbuf("cu", [128, T])
        ub = P.sbuf("cub", [128, T], BF16)
        rr = P.sbuf("crr", [128, T])
        ii = P.sbuf("cii", [128, T])
        aa = P.sbuf("caa", [128, T])
        vv = P.sbuf("cvv", [128, T])
        hh = [P.sbuf(f"chh{d}", [128, T]) for d in range(2)]
        gy = P.sbuf("cgy", [128, T], BF16)
        oo = P.sbuf("coo", [128, T], BF16)
        ps = [P.psum(f"cps{i}") for i in range(4)]
        ip = 0
        for ch in range(8):
            P.dma("sp", x[:], C.CXT[ch * 128:(ch + 1) * 128, :], w=[x])
            P.dma("sp", gy[:], C.GCY[ch * 128:(ch + 1) * 128, :], w=[gy])
            P.op("dve", lambda e, ch=ch: e.tensor_scalar(out=u[:], in0=x[:], scalar1=LP.convw[:, 2, ch:ch + 1],
                                                         scalar2=LP.convb[:, ch:ch + 1], op0=ALU.mult, op1=ALU.add),
                 r=[x, LP.convw, LP.convb], w=[u])
            for (s0, s1) in ((0, LC), (LC, T)):
                for tap in (0, 1, 3):
                    off = tap - 2
                    lo = s0 + max(0, -off)
                    hi = s1 - max(0, off)
                    P.op("dve", lambda e, ch=ch, tap=tap, lo=lo, hi=hi, off=off: e.scalar_tensor_tensor(
                        out=u[:, lo:hi], in0=x[:, lo + off:hi + off], scalar=LP.convw[:, tap, ch:ch + 1],
                        in1=u[:, lo:hi], op0=ALU.mult, op1=ALU.add), r=[x, u, LP.convw], w=[u])
            P.op("act", lambda e: e.activation(out=ub[:], in_=u[:], func=AF.Copy), r=[u], w=[ub])
            for d in range(2):
                for (t0, tn) in TG:
                    pr_, pi_ = ps[ip % 4], ps[(ip + 1) % 4]
                    ip += 2
                    P.op("pe", lambda e, pr_=pr_, d=d, ch=ch, t0=t0, tn=tn: e.matmul(
                        pr_[:, :tn], lhsT=wr[:, d * 8 + ch, :], rhs=ub[:, t0:t0 + tn], start=True, stop=True),
                        r=[wr, ub], w=[pr_])
                    P.op("pe", lambda e, pi_=pi_, d=d, ch=ch, t0=t0, tn=tn: e.matmul(
                        pi_[:, :tn], lhsT=wi[:, d * 8 + ch, :], rhs=ub[:, t0:t0 + tn], start=True, stop=True),
                        r=[wi, ub], w=[pi_])
                    P.op("act", lambda e, pr_=pr_, d=d, ch=ch, t0=t0, tn=tn: e.activation(
                        out=rr[:, t0:t0 + tn], in_=pr_[:, :tn], func=AF.Sigmoid, bias=LP.brg[:, d, ch:ch + 1]),
                        r=[pr_, LP.brg], w=[rr])
                    P.op("act", lambda e, pi_=pi_, d=d, ch=ch, t0=t0, tn=tn: e.activation(
                        out=ii[:, t0:t0 + tn], in_=pi_[:, :tn], func=AF.Sigmoid, bias=LP.big[:, d, ch:ch + 1]),
                        r=[pi_, LP.big], w=[ii])
                P.op("act", lambda e, d=d, ch=ch: e.activation(out=aa[:], in_=rr[:], func=AF.Exp,
                                                               scale=LP.sp8[:, d, ch:ch + 1]), r=[rr, LP.sp8], w=[aa])
                P.op("act", lambda e, d=d, ch=ch: e.activation(out=vv[:], in_=rr[:], func=AF.Exp,
                                                               scale=LP.sp16[:, d, ch:ch + 1]), r=[rr, LP.sp16], w=[vv])
                P.op("act", lambda e: e.activation(out=vv[:], in_=vv[:], func=AF.Sqrt, scale=-1.0, bias=1.0),
                     r=[vv], w=[vv])
                P.op("dve", lambda e: e.tensor_tensor(out=vv[:], in0=vv[:], in1=ii[:], op=ALU.mult), r=[vv, ii], w=[vv])
                P.op("dve", lambda e: e.tensor_tensor(out=vv[:], in0=vv[:], in1=u[:], op=ALU.mult), r=[vv, u], w=[vv])
                H = hh[d]
                if d == 0:
                    P.op("dve", lambda e, H=H: e.tensor_tensor_scan(out=H[:], data0=aa[:], data1=vv[:], initial=0.0,
                                                                    op0=ALU.mult, op1=ALU.add), r=[aa, vv], w=[H])
                else:
                    P.op("dve", lambda e, H=H: e.tensor_tensor_scan(
                        out=rev(H[:, 0:LC]), data0=rev(aa[:, 0:LC]), data1=rev(vv[:, 0:LC]), initial=0.0,
                        op0=ALU.mult, op1=ALU.add), r=[aa, vv], w=[H])
                    P.op("dve", lambda e, H=H: e.tensor_tensor_scan(
                        out=rev(H[:, LC:T]), data0=rev(aa[:, LC:T]), data1=rev(vv[:, LC:T]), initial=H[:, 0:1],
                        op0=ALU.mult, op1=ALU.add), r=[aa, vv, H], w=[H])
            P.op("dve", lambda e: e.tensor_tensor(out=hh[0][:], in0=hh[0][:], in1=hh[1][:], op=ALU.add),
                 r=[hh[0], hh[1]], w=[hh[0]])
            P.op("dve", lambda e: e.tensor_tensor(out=oo[:], in0=hh[0][:], in1=gy[:], op=ALU.mult),
                 r=[hh[0], gy], w=[oo])
            P.dma("sp", C.OT[2, ch * 128:(ch + 1) * 128, :], oo[:], r=[oo])


def stage_hgrn(P, C, l, LP):
    NCK = T // 64
    with P.scope():
        o1 = C.ones_f[:, 0:1]
        ones_bc = mk(o1, [o1.ap[0], [0, T]])
        mf = P.sbuf("mf", [64, 64], BF16)
        mb = P.sbuf("mb", [64, 64], BF16)
        P.op("dve", lambda e: e.tensor_copy(out=mf[:], in_=C.m_le[0:64, 0:64]), r=[C.m_le], w=[mf])
        P.op("dve", lambda e: e.tensor_copy(out=mb[:], in_=C.m_ge[0:64, 0:64]), r=[C.m_ge], w=[mb])
        z = P.sbuf("dz", [128, T])
        sg = P.sbuf("dsg", [128, T])
        kk = P.sbuf("dkk", [128, T])
        Bi = P.sbuf("dBi", [128, T])
        Be_ = P.sbuf("dBe", [128, T])
        E = P.sbuf("dE", [128, T])
        E2 = z
        qT = P.sbuf("dqT", [128, T], BF16)
        vt = P.sbuf("dvt", [64, NCK, 128], BF16)
        sgd = P.sbuf("dsgd", [64, NCK, 128], BF16)
        qt_ = [P.sbuf(f"dqt{d}", [128, T], BF16) for d in range(2)]
        kt_ = [P.sbuf(f"dkt{d}", [128, T], BF16) for d in range(2)]
        qh_ = [P.sbuf(f"dqh{d}", [128, T], BF16) for d in range(2)]
        khf = P.sbuf("dkhf", [128, T], BF16)
        kh_ = [khf, khf]
        kx_ = [P.sbuf(f"dkx{d}", [128, T], BF16) for d in range(2)]
        dec = [P.sbuf(f"ddec{d}", [128, NCK]) for d in range(2)]
        Sf = [P.sbuf(f"dSf{d}", [128, 128]) for d in range(2)]
        Sb = [P.sbuf(f"dSb{d}", [128, 128], BF16) for d in range(2)]
        Od = [P.sbuf(f"dO{d}", [64, NCK, 128]) for d in range(2)]
        Od_tok = [[Tok(f"Od{d}_{c}") for c in range(NCK // 4)] for d in range(2)]
        A_all = [P.sbuf(f"dAall{d}", [64, NCK, 64], BF16) for d in range(2)]
        KH_all = [P.sbuf(f"dKHall{d}", [64, NCK, 128], BF16) for d in range(2)]
        ps_att = P.psum("dpsatt")
        P.op("dve", lambda e: e.memset(ps_att[:], 0.0), w=[ps_att])
        ps_kh = P.psum("dpskh", [128, 1024], BF16)
        ps_o = [[P.psum(f"dpso{d}{i}") for i in range(2)] for d in range(2)]
        ps_s = [P.psum(f"dpss{d}") for d in range(2)]
        ps_s_tok = [[Tok(f"pss{d}{i}") for i in range(2)] for d in range(2)]
        osq = Od[1]
        oss = P.sbuf("doss", [64, NCK])
        oy = KH_all[0]
        odT = P.sbuf("dodT", [128, T], BF16)

        c3 = lambda tl, off=0: tl[:, off:off + T].rearrange("p (c i) -> p c i", i=64)
        for h in range(8):
            P.dma("sp", qT[:], C.DQT[h * 128:(h + 1) * 128, :], w=[qT])
            P.dma("sp", vt[:], C.VD[:, h * 128:(h + 1) * 128].rearrange("(c p) v -> p c v", p=64), w=[vt])
            P.dma("sp", sgd[:], C.SGD[:, h * 128:(h + 1) * 128].rearrange("(c p) v -> p c v", p=64), w=[sgd])
            def gate_math(d, h):
                P.dma("sp", z[:], C.ZF[d, h * 128:(h + 1) * 128, :], w=[z])
                P.op("act", lambda e: e.activation(out=sg[:], in_=z[:], func=AF.Sigmoid), r=[z], w=[sg])
                P.op("dve", lambda e, d=d, h=h: e.tensor_scalar(
                    out=kk[:], in0=sg[:], scalar1=LP.noml[:, d, h:h + 1], scalar2=LP.oml[:, d, h:h + 1], op0=ALU.mult,
                    op1=ALU.add), r=[sg, LP.noml, LP.oml], w=[kk])
                P.op("act", lambda e, d=d, h=h: e.activation(out=sg[:], in_=sg[:], func=AF.Ln,
                                                             scale=LP.oml[:, d, h:h + 1], bias=LP.lb[:, d, h:h + 1]),
                     r=[sg, LP.oml, LP.lb], w=[sg])
                P.op("dve", lambda e: e.tensor_tensor_scan(out=Bi[:], data0=ones_bc, data1=sg[:],
                                                           initial=0.0, op0=ALU.mult, op1=ALU.add),
                     r=[C.ones_f, sg], w=[Bi])
                P.op("dve", lambda e: e.tensor_tensor(out=Be_[:], in0=Bi[:], in1=sg[:], op=ALU.subtract),
                     r=[Bi, sg], w=[Be_])
                Bx = Bi
                Bsrc = Bi if d == 0 else Be_
                Bv = c3(Bsrc)
                Bs1 = c3(Be_)[:, :, 0:1]
                Be = c3(Bi)[:, :, 63:64]
                c32 = lambda tl, off=0: tl[:, off:off + T].rearrange("p (c i) -> p c i", i=32)
                Bv32 = c32(Bsrc)
                Bref = c32(Bsrc)[:, :, 16:17]
                bcl = lambda a: mk(a, [a.ap[0], a.ap[1], [0, 64]])
                bcl32 = lambda a: mk(a, [a.ap[0], a.ap[1], [0, 32]])
                if d == 0:
                    P.op("dve", lambda e: e.tensor_tensor(out=c32(E), in0=Bv32, in1=bcl32(Bref), op=ALU.subtract),
                         r=[Bi, Be_], w=[E])
                else:
                    P.op("dve", lambda e: e.tensor_tensor(out=c32(E), in0=bcl32(Bref), in1=Bv32, op=ALU.subtract),
                         r=[Bi, Be_], w=[E])
                P.op("act", lambda e: e.activation(out=E2[:], in_=E[:], func=AF.Exp, scale=-1.0), r=[E], w=[E2])
                P.op("act", lambda e: e.activation(out=E[:], in_=E[:], func=AF.Exp), r=[E], w=[E])
                P.op("dve", lambda e, d=d: e.tensor_tensor(out=qt_[d][:], in0=qT[:], in1=E[:], op=ALU.mult),
                     r=[qT, E], w=[qt_[d]])
                P.op("pool", lambda e, d=d: e.tensor_tensor(out=kt_[d][:], in0=kk[:], in1=E2[:], op=ALU.mult),
                     r=[kk, E2], w=[kt_[d]])
                if d == 0:
                    P.op("pool", lambda e: e.tensor_tensor(out=c3(E), in0=Bv, in1=bcl(Bs1), op=ALU.subtract),
                         r=[Bi, Be_], w=[E])
                    P.op("pool", lambda e: e.tensor_tensor(out=c3(E2), in0=bcl(Be), in1=Bv, op=ALU.subtract),
                         r=[Bi, Be_], w=[E2])
                else:
                    P.op("pool", lambda e: e.tensor_tensor(out=c3(E), in0=bcl(Be), in1=Bv, op=ALU.subtract),
                         r=[Bi, Be_], w=[E])
                    P.op("pool", lambda e: e.tensor_tensor(out=c3(E2), in0=Bv, in1=bcl(Bs1), op=ALU.subtract),
                         r=[Bi, Be_], w=[E2])
                P.op("act", lambda e: e.activation(out=sg[:], in_=E[:], func=AF.Exp, scale=-1.0), r=[E], w=[sg])
                P.op("act", lambda e: e.activation(out=E[:], in_=E[:], func=AF.Exp), r=[E], w=[E])
                P.op("act", lambda e: e.activation(out=E2[:], in_=E2[:], func=AF.Exp), r=[E2], w=[E2])
                P.op("dve", lambda e, d=d: e.tensor_tensor(out=qh_[d][:], in0=qT[:], in1=E[:], op=ALU.mult),
                     r=[qT, E], w=[qh_[d]])
                P.op("dve", lambda e, d=d: e.tensor_tensor(out=kx_[d][:], in0=kk[:], in1=sg[:], op=ALU.mult),
                     r=[kk, sg], w=[kx_[d]])
                P.op("pool", lambda e, d=d: e.tensor_tensor(out=kh_[d][:], in0=kk[:], in1=E2[:], op=ALU.mult),
                     r=[kk, E2], w=[kh_[d]])
                P.op("dve", lambda e, d=d: e.tensor_tensor(out=dec[d][:].rearrange("p (c o) -> p c o", o=1), in0=Be,
                                                           in1=Bs1, op=ALU.subtract), r=[Bi, Be_], w=[dec[d]])
                P.op("act", lambda e, d=d: e.activation(out=dec[d][:], in_=dec[d][:], func=AF.Exp),
                     r=[dec[d]], w=[dec[d]])
                P.op("pool", lambda e, d=d: e.memset(Sf[d][:], 0.0), w=[Sf[d]])
                P.op("pool", lambda e, d=d: e.memset(Sb[d][:], 0.0), w=[Sb[d]])

            def pre_phase(d):
                mask = mf if d == 0 else mb
                jh, ih = (0, 1) if d == 0 else (1, 0)
                for c8 in range(0, NCK, 8):
                    n8 = min(8, NCK - c8)
                    for j in range(n8):
                        c0 = (c8 + j) * 64
                        for hb in range(2):
                            P.op("pe", lambda e, c0=c0, hb=hb, j=j: e.matmul(
                                ps_att[hb * 32:(hb + 1) * 32, j * 64 + hb * 32:j * 64 + (hb + 1) * 32],
                                lhsT=kt_[d][:, c0 + hb * 32:c0 + (hb + 1) * 32],
                                rhs=qt_[d][:, c0 + hb * 32:c0 + (hb + 1) * 32], start=True, stop=True),
                                r=[kt_[d], qt_[d]], w=[ps_att])
                        P.op("pe", lambda e, c0=c0, j=j: e.matmul(
                            ps_att[jh * 32:(jh + 1) * 32, j * 64 + ih * 32:j * 64 + (ih + 1) * 32],
                            lhsT=kx_[d][:, c0 + jh * 32:c0 + (jh + 1) * 32],
                            rhs=qh_[d][:, c0 + ih * 32:c0 + (ih + 1) * 32], start=True, stop=True),
                            r=[kx_[d], qh_[d]], w=[ps_att])
                    P.op("dve", lambda e, c8=c8, n8=n8: e.tensor_tensor(
                        out=A_all[d][:, c8:c8 + n8, :],
                        in0=ps_att[0:64, 0:n8 * 64].rearrange("p (c i) -> p c i", i=64),
                        in1=bc_mid(mask[:, :], n8), op=ALU.mult), r=[ps_att, mask], w=[A_all[d]])
                    for j in range(n8):
                        c0 = (c8 + j) * 64
                        P.op("pe", lambda e, c0=c0, j=j: e.transpose(
                            out=ps_kh[0:64, j * 128:(j + 1) * 128], in_=khf[:, c0:c0 + 64], identity=C.ident[:]),
                            r=[khf, C.ident], w=[ps_kh])
                    P.op("act", lambda e, c8=c8, n8=n8: e.activation(
                        out=KH_all[d][:, c8:c8 + n8, :],
                        in_=ps_kh[0:64, 0:n8 * 128].rearrange("p (c k) -> p c k", k=128), func=AF.Copy),
                        r=[ps_kh], w=[KH_all[d]])

            for d in range(2):
                gate_math(d, h)
                pre_phase(d)
            order = [list(range(NCK)), [3, 2, 1, 0] + list(range(NCK - 1, 3, -1))]
            for step in range(NCK):
                for d in range(2):
                    c = order[d][step]
                    sl = slice(c * 64, (c + 1) * 64)
                    b4 = step // 4
                    grp = order[d][b4 * 4:b4 * 4 + 4]
                    lo = min(grp)
                    slot = c - lo
                    po = ps_o[d][b4 % 2]
                    ot = Od_tok[d][lo // 4]
                    P.op("pe", lambda e, d=d, c=c, po=po, slot=slot: e.matmul(
                        po[0:64, slot * 128:(slot + 1) * 128], lhsT=A_all[d][:, c, :], rhs=vt[:, c, :], start=True,
                        stop=False), r=[A_all[d], vt], w=[po])
                    P.op("pe", lambda e, d=d, sl=sl, po=po, slot=slot: e.matmul(
                        po[0:64, slot * 128:(slot + 1) * 128], lhsT=qh_[d][:, sl], rhs=Sb[d][:], start=False,
                        stop=True), r=[qh_[d], Sb[d]], w=[po])
                    if step % 4 == 3:
                        P.op("act", lambda e, d=d, lo=lo, po=po: e.activation(
                            out=Od[d][:, lo:lo + 4, :], in_=po[0:64, 0:512].rearrange("p (c v) -> p c v", v=128),
                            func=AF.Copy), r=[po], w=[ot])
                    ss_ = step % 2
                    stok = ps_s_tok[d][ss_]
                    P.op("pe", lambda e, d=d, c=c, ss_=ss_: e.matmul(
                        ps_s[d][:, ss_ * 128:(ss_ + 1) * 128], lhsT=KH_all[d][:, c, :], rhs=vt[:, c, :], start=True,
                        stop=True), r=[KH_all[d], vt], w=[stok])
                    P.op("dve", lambda e, d=d, c=c, ss_=ss_: e.scalar_tensor_tensor(
                        out=Sf[d][:], in0=Sf[d][:], scalar=dec[d][:, c:c + 1], in1=ps_s[d][:, ss_ * 128:(ss_ + 1) * 128],
                        op0=ALU.mult, op1=ALU.add), r=[Sf[d], dec[d], stok], w=[Sf[d]])
                    P.op("act", lambda e, d=d: e.activation(out=Sb[d][:], in_=Sf[d][:], func=AF.Copy),
                         r=[Sf[d]], w=[Sb[d]])
            allO = [tk for d in range(2) for tk in Od_tok[d]]
            P.op("dve", lambda e: e.tensor_tensor(out=Od[0][:], in0=Od[0][:], in1=Od[1][:], op=ALU.add),
                 r=allO, w=Od_tok[0])
            P.op("act", lambda e: e.activation(out=osq[:], in_=Od[0][:], func=AF.Square), r=Od_tok[0], w=Od_tok[1])
            P.op("dve", lambda e: e.tensor_reduce(out=oss[:], in_=osq[:], axis=AX.X, op=ALU.add), r=Od_tok[1], w=[oss])
            P.op("act", lambda e: e.activation(out=oss[:], in_=oss[:], func=AF.Sqrt, scale=1.0 / 128, bias=EPS),
                 r=[oss], w=[oss])
            P.op("dve", lambda e: e.reciprocal(out=oss[:], in_=oss[:]), r=[oss], w=[oss])
            P.op("dve", lambda e: e.tensor_tensor(out=osq[:], in0=Od[0][:], in1=bc_last(oss[:, :], 128), op=ALU.mult),
                 r=Od_tok[0] + [oss], w=Od_tok[1])
            P.op("pool", lambda e: e.tensor_tensor(out=osq[:], in0=osq[:], in1=bc_mid(LP.onorm[0:64, :], NCK),
                                                   op=ALU.mult), r=Od_tok[1] + [LP.onorm], w=Od_tok[1])
            P.op("dve", lambda e: e.tensor_tensor(out=oy[:], in0=osq[:], in1=sgd[:], op=ALU.mult),
                 r=Od_tok[1] + [sgd], w=[oy])
            for c8 in range(0, NCK, 8):
                n8 = min(8, NCK - c8)
                pt = ps_kh
                for j in range(n8):
                    P.op("pe", lambda e, pt=pt, j=j, c8=c8: e.transpose(
                        out=pt[:, j * 64:(j + 1) * 64], in_=oy[:, c8 + j, :], identity=C.ident[0:64, 0:64]),
                        r=[oy, C.ident], w=[pt])
                P.op("act", lambda e, pt=pt, c8=c8, n8=n8: e.activation(
                    out=odT[:, c8 * 64:(c8 + n8) * 64], in_=pt[:, 0:n8 * 64], func=AF.Copy), r=[pt], w=[odT])
            P.dma("sp", C.OT[3, h * 128:(h + 1) * 128, :], odT[:], r=[odT])
            if h == 7 and getattr(C, "DBG", None) is not None:
                P.dma("sp", C.DBG["O"][:, :, :], Od[0][:], r=[Od[0]])
                P.dma("sp", C.DBG["O1"][:, :, :], Od[1][:], r=[Od[1]])
                for d in range(2):
                    P.dma("sp", C.DBG["qt"][d, :, :], qt_[d][:], r=[qt_[d]])
                    P.dma("sp", C.DBG["kt"][d, :, :], kt_[d][:], r=[kt_[d]])
                    P.dma("sp", C.DBG["qh"][d, :, :], qh_[d][:], r=[qh_[d]])
                    P.dma("sp", C.DBG["dec"][d, :, :], dec[d][:], r=[dec[d]])
                P.dma("sp", C.DBG["kk"][:, :], kk[:], r=[kk])


def stage_merge(P, C, l, LP):
    SG = 1152
    SUB = [(0, 384), (384, 384), (768, 384)]
    with P.scope():
        oT = P.sbuf("moT", [128, 4, 8, SG], BF16)
        yT = P.sbuf("myT", [128, SG], BF16)
        sgt = [P.sbuf(f"msgt{i}", [128, 4, SG], BF16) for i in range(2)]
        wb = [P.sbuf(f"mwb{i}", [128, 4, 8, 128], BF16) for i in range(2)]
        acc = [P.sbuf(f"macc{i}", [128, 384]) for i in range(2)]
        tmp = [P.sbuf(f"mtmp{i}", [128, 384]) for i in range(2)]
        ps = [P.psum(f"mps{i}") for i in range(4)]
        ip = 0
        ia = 0
        for sgi in range(2):
            g0 = sgi * SG
            for n in range(4):
                P.dma("sp", oT[:, n, :, :], C.OT[n, :, g0:g0 + SG].rearrange("(c p) t -> p c t", p=128), w=[oT])
            for dmb in range(16):
                k = dmb % 2
                for n in range(4):
                    P.dma("pool", wb[k][:, n, :, :],
                          C.w_branch[l, n, :, dmb * 128:(dmb + 1) * 128].rearrange("(c p) o -> p c o", p=128),
                          w=[wb[k]])
                P.dma("sp", sgt[k][:], bass.AP(C.SGT.t, (dmb * 128) * T + g0, [[T, 128], [D * T, 4], [1, SG]]),
                      w=[sgt[k]])
                for (s0, sn) in SUB:
                    A = acc[ia % 2]
                    TM = tmp[ia % 2]
                    ia += 1
                    for n in range(4):
                        pt = ps[ip % 4]
                        ip += 1
                        for c in range(8):
                            P.op("pe", lambda e, pt=pt, k=k, n=n, c=c, s0=s0, sn=sn: e.matmul(
                                pt[:, :sn], lhsT=wb[k][:, n, c, :], rhs=oT[:, n, c, s0:s0 + sn], start=(c == 0),
                                stop=(c == 7)), r=[wb[k], oT], w=[pt])
                        if n == 0:
                            P.op("dve", lambda e, pt=pt, k=k, A=A, s0=s0, sn=sn: e.tensor_tensor(
                                out=A[:, :sn], in0=pt[:, :sn], in1=sgt[k][:, 0, s0:s0 + sn], op=ALU.mult),
                                r=[pt, sgt[k]], w=[A])
                        else:
                            P.op("dve", lambda e, pt=pt, k=k, n=n, TM=TM, s0=s0, sn=sn: e.tensor_tensor(
                                out=TM[:, :sn], in0=pt[:, :sn], in1=sgt[k][:, n, s0:s0 + sn], op=ALU.mult),
                                r=[pt, sgt[k]], w=[TM])
                            if n < 3:
                                P.op("pool", lambda e, A=A, TM=TM, sn=sn: e.tensor_tensor(
                                    out=A[:, :sn], in0=A[:, :sn], in1=TM[:, :sn], op=ALU.add), r=[A, TM], w=[A])
                            else:
                                P.op("pool", lambda e, A=A, TM=TM, s0=s0, sn=sn: e.tensor_tensor(
                                    out=yT[:, s0:s0 + sn], in0=A[:, :sn], in1=TM[:, :sn], op=ALU.add),
                                    r=[A, TM], w=[yT])
                P.dma("sp", C.YT[dmb * 128:(dmb + 1) * 128, g0:g0 + SG], yT[:], r=[yT])


def stage_out_norm2(P, C, l, LP):
    with P.scope():
        yT = P.sbuf("oyT", [128, 16, T], BF16)
        P.dma("sp", yT[:], C.YT[:, :].rearrange("(c p) t -> p c t", p=128), w=[yT])
        wo = P.sbuf("owo", [128, 16, D], BF16)
        for c4 in range(4):
            P.dma("pool", wo[:, c4 * 4:(c4 + 1) * 4, :],
                  C.w_out[l, c4 * 512:(c4 + 1) * 512, :].rearrange("(c p) o -> p c o", p=128), w=[wo])
        g1 = P.sbuf("og1", [128, D])
        P.dma("sp", g1[:], pbc(C.MOD, 1 * 6 * D + 2 * D, D), w=[g1])
        xt = [P.sbuf(f"oxt{i}", [128, D]) for i in range(2)]
        tmp = [P.sbuf(f"otmp{i}", [128, 512]) for i in range(2)]
        sq = P.sbuf("osq", [128, D])
        xn = P.sbuf("oxn", [128, D])
        ss = P.sbuf("oss", [128, 1])
        hf = P.sbuf("ohf", [128, 16, 128])
        hb = [P.sbuf(f"ohb{i}", [128, 16, 128], BF16) for i in range(2)]
        ps = [P.psum(f"ops{i}") for i in range(4)]
        pst = [P.psum(f"opst{i}") for i in range(2)]
        psr = P.psum("opsr")
        aff = P.sbuf("raff", [128, NE])
        sel = P.sbuf("rsel", [128, NE])
        r4 = [P.sbuf(f"r4_{i}", [128, 4]) for i in range(8)]
        msk = P.sbuf("rmsk", [128, NE])
        gm = P.sbuf("rgm", [128, 1])
        gt_ = [P.sbuf(f"rgt{i}", [128, NE]) for i in range(2)]
        P.dma("sp", xt[0][:], C.XRES[0:128, :], w=[xt[0]])
        for t in range(NT):
            if t + 1 < NT:
                P.dma("sp", xt[(t + 1) % 2][:], C.XRES[(t + 1) * 128:(t + 2) * 128, :], w=[xt[(t + 1) % 2]])
            X = xt[t % 2]
            r = 1 if t < 2 else 0
            if t == 2:
                P.dma("sp", g1[:], pbc(C.MOD, 0 * 6 * D + 2 * D, D), w=[g1])
            for cg in range(4):
                pt = ps[cg]
                for c in range(16):
                    P.op("pe", lambda e, pt=pt, c=c, t=t, cg=cg: e.matmul(
                        pt[:], lhsT=yT[:, c, t * 128:(t + 1) * 128], rhs=wo[:, c, cg * 512:(cg + 1) * 512],
                        start=(c == 0), stop=(c == 15)), r=[yT, wo], w=[pt])
                TM = tmp[cg % 2]
                P.op("dve", lambda e, pt=pt, cg=cg, TM=TM: e.tensor_tensor(
                    out=TM[:], in0=pt[:], in1=g1[:, cg * 512:(cg + 1) * 512],
                    op=ALU.mult), r=[pt, g1], w=[TM])
                P.op("pool", lambda e, X=X, TM=TM, cg=cg: e.tensor_tensor(
                    out=X[:, cg * 512:(cg + 1) * 512], in0=X[:, cg * 512:(cg + 1) * 512], in1=TM[:], op=ALU.add),
                    r=[X, TM], w=[X])
            P.dma("sp", C.XRES[t * 128:(t + 1) * 128, :], X[:], r=[X])
            norm_tile(P, X, xn, sq, ss, D)
            HB = hb[t % 2]
            for c4 in range(4):
                pt = pst[c4 % 2]
                for j in range(4):
                    c = c4 * 4 + j
                    P.op("pe", lambda e, c=c, j=j, pt=pt: e.transpose(
                        out=pt[:, j * 128:(j + 1) * 128], in_=xn[:, c * 128:(c + 1) * 128], identity=C.ident_f[:]),
                        r=[xn, C.ident_f], w=[pt])
                for j in range(4):
                    c = c4 * 4 + j
                    if j % 2 == 0:
                        P.op("act", lambda e, c=c, j=j, pt=pt, r=r: e.activation(
                            out=hf[:, c, :], in_=pt[:, j * 128:(j + 1) * 128], func=AF.Identity,
                            scale=LP.s2[:, r, c:c + 1], bias=LP.modP[:, r, 3, c:c + 1]),
                            r=[pt, LP.s2, LP.modP], w=[hf])
                    else:
                        P.op("dve", lambda e, c=c, j=j, pt=pt, r=r: e.tensor_scalar(
                            out=hf[:, c, :], in0=pt[:, j * 128:(j + 1) * 128], scalar1=LP.s2[:, r, c:c + 1],
                            scalar2=LP.modP[:, r, 3, c:c + 1], op0=ALU.mult, op1=ALU.add),
                            r=[pt, LP.s2, LP.modP], w=[hf])
            P.op("act", lambda e, HB=HB: e.activation(out=HB[:], in_=hf[:], func=AF.Copy), r=[hf], w=[HB])
            P.dma("sp", C.H2T[:, t * 128:(t + 1) * 128].rearrange("(c p) t -> p c t", p=128), HB[:], r=[HB])
            for c in range(16):
                P.op("pe", lambda e, c=c: e.matmul(psr[:, 0:NE], lhsT=hf[:, c, :], rhs=LP.wr[:, c, :], start=(c == 0),
                                                   stop=(c == 15)), r=[hf, LP.wr], w=[psr])
            P.op("act", lambda e: e.activation(out=aff[:], in_=psr[:, 0:NE], func=AF.Sigmoid), r=[psr], w=[aff])
            P.op("dve", lambda e: e.tensor_tensor(out=sel[:], in0=aff[:], in1=LP.br[:], op=ALU.add),
                 r=[aff, LP.br], w=[sel])
            s3 = sel[:].rearrange("p (g k) -> p g k", k=4)
            hi1, lo1, hi2, lo2, top1, sec, gs, ing = r4
            tt = lambda o, a, b, op, rr, ww: P.op("dve", lambda e: e.tensor_tensor(out=o, in0=a, in1=b, op=op),
                                                  r=rr, w=ww)
            tt(hi1[:], s3[:, :, 0], s3[:, :, 1], ALU.max, [sel], [hi1])
            tt(lo1[:], s3[:, :, 0], s3[:, :, 1], ALU.min, [sel], [lo1])
            tt(hi2[:], s3[:, :, 2], s3[:, :, 3], ALU.max, [sel], [hi2])
            tt(lo2[:], s3[:, :, 2], s3[:, :, 3], ALU.min, [sel], [lo2])
            tt(top1[:], hi1[:], hi2[:], ALU.max, [hi1, hi2], [top1])
            tt(sec[:], hi1[:], hi2[:], ALU.min, [hi1, hi2], [sec])
            tt(lo1[:], lo1[:], lo2[:], ALU.max, [lo1, lo2], [lo1])
            tt(sec[:], sec[:], lo1[:], ALU.max, [sec, lo1], [sec])
            tt(gs[:], top1[:], sec[:], ALU.add, [top1, sec], [gs])
            P.op("dve", lambda e: e.tensor_reduce(out=gm[:], in_=gs[:], axis=AX.X, op=ALU.max), r=[gs], w=[gm])
            P.op("dve", lambda e: e.tensor_scalar(out=ing[:], in0=gs[:], scalar1=gm[:, 0:1], scalar2=None,
                                                  op0=ALU.is_equal), r=[gs, gm], w=[ing])
            m3 = msk[:].rearrange("p (g k) -> p g k", k=4)
            P.op("dve", lambda e: e.tensor_tensor(out=m3, in0=s3, in1=bc_last(sec[:, :], 4), op=ALU.is_ge),
                 r=[sel, sec], w=[msk])
            P.op("dve", lambda e: e.tensor_tensor(out=m3, in0=m3, in1=bc_last(ing[:, :], 4), op=ALU.mult),
                 r=[msk, ing], w=[msk])
            P.op("dve", lambda e: e.tensor_tensor(out=msk[:], in0=msk[:], in1=aff[:], op=ALU.mult),
                 r=[msk, aff], w=[msk])
            P.op("dve", lambda e: e.tensor_reduce(out=gm[:], in_=msk[:], axis=AX.X, op=ALU.add), r=[msk], w=[gm])
            P.op("dve", lambda e: e.reciprocal(out=gm[:], in_=gm[:]), r=[gm], w=[gm])
            G = gt_[t % 2]
            P.op("dve", lambda e, G=G: e.tensor_scalar(out=G[:], in0=msk[:], scalar1=gm[:, 0:1], scalar2=None,
                                                       op0=ALU.mult), r=[msk, gm], w=[G])
            P.dma("sp", C.GATES[t * 128:(t + 1) * 128, :], G[:], r=[G])


def stage_moe(P, C, l, LP, last=False):
    GT = 768
    with P.scope():
        g2 = P.sbuf("eg2", [128, D])
        hT = P.sbuf("ehT", [128, 16, GT], BF16)
        gts = P.sbuf("egts", [128, 6, NE])
        yacc = P.sbuf("eyacc", [128, 6, D])
        yacc_tok = [Tok(f"yacc{i}") for i in range(6)]
        wg = [P.sbuf(f"ewg{i}", [128, 16, DFF], BF16) for i in range(2)]
        wu = [P.sbuf(f"ewu{i}", [128, 16, DFF], BF16) for i in range(2)]
        wd = [P.sbuf(f"ewd{i}", [128, 4, D], BF16) for i in range(2)]
        sg = [P.sbuf(f"esg{i}", [128, DFF]) for i in range(2)]
        hid = [P.sbuf(f"ehid{i}", [128, DFF], BF16) for i in range(2)]
        hidT = [P.sbuf(f"ehidT{i}", [128, DFF], BF16) for i in range(2)]
        xt = P.sbuf("ext", [128, D])
        psg = [P.psum(f"epsg{i}") for i in range(2)]
        psu = [P.psum(f"epsu{i}") for i in range(2)]
        pst = P.psum("epst", [128, 1024], BF16)
        psy = [P.psum(f"epsy{i}") for i in range(2)]
        cnt = {"it": 0, "iy": 0}

        def issue_w(ex):
            k = ex % 2
            P.dma("pool", wg[k][:], C.w_gate[l, ex, :, :].rearrange("(c p) f -> p c f", p=128), w=[wg[k]])
            P.dma("pool", wu[k][:], C.w_up[l, ex, :, :].rearrange("(c p) f -> p c f", p=128), w=[wu[k]])
            P.dma("pool", wd[k][:], C.w_down[l, ex, :, :].rearrange("(c p) o -> p c o", p=128), w=[wd[k]])

        def emit_gu(st):
            ex, ti, pg, pu = st["ex"], st["ti"], st["pg"], st["pu"]
            k = ex % 2
            for c in range(16):
                P.op("pe", lambda e, c=c: e.matmul(pg[:], lhsT=hT[:, c, ti * 128:(ti + 1) * 128], rhs=wg[k][:, c, :],
                                                   start=(c == 0), stop=(c == 15)), r=[hT, wg[k]], w=[pg])
            for c in range(16):
                P.op("pe", lambda e, c=c: e.matmul(pu[:], lhsT=hT[:, c, ti * 128:(ti + 1) * 128], rhs=wu[k][:, c, :],
                                                   start=(c == 0), stop=(c == 15)), r=[hT, wu[k]], w=[pu])

        def emit_rest(st):
            ex, ti, pg, pu, SG_, H, HT = st["ex"], st["ti"], st["pg"], st["pu"], st["sg"], st["hid"], st["hidT"]
            k = ex % 2
            P.op("act", lambda e: e.activation(out=SG_[:], in_=pg[:], func=AF.Silu), r=[pg], w=[SG_])
            P.op("dve", lambda e: e.scalar_tensor_tensor(out=H[:], in0=SG_[:], scalar=gts[:, ti, ex:ex + 1], in1=pu[:],
                                                         op0=ALU.mult, op1=ALU.mult), r=[SG_, pu, gts], w=[H])
            for j in range(4):
                P.op("pe", lambda e, j=j: e.transpose(out=pst[:, j * 128:(j + 1) * 128],
                                                      in_=H[:, j * 128:(j + 1) * 128], identity=C.ident[:]),
                     r=[H, C.ident], w=[pst])
            P.op("act", lambda e: e.activation(out=HT[:], in_=pst[:, 0:512], func=AF.Copy), r=[pst], w=[HT])
            for cg in range(4):
                py = psy[cnt["iy"] % 2]
                cnt["iy"] += 1
                for f in range(4):
                    P.op("pe", lambda e, f=f, cg=cg, py=py: e.matmul(
                        py[:], lhsT=HT[:, f * 128:(f + 1) * 128], rhs=wd[k][:, f, cg * 512:(cg + 1) * 512],
                        start=(f == 0), stop=(f == 3)), r=[HT, wd[k]], w=[py])
                if st["firstex"]:
                    P.op("act", lambda e, py=py, cg=cg: e.activation(
                        out=yacc[:, ti, cg * 512:(cg + 1) * 512], in_=py[:], func=AF.Copy),
                        r=[py], w=[yacc_tok[ti]])
                else:
                    P.op("dve", lambda e, py=py, cg=cg: e.tensor_tensor(
                        out=yacc[:, ti, cg * 512:(cg + 1) * 512], in0=yacc[:, ti, cg * 512:(cg + 1) * 512],
                        in1=py[:], op=ALU.add), r=[py, yacc_tok[ti]], w=[yacc_tok[ti]])

        cur_g2 = None
        for t0 in range(0, T, GT):
            nt = GT // 128
            tiles = [ti for ti in range(nt) if not (last and (t0 // 128 + ti) < 2)]
            P.dma("sp", hT[:], C.H2T[:, t0:t0 + GT].rearrange("(c p) t -> p c t", p=128), w=[hT])
            P.dma("sp", gts[:], C.GATES[t0:t0 + GT, :].rearrange("(n p) e -> p n e", p=128), w=[gts])
            issue_w(0)
            steps = []
            for ex in range(NE):
                for ti in tiles:
                    i = cnt["it"]
                    cnt["it"] += 1
                    steps.append(dict(ex=ex, ti=ti, pg=psg[i % 2], pu=psu[i % 2], sg=sg[i % 2], hid=hid[i % 2],
                                      hidT=hidT[i % 2], firstex=(ex == 0), wfirst=(ti == tiles[0])))
            emit_gu(steps[0])
            for i, st in enumerate(steps):
                if st["wfirst"] and st["ex"] + 1 < NE:
                    issue_w(st["ex"] + 1)
                if i + 1 < len(steps):
                    emit_gu(steps[i + 1])
                emit_rest(st)
            for ti in tiles:
                tt_ = t0 // 128 + ti
                r = 1 if tt_ < 2 else 0
                if cur_g2 != r:
                    P.dma("sp", g2[:], pbc(C.MOD, r * 6 * D + 5 * D, D), w=[g2])
                    cur_g2 = r
                P.dma("sp", xt[:], C.XRES[tt_ * 128:(tt_ + 1) * 128, :], w=[xt])
                P.op("dve", lambda e, ti=ti: e.tensor_tensor(out=yacc[:, ti, :], in0=yacc[:, ti, :], in1=g2[:],
                                                             op=ALU.mult), r=[yacc_tok[ti], g2], w=[yacc_tok[ti]])
                P.op("pool", lambda e, ti=ti: e.tensor_tensor(out=xt[:], in0=xt[:], in1=yacc[:, ti, :], op=ALU.add),
                     r=[xt, yacc_tok[ti]], w=[xt])
                if last:
                    P.dma("sp", C.out[(tt_ - 2) * 128:(tt_ - 1) * 128, :], xt[:], r=[xt])
                else:
                    P.dma("sp", C.XRES[tt_ * 128:(tt_ + 1) * 128, :], xt[:], r=[xt])


_CACHE = {}


def make_in_maps(inputs, ncores=8):
    ca, sa, cb, sb = rope_tables()
    f = lambda a: np.ascontiguousarray(np.asarray(a, dtype=np.float32))
    shared = {
        "c_ctx": f(inputs["c_ctx"]).reshape(1, D),
        "w_ada": f(inputs["w_ada"]), "b_ada": f(inputs["b_ada"]),
        "norm1_g": f(inputs["norm1_g"]), "norm2_g": f(inputs["norm2_g"]),
        "w_in": f(inputs["w_in"]), "qn_a": f(inputs["qn_a"]), "kn_a": f(inputs["kn_a"]),
        "sink_a": f(inputs["sink_a"]), "qn_b": f(inputs["qn_b"]), "kn_b": f(inputs["kn_b"]),
        "lam_b": f(inputs["lam_b"]).reshape(DEPTH, 256), "subln_b": f(inputs["subln_b"]),
        "conv_w": f(inputs["conv_w"]), "conv_b": f(inputs["conv_b"]),
        "w_rg": f(inputs["w_rg"]), "b_rg": f(inputs["b_rg"]), "w_ig": f(inputs["w_ig"]), "b_ig": f(inputs["b_ig"]),
        "lru_lambda": f(inputs["lru_lambda"]), "lb_d": f(inputs["lb_d"]), "onorm_d": f(inputs["onorm_d"]),
        "w_branch": f(inputs["w_branch"]), "w_out": f(inputs["w_out"]),
        "w_router": f(inputs["w_router"]), "b_router": f(inputs["b_router"]).reshape(1, NE),
        "w_gate": f(inputs["w_gate"]), "w_up": f(inputs["w_up"]), "w_down": f(inputs["w_down"]),
        "ropeA_c": ca, "ropeA_s": sa, "ropeB_c": cb, "ropeB_s": sb,
    }
    x = f(inputs["x"])
    ctx = f(inputs["ctx"])
    c = f(inputs["c"])
    maps = []
    for b in range(ncores):
        m = dict(shared)
        m["x"] = x[b]
        m["ctx"] = ctx[b]
        m["c"] = c[b].reshape(1, D)
        maps.append(m)
    return maps


def kernel(**inputs):
    if "nc" not in _CACHE:
        _CACHE["nc"] = build()[0]
    nc = _CACHE["nc"]
    maps = make_in_maps(inputs, 8)
    res = run_bass_kernel_spmd(nc, maps, core_ids=list(range(8)))
    return np.stack([np.asarray(r["out"], dtype=np.float32) for r in res.results], axis=0)
```

```python
import contextlib
import math
import numpy as np
import concourse.bass as bass
import concourse.mybir as mybir
from concourse.bass_utils import run_bass_kernel_spmd

F32 = mybir.dt.float32
BF16 = mybir.dt.bfloat16
AF = mybir.ActivationFunctionType
ALU = mybir.AluOpType
AX = mybir.AxisListType

ENGS = ("pe", "act", "dve", "pool", "sp")
DMA_RING = 6


class Tok:
    __slots__ = ("lw", "rd", "name")

    def __init__(self, name=""):
        self.lw = None
        self.rd = []
        self.name = name


class Ev:
    __slots__ = ("eng", "kind", "idx", "needed", "sem", "val")

    def __init__(self, eng, kind, idx):
        self.eng = eng
        self.kind = kind
        self.idx = idx
        self.needed = False
        self.sem = None
        self.val = None


class Tile(Tok):
    __slots__ = ("t", "shape", "dtype")

    def __init__(self, t, shape, dtype, name=""):
        super().__init__(name)
        self.t = t
        self.shape = shape
        self.dtype = dtype

    def __getitem__(self, k):
        return self.t[k]


class Prog:
    def __init__(self, nc):
        self.nc = nc
        self.q = {e: [] for e in ENGS}
        self.ndma = {e: 0 for e in ENGS}
        self.dma_evs = {e: [] for e in ENGS}
        self.last_ev = {e: None for e in ENGS}
        self.stack = contextlib.ExitStack()
        self.scopes = []
        self.uid = 0
        self.all_dma_evs = []

    def _name(self, name):
        self.uid += 1
        return f"{name}_{self.uid}"

    def _cur(self):
        return self.scopes[-1] if self.scopes else self.stack

    def sbuf(self, name, shape, dtype=F32):
        t = self._cur().enter_context(self.nc.sbuf_tensor(self._name(name), list(shape), dtype))
        return Tile(t, shape, dtype, name)

    def psum(self, name, shape=(128, 512), dtype=F32):
        t = self._cur().enter_context(self.nc.psum_tensor(self._name(name), list(shape), dtype))
        return Tile(t, shape, dtype, name)

    def dram(self, name, shape, dtype=F32, kind="Internal"):
        t = self.nc.dram_tensor(name, list(shape), dtype, kind=kind)
        return Tile(t, shape, dtype, name)

    @contextlib.contextmanager
    def scope(self):
        self.barrier()
        es = contextlib.ExitStack()
        self.scopes.append(es)
        try:
            yield
        finally:
            self.barrier()
            self.scopes.pop()
            es.close()

    def _emit(self, eng, fn, r, w, kind):
        deps = []
        for t in r:
            if t.lw is not None:
                deps.append(t.lw)
        for t in w:
            if t.lw is not None:
                deps.append(t.lw)
            deps.extend(t.rd)
        if kind == "d":
            i = self.ndma[eng]
            self.ndma[eng] += 1
            ev = Ev(eng, "d", i)
            if i >= DMA_RING:
                deps.append(self.dma_evs[eng][i - DMA_RING])
            self.dma_evs[eng].append(ev)
            self.all_dma_evs.append(ev)
        else:
            ev = Ev(eng, "c", len(self.q[eng]))
        dd = []
        seen = set()
        for d in deps:
            if id(d) in seen:
                continue
            seen.add(id(d))
            if d.kind == "c" and d.eng == eng and eng == "pe":
                continue
            dd.append(d)
            d.needed = True
        self.q[eng].append([dd, fn, ev])
        for t in r:
            t.rd.append(ev)
        for t in w:
            t.lw = ev
            t.rd = []
        if kind == "c":
            self.last_ev[eng] = ev
        return ev

    def op(self, eng, fn, r=(), w=()):
        return self._emit(eng, fn, list(r), list(w), "c")

    def dma(self, eng, out, in_, r=(), w=(), **kw):
        return self._emit(eng, lambda e: e.dma_start(out=out, in_=in_, **kw), list(r), list(w), "d")

    def barrier(self):
        evs = [self.last_ev[e] for e in ENGS if self.last_ev[e] is not None]
        evs += self.all_dma_evs
        self.all_dma_evs = []
        if not evs:
            return
        for e in ENGS:
            deps = []
            for d in evs:
                if d.kind == "c" and d.eng == e:
                    continue
                d.needed = True
                deps.append(d)
            self.q[e].append([deps, None, None])

    def finalize(self):
        nc = self.nc
        st = self.stack
        self.barrier()
        sem_c = {e: st.enter_context(nc.semaphore(f"c_{e}")) for e in ENGS}
        sem_d = {e: [st.enter_context(nc.semaphore(f"d_{e}_{k}")) for k in range(DMA_RING)]
                 for e in ENGS if self.ndma[e] > 0}
        for e in ENGS:
            cnt = 0
            for deps, fn, ev in self.q[e]:
                if ev is None:
                    continue
                if ev.kind == "c":
                    if ev.needed:
                        cnt += 1
                        ev.sem = sem_c[e]
                        ev.val = cnt
                else:
                    ev.sem = sem_d[e][ev.idx % DMA_RING]
                    ev.val = 16 * (ev.idx // DMA_RING + 1)
        getter = {"pe": "tensor", "act": "scalar", "dve": "vector", "pool": "gpsimd", "sp": "sync"}
        stats = [0, 0]
        with nc.Block() as block:
            for e in ENGS:
                items = self.q[e]
                if not items:
                    continue

                def body(eng, items=items):
                    seen = {}
                    for deps, fn, ev in items:
                        for d in deps:
                            key = id(d.sem)
                            if seen.get(key, 0) >= d.val:
                                continue
                            seen[key] = d.val
                            eng.wait_ge(d.sem, d.val)
                            stats[1] += 1
                        if fn is None:
                            continue
                        ins = fn(eng)
                        stats[0] += 1
                        if ev.kind == "d":
                            ins.then_inc(ev.sem, 16)
                        elif ev.needed:
                            ins.then_inc(ev.sem, 1)

                getattr(block, getter[e])(body)
        self.stats = tuple(stats)
        st.close()
        return nc


D = 2048
SEQ = 2048
LC = 256
T = SEQ + LC
NT = T // 128
NCH = D // 128
DEPTH = 2
EPS = 1e-6
IN_W = 19968
NE = 16
DFF = 512
GRID_W = 64
TG = [(0, 512), (512, 512), (1024, 512), (1536, 512), (2048, 256)]


def mk(base, pairs):
    return bass.AP(base.tensor, base.offset, [list(p) for p in pairs])


def bc_last(ap2, n):
    return mk(ap2, [ap2.ap[0], ap2.ap[1], [0, n]])


def bc_mid(ap2, k):
    return mk(ap2, [ap2.ap[0], [0, k], ap2.ap[1]])


def pbc(dt_tile, offset, n, parts=128):
    return bass.AP(dt_tile.t, offset, [[0, parts], [1, n]])


class Ctx:
    pass


def rope_tables():
    pos = np.arange(SEQ)
    rows = (pos // GRID_W).astype(np.float32)
    cols = (pos % GRID_W).astype(np.float32)

    def tab(half):
        inv = (10000.0 ** (-np.arange(half, dtype=np.float32) / half)).astype(np.float32)
        ar = rows[:, None] * inv[None, :]
        ac = cols[:, None] * inv[None, :]
        c = np.concatenate([np.cos(ar), np.cos(ar), np.cos(ac), np.cos(ac)], axis=1)
        s = np.concatenate([-np.sin(ar), np.sin(ar), -np.sin(ac), np.sin(ac)], axis=1)
        return c.astype(np.float32), s.astype(np.float32)

    ca, sa = tab(32)
    cb, sb = tab(16)
    return ca, sa, cb, sb


def build(nlayers=DEPTH, debug=(), only=None, feed=()):
    nc = bass.Bass("TRN2", target_bir_lowering=False)
    P = Prog(nc)
    C = Ctx()
    ext = lambda n, s, d=F32: P.dram(n, s, d, kind="ExternalInput")
    C.x = ext("x", [SEQ, D])
    C.ctx = ext("ctx", [LC, D])
    C.c = ext("c", [1, D])
    C.c_ctx = ext("c_ctx", [1, D])
    C.w_ada = ext("w_ada", [DEPTH, D, 6 * D])
    C.b_ada = ext("b_ada", [DEPTH, 6 * D])
    C.norm1_g = ext("norm1_g", [DEPTH, D])
    C.norm2_g = ext("norm2_g", [DEPTH, D])
    C.w_in = ext("w_in", [DEPTH, D, IN_W])
    C.qn_a = ext("qn_a", [DEPTH, 128])
    C.kn_a = ext("kn_a", [DEPTH, 128])
    C.sink_a = ext("sink_a", [DEPTH, 8])
    C.qn_b = ext("qn_b", [DEPTH, 64])
    C.kn_b = ext("kn_b", [DEPTH, 64])
    C.lam_b = ext("lam_b", [DEPTH, 256])
    C.subln_b = ext("subln_b", [DEPTH, 128])
    C.conv_w = ext("conv_w", [DEPTH, 4, 1024])
    C.conv_b = ext("conv_b", [DEPTH, 1024])
    C.w_rg = ext("w_rg", [DEPTH, 2, 8, 128, 128])
    C.b_rg = ext("b_rg", [DEPTH, 2, 1024])
    C.w_ig = ext("w_ig", [DEPTH, 2, 8, 128, 128])
    C.b_ig = ext("b_ig", [DEPTH, 2, 1024])
    C.lru_lambda = ext("lru_lambda", [DEPTH, 2, 1024])
    C.lb_d = ext("lb_d", [DEPTH, 2, 1024])
    C.onorm_d = ext("onorm_d", [DEPTH, 128])
    C.w_branch = ext("w_branch", [DEPTH, 4, 1024, D])
    C.w_out = ext("w_out", [DEPTH, D, D])
    C.w_router = ext("w_router", [D, NE])
    C.b_router = ext("b_router", [1, NE])
    C.w_gate = ext("w_gate", [DEPTH, NE, D, DFF])
    C.w_up = ext("w_up", [DEPTH, NE, D, DFF])
    C.w_down = ext("w_down", [DEPTH, NE, DFF, D])
    C.ropeA_c = ext("ropeA_c", [SEQ, 128])
    C.ropeA_s = ext("ropeA_s", [SEQ, 128])
    C.ropeB_c = ext("ropeB_c", [SEQ, 64])
    C.ropeB_s = ext("ropeB_s", [SEQ, 64])
    C.out = P.dram("out", [SEQ, D], F32, kind="ExternalOutput")

    def scratch(name, shape, dt):
        kind = "ExternalInput" if name in feed else ("ExternalOutput" if name in debug else "Internal")
        return P.dram(name, shape, dt, kind=kind)

    C.XRES = scratch("XRES", [T, D], F32)
    C.MOD = scratch("MOD", [2, 6 * D], F32)
    C.QAT = scratch("QAT", [8, 128, T], BF16)
    C.KAT = scratch("KAT", [2, 128, T], BF16)
    C.VA = scratch("VA", [T, 256], BF16)
    C.QBT = scratch("QBT", [8, 128, T], BF16)
    C.KBT = scratch("KBT", [8, 128, T], BF16)
    C.VB = scratch("VB", [T, 1024], BF16)
    C.CXT = scratch("CXT", [1024, T], F32)
    C.GCY = scratch("GCY", [1024, T], BF16)
    C.DQT = scratch("DQT", [1024, T], BF16)
    C.ZF = scratch("ZF", [2, 1024, T], F32)
    C.VD = scratch("VD", [T, 1024], BF16)
    C.SGD = scratch("SGD", [T, 1024], BF16)
    C.SGT = scratch("SGT", [4 * D, T], BF16)
    C.OT = scratch("OT", [4, 1024, T], BF16)
    C.YT = scratch("YT", [D, T], BF16)
    C.H2T = scratch("H2T", [D, T], BF16)
    C.GATES = scratch("GATES", [T, NE], F32)
    C.DBG = None
    if "DBGHG" in debug:
        eo = lambda n, sh, dt=F32: P.dram(n, sh, dt, kind="ExternalOutput")
        C.DBG = {"O": eo("dbgO", [64, 36, 128]), "O1": eo("dbgO1", [64, 36, 128]),
                 "qt": eo("dbgqt", [2, 128, T], BF16), "kt": eo("dbgkt", [2, 128, T], BF16),
                 "qh": eo("dbgqh", [2, 128, T], BF16), "kh": eo("dbgkh", [2, 128, T], BF16),
                 "dec": eo("dbgdec", [2, 128, 36]), "Bx": eo("dbgBx", [128, T + 1]), "kk": eo("dbgkk", [128, T])}

    ident_f = P.sbuf("ident_f", [128, 128], F32)
    ident = P.sbuf("ident", [128, 128], BF16)
    ones_f = P.sbuf("ones_f", [128, 128], F32)
    ones = P.sbuf("ones", [128, 128], BF16)
    m_ge = P.sbuf("m_ge", [128, 128], BF16)
    m_le = P.sbuf("m_le", [128, 128], BF16)
    tmpc = P.sbuf("tmpc", [128, 128], F32)
    P.op("pool", lambda e: e.memset(ident_f[:], 0.0), w=[ident_f])
    P.op("pool", lambda e: e.affine_select(out=ident_f[:], in_=ident_f[:], pattern=[[-1, 128]],
                                           compare_op=ALU.not_equal, fill=1.0, base=0, channel_multiplier=1),
         r=[ident_f], w=[ident_f])
    P.op("dve", lambda e: e.tensor_copy(out=ident[:], in_=ident_f[:]), r=[ident_f], w=[ident])
    P.op("pool", lambda e: e.memset(ones_f[:], 1.0), w=[ones_f])
    P.op("dve", lambda e: e.tensor_copy(out=ones[:], in_=ones_f[:]), r=[ones_f], w=[ones])
    P.op("pool", lambda e: e.affine_select(out=tmpc[:], in_=ones_f[:], pattern=[[-1, 128]],
                                           compare_op=ALU.is_ge, fill=0.0, base=0, channel_multiplier=1),
         r=[ones_f], w=[tmpc])
    P.op("dve", lambda e: e.tensor_copy(out=m_ge[:], in_=tmpc[:]), r=[tmpc], w=[m_ge])
    P.op("pool", lambda e: e.affine_select(out=tmpc[:], in_=ones_f[:], pattern=[[1, 128]],
                                           compare_op=ALU.is_ge, fill=0.0, base=0, channel_multiplier=-1),
         r=[ones_f], w=[tmpc])
    P.op("dve", lambda e: e.tensor_copy(out=m_le[:], in_=tmpc[:]), r=[tmpc], w=[m_le])
    C.ident, C.ident_f, C.ones, C.ones_f, C.m_ge, C.m_le = ident, ident_f, ones, ones_f, m_ge, m_le

    P.dma("sp", C.XRES[0:LC, :], C.ctx[:, :])
    for i in range(4):
        P.dma("sp", C.XRES[LC + i * 512:LC + (i + 1) * 512, :], C.x[i * 512:(i + 1) * 512, :])
    P.barrier()

    if only is not None:
        with P.scope():
            LP = layer_consts(P, C, 0)
            if "mod" in only:
                stage_mod(P, C, 0, LP)
            for nm in only:
                if nm == "np":
                    with P.scope():
                        hT, hT_tok = stage_norm_T(P, C, 0, LP)
                        stage_proj(P, C, 0, LP, hT, hT_tok)
                elif nm != "mod":
                    globals()["stage_" + nm](P, C, 0, LP)
        P.finalize()
        return nc, P
    for l in range(nlayers):
        last = (l == DEPTH - 1)
        with P.scope():
            LP = layer_consts(P, C, l)
            stage_mod(P, C, l, LP)
            with P.scope():
                hT, hT_tok = stage_norm_T(P, C, l, LP)
                stage_proj(P, C, l, LP, hT, hT_tok)
            stage_attn_a(P, C, l, LP)
            stage_attn_b(P, C, l, LP)
            stage_rglru(P, C, l, LP)
            stage_hgrn(P, C, l, LP)
            stage_merge(P, C, l, LP)
            stage_out_norm2(P, C, l, LP)
            stage_moe(P, C, l, LP, last)
    P.finalize()
    return nc, P


def col_load(P, dst_ap, src_tile, off, n, w, eng="sp"):
    k = n // 128
    src = bass.AP(src_tile.t, off, [[1, 128], [128, k]])
    P.dma(eng, dst_ap, src, w=w, allow_slow_non_contiguous=True)


def layer_consts(P, C, l):
    LP = Ctx()
    LP.n1 = P.sbuf("n1", [128, 16])
    LP.n2 = P.sbuf("n2", [128, 16])
    col_load(P, LP.n1[:], C.norm1_g, l * D, D, [LP.n1])
    col_load(P, LP.n2[:], C.norm2_g, l * D, D, [LP.n2])
    LP.qn_a = P.sbuf("qn_a", [128, 128])
    LP.kn_a = P.sbuf("kn_a", [128, 128])
    LP.qn_b = P.sbuf("qn_b", [128, 64])
    LP.kn_b = P.sbuf("kn_b", [128, 64])
    LP.onorm = P.sbuf("onorm", [128, 128])
    P.dma("sp", LP.qn_a[:], pbc(C.qn_a, l * 128, 128), w=[LP.qn_a])
    P.dma("sp", LP.kn_a[:], pbc(C.kn_a, l * 128, 128), w=[LP.kn_a])
    P.dma("sp", LP.qn_b[:], pbc(C.qn_b, l * 64, 64), w=[LP.qn_b])
    P.dma("sp", LP.kn_b[:], pbc(C.kn_b, l * 64, 64), w=[LP.kn_b])
    P.dma("sp", LP.onorm[:], pbc(C.onorm_d, l * 128, 128), w=[LP.onorm])
    LP.esink = P.sbuf("esink", [128, 8])
    P.dma("sp", LP.esink[:], pbc(C.sink_a, l * 8, 8), w=[LP.esink])
    P.op("act", lambda e: e.activation(out=LP.esink[:], in_=LP.esink[:], func=AF.Exp), r=[LP.esink], w=[LP.esink])
    lam_init = 0.8 - 0.6 * math.exp(-0.3 * l)
    LP.lam_init = lam_init
    lb_ = P.sbuf("lamb", [128, 256])
    P.dma("sp", lb_[:], pbc(C.lam_b, l * 256, 256), w=[lb_])
    pr = P.sbuf("lampr", [128, 2, 64])
    lb4 = lb_[:].rearrange("p (a b d) -> p a b d", a=2, b=2)
    P.op("dve", lambda e: e.tensor_tensor(out=pr[:], in0=lb4[:, :, 0, :], in1=lb4[:, :, 1, :], op=ALU.mult),
         r=[lb_], w=[pr])
    s2 = P.sbuf("lams2", [128, 2])
    P.op("dve", lambda e: e.tensor_reduce(out=s2[:], in_=pr[:], axis=AX.X, op=ALU.add), r=[pr], w=[s2])
    P.op("act", lambda e: e.activation(out=s2[:], in_=s2[:], func=AF.Exp), r=[s2], w=[s2])
    LP.nlam = P.sbuf("nlam", [128, 1])
    P.op("dve", lambda e: e.scalar_tensor_tensor(out=LP.nlam[:], in0=s2[:, 1:2], scalar=-lam_init, in1=s2[:, 0:1],
                                                 op0=ALU.add, op1=ALU.subtract), r=[s2], w=[LP.nlam])
    LP.subln = P.sbuf("subln", [128, 1])
    col_load(P, LP.subln[:], C.subln_b, l * 128, 128, [LP.subln])
    P.op("dve", lambda e: e.tensor_scalar(out=LP.subln[:], in0=LP.subln[:], scalar1=(1.0 - lam_init), scalar2=None,
                                          op0=ALU.mult), r=[LP.subln], w=[LP.subln])
    LP.convw = P.sbuf("convw", [128, 4, 8])
    for tap in range(4):
        col_load(P, LP.convw[:, tap, :], C.conv_w, (l * 4 + tap) * 1024, 1024, [LP.convw])
    LP.convb = P.sbuf("convb", [128, 8])
    col_load(P, LP.convb[:], C.conv_b, l * 1024, 1024, [LP.convb])
    LP.brg = P.sbuf("brg", [128, 2, 8])
    LP.big = P.sbuf("big", [128, 2, 8])
    lam = P.sbuf("lrulam", [128, 2, 8])
    for d in range(2):
        col_load(P, LP.brg[:, d, :], C.b_rg, (l * 2 + d) * 1024, 1024, [LP.brg])
        col_load(P, LP.big[:, d, :], C.b_ig, (l * 2 + d) * 1024, 1024, [LP.big])
        col_load(P, lam[:, d, :], C.lru_lambda, (l * 2 + d) * 1024, 1024, [lam])
    P.op("act", lambda e: e.activation(out=lam[:], in_=lam[:], func=AF.Exp, scale=-1.0), r=[lam], w=[lam])
    P.op("act", lambda e: e.activation(out=lam[:], in_=lam[:], func=AF.Ln, bias=1.0, scale=1.0), r=[lam], w=[lam])
    LP.sp8 = P.sbuf("sp8", [128, 2, 8])
    LP.sp16 = P.sbuf("sp16", [128, 2, 8])
    P.op("dve", lambda e: e.tensor_scalar(out=LP.sp8[:], in0=lam[:], scalar1=-8.0, scalar2=None, op0=ALU.mult),
         r=[lam], w=[LP.sp8])
    P.op("dve", lambda e: e.tensor_scalar(out=LP.sp16[:], in0=lam[:], scalar1=-16.0, scalar2=None, op0=ALU.mult),
         r=[lam], w=[LP.sp16])
    LP.lb = P.sbuf("lb", [128, 2, 8])
    LP.oml = P.sbuf("oml", [128, 2, 8])
    LP.noml = P.sbuf("noml", [128, 2, 8])
    if l == 0:
        P.op("pool", lambda e: e.memset(LP.lb[:], 0.0), w=[LP.lb])
    else:
        d0 = P.sbuf("lbd0", [128, 2, 8])
        for d in range(2):
            col_load(P, d0[:, d, :], C.lb_d, (0 * 2 + d) * 1024, 1024, [d0])
            col_load(P, LP.lb[:, d, :], C.lb_d, (1 * 2 + d) * 1024, 1024, [LP.lb])
        P.op("dve", lambda e: e.tensor_tensor(out=LP.lb[:], in0=LP.lb[:], in1=d0[:], op=ALU.subtract),
             r=[LP.lb, d0], w=[LP.lb])
        P.op("act", lambda e: e.activation(out=LP.lb[:], in_=LP.lb[:], func=AF.Sigmoid), r=[LP.lb], w=[LP.lb])
    P.op("dve", lambda e: e.tensor_scalar(out=LP.oml[:], in0=LP.lb[:], scalar1=-1.0, scalar2=1.0, op0=ALU.mult,
                                          op1=ALU.add), r=[LP.lb], w=[LP.oml])
    P.op("dve", lambda e: e.tensor_scalar(out=LP.noml[:], in0=LP.oml[:], scalar1=-1.0, scalar2=None, op0=ALU.mult),
         r=[LP.oml], w=[LP.noml])
    LP.wr = P.sbuf("wr", [128, 16, NE])
    P.dma("sp", LP.wr[:], C.w_router[:, :].rearrange("(c p) e -> p c e", p=128), w=[LP.wr])
    LP.br = P.sbuf("br", [128, NE])
    P.dma("sp", LP.br[:], pbc(C.b_router, 0, NE), w=[LP.br])
    LP.modP = P.sbuf("modP", [128, 2, 6, 16])
    LP.s1 = P.sbuf("s1", [128, 2, 16])
    LP.s2 = P.sbuf("s2", [128, 2, 16])
    return LP


def stage_mod(P, C, l, LP):
    with P.scope():
        cc = P.sbuf("cc", [128, 16, 2])
        col_load(P, cc[:, :, 0], C.c, 0, D, [cc])
        col_load(P, cc[:, :, 1], C.c_ctx, 0, D, [cc])
        scT = P.sbuf("scT", [128, 16, 2], BF16)
        P.op("act", lambda e: e.activation(out=scT[:], in_=cc[:], func=AF.Silu), r=[cc], w=[scT])
        bada = P.sbuf("bada", [2, 6 * D])
        P.dma("sp", bada[:], pbc(C.b_ada, l * 6 * D, 6 * D, parts=2), w=[bada])
        modsb = P.sbuf("modsb", [2, 6 * D])
        wsrc = C.w_ada[l, :, :].rearrange("(c p) n -> p c n", p=128)
        W = [P.sbuf(f"wada{i}", [128, 16, 512], BF16) for i in range(2)]
        ps = [P.psum(f"psmod{i}") for i in range(2)]
        for g in range(24):
            wt = W[g % 2]
            pt = ps[g % 2]
            P.dma("pool", wt[:], wsrc[:, :, g * 512:(g + 1) * 512], w=[wt])
            for c in range(16):
                P.op("pe", lambda e, c=c, wt=wt, pt=pt: e.matmul(pt[0:2, :], lhsT=scT[:, c, :], rhs=wt[:, c, :],
                                                               start=(c == 0), stop=(c == 15)),
                     r=[scT, wt], w=[pt])
            P.op("dve", lambda e, g=g, pt=pt: e.tensor_tensor(out=modsb[:, g * 512:(g + 1) * 512], in0=pt[0:2, :],
                                                              in1=bada[:, g * 512:(g + 1) * 512], op=ALU.add),
                 r=[pt, bada], w=[modsb])
        P.dma("sp", C.MOD[:, :], modsb[:], r=[modsb])
    for r in range(2):
        for k in range(6):
            col_load(P, LP.modP[:, r, k, :], C.MOD, r * 6 * D + k * D, D, [LP.modP])
    for (s, n, k) in ((LP.s1, LP.n1, 1), (LP.s2, LP.n2, 4)):
        for r in range(2):
            P.op("dve", lambda e, s=s, n=n, k=k, r=r: e.scalar_tensor_tensor(
                out=s[:, r, :], in0=LP.modP[:, r, k, :], scalar=1.0, in1=n[:], op0=ALU.add, op1=ALU.mult),
                r=[LP.modP, n], w=[s])


def norm_tile(P, xt, xn_out_dtype_tile, sq, ss, n):
    P.op("act", lambda e: e.activation(out=sq[:], in_=xt[:], func=AF.Square), r=[xt], w=[sq])
    P.op("dve", lambda e: e.tensor_reduce(out=ss[:], in_=sq[:], axis=AX.X, op=ALU.add), r=[sq], w=[ss])
    P.op("act", lambda e: e.activation(out=ss[:], in_=ss[:], func=AF.Sqrt, scale=1.0 / n, bias=EPS), r=[ss], w=[ss])
    P.op("dve", lambda e: e.reciprocal(out=ss[:], in_=ss[:]), r=[ss], w=[ss])
    P.op("dve", lambda e: e.tensor_scalar(out=xn_out_dtype_tile[:], in0=xt[:], scalar1=ss[:, 0:1], scalar2=None,
                                          op0=ALU.mult), r=[xt, ss], w=[xn_out_dtype_tile])


def stage_norm_T(P, C, l, LP):
    hT = P.sbuf("hT", [128, 16, T], BF16)
    hT_tok = [Tok(f"hT{t}") for t in range(NT)]
    with P.scope():
        xt = [P.sbuf(f"xt{i}", [128, D]) for i in range(2)]
        sq = P.sbuf("sq", [128, D])
        xn = [P.sbuf(f"xn{i}", [128, D], BF16) for i in range(2)]
        ss = [P.sbuf(f"ss{i}", [128, 1]) for i in range(2)]
        pst = [P.psum(f"pst{i}", [128, 1024], BF16) for i in range(2)]
        P.dma("sp", xt[0][:], C.XRES[0:128, :], w=[xt[0]])
        for t in range(NT):
            if t + 1 < NT:
                P.dma("sp", xt[(t + 1) % 2][:], C.XRES[(t + 1) * 128:(t + 2) * 128, :], w=[xt[(t + 1) % 2]])
            X, XN, SS = xt[t % 2], xn[t % 2], ss[t % 2]
            norm_tile(P, X, XN, sq, SS, D)
            r = 1 if t < 2 else 0
            for c4 in range(4):
                pt = pst[c4 % 2]
                for j in range(4):
                    c = c4 * 4 + j
                    P.op("pe", lambda e, c=c, j=j, pt=pt, XN=XN: e.transpose(
                        out=pt[:, j * 128:(j + 1) * 128], in_=XN[:, c * 128:(c + 1) * 128], identity=C.ident[:]),
                        r=[XN, C.ident], w=[pt])
                for j in range(4):
                    c = c4 * 4 + j
                    eng = "act" if j % 2 == 0 else "dve"
                    if eng == "act":
                        P.op("act", lambda e, c=c, j=j, pt=pt, t=t, r=r: e.activation(
                            out=hT[:, c, t * 128:(t + 1) * 128], in_=pt[:, j * 128:(j + 1) * 128], func=AF.Identity,
                            scale=LP.s1[:, r, c:c + 1], bias=LP.modP[:, r, 0, c:c + 1]),
                            r=[pt, LP.s1, LP.modP], w=[hT_tok[t]])
                    else:
                        P.op("dve", lambda e, c=c, j=j, pt=pt, t=t, r=r: e.tensor_scalar(
                            out=hT[:, c, t * 128:(t + 1) * 128], in0=pt[:, j * 128:(j + 1) * 128],
                            scalar1=LP.s1[:, r, c:c + 1], scalar2=LP.modP[:, r, 0, c:c + 1], op0=ALU.mult,
                            op1=ALU.add), r=[pt, LP.s1, LP.modP], w=[hT_tok[t]])
    return hT, hT_tok


def proj_groups():
    g = []
    g += [("aq", 0), ("aq", 1), ("akv", 0)]
    g += [("bq", 0), ("bq", 1), ("bk", 0), ("bk", 1), ("bv", 0), ("bv", 1)]
    g += [("cx", 0), ("cx", 1), ("cy", 0), ("cy", 1)]
    g += [("dq", 0), ("dq", 1), ("dff", 0), ("dff", 1), ("dfb", 0), ("dfb", 1)]
    g += [("di", 0), ("di", 1), ("dg", 0), ("dg", 1)]
    g += [("gt", i) for i in range(16)]
    return g


def stage_proj(P, C, l, LP, hT, hT_tok):
    groups = proj_groups()
    assert len(groups) * 512 == IN_W
    with P.scope():
        wsrc = C.w_in[l, :, :].rearrange("(c p) n -> p c n", p=128)
        W = [P.sbuf(f"win{i}", [128, 16, 512], BF16) for i in range(3)]
        ps = [P.psum(f"psproj{i}") for i in range(5)]
        pst = [P.psum(f"pstq{i}", [128, 1024], BF16) for i in range(2)]
        ca = [P.sbuf(f"ca{i}", [128, 128]) for i in range(2)]
        sa = [P.sbuf(f"sa{i}", [128, 128]) for i in range(2)]
        cb = [P.sbuf(f"cb{i}", [128, 64]) for i in range(2)]
        sb = [P.sbuf(f"sb{i}", [128, 64]) for i in range(2)]
        sq = P.sbuf("qsq", [128, 512])
        ss = P.sbuf("qss", [128, 8])
        qn = P.sbuf("qn", [128, 512])
        t1 = P.sbuf("qt1", [128, 512])
        t2 = P.sbuf("qt2", [128, 512])
        qb = [P.sbuf(f"qb{i}", [128, 512], BF16) for i in range(4)]
        qT = [P.sbuf(f"qT{i}", [128, 512], BF16) for i in range(2)]
        ob = [P.sbuf(f"ob{i}", [128, 512], BF16) for i in range(3)]
        of = [P.sbuf(f"of{i}", [128, 512], F32) for i in range(2)]
        gl = [P.sbuf(f"gl{i}", [128, 512], F32) for i in range(2)]
        cnt = {"ps": 0, "ob": 0, "of": 0, "qb": 0, "pst": 0, "qT": 0, "rope": 0}

        def nxt(key, lst):
            i = cnt[key]
            cnt[key] += 1
            return lst[i % len(lst)]

        pending = []

        def flush_pending(keep):
            while len(pending) > keep:
                pending.pop(0)()

        def qk_epi(pt, t, ncomp, dim, gain, cosT, sinT, dst, dst_h0, nheads_out):
            w = ncomp * dim
            P.op("act", lambda e: e.activation(out=sq[:, :w], in_=pt[:, :w], func=AF.Square), r=[pt], w=[sq])
            P.op("dve", lambda e: e.tensor_reduce(out=ss[:, :ncomp], in_=sq[:, :w].rearrange("p (h d) -> p h d", d=dim),
                                                  axis=AX.X, op=ALU.add), r=[sq], w=[ss])
            P.op("act", lambda e: e.activation(out=ss[:, :ncomp], in_=ss[:, :ncomp], func=AF.Sqrt, scale=1.0 / dim,
                                               bias=EPS), r=[ss], w=[ss])
            P.op("dve", lambda e: e.reciprocal(out=ss[:, :ncomp], in_=ss[:, :ncomp]), r=[ss], w=[ss])
            v3 = lambda tl: tl[:, :w].rearrange("p (h d) -> p h d", d=dim)
            P.op("dve", lambda e: e.tensor_tensor(out=v3(qn), in0=v3(pt), in1=bc_last(ss[:, :ncomp], dim),
                                                  op=ALU.mult), r=[pt, ss], w=[qn])
            QB = nxt("qb", qb)
            if t >= 2:
                P.op("pool", lambda e: e.tensor_tensor(out=v3(qn), in0=v3(qn), in1=bc_mid(gain[:, :dim], ncomp),
                                                       op=ALU.mult), r=[qn, gain], w=[qn])
                hd = dim // 4
                ng = w // (2 * hd)
                v4 = lambda tl: tl[:, :w].rearrange("p (g s i) -> p g s i", s=2, i=hd)
                def tb(tab, s):
                    b = tab[:, :].rearrange("p (g s i) -> p g s i", s=2, i=hd)[:, :, s, :]
                    return mk(b, [b.ap[0], [0, ncomp], b.ap[1], b.ap[2]])
                q5 = lambda tl, s: tl[:, :w].rearrange("p (h g s i) -> p h g s i", h=ncomp, s=2, i=hd)[:, :, :, s, :]
                P.op("dve", lambda e: e.tensor_tensor(out=v3(t1), in0=v3(qn), in1=bc_mid(cosT[:, :dim], ncomp),
                                                      op=ALU.mult), r=[qn, cosT], w=[t1])
                for s in range(2):
                    P.op("pool", lambda e, s=s: e.tensor_tensor(out=q5(t2, s), in0=q5(qn, 1 - s), in1=tb(sinT, s),
                                                                op=ALU.mult), r=[qn, sinT], w=[t2])
                P.op("dve", lambda e: e.tensor_tensor(out=QB[:, :w], in0=t1[:, :w], in1=t2[:, :w], op=ALU.add),
                     r=[t1, t2], w=[QB])
            else:
                P.op("pool", lambda e: e.tensor_tensor(out=v3(QB), in0=v3(qn), in1=bc_mid(gain[:, :dim], ncomp),
                                                       op=ALU.mult), r=[qn, gain], w=[QB])
            nblk = w // 128

            def part_b():
                PT = nxt("pst", pst)
                for j in range(nblk):
                    P.op("pe", lambda e, j=j: e.transpose(out=PT[:, j * 128:(j + 1) * 128],
                                                          in_=QB[:, j * 128:(j + 1) * 128], identity=C.ident[:]),
                         r=[QB, C.ident], w=[PT])
                QT = nxt("qT", qT)
                P.op("act", lambda e: e.activation(out=QT[:, :w], in_=PT[:, :w], func=AF.Copy), r=[PT], w=[QT])
                dsta = bass.AP(dst.t, dst_h0 * 128 * T + t * 128, [[T, 128], [128 * T, nblk], [1, 128]])
                P.dma("sp", dsta, QT[:, :w].rearrange("p (h q) -> p h q", q=128), r=[QT])

            pending.append(part_b)

        for gi, (kind, idx) in enumerate(groups):
            wt = W[gi % 3]
            P.dma("pool", wt[:], wsrc[:, :, gi * 512:(gi + 1) * 512], w=[wt])
            tokmajor = kind in ("aq", "akv", "bq", "bk", "bv", "di", "dg")
            if tokmajor:
                for t in range(NT):
                    pt = nxt("ps", ps)
                    for c in range(16):
                        P.op("pe", lambda e, c=c, t=t, pt=pt, wt=wt: e.matmul(
                            pt[:], lhsT=hT[:, c, t * 128:(t + 1) * 128], rhs=wt[:, c, :], start=(c == 0),
                            stop=(c == 15)), r=[hT_tok[t], wt], w=[pt])
                    flush_pending(1)
                    lat = t >= 2
                    if lat and kind in ("aq", "akv", "bq", "bk"):
                        k = cnt["rope"] % 2
                        cnt["rope"] += 1
                        r0 = (t - 2) * 128
                        if kind in ("aq", "akv"):
                            P.dma("sp", ca[k][:], C.ropeA_c[r0:r0 + 128, :], w=[ca[k]])
                            P.dma("sp", sa[k][:], C.ropeA_s[r0:r0 + 128, :], w=[sa[k]])
                            cT, sT = ca[k], sa[k]
                        else:
                            P.dma("sp", cb[k][:], C.ropeB_c[r0:r0 + 128, :], w=[cb[k]])
                            P.dma("sp", sb[k][:], C.ropeB_s[r0:r0 + 128, :], w=[sb[k]])
                            cT, sT = cb[k], sb[k]
                    else:
                        cT = sT = None
                    if kind == "aq":
                        qk_epi(pt, t, 4, 128, LP.qn_a, cT, sT, C.QAT, idx * 4, 4)
                    elif kind == "akv":
                        qk_epi(pt, t, 2, 128, LP.kn_a, cT, sT, C.KAT, 0, 2)
                        O = nxt("ob", ob)
                        P.op("act", lambda e, O=O, pt=pt: e.activation(out=O[:, :256], in_=pt[:, 256:512],
                                                                       func=AF.Copy), r=[pt], w=[O])
                        P.dma("sp", C.VA[t * 128:(t + 1) * 128, :], O[:, :256], r=[O])
                    elif kind == "bq":
                        qk_epi(pt, t, 8, 64, LP.qn_b, cT, sT, C.QBT, idx * 4, 4)
                    elif kind == "bk":
                        qk_epi(pt, t, 8, 64, LP.kn_b, cT, sT, C.KBT, idx * 4, 4)
                    else:
                        O = nxt("ob", ob)
                        dst = {"bv": C.VB, "di": C.VD, "dg": C.SGD}[kind]
                        fn = AF.Silu if kind == "dg" else AF.Copy
                        P.op("act", lambda e, O=O, pt=pt, fn=fn: e.activation(out=O[:], in_=pt[:], func=fn),
                             r=[pt], w=[O])
                        P.dma("sp", dst[t * 128:(t + 1) * 128, idx * 512:(idx + 1) * 512], O[:], r=[O])
            else:
                flush_pending(0)
                for cbk in range(4):
                    for (t0, tn) in TG:
                        pt = nxt("ps", ps)
                        toks = [hT_tok[t] for t in range(t0 // 128, (t0 + tn) // 128)]
                        for c in range(16):
                            P.op("pe", lambda e, c=c, pt=pt, wt=wt, cbk=cbk, t0=t0, tn=tn: e.matmul(
                                pt[:, :tn], lhsT=wt[:, c, cbk * 128:(cbk + 1) * 128], rhs=hT[:, c, t0:t0 + tn],
                                start=(c == 0), stop=(c == 15)), r=toks + [wt], w=[pt])
                        row = idx * 512 + cbk * 128
                        if kind in ("cx", "dff", "dfb"):
                            O = nxt("of", of)
                            P.op("act", lambda e, O=O, pt=pt, tn=tn: e.activation(out=O[:, :tn], in_=pt[:, :tn],
                                                                                  func=AF.Copy), r=[pt], w=[O])
                            if kind == "cx":
                                dst = C.CXT[row:row + 128, t0:t0 + tn]
                            else:
                                dst = C.ZF[0 if kind == "dff" else 1, row:row + 128, t0:t0 + tn]
                            P.dma("sp", dst, O[:, :tn], r=[O])
                        elif kind == "cy":
                            G = nxt("of", gl)
                            O = nxt("ob", ob)
                            P.op("act", lambda e, G=G, pt=pt, tn=tn: e.activation(out=G[:, :tn], in_=pt[:, :tn],
                                                                                  func=AF.Square), r=[pt], w=[G])
                            P.op("dve", lambda e, G=G, tn=tn: e.tensor_scalar(
                                out=G[:, :tn], in0=G[:, :tn], scalar1=0.044715 * 1.5957691216, scalar2=1.5957691216,
                                op0=ALU.mult, op1=ALU.add), r=[G], w=[G])
                            P.op("dve", lambda e, G=G, pt=pt, tn=tn: e.tensor_tensor(
                                out=G[:, :tn], in0=G[:, :tn], in1=pt[:, :tn], op=ALU.mult), r=[G, pt], w=[G])
                            P.op("act", lambda e, G=G, tn=tn: e.activation(out=G[:, :tn], in_=G[:, :tn],
                                                                           func=AF.Sigmoid), r=[G], w=[G])
                            P.op("dve", lambda e, G=G, O=O, pt=pt, tn=tn: e.tensor_tensor(
                                out=O[:, :tn], in0=G[:, :tn], in1=pt[:, :tn], op=ALU.mult), r=[G, pt], w=[O])
                            P.dma("sp", C.GCY[row:row + 128, t0:t0 + tn], O[:, :tn], r=[O])
                        else:
                            O = nxt("ob", ob)
                            fn = AF.Sigmoid if kind == "gt" else AF.Copy
                            P.op("act", lambda e, O=O, pt=pt, tn=tn, fn=fn: e.activation(
                                out=O[:, :tn], in_=pt[:, :tn], func=fn), r=[pt], w=[O])
                            dst = (C.SGT if kind == "gt" else C.DQT)[row:row + 128, t0:t0 + tn]
                            P.dma("sp", dst, O[:, :tn], r=[O])


def stage_attn_a(P, C, l, LP):
    scale = 128 ** -0.5
    with P.scope():
        QA = P.sbuf("QA", [128, 8, T], BF16)
        KA = P.sbuf("KA", [128, 2, T], BF16)
        VAs = P.sbuf("VAs", [128, NT, 256], BF16)
        OA = P.sbuf("OA", [128, 8, T], BF16)
        P.dma("sp", QA[:], C.QAT[:, :, :].rearrange("h d t -> d h t"), w=[QA])
        P.dma("sp", KA[:], C.KAT[:, :, :].rearrange("h d t -> d h t"), w=[KA])
        P.dma("sp", VAs[:], C.VA[:, :].rearrange("(n p) v -> p n v", p=128), w=[VAs])
        pss = [P.psum(f"pss{i}") for i in range(2)]
        pso = [P.psum(f"pso{i}") for i in range(2)]
        psz = [P.psum(f"psz{i}") for i in range(2)]
        pT = [P.sbuf(f"pT{i}", [128, 512], BF16) for i in range(3)]
        zs = P.sbuf("zs", [128, 512])
        ot = P.sbuf("ot", [128, 512])
        OA_tok = Tok("OA")
        steps = []
        it = 0
        for qb in range(NT):
            if qb < 2:
                kbs = [(0, None), (1, None)]
            else:
                n = qb - 2
                kbs = [(0, None), (1, None)]
                if n >= 1:
                    kbs.append((qb - 1, C.m_ge))
                kbs.append((qb, None))
                if n <= 14:
                    kbs.append((qb + 1, C.m_le))
            for hk in range(2):
                po, pz = pso[it % 2], psz[it % 2]
                it += 1
                for ki, (kb, mask) in enumerate(kbs):
                    steps.append(dict(qb=qb, hk=hk, kb=kb, mask=mask, po=po, pz=pz, first=(ki == 0),
                                      last=(ki == len(kbs) - 1)))
        for i, st in enumerate(steps):
            st["psx"] = pss[i % 2]
            st["pt"] = pT[i % 3]

        def emit_s(st):
            psx, kb, hk, qb = st["psx"], st["kb"], st["hk"], st["qb"]
            rhs_q = QA[:, 4 * hk:4 * hk + 4, qb * 128:(qb + 1) * 128]
            P.op("pe", lambda e: e.matmul(psx[:].rearrange("p (h q) -> p h q", h=4),
                                          lhsT=KA[:, hk, kb * 128:(kb + 1) * 128], rhs=rhs_q, start=True, stop=True),
                 r=[KA, QA], w=[psx])

        def emit_rest(st):
            psx, pt_, kb, hk, qb, mask, po, pz = (st["psx"], st["pt"], st["kb"], st["hk"], st["qb"], st["mask"],
                                                  st["po"], st["pz"])
            f, l_ = st["first"], st["last"]
            P.op("act", lambda e: e.activation(out=pt_[:], in_=psx[:], func=AF.Exp, scale=scale), r=[psx], w=[pt_])
            if mask is not None:
                P.op("pool", lambda e: e.tensor_tensor(
                    out=pt_[:].rearrange("p (h q) -> p h q", h=4), in0=pt_[:].rearrange("p (h q) -> p h q", h=4),
                    in1=bc_mid(mask[:, :], 4), op=ALU.mult), r=[pt_, mask], w=[pt_])
            P.op("pe", lambda e: e.matmul(po[:], lhsT=VAs[:, kb, hk * 128:(hk + 1) * 128], rhs=pt_[:], start=f,
                                          stop=l_), r=[VAs, pt_], w=[po])
            P.op("pe", lambda e: e.matmul(pz[:], lhsT=C.ones[:], rhs=pt_[:], start=f, stop=l_),
                 r=[C.ones, pt_], w=[pz])
            if l_:
                P.op("dve", lambda e: e.tensor_tensor(
                    out=zs[:].rearrange("p (h q) -> p h q", h=4), in0=pz[:].rearrange("p (h q) -> p h q", h=4),
                    in1=bc_last(LP.esink[:, 4 * hk:4 * hk + 4], 128), op=ALU.add), r=[pz, LP.esink], w=[zs])
                P.op("dve", lambda e: e.reciprocal(out=zs[:], in_=zs[:]), r=[zs], w=[zs])
                P.op("dve", lambda e: e.tensor_tensor(
                    out=OA[:, 4 * hk:4 * hk + 4, qb * 128:(qb + 1) * 128],
                    in0=po[:].rearrange("p (h q) -> p h q", h=4), in1=zs[:].rearrange("p (h q) -> p h q", h=4),
                    op=ALU.mult), r=[po, zs], w=[OA_tok])

        emit_s(steps[0])
        for i, st in enumerate(steps):
            if i + 1 < len(steps):
                emit_s(steps[i + 1])
            emit_rest(st)
        P.dma("sp", C.OT[0, :, :].rearrange("(h d) t -> d h t", d=128), OA[:], r=[OA_tok])


def stage_attn_b(P, C, l, LP):
    scale = 64 ** -0.5
    with P.scope():
        QB_ = [P.sbuf(f"QBh{i}", [128, T], BF16) for i in range(2)]
        KB_ = [P.sbuf(f"KBh{i}", [128, T], BF16) for i in range(2)]
        VB_ = [P.sbuf(f"VBh{i}", [128, NT, 128], BF16) for i in range(2)]
        OB_ = [P.sbuf(f"OBh{i}", [128, T], BF16) for i in range(2)]
        pss = [P.psum(f"bpss{i}") for i in range(2)]
        pso = [P.psum(f"bpso{i}") for i in range(2)]
        psz = [P.psum(f"bpsz{i}") for i in range(2)]
        psn = P.psum("bpsn")
        pT = [P.sbuf(f"bpT{i}", [128, 512], BF16) for i in range(3)]
        r1 = P.sbuf("br1", [128, 512])
        r2 = P.sbuf("br2", [128, 512])
        o1 = P.sbuf("bo1", [128, 512])
        o2 = P.sbuf("bo2", [128, 512])
        osq = P.sbuf("bosq", [128, 512], BF16)
        ip = 0

        def load(h):
            k = h % 2
            P.dma("sp", QB_[k][:], C.QBT[h, :, :], w=[QB_[k]])
            P.dma("sp", KB_[k][:], C.KBT[h, :, :], w=[KB_[k]])
            P.dma("sp", VB_[k][:], C.VB[:, h * 128:(h + 1) * 128].rearrange("(n p) v -> p n v", p=128), w=[VB_[k]])

        load(0)
        for h in range(8):
            if h + 1 < 8:
                load(h + 1)
            Q, K, V, O = QB_[h % 2], KB_[h % 2], VB_[h % 2], OB_[h % 2]
            steps = []
            for (q0, qn_, nkb) in [(0, 256, 2)] + [(LC + i * 512, 512, NT) for i in range(4)]:
                for c in range(2):
                    for kb in range(nkb):
                        steps.append(dict(q0=q0, qn=qn_, c=c, kb=kb, first=(kb == 0), last=(kb == nkb - 1),
                                          fin=(c == 1 and kb == nkb - 1)))
            for st in steps:
                st["psx"] = pss[ip % 2]
                st["pt"] = pT[ip % 3]
                ip += 1

            def emit_s(st, K=K, Q=Q):
                psx, c, kb, q0, qn_ = st["psx"], st["c"], st["kb"], st["q0"], st["qn"]
                P.op("pe", lambda e: e.matmul(psx[:, :qn_], lhsT=K[c * 64:(c + 1) * 64, kb * 128:(kb + 1) * 128],
                                              rhs=Q[c * 64:(c + 1) * 64, q0:q0 + qn_], start=True, stop=True),
                     r=[K, Q], w=[psx])

            def emit_rest(st, V=V, O=O):
                psx, pt_, c, kb, q0, qn_ = st["psx"], st["pt"], st["c"], st["kb"], st["q0"], st["qn"]
                f, l_ = st["first"], st["last"]
                po, pz = pso[c], psz[c]
                P.op("act", lambda e: e.activation(out=pt_[:, :qn_], in_=psx[:, :qn_], func=AF.Exp, scale=scale),
                     r=[psx], w=[pt_])
                P.op("pe", lambda e: e.matmul(po[:, :qn_], lhsT=V[:, kb, :], rhs=pt_[:, :qn_], start=f, stop=l_),
                     r=[V, pt_], w=[po])
                P.op("pe", lambda e: e.matmul(pz[:, :qn_], lhsT=C.ones[:], rhs=pt_[:, :qn_], start=f, stop=l_),
                     r=[C.ones, pt_], w=[pz])
                if not st["fin"]:
                    return
                P.op("dve", lambda e: e.reciprocal(out=r1[:, :qn_], in_=psz[0][:, :qn_]), r=[psz[0]], w=[r1])
                P.op("dve", lambda e: e.reciprocal(out=r2[:, :qn_], in_=psz[1][:, :qn_]), r=[psz[1]], w=[r2])
                P.op("dve", lambda e: e.tensor_tensor(out=o1[:, :qn_], in0=pso[0][:, :qn_], in1=r1[:, :qn_],
                                                      op=ALU.mult), r=[pso[0], r1], w=[o1])
                P.op("dve", lambda e: e.tensor_tensor(out=o2[:, :qn_], in0=pso[1][:, :qn_], in1=r2[:, :qn_],
                                                      op=ALU.mult), r=[pso[1], r2], w=[o2])
                P.op("dve", lambda e: e.scalar_tensor_tensor(
                    out=o1[:, :qn_], in0=o2[:, :qn_], scalar=LP.nlam[:, 0:1], in1=o1[:, :qn_], op0=ALU.mult,
                    op1=ALU.add), r=[o1, o2, LP.nlam], w=[o1])
                P.op("act", lambda e: e.activation(out=osq[:, :qn_], in_=o1[:, :qn_], func=AF.Square),
                     r=[o1], w=[osq])
                P.op("pe", lambda e: e.matmul(psn[:, :qn_], lhsT=C.ones[:], rhs=osq[:, :qn_], start=True, stop=True),
                     r=[C.ones, osq], w=[psn])
                P.op("act", lambda e: e.activation(out=r1[:, :qn_], in_=psn[:, :qn_], func=AF.Sqrt,
                                                   scale=1.0 / 128, bias=EPS), r=[psn], w=[r1])
                P.op("dve", lambda e: e.reciprocal(out=r1[:, :qn_], in_=r1[:, :qn_]), r=[r1], w=[r1])
                P.op("dve", lambda e: e.scalar_tensor_tensor(
                    out=O[:, q0:q0 + qn_], in0=o1[:, :qn_], scalar=LP.subln[:, 0:1], in1=r1[:, :qn_], op0=ALU.mult,
                    op1=ALU.mult), r=[o1, r1, LP.subln], w=[O])

            emit_s(steps[0])
            for i, st in enumerate(steps):
                if i + 1 < len(steps):
                    emit_s(steps[i + 1])
                emit_rest(st)
            P.dma("sp", C.OT[1, h * 128:(h + 1) * 128, :], O[:], r=[O])


def rev(ap2):
    (ps, pn), (s, n) = ap2.ap
    return bass.AP(ap2.tensor, ap2.offset + s * (n - 1), [[ps, pn], [-s, n]])


def stage_rglru(P, C, l, LP):
    with P.scope():
        wr = P.sbuf("wrg", [128, 16, 128], BF16)
        wi = P.sbuf("wig", [128, 16, 128], BF16)
        P.dma("pool", wr[:], C.w_rg[l, :, :, :, :].rearrange("d b i o -> i (d b) o"), w=[wr])
        P.dma("pool", wi[:], C.w_ig[l, :, :, :, :].rearrange("d b i o -> i (d b) o"), w=[wi])
        x = P.sbuf("cx", [128, T])
        u = P.sbuf("cu", [128, T])
        ub = P.sbuf("cub", [128, T], BF16)
        rr = P.sbuf("crr", [128, T])
        ii = P.sbuf("cii", [128, T])
        aa = P.sbuf("caa", [128, T])
        vv = P.sbuf("cvv", [128, T])
        hh = [P.sbuf(f"chh{d}", [128, T]) for d in range(2)]
        gy = P.sbuf("cgy", [128, T], BF16)
        oo = P.sbuf("coo", [128, T], BF16)
        ps = [P.psum(f"cps{i}") for i in range(4)]
        ip = 0
        for ch in range(8):
            P.dma("sp", x[:], C.CXT[ch * 128:(ch + 1) * 128, :], w=[x])
            P.dma("sp", gy[:], C.GCY[ch * 128:(ch + 1) * 128, :], w=[gy])
            P.op("dve", lambda e, ch=ch: e.tensor_scalar(out=u[:], in0=x[:], scalar1=LP.convw[:, 2, ch:ch + 1],
                                                         scalar2=LP.convb[:, ch:ch + 1], op0=ALU.mult, op1=ALU.add),
                 r=[x, LP.convw, LP.convb], w=[u])
            for (s0, s1) in ((0, LC), (LC, T)):
                for tap in (0, 1, 3):
                    off = tap - 2
                    lo = s0 + max(0, -off)
                    hi = s1 - max(0, off)
                    P.op("dve", lambda e, ch=ch, tap=tap, lo=lo, hi=hi, off=off: e.scalar_tensor_tensor(
                        out=u[:, lo:hi], in0=x[:, lo + off:hi + off], scalar=LP.convw[:, tap, ch:ch + 1],
                        in1=u[:, lo:hi], op0=ALU.mult, op1=ALU.add), r=[x, u, LP.convw], w=[u])
            P.op("act", lambda e: e.activation(out=ub[:], in_=u[:], func=AF.Copy), r=[u], w=[ub])
            for d in range(2):
                for (t0, tn) in TG:
                    pr_, pi_ = ps[ip % 4], ps[(ip + 1) % 4]
                    ip += 2
                    P.op("pe", lambda e, pr_=pr_, d=d, ch=ch, t0=t0, tn=tn: e.matmul(
                        pr_[:, :tn], lhsT=wr[:, d * 8 + ch, :], rhs=ub[:, t0:t0 + tn], start=True, stop=True),
                        r=[wr, ub], w=[pr_])
                    P.op("pe", lambda e, pi_=pi_, d=d, ch=ch, t0=t0, tn=tn: e.matmul(
                        pi_[:, :tn], lhsT=wi[:, d * 8 + ch, :], rhs=ub[:, t0:t0 + tn], start=True, stop=True),
                        r=[wi, ub], w=[pi_])
                    P.op("act", lambda e, pr_=pr_, d=d, ch=ch, t0=t0, tn=tn: e.activation(
                        out=rr[:, t0:t0 + tn], in_=pr_[:, :tn], func=AF.Sigmoid, bias=LP.brg[:, d, ch:ch + 1]),
                        r=[pr_, LP.brg], w=[rr])
                    P.op("act", lambda e, pi_=pi_, d=d, ch=ch, t0=t0, tn=tn: e.activation(
                        out=ii[:, t0:t0 + tn], in_=pi_[:, :tn], func=AF.Sigmoid, bias=LP.big[:, d, ch:ch + 1]),
                        r=[pi_, LP.big], w=[ii])
                P.op("act", lambda e, d=d, ch=ch: e.activation(out=aa[:], in_=rr[:], func=AF.Exp,
                                                               scale=LP.sp8[:, d, ch:ch + 1]), r=[rr, LP.sp8], w=[aa])
                P.op("act", lambda e, d=d, ch=ch: e.activation(out=vv[:], in_=rr[:], func=AF.Exp,
                                                               scale=LP.sp16[:, d, ch:ch + 1]), r=[rr, LP.sp16], w=[vv])
                P.op("act", lambda e: e.activation(out=vv[:], in_=vv[:], func=AF.Sqrt, scale=-1.0, bias=1.0),
                     r=[vv], w=[vv])
                P.op("dve", lambda e: e.tensor_tensor(out=vv[:], in0=vv[:], in1=ii[:], op=ALU.mult), r=[vv, ii], w=[vv])
                P.op("dve", lambda e: e.tensor_tensor(out=vv[:], in0=vv[:], in1=u[:], op=ALU.mult), r=[vv, u], w=[vv])
                H = hh[d]
                if d == 0:
                    P.op("dve", lambda e, H=H: e.tensor_tensor_scan(out=H[:], data0=aa[:], data1=vv[:], initial=0.0,
                                                                    op0=ALU.mult, op1=ALU.add), r=[aa, vv], w=[H])
                else:
                    P.op("dve", lambda e, H=H: e.tensor_tensor_scan(
                        out=rev(H[:, 0:LC]), data0=rev(aa[:, 0:LC]), data1=rev(vv[:, 0:LC]), initial=0.0,
                        op0=ALU.mult, op1=ALU.add), r=[aa, vv], w=[H])
                    P.op("dve", lambda e, H=H: e.tensor_tensor_scan(
                        out=rev(H[:, LC:T]), data0=rev(aa[:, LC:T]), data1=rev(vv[:, LC:T]), initial=H[:, 0:1],
                        op0=ALU.mult, op1=ALU.add), r=[aa, vv, H], w=[H])
            P.op("dve", lambda e: e.tensor_tensor(out=hh[0][:], in0=hh[0][:], in1=hh[1][:], op=ALU.add),
                 r=[hh[0], hh[1]], w=[hh[0]])
            P.op("dve", lambda e: e.tensor_tensor(out=oo[:], in0=hh[0][:], in1=gy[:], op=ALU.mult),
                 r=[hh[0], gy], w=[oo])
            P.dma("sp", C.OT[2, ch * 128:(ch + 1) * 128, :], oo[:], r=[oo])


def stage_hgrn(P, C, l, LP):
    NCK = T // 64
    with P.scope():
        o1 = C.ones_f[:, 0:1]
        ones_bc = mk(o1, [o1.ap[0], [0, T]])
        mf = P.sbuf("mf", [64, 64], BF16)
        mb = P.sbuf("mb", [64, 64], BF16)
        P.op("dve", lambda e: e.tensor_copy(out=mf[:], in_=C.m_le[0:64, 0:64]), r=[C.m_le], w=[mf])
        P.op("dve", lambda e: e.tensor_copy(out=mb[:], in_=C.m_ge[0:64, 0:64]), r=[C.m_ge], w=[mb])
        z = P.sbuf("dz", [128, T])
        sg = P.sbuf("dsg", [128, T])
        kk = P.sbuf("dkk", [128, T])
        Bi = P.sbuf("dBi", [128, T])
        Be_ = P.sbuf("dBe", [128, T])
        E = P.sbuf("dE", [128, T])
        E2 = z
        qT = P.sbuf("dqT", [128, T], BF16)
        vt = P.sbuf("dvt", [64, NCK, 128], BF16)
        sgd = P.sbuf("dsgd", [64, NCK, 128], BF16)
        qt_ = [P.sbuf(f"dqt{d}", [128, T], BF16) for d in range(2)]
        kt_ = [P.sbuf(f"dkt{d}", [128, T], BF16) for d in range(2)]
        qh_ = [P.sbuf(f"dqh{d}", [128, T], BF16) for d in range(2)]
        khf = P.sbuf("dkhf", [128, T], BF16)
        kh_ = [khf, khf]
        kx_ = [P.sbuf(f"dkx{d}", [128, T], BF16) for d in range(2)]
        dec = [P.sbuf(f"ddec{d}", [128, NCK]) for d in range(2)]
        Sf = [P.sbuf(f"dSf{d}", [128, 128]) for d in range(2)]
        Sb = [P.sbuf(f"dSb{d}", [128, 128], BF16) for d in range(2)]
        Od = [P.sbuf(f"dO{d}", [64, NCK, 128]) for d in range(2)]
        Od_tok = [[Tok(f"Od{d}_{c}") for c in range(NCK // 4)] for d in range(2)]
        A_all = [P.sbuf(f"dAall{d}", [64, NCK, 64], BF16) for d in range(2)]
        KH_all = [P.sbuf(f"dKHall{d}", [64, NCK, 128], BF16) for d in range(2)]
        ps_att = P.psum("dpsatt")
        P.op("dve", lambda e: e.memset(ps_att[:], 0.0), w=[ps_att])
        ps_kh = P.psum("dpskh", [128, 1024], BF16)
        ps_o = [[P.psum(f"dpso{d}{i}") for i in range(2)] for d in range(2)]
        ps_s = [P.psum(f"dpss{d}") for d in range(2)]
        ps_s_tok = [[Tok(f"pss{d}{i}") for i in range(2)] for d in range(2)]
        osq = Od[1]
        oss = P.sbuf("doss", [64, NCK])
        oy = KH_all[0]
        odT = P.sbuf("dodT", [128, T], BF16)

        c3 = lambda tl, off=0: tl[:, off:off + T].rearrange("p (c i) -> p c i", i=64)
        for h in range(8):
            P.dma("sp", qT[:], C.DQT[h * 128:(h + 1) * 128, :], w=[qT])
            P.dma("sp", vt[:], C.VD[:, h * 128:(h + 1) * 128].rearrange("(c p) v -> p c v", p=64), w=[vt])
            P.dma("sp", sgd[:], C.SGD[:, h * 128:(h + 1) * 128].rearrange("(c p) v -> p c v", p=64), w=[sgd])
            def gate_math(d, h):
                P.dma("sp", z[:], C.ZF[d, h * 128:(h + 1) * 128, :], w=[z])
                P.op("act", lambda e: e.activation(out=sg[:], in_=z[:], func=AF.Sigmoid), r=[z], w=[sg])
                P.op("dve", lambda e, d=d, h=h: e.tensor_scalar(
                    out=kk[:], in0=sg[:], scalar1=LP.noml[:, d, h:h + 1], scalar2=LP.oml[:, d, h:h + 1], op0=ALU.mult,
                    op1=ALU.add), r=[sg, LP.noml, LP.oml], w=[kk])
                P.op("act", lambda e, d=d, h=h: e.activation(out=sg[:], in_=sg[:], func=AF.Ln,
                                                             scale=LP.oml[:, d, h:h + 1], bias=LP.lb[:, d, h:h + 1]),
                     r=[sg, LP.oml, LP.lb], w=[sg])
                P.op("dve", lambda e: e.tensor_tensor_scan(out=Bi[:], data0=ones_bc, data1=sg[:],
                                                           initial=0.0, op0=ALU.mult, op1=ALU.add),
                     r=[C.ones_f, sg], w=[Bi])
                P.op("dve", lambda e: e.tensor_tensor(out=Be_[:], in0=Bi[:], in1=sg[:], op=ALU.subtract),
                     r=[Bi, sg], w=[Be_])
                Bx = Bi
                Bsrc = Bi if d == 0 else Be_
                Bv = c3(Bsrc)
                Bs1 = c3(Be_)[:, :, 0:1]
                Be = c3(Bi)[:, :, 63:64]
                c32 = lambda tl, off=0: tl[:, off:off + T].rearrange("p (c i) -> p c i", i=32)
                Bv32 = c32(Bsrc)
                Bref = c32(Bsrc)[:, :, 16:17]
                bcl = lambda a: mk(a, [a.ap[0], a.ap[1], [0, 64]])
                bcl32 = lambda a: mk(a, [a.ap[0], a.ap[1], [0, 32]])
                if d == 0:
                    P.op("dve", lambda e: e.tensor_tensor(out=c32(E), in0=Bv32, in1=bcl32(Bref), op=ALU.subtract),
                         r=[Bi, Be_], w=[E])
                else:
                    P.op("dve", lambda e: e.tensor_tensor(out=c32(E), in0=bcl32(Bref), in1=Bv32, op=ALU.subtract),
                         r=[Bi, Be_], w=[E])
                P.op("act", lambda e: e.activation(out=E2[:], in_=E[:], func=AF.Exp, scale=-1.0), r=[E], w=[E2])
                P.op("act", lambda e: e.activation(out=E[:], in_=E[:], func=AF.Exp), r=[E], w=[E])
                P.op("dve", lambda e, d=d: e.tensor_tensor(out=qt_[d][:], in0=qT[:], in1=E[:], op=ALU.mult),
                     r=[qT, E], w=[qt_[d]])
                P.op("pool", lambda e, d=d: e.tensor_tensor(out=kt_[d][:], in0=kk[:], in1=E2[:], op=ALU.mult),
                     r=[kk, E2], w=[kt_[d]])
                if d == 0:
                    P.op("pool", lambda e: e.tensor_tensor(out=c3(E), in0=Bv, in1=bcl(Bs1), op=ALU.subtract),
                         r=[Bi, Be_], w=[E])
                    P.op("pool", lambda e: e.tensor_tensor(out=c3(E2), in0=bcl(Be), in1=Bv, op=ALU.subtract),
                         r=[Bi, Be_], w=[E2])
                else:
                    P.op("pool", lambda e: e.tensor_tensor(out=c3(E), in0=bcl(Be), in1=Bv, op=ALU.subtract),
                         r=[Bi, Be_], w=[E])
                    P.op("pool", lambda e: e.tensor_tensor(out=c3(E2), in0=Bv, in1=bcl(Bs1), op=ALU.subtract),
                         r=[Bi, Be_], w=[E2])
                P.op("act", lambda e: e.activation(out=sg[:], in_=E[:], func=AF.Exp, scale=-1.0), r=[E], w=[sg])
                P.op("act", lambda e: e.activation(out=E[:], in_=E[:], func=AF.Exp), r=[E], w=[E])
                P.op("act", lambda e: e.activation(out=E2[:], in_=E2[:], func=AF.Exp), r=[E2], w=[E2])
                P.op("dve", lambda e, d=d: e.tensor_tensor(out=qh_[d][:], in0=qT[:], in1=E[:], op=ALU.mult),
                     r=[qT, E], w=[qh_[d]])
                P.op("dve", lambda e, d=d: e.tensor_tensor(out=kx_[d][:], in0=kk[:], in1=sg[:], op=ALU.mult),
                     r=[kk, sg], w=[kx_[d]])
                P.op("pool", lambda e, d=d: e.tensor_tensor(out=kh_[d][:], in0=kk[:], in1=E2[:], op=ALU.mult),
                     r=[kk, E2], w=[kh_[d]])
                P.op("dve", lambda e, d=d: e.tensor_tensor(out=dec[d][:].rearrange("p (c o) -> p c o", o=1), in0=Be,
                                                           in1=Bs1, op=ALU.subtract), r=[Bi, Be_], w=[dec[d]])
                P.op("act", lambda e, d=d: e.activation(out=dec[d][:], in_=dec[d][:], func=AF.Exp),
                     r=[dec[d]], w=[dec[d]])
                P.op("pool", lambda e, d=d: e.memset(Sf[d][:], 0.0), w=[Sf[d]])
                P.op("pool", lambda e, d=d: e.memset(Sb[d][:], 0.0), w=[Sb[d]])

            def pre_phase(d):
                mask = mf if d == 0 else mb
                jh, ih = (0, 1) if d == 0 else (1, 0)
                for c8 in range(0, NCK, 8):
                    n8 = min(8, NCK - c8)
                    for j in range(n8):
                        c0 = (c8 + j) * 64
                        for hb in range(2):
                            P.op("pe", lambda e, c0=c0, hb=hb, j=j: e.matmul(
                                ps_att[hb * 32:(hb + 1) * 32, j * 64 + hb * 32:j * 64 + (hb + 1) * 32],
                                lhsT=kt_[d][:, c0 + hb * 32:c0 + (hb + 1) * 32],
                                rhs=qt_[d][:, c0 + hb * 32:c0 + (hb + 1) * 32], start=True, stop=True),
                                r=[kt_[d], qt_[d]], w=[ps_att])
                        P.op("pe", lambda e, c0=c0, j=j: e.matmul(
                            ps_att[jh * 32:(jh + 1) * 32, j * 64 + ih * 32:j * 64 + (ih + 1) * 32],
                            lhsT=kx_[d][:, c0 + jh * 32:c0 + (jh + 1) * 32],
                            rhs=qh_[d][:, c0 + ih * 32:c0 + (ih + 1) * 32], start=True, stop=True),
                            r=[kx_[d], qh_[d]], w=[ps_att])
                    P.op("dve", lambda e, c8=c8, n8=n8: e.tensor_tensor(
                        out=A_all[d][:, c8:c8 + n8, :],
                        in0=ps_att[0:64, 0:n8 * 64].rearrange("p (c i) -> p c i", i=64),
                        in1=bc_mid(mask[:, :], n8), op=ALU.mult), r=[ps_att, mask], w=[A_all[d]])
                    for j in range(n8):
                        c0 = (c8 + j) * 64
                        P.op("pe", lambda e, c0=c0, j=j: e.transpose(
                            out=ps_kh[0:64, j * 128:(j + 1) * 128], in_=khf[:, c0:c0 + 64], identity=C.ident[:]),
                            r=[khf, C.ident], w=[ps_kh])
                    P.op("act", lambda e, c8=c8, n8=n8: e.activation(
                        out=KH_all[d][:, c8:c8 + n8, :],
                        in_=ps_kh[0:64, 0:n8 * 128].rearrange("p (c k) -> p c k", k=128), func=AF.Copy),
                        r=[ps_kh], w=[KH_all[d]])

            for d in range(2):
                gate_math(d, h)
                pre_phase(d)
            order = [list(range(NCK)), [3, 2, 1, 0] + list(range(NCK - 1, 3, -1))]
            for step in range(NCK):
                for d in range(2):
                    c = order[d][step]
                    sl = slice(c * 64, (c + 1) * 64)
                    b4 = step // 4
                    grp = order[d][b4 * 4:b4 * 4 + 4]
                    lo = min(grp)
                    slot = c - lo
                    po = ps_o[d][b4 % 2]
                    ot = Od_tok[d][lo // 4]
                    P.op("pe", lambda e, d=d, c=c, po=po, slot=slot: e.matmul(
                        po[0:64, slot * 128:(slot + 1) * 128], lhsT=A_all[d][:, c, :], rhs=vt[:, c, :], start=True,
                        stop=False), r=[A_all[d], vt], w=[po])
                    P.op("pe", lambda e, d=d, sl=sl, po=po, slot=slot: e.matmul(
                        po[0:64, slot * 128:(slot + 1) * 128], lhsT=qh_[d][:, sl], rhs=Sb[d][:], start=False,
                        stop=True), r=[qh_[d], Sb[d]], w=[po])
                    if step % 4 == 3:
                        P.op("act", lambda e, d=d, lo=lo, po=po: e.activation(
                            out=Od[d][:, lo:lo + 4, :], in_=po[0:64, 0:512].rearrange("p (c v) -> p c v", v=128),
                            func=AF.Copy), r=[po], w=[ot])
                    ss_ = step % 2
                    stok = ps_s_tok[d][ss_]
                    P.op("pe", lambda e, d=d, c=c, ss_=ss_: e.matmul(
                        ps_s[d][:, ss_ * 128:(ss_ + 1) * 128], lhsT=KH_all[d][:, c, :], rhs=vt[:, c, :], start=True,
                        stop=True), r=[KH_all[d], vt], w=[stok])
                    P.op("dve", lambda e, d=d, c=c, ss_=ss_: e.scalar_tensor_tensor(
                        out=Sf[d][:], in0=Sf[d][:], scalar=dec[d][:, c:c + 1], in1=ps_s[d][:, ss_ * 128:(ss_ + 1) * 128],
                        op0=ALU.mult, op1=ALU.add), r=[Sf[d], dec[d], stok], w=[Sf[d]])
                    P.op("act", lambda e, d=d: e.activation(out=Sb[d][:], in_=Sf[d][:], func=AF.Copy),
                         r=[Sf[d]], w=[Sb[d]])
            allO = [tk for d in range(2) for tk in Od_tok[d]]
            P.op("dve", lambda e: e.tensor_tensor(out=Od[0][:], in0=Od[0][:], in1=Od[1][:], op=ALU.add),
                 r=allO, w=Od_tok[0])
            P.op("act", lambda e: e.activation(out=osq[:], in_=Od[0][:], func=AF.Square), r=Od_tok[0], w=Od_tok[1])
            P.op("dve", lambda e: e.tensor_reduce(out=oss[:], in_=osq[:], axis=AX.X, op=ALU.add), r=Od_tok[1], w=[oss])
            P.op("act", lambda e: e.activation(out=oss[:], in_=oss[:], func=AF.Sqrt, scale=1.0 / 128, bias=EPS),
                 r=[oss], w=[oss])
            P.op("dve", lambda e: e.reciprocal(out=oss[:], in_=oss[:]), r=[oss], w=[oss])
            P.op("dve", lambda e: e.tensor_tensor(out=osq[:], in0=Od[0][:], in1=bc_last(oss[:, :], 128), op=ALU.mult),
                 r=Od_tok[0] + [oss], w=Od_tok[1])
            P.op("pool", lambda e: e.tensor_tensor(out=osq[:], in0=osq[:], in1=bc_mid(LP.onorm[0:64, :], NCK),
                                                   op=ALU.mult), r=Od_tok[1] + [LP.onorm], w=Od_tok[1])
            P.op("dve", lambda e: e.tensor_tensor(out=oy[:], in0=osq[:], in1=sgd[:], op=ALU.mult),
                 r=Od_tok[1] + [sgd], w=[oy])
            for c8 in range(0, NCK, 8):
                n8 = min(8, NCK - c8)
                pt = ps_kh
                for j in range(n8):
                    P.op("pe", lambda e, pt=pt, j=j, c8=c8: e.transpose(
                        out=pt[:, j * 64:(j + 1) * 64], in_=oy[:, c8 + j, :], identity=C.ident[0:64, 0:64]),
                        r=[oy, C.ident], w=[pt])
                P.op("act", lambda e, pt=pt, c8=c8, n8=n8: e.activation(
                    out=odT[:, c8 * 64:(c8 + n8) * 64], in_=pt[:, 0:n8 * 64], func=AF.Copy), r=[pt], w=[odT])
            P.dma("sp", C.OT[3, h * 128:(h + 1) * 128, :], odT[:], r=[odT])
            if h == 7 and getattr(C, "DBG", None) is not None:
                P.dma("sp", C.DBG["O"][:, :, :], Od[0][:], r=[Od[0]])
                P.dma("sp", C.DBG["O1"][:, :, :], Od[1][:], r=[Od[1]])
                for d in range(2):
                    P.dma("sp", C.DBG["qt"][d, :, :], qt_[d][:], r=[qt_[d]])
                    P.dma("sp", C.DBG["kt"][d, :, :], kt_[d][:], r=[kt_[d]])
                    P.dma("sp", C.DBG["qh"][d, :, :], qh_[d][:], r=[qh_[d]])
                    P.dma("sp", C.DBG["dec"][d, :, :], dec[d][:], r=[dec[d]])
                P.dma("sp", C.DBG["kk"][:, :], kk[:], r=[kk])


def stage_merge(P, C, l, LP):
    SG = 1152
    SUB = [(0, 384), (384, 384), (768, 384)]
    with P.scope():
        oT = P.sbuf("moT", [128, 4, 8, SG], BF16)
        yT = P.sbuf("myT", [128, SG], BF16)
        sgt = [P.sbuf(f"msgt{i}", [128, 4, SG], BF16) for i in range(2)]
        wb = [P.sbuf(f"mwb{i}", [128, 4, 8, 512], BF16) for i in range(2)]
        acc = [P.sbuf(f"macc{i}", [128, 384]) for i in range(2)]
        tmp = [P.sbuf(f"mtmp{i}", [128, 384]) for i in range(2)]
        ps = [P.psum(f"mps{i}") for i in range(4)]
        ip = 0
        ia = 0

        def load_wb(gidx):
            g4 = gidx % 4
            for n in range(4):
                P.dma("pool", wb[g4 % 2][:, n, :, :],
                      C.w_branch[l, n, :, g4 * 512:(g4 + 1) * 512].rearrange("(c p) o -> p c o", p=128),
                      w=[wb[g4 % 2]])

        for sgi in range(2):
            g0 = sgi * SG
            for n in range(4):
                P.dma("sp", oT[:, n, :, :], C.OT[n, :, g0:g0 + SG].rearrange("(c p) t -> p c t", p=128), w=[oT])
            for dmb in range(16):
                k = dmb % 2
                kw = (dmb // 4) % 2
                jw = dmb % 4
                if jw == 0:
                    if sgi == 0 and dmb == 0:
                        load_wb(0)
                    nxt_g = sgi * 4 + dmb // 4 + 1
                    if nxt_g < 8:
                        load_wb(nxt_g)
                P.dma("sp", sgt[k][:], bass.AP(C.SGT.t, (dmb * 128) * T + g0, [[T, 128], [D * T, 4], [1, SG]]),
                      w=[sgt[k]])
                for (s0, sn) in SUB:
                    A = acc[ia % 2]
                    TM = tmp[ia % 2]
                    ia += 1
                    for n in range(4):
                        pt = ps[ip % 4]
                        ip += 1
                        for c in range(8):
                            P.op("pe", lambda e, pt=pt, kw=kw, jw=jw, n=n, c=c, s0=s0, sn=sn: e.matmul(
                                pt[:, :sn], lhsT=wb[kw][:, n, c, jw * 128:(jw + 1) * 128], rhs=oT[:, n, c, s0:s0 + sn],
                                start=(c == 0), stop=(c == 7)), r=[wb[kw], oT], w=[pt])
                        if n == 0:
                            P.op("dve", lambda e, pt=pt, k=k, A=A, s0=s0, sn=sn: e.tensor_tensor(
                                out=A[:, :sn], in0=pt[:, :sn], in1=sgt[k][:, 0, s0:s0 + sn], op=ALU.mult),
                                r=[pt, sgt[k]], w=[A])
                        else:
                            P.op("dve", lambda e, pt=pt, k=k, n=n, TM=TM, s0=s0, sn=sn: e.tensor_tensor(
                                out=TM[:, :sn], in0=pt[:, :sn], in1=sgt[k][:, n, s0:s0 + sn], op=ALU.mult),
                                r=[pt, sgt[k]], w=[TM])
                            if n < 3:
                                P.op("pool", lambda e, A=A, TM=TM, sn=sn: e.tensor_tensor(
                                    out=A[:, :sn], in0=A[:, :sn], in1=TM[:, :sn], op=ALU.add), r=[A, TM], w=[A])
                            else:
                                P.op("pool", lambda e, A=A, TM=TM, s0=s0, sn=sn: e.tensor_tensor(
                                    out=yT[:, s0:s0 + sn], in0=A[:, :sn], in1=TM[:, :sn], op=ALU.add),
                                    r=[A, TM], w=[yT])
                P.dma("sp", C.YT[dmb * 128:(dmb + 1) * 128, g0:g0 + SG], yT[:], r=[yT])


def stage_out_norm2(P, C, l, LP):
    with P.scope():
        yT = P.sbuf("oyT", [128, 16, T], BF16)
        P.dma("sp", yT[:], C.YT[:, :].rearrange("(c p) t -> p c t", p=128), w=[yT])
        wo = P.sbuf("owo", [128, 16, D], BF16)
        for c4 in range(4):
            P.dma("pool", wo[:, c4 * 4:(c4 + 1) * 4, :],
                  C.w_out[l, c4 * 512:(c4 + 1) * 512, :].rearrange("(c p) o -> p c o", p=128), w=[wo])
        g1 = P.sbuf("og1", [128, D])
        P.dma("sp", g1[:], pbc(C.MOD, 1 * 6 * D + 2 * D, D), w=[g1])
        xt = [P.sbuf(f"oxt{i}", [128, D]) for i in range(2)]
        tmp = [P.sbuf(f"otmp{i}", [128, 512]) for i in range(2)]
        sq = P.sbuf("osq", [128, D])
        xn = P.sbuf("oxn", [128, D])
        ss = P.sbuf("oss", [128, 1])
        hf = P.sbuf("ohf", [128, 16, 128])
        hb = [P.sbuf(f"ohb{i}", [128, 16, 128], BF16) for i in range(2)]
        ps = [P.psum(f"ops{i}") for i in range(4)]
        pst = [P.psum(f"opst{i}") for i in range(2)]
        psr = P.psum("opsr")
        aff = P.sbuf("raff", [128, NE])
        sel = P.sbuf("rsel", [128, NE])
        r4 = [P.sbuf(f"r4_{i}", [128, 4]) for i in range(8)]
        msk = P.sbuf("rmsk", [128, NE])
        gm = P.sbuf("rgm", [128, 1])
        gt_ = [P.sbuf(f"rgt{i}", [128, NE]) for i in range(2)]
        P.dma("sp", xt[0][:], C.XRES[0:128, :], w=[xt[0]])
        for t in range(NT):
            if t + 1 < NT:
                P.dma("sp", xt[(t + 1) % 2][:], C.XRES[(t + 1) * 128:(t + 2) * 128, :], w=[xt[(t + 1) % 2]])
            X = xt[t % 2]
            r = 1 if t < 2 else 0
            if t == 2:
                P.dma("sp", g1[:], pbc(C.MOD, 0 * 6 * D + 2 * D, D), w=[g1])
            for cg in range(4):
                pt = ps[cg]
                for c in range(16):
                    P.op("pe", lambda e, pt=pt, c=c, t=t, cg=cg: e.matmul(
                        pt[:], lhsT=yT[:, c, t * 128:(t + 1) * 128], rhs=wo[:, c, cg * 512:(cg + 1) * 512],
                        start=(c == 0), stop=(c == 15)), r=[yT, wo], w=[pt])
                TM = tmp[cg % 2]
                P.op("dve", lambda e, pt=pt, cg=cg, TM=TM: e.tensor_tensor(
                    out=TM[:], in0=pt[:], in1=g1[:, cg * 512:(cg + 1) * 512],
                    op=ALU.mult), r=[pt, g1], w=[TM])
                P.op("pool", lambda e, X=X, TM=TM, cg=cg: e.tensor_tensor(
                    out=X[:, cg * 512:(cg + 1) * 512], in0=X[:, cg * 512:(cg + 1) * 512], in1=TM[:], op=ALU.add),
                    r=[X, TM], w=[X])
            P.dma("sp", C.XRES[t * 128:(t + 1) * 128, :], X[:], r=[X])
            norm_tile(P, X, xn, sq, ss, D)
            HB = hb[t % 2]
            for c4 in range(4):
                pt = pst[c4 % 2]
                for j in range(4):
                    c = c4 * 4 + j
                    P.op("pe", lambda e, c=c, j=j, pt=pt: e.transpose(
                        out=pt[:, j * 128:(j + 1) * 128], in_=xn[:, c * 128:(c + 1) * 128], identity=C.ident_f[:]),
                        r=[xn, C.ident_f], w=[pt])
                for j in range(4):
                    c = c4 * 4 + j
                    if j % 2 == 0:
                        P.op("act", lambda e, c=c, j=j, pt=pt, r=r: e.activation(
                            out=hf[:, c, :], in_=pt[:, j * 128:(j + 1) * 128], func=AF.Identity,
                            scale=LP.s2[:, r, c:c + 1], bias=LP.modP[:, r, 3, c:c + 1]),
                            r=[pt, LP.s2, LP.modP], w=[hf])
                    else:
                        P.op("dve", lambda e, c=c, j=j, pt=pt, r=r: e.tensor_scalar(
                            out=hf[:, c, :], in0=pt[:, j * 128:(j + 1) * 128], scalar1=LP.s2[:, r, c:c + 1],
                            scalar2=LP.modP[:, r, 3, c:c + 1], op0=ALU.mult, op1=ALU.add),
                            r=[pt, LP.s2, LP.modP], w=[hf])
            P.op("act", lambda e, HB=HB: e.activation(out=HB[:], in_=hf[:], func=AF.Copy), r=[hf], w=[HB])
            P.dma("sp", C.H2T[:, t * 128:(t + 1) * 128].rearrange("(c p) t -> p c t", p=128), HB[:], r=[HB])
            for c in range(16):
                P.op("pe", lambda e, c=c: e.matmul(psr[:, 0:NE], lhsT=hf[:, c, :], rhs=LP.wr[:, c, :], start=(c == 0),
                                                   stop=(c == 15)), r=[hf, LP.wr], w=[psr])
            P.op("act", lambda e: e.activation(out=aff[:], in_=psr[:, 0:NE], func=AF.Sigmoid), r=[psr], w=[aff])
            P.op("dve", lambda e: e.tensor_tensor(out=sel[:], in0=aff[:], in1=LP.br[:], op=ALU.add),
                 r=[aff, LP.br], w=[sel])
            s3 = sel[:].rearrange("p (g k) -> p g k", k=4)
            hi1, lo1, hi2, lo2, top1, sec, gs, ing = r4
            tt = lambda o, a, b, op, rr, ww: P.op("dve", lambda e: e.tensor_tensor(out=o, in0=a, in1=b, op=op),
                                                  r=rr, w=ww)
            tt(hi1[:], s3[:, :, 0], s3[:, :, 1], ALU.max, [sel], [hi1])
            tt(lo1[:], s3[:, :, 0], s3[:, :, 1], ALU.min, [sel], [lo1])
            tt(hi2[:], s3[:, :, 2], s3[:, :, 3], ALU.max, [sel], [hi2])
            tt(lo2[:], s3[:, :, 2], s3[:, :, 3], ALU.min, [sel], [lo2])
            tt(top1[:], hi1[:], hi2[:], ALU.max, [hi1, hi2], [top1])
            tt(sec[:], hi1[:], hi2[:], ALU.min, [hi1, hi2], [sec])
            tt(lo1[:], lo1[:], lo2[:], ALU.max, [lo1, lo2], [lo1])
            tt(sec[:], sec[:], lo1[:], ALU.max, [sec, lo1], [sec])
            tt(gs[:], top1[:], sec[:], ALU.add, [top1, sec], [gs])
            P.op("dve", lambda e: e.tensor_reduce(out=gm[:], in_=gs[:], axis=AX.X, op=ALU.max), r=[gs], w=[gm])
            P.op("dve", lambda e: e.tensor_scalar(out=ing[:], in0=gs[:], scalar1=gm[:, 0:1], scalar2=None,
                                                  op0=ALU.is_equal), r=[gs, gm], w=[ing])
            m3 = msk[:].rearrange("p (g k) -> p g k", k=4)
            P.op("dve", lambda e: e.tensor_tensor(out=m3, in0=s3, in1=bc_last(sec[:, :], 4), op=ALU.is_ge),
                 r=[sel, sec], w=[msk])
            P.op("dve", lambda e: e.tensor_tensor(out=m3, in0=m3, in1=bc_last(ing[:, :], 4), op=ALU.mult),
                 r=[msk, ing], w=[msk])
            P.op("dve", lambda e: e.tensor_tensor(out=msk[:], in0=msk[:], in1=aff[:], op=ALU.mult),
                 r=[msk, aff], w=[msk])
            P.op("dve", lambda e: e.tensor_reduce(out=gm[:], in_=msk[:], axis=AX.X, op=ALU.add), r=[msk], w=[gm])
            P.op("dve", lambda e: e.reciprocal(out=gm[:], in_=gm[:]), r=[gm], w=[gm])
            G = gt_[t % 2]
            P.op("dve", lambda e, G=G: e.tensor_scalar(out=G[:], in0=msk[:], scalar1=gm[:, 0:1], scalar2=None,
                                                       op0=ALU.mult), r=[msk, gm], w=[G])
            P.dma("sp", C.GATES[t * 128:(t + 1) * 128, :], G[:], r=[G])


def stage_moe(P, C, l, LP, last=False):
    GT = 768
    with P.scope():
        g2 = P.sbuf("eg2", [128, D])
        hT = P.sbuf("ehT", [128, 16, GT], BF16)
        gts = P.sbuf("egts", [128, 6, NE])
        yacc = P.sbuf("eyacc", [128, 6, D])
        yacc_tok = [Tok(f"yacc{i}") for i in range(6)]
        wg = [P.sbuf(f"ewg{i}", [128, 16, DFF], BF16) for i in range(2)]
        wu = [P.sbuf(f"ewu{i}", [128, 16, DFF], BF16) for i in range(2)]
        wd = [P.sbuf(f"ewd{i}", [128, 4, D], BF16) for i in range(2)]
        sg = [P.sbuf(f"esg{i}", [128, DFF]) for i in range(2)]
        hid = [P.sbuf(f"ehid{i}", [128, DFF], BF16) for i in range(2)]
        hidT = [P.sbuf(f"ehidT{i}", [128, DFF], BF16) for i in range(2)]
        xt = P.sbuf("ext", [128, D])
        psg = [P.psum(f"epsg{i}") for i in range(2)]
        psu = [P.psum(f"epsu{i}") for i in range(2)]
        pst = P.psum("epst", [128, 1024], BF16)
        psy = [P.psum(f"epsy{i}") for i in range(2)]
        cnt = {"it": 0, "iy": 0}

        def issue_w(ex):
            k = ex % 2
            P.dma("pool", wg[k][:], C.w_gate[l, ex, :, :].rearrange("(c p) f -> p c f", p=128), w=[wg[k]])
            P.dma("pool", wu[k][:], C.w_up[l, ex, :, :].rearrange("(c p) f -> p c f", p=128), w=[wu[k]])
            P.dma("pool", wd[k][:], C.w_down[l, ex, :, :].rearrange("(c p) o -> p c o", p=128), w=[wd[k]])

        def emit_gu(st):
            ex, ti, pg, pu = st["ex"], st["ti"], st["pg"], st["pu"]
            k = ex % 2
            for c in range(16):
                P.op("pe", lambda e, c=c: e.matmul(pg[:], lhsT=hT[:, c, ti * 128:(ti + 1) * 128], rhs=wg[k][:, c, :],
                                                   start=(c == 0), stop=(c == 15)), r=[hT, wg[k]], w=[pg])
            for c in range(16):
                P.op("pe", lambda e, c=c: e.matmul(pu[:], lhsT=hT[:, c, ti * 128:(ti + 1) * 128], rhs=wu[k][:, c, :],
                                                   start=(c == 0), stop=(c == 15)), r=[hT, wu[k]], w=[pu])

        def emit_rest(st):
            ex, ti, pg, pu, SG_, H, HT = st["ex"], st["ti"], st["pg"], st["pu"], st["sg"], st["hid"], st["hidT"]
            k = ex % 2
            P.op("act", lambda e: e.activation(out=SG_[:], in_=pg[:], func=AF.Silu), r=[pg], w=[SG_])
            P.op("dve", lambda e: e.scalar_tensor_tensor(out=H[:], in0=SG_[:], scalar=gts[:, ti, ex:ex + 1], in1=pu[:],
                                                         op0=ALU.mult, op1=ALU.mult), r=[SG_, pu, gts], w=[H])
            for j in range(4):
                P.op("pe", lambda e, j=j: e.transpose(out=pst[:, j * 128:(j + 1) * 128],
                                                      in_=H[:, j * 128:(j + 1) * 128], identity=C.ident[:]),
                     r=[H, C.ident], w=[pst])
            P.op("act", lambda e: e.activation(out=HT[:], in_=pst[:, 0:512], func=AF.Copy), r=[pst], w=[HT])
            for cg in range(4):
                py = psy[cnt["iy"] % 2]
                cnt["iy"] += 1
                for f in range(4):
                    P.op("pe", lambda e, f=f, cg=cg, py=py: e.matmul(
                        py[:], lhsT=HT[:, f * 128:(f + 1) * 128], rhs=wd[k][:, f, cg * 512:(cg + 1) * 512],
                        start=(f == 0), stop=(f == 3)), r=[HT, wd[k]], w=[py])
                if st["firstex"]:
                    P.op("act", lambda e, py=py, cg=cg: e.activation(
                        out=yacc[:, ti, cg * 512:(cg + 1) * 512], in_=py[:], func=AF.Copy),
                        r=[py], w=[yacc_tok[ti]])
                else:
                    P.op("dve", lambda e, py=py, cg=cg: e.tensor_tensor(
                        out=yacc[:, ti, cg * 512:(cg + 1) * 512], in0=yacc[:, ti, cg * 512:(cg + 1) * 512],
                        in1=py[:], op=ALU.add), r=[py, yacc_tok[ti]], w=[yacc_tok[ti]])

        cur_g2 = None
        for t0 in range(0, T, GT):
            nt = GT // 128
            tiles = [ti for ti in range(nt) if not (last and (t0 // 128 + ti) < 2)]
            P.dma("sp", hT[:], C.H2T[:, t0:t0 + GT].rearrange("(c p) t -> p c t", p=128), w=[hT])
            P.dma("sp", gts[:], C.GATES[t0:t0 + GT, :].rearrange("(n p) e -> p n e", p=128), w=[gts])
            issue_w(0)
            steps = []
            for ex in range(NE):
                for ti in tiles:
                    i = cnt["it"]
                    cnt["it"] += 1
                    steps.append(dict(ex=ex, ti=ti, pg=psg[i % 2], pu=psu[i % 2], sg=sg[i % 2], hid=hid[i % 2],
                                      hidT=hidT[i % 2], firstex=(ex == 0), wfirst=(ti == tiles[0])))
            emit_gu(steps[0])
            for i, st in enumerate(steps):
                if st["wfirst"] and st["ex"] + 1 < NE:
                    issue_w(st["ex"] + 1)
                if i + 1 < len(steps):
                    emit_gu(steps[i + 1])
                emit_rest(st)
            for ti in tiles:
                tt_ = t0 // 128 + ti
                r = 1 if tt_ < 2 else 0
                if cur_g2 != r:
                    P.dma("sp", g2[:], pbc(C.MOD, r * 6 * D + 5 * D, D), w=[g2])
                    cur_g2 = r
                P.dma("sp", xt[:], C.XRES[tt_ * 128:(tt_ + 1) * 128, :], w=[xt])
                P.op("dve", lambda e, ti=ti: e.tensor_tensor(out=yacc[:, ti, :], in0=yacc[:, ti, :], in1=g2[:],
                                                             op=ALU.mult), r=[yacc_tok[ti], g2], w=[yacc_tok[ti]])
                P.op("pool", lambda e, ti=ti: e.tensor_tensor(out=xt[:], in0=xt[:], in1=yacc[:, ti, :], op=ALU.add),
                     r=[xt, yacc_tok[ti]], w=[xt])
                if last:
                    P.dma("sp", C.out[(tt_ - 2) * 128:(tt_ - 1) * 128, :], xt[:], r=[xt])
                else:
                    P.dma("sp", C.XRES[tt_ * 128:(tt_ + 1) * 128, :], xt[:], r=[xt])


_CACHE = {}


def make_in_maps(inputs, ncores=8):
    ca, sa, cb, sb = rope_tables()
    f = lambda a: np.ascontiguousarray(np.asarray(a, dtype=np.float32))
    shared = {
        "c_ctx": f(inputs["c_ctx"]).reshape(1, D),
        "w_ada": f(inputs["w_ada"]), "b_ada": f(inputs["b_ada"]),
        "norm1_g": f(inputs["norm1_g"]), "norm2_g": f(inputs["norm2_g"]),
        "w_in": f(inputs["w_in"]), "qn_a": f(inputs["qn_a"]), "kn_a": f(inputs["kn_a"]),
        "sink_a": f(inputs["sink_a"]), "qn_b": f(inputs["qn_b"]), "kn_b": f(inputs["kn_b"]),
        "lam_b": f(inputs["lam_b"]).reshape(DEPTH, 256), "subln_b": f(inputs["subln_b"]),
        "conv_w": f(inputs["conv_w"]), "conv_b": f(inputs["conv_b"]),
        "w_rg": f(inputs["w_rg"]), "b_rg": f(inputs["b_rg"]), "w_ig": f(inputs["w_ig"]), "b_ig": f(inputs["b_ig"]),
        "lru_lambda": f(inputs["lru_lambda"]), "lb_d": f(inputs["lb_d"]), "onorm_d": f(inputs["onorm_d"]),
        "w_branch": f(inputs["w_branch"]), "w_out": f(inputs["w_out"]),
        "w_router": f(inputs["w_router"]), "b_router": f(inputs["b_router"]).reshape(1, NE),
        "w_gate": f(inputs["w_gate"]), "w_up": f(inputs["w_up"]), "w_down": f(inputs["w_down"]),
        "ropeA_c": ca, "ropeA_s": sa, "ropeB_c": cb, "ropeB_s": sb,
    }
    x = f(inputs["x"])
    ctx = f(inputs["ctx"])
    c = f(inputs["c"])
    maps = []
    for b in range(ncores):
        m = dict(shared)
        m["x"] = x[b]
        m["ctx"] = ctx[b]
        m["c"] = c[b].reshape(1, D)
        maps.append(m)
    return maps


def kernel(**inputs):
    if "nc" not in _CACHE:
        _CACHE["nc"] = build()[0]
    nc = _CACHE["nc"]
    maps = make_in_maps(inputs, 8)
    res = run_bass_kernel_spmd(nc, maps, core_ids=list(range(8)))
    return np.stack([np.asarray(r["out"], dtype=np.float32) for r in res.results], axis=0)
```

```python
import contextlib
import math
import numpy as np
import concourse.bass as bass
import concourse.mybir as mybir
from concourse.bass_utils import run_bass_kernel_spmd

F32 = mybir.dt.float32
BF16 = mybir.dt.bfloat16
AF = mybir.ActivationFunctionType
ALU = mybir.AluOpType
AX = mybir.AxisListType

ENGS = ("pe", "act", "dve", "pool", "sp")
DMA_RING = 6


class Tok:
    __slots__ = ("lw", "rd", "name")

    def __init__(self, name=""):
        self.lw = None
        self.rd = []
        self.name = name


class Ev:
    __slots__ = ("eng", "kind", "idx", "needed", "sem", "val")

    def __init__(self, eng, kind, idx):
        self.eng = eng
        self.kind = kind
        self.idx = idx
        self.needed = False
        self.sem = None
        self.val = None


class Tile(Tok):
    __slots__ = ("t", "shape", "dtype")

    def __init__(self, t, shape, dtype, name=""):
        super().__init__(name)
        self.t = t
        self.shape = shape
        self.dtype = dtype

    def __getitem__(self, k):
        return self.t[k]


class Prog:
    def __init__(self, nc):
        self.nc = nc
        self.q = {e: [] for e in ENGS}
        self.ndma = {e: 0 for e in ENGS}
        self.dma_evs = {e: [] for e in ENGS}
        self.last_ev = {e: None for e in ENGS}
        self.stack = contextlib.ExitStack()
        self.scopes = []
        self.uid = 0
        self.all_dma_evs = []

    def _name(self, name):
        self.uid += 1
        return f"{name}_{self.uid}"

    def _cur(self):
        return self.scopes[-1] if self.scopes else self.stack

    def sbuf(self, name, shape, dtype=F32):
        t = self._cur().enter_context(self.nc.sbuf_tensor(self._name(name), list(shape), dtype))
        return Tile(t, shape, dtype, name)

    def psum(self, name, shape=(128, 512), dtype=F32):
        t = self._cur().enter_context(self.nc.psum_tensor(self._name(name), list(shape), dtype))
        return Tile(t, shape, dtype, name)

    def dram(self, name, shape, dtype=F32, kind="Internal"):
        t = self.nc.dram_tensor(name, list(shape), dtype, kind=kind)
        return Tile(t, shape, dtype, name)

    @contextlib.contextmanager
    def scope(self):
        self.barrier()
        es = contextlib.ExitStack()
        self.scopes.append(es)
        try:
            yield
        finally:
            self.barrier()
            self.scopes.pop()
            es.close()

    def _emit(self, eng, fn, r, w, kind):
        deps = []
        for t in r:
            if t.lw is not None:
                deps.append(t.lw)
        for t in w:
            if t.lw is not None:
                deps.append(t.lw)
            deps.extend(t.rd)
        if kind == "d":
            i = self.ndma[eng]
            self.ndma[eng] += 1
            ev = Ev(eng, "d", i)
            if i >= DMA_RING:
                deps.append(self.dma_evs[eng][i - DMA_RING])
            self.dma_evs[eng].append(ev)
            self.all_dma_evs.append(ev)
        else:
            ev = Ev(eng, "c", len(self.q[eng]))
        dd = []
        seen = set()
        for d in deps:
            if id(d) in seen:
                continue
            seen.add(id(d))
            if d.kind == "c" and d.eng == eng and eng == "pe":
                continue
            dd.append(d)
            d.needed = True
        self.q[eng].append([dd, fn, ev])
        for t in r:
            t.rd.append(ev)
        for t in w:
            t.lw = ev
            t.rd = []
        if kind == "c":
            self.last_ev[eng] = ev
        return ev

    def op(self, eng, fn, r=(), w=()):
        return self._emit(eng, fn, list(r), list(w), "c")

    def dma(self, eng, out, in_, r=(), w=(), **kw):
        return self._emit(eng, lambda e: e.dma_start(out=out, in_=in_, **kw), list(r), list(w), "d")

    def barrier(self):
        evs = [self.last_ev[e] for e in ENGS if self.last_ev[e] is not None]
        evs += self.all_dma_evs
        self.all_dma_evs = []
        if not evs:
            return
        for e in ENGS:
            deps = []
            for d in evs:
                if d.kind == "c" and d.eng == e:
                    continue
                d.needed = True
                deps.append(d)
            self.q[e].append([deps, None, None])

    def finalize(self):
        nc = self.nc
        st = self.stack
        self.barrier()
        sem_c = {e: st.enter_context(nc.semaphore(f"c_{e}")) for e in ENGS}
        sem_d = {e: [st.enter_context(nc.semaphore(f"d_{e}_{k}")) for k in range(DMA_RING)]
                 for e in ENGS if self.ndma[e] > 0}
        for e in ENGS:
            cnt = 0
            for deps, fn, ev in self.q[e]:
                if ev is None:
                    continue
                if ev.kind == "c":
                    if ev.needed:
                        cnt += 1
                        ev.sem = sem_c[e]
                        ev.val = cnt
                else:
                    ev.sem = sem_d[e][ev.idx % DMA_RING]
                    ev.val = 16 * (ev.idx // DMA_RING + 1)
        getter = {"pe": "tensor", "act": "scalar", "dve": "vector", "pool": "gpsimd", "sp": "sync"}
        stats = [0, 0]
        with nc.Block() as block:
            for e in ENGS:
                items = self.q[e]
                if not items:
                    continue

                def body(eng, items=items):
                    seen = {}
                    for deps, fn, ev in items:
                        for d in deps:
                            key = id(d.sem)
                            if seen.get(key, 0) >= d.val:
                                continue
                            seen[key] = d.val
                            eng.wait_ge(d.sem, d.val)
                            stats[1] += 1
                        if fn is None:
                            continue
                        ins = fn(eng)
                        stats[0] += 1
                        if ev.kind == "d":
                            ins.then_inc(ev.sem, 16)
                        elif ev.needed:
                            ins.then_inc(ev.sem, 1)

                getattr(block, getter[e])(body)
        self.stats = tuple(stats)
        st.close()
        return nc


D = 2048
SEQ = 2048
LC = 256
T = SEQ + LC
NT = T // 128
NCH = D // 128
DEPTH = 2
EPS = 1e-6
IN_W = 19968
NE = 16
DFF = 512
GRID_W = 64
TG = [(0, 512), (512, 512), (1024, 512), (1536, 512), (2048, 256)]


def mk(base, pairs):
    return bass.AP(base.tensor, base.offset, [list(p) for p in pairs])


def bc_last(ap2, n):
    return mk(ap2, [ap2.ap[0], ap2.ap[1], [0, n]])


def bc_mid(ap2, k):
    return mk(ap2, [ap2.ap[0], [0, k], ap2.ap[1]])


def pbc(dt_tile, offset, n, parts=128):
    return bass.AP(dt_tile.t, offset, [[0, parts], [1, n]])


class Ctx:
    pass


def rope_tables():
    pos = np.arange(SEQ)
    rows = (pos // GRID_W).astype(np.float32)
    cols = (pos % GRID_W).astype(np.float32)

    def tab(half):
        inv = (10000.0 ** (-np.arange(half, dtype=np.float32) / half)).astype(np.float32)
        ar = rows[:, None] * inv[None, :]
        ac = cols[:, None] * inv[None, :]
        c = np.concatenate([np.cos(ar), np.cos(ar), np.cos(ac), np.cos(ac)], axis=1)
        s = np.concatenate([-np.sin(ar), np.sin(ar), -np.sin(ac), np.sin(ac)], axis=1)
        return c.astype(np.float32), s.astype(np.float32)

    ca, sa = tab(32)
    cb, sb = tab(16)
    return ca, sa, cb, sb


def build(nlayers=DEPTH, debug=(), only=None, feed=()):
    nc = bass.Bass("TRN2", target_bir_lowering=False)
    P = Prog(nc)
    C = Ctx()
    ext = lambda n, s, d=F32: P.dram(n, s, d, kind="ExternalInput")
    C.x = ext("x", [SEQ, D])
    C.ctx = ext("ctx", [LC, D])
    C.c = ext("c", [1, D])
    C.c_ctx = ext("c_ctx", [1, D])
    C.w_ada = ext("w_ada", [DEPTH, D, 6 * D])
    C.b_ada = ext("b_ada", [DEPTH, 6 * D])
    C.norm1_g = ext("norm1_g", [DEPTH, D])
    C.norm2_g = ext("norm2_g", [DEPTH, D])
    C.w_in = ext("w_in", [DEPTH, D, IN_W])
    C.qn_a = ext("qn_a", [DEPTH, 128])
    C.kn_a = ext("kn_a", [DEPTH, 128])
    C.sink_a = ext("sink_a", [DEPTH, 8])
    C.qn_b = ext("qn_b", [DEPTH, 64])
    C.kn_b = ext("kn_b", [DEPTH, 64])
    C.lam_b = ext("lam_b", [DEPTH, 256])
    C.subln_b = ext("subln_b", [DEPTH, 128])
    C.conv_w = ext("conv_w", [DEPTH, 4, 1024])
    C.conv_b = ext("conv_b", [DEPTH, 1024])
    C.w_rg = ext("w_rg", [DEPTH, 2, 8, 128, 128])
    C.b_rg = ext("b_rg", [DEPTH, 2, 1024])
    C.w_ig = ext("w_ig", [DEPTH, 2, 8, 128, 128])
    C.b_ig = ext("b_ig", [DEPTH, 2, 1024])
    C.lru_lambda = ext("lru_lambda", [DEPTH, 2, 1024])
    C.lb_d = ext("lb_d", [DEPTH, 2, 1024])
    C.onorm_d = ext("onorm_d", [DEPTH, 128])
    C.w_branch = ext("w_branch", [DEPTH, 4, 1024, D])
    C.w_out = ext("w_out", [DEPTH, D, D])
    C.w_router = ext("w_router", [D, NE])
    C.b_router = ext("b_router", [1, NE])
    C.w_gate = ext("w_gate", [DEPTH, NE, D, DFF])
    C.w_up = ext("w_up", [DEPTH, NE, D, DFF])
    C.w_down = ext("w_down", [DEPTH, NE, DFF, D])
    C.ropeA_c = ext("ropeA_c", [SEQ, 128])
    C.ropeA_s = ext("ropeA_s", [SEQ, 128])
    C.ropeB_c = ext("ropeB_c", [SEQ, 64])
    C.ropeB_s = ext("ropeB_s", [SEQ, 64])
    C.out = P.dram("out", [SEQ, D], F32, kind="ExternalOutput")

    def scratch(name, shape, dt):
        kind = "ExternalInput" if name in feed else ("ExternalOutput" if name in debug else "Internal")
        return P.dram(name, shape, dt, kind=kind)

    C.XRES = scratch("XRES", [T, D], F32)
    C.MOD = scratch("MOD", [2, 6 * D], F32)
    C.QAT = scratch("QAT", [8, 128, T], BF16)
    C.KAT = scratch("KAT", [2, 128, T], BF16)
    C.VA = scratch("VA", [T, 256], BF16)
    C.QBT = scratch("QBT", [8, 128, T], BF16)
    C.KBT = scratch("KBT", [8, 128, T], BF16)
    C.VB = scratch("VB", [T, 1024], BF16)
    C.CXT = scratch("CXT", [1024, T], F32)
    C.GCY = scratch("GCY", [1024, T], BF16)
    C.DQT = scratch("DQT", [1024, T], BF16)
    C.ZF = scratch("ZF", [2, 1024, T], F32)
    C.VD = scratch("VD", [T, 1024], BF16)
    C.SGD = scratch("SGD", [T, 1024], BF16)
    C.SGT = scratch("SGT", [4 * D, T], BF16)
    C.OT = scratch("OT", [4, 1024, T], BF16)
    C.YT = scratch("YT", [D, T], BF16)
    C.H2T = scratch("H2T", [D, T], BF16)
    C.GATES = scratch("GATES", [T, NE], F32)
    C.DBG = None
    if "DBGHG" in debug:
        eo = lambda n, sh, dt=F32: P.dram(n, sh, dt, kind="ExternalOutput")
        C.DBG = {"O": eo("dbgO", [64, 36, 128]), "O1": eo("dbgO1", [64, 36, 128]),
                 "qt": eo("dbgqt", [2, 128, T], BF16), "kt": eo("dbgkt", [2, 128, T], BF16),
                 "qh": eo("dbgqh", [2, 128, T], BF16), "kh": eo("dbgkh", [2, 128, T], BF16),
                 "dec": eo("dbgdec", [2, 128, 36]), "Bx": eo("dbgBx", [128, T + 1]), "kk": eo("dbgkk", [128, T])}

    ident_f = P.sbuf("ident_f", [128, 128], F32)
    ident = P.sbuf("ident", [128, 128], BF16)
    ones_f = P.sbuf("ones_f", [128, 128], F32)
    ones = P.sbuf("ones", [128, 128], BF16)
    m_ge = P.sbuf("m_ge", [128, 128], BF16)
    m_le = P.sbuf("m_le", [128, 128], BF16)
    tmpc = P.sbuf("tmpc", [128, 128], F32)
    P.op("pool", lambda e: e.memset(ident_f[:], 0.0), w=[ident_f])
    P.op("pool", lambda e: e.affine_select(out=ident_f[:], in_=ident_f[:], pattern=[[-1, 128]],
                                           compare_op=ALU.not_equal, fill=1.0, base=0, channel_multiplier=1),
         r=[ident_f], w=[ident_f])
    P.op("dve", lambda e: e.tensor_copy(out=ident[:], in_=ident_f[:]), r=[ident_f], w=[ident])
    P.op("pool", lambda e: e.memset(ones_f[:], 1.0), w=[ones_f])
    P.op("dve", lambda e: e.tensor_copy(out=ones[:], in_=ones_f[:]), r=[ones_f], w=[ones])
    P.op("pool", lambda e: e.affine_select(out=tmpc[:], in_=ones_f[:], pattern=[[-1, 128]],
                                           compare_op=ALU.is_ge, fill=0.0, base=0, channel_multiplier=1),
         r=[ones_f], w=[tmpc])
    P.op("dve", lambda e: e.tensor_copy(out=m_ge[:], in_=tmpc[:]), r=[tmpc], w=[m_ge])
    P.op("pool", lambda e: e.affine_select(out=tmpc[:], in_=ones_f[:], pattern=[[1, 128]],
                                           compare_op=ALU.is_ge, fill=0.0, base=0, channel_multiplier=-1),
         r=[ones_f], w=[tmpc])
    P.op("dve", lambda e: e.tensor_copy(out=m_le[:], in_=tmpc[:]), r=[tmpc], w=[m_le])
    C.ident, C.ident_f, C.ones, C.ones_f, C.m_ge, C.m_le = ident, ident_f, ones, ones_f, m_ge, m_le

    P.dma("sp", C.XRES[0:LC, :], C.ctx[:, :])
    for i in range(4):
        P.dma("sp", C.XRES[LC + i * 512:LC + (i + 1) * 512, :], C.x[i * 512:(i + 1) * 512, :])
    P.barrier()

    if only is not None:
        with P.scope():
            LP = layer_consts(P, C, 0)
            if "mod" in only:
                stage_mod(P, C, 0, LP)
            for nm in only:
                if nm == "np":
                    with P.scope():
                        hT, hT_tok = stage_norm_T(P, C, 0, LP)
                        stage_proj(P, C, 0, LP, hT, hT_tok)
                elif nm != "mod":
                    globals()["stage_" + nm](P, C, 0, LP)
        P.finalize()
        return nc, P
    for l in range(nlayers):
        last = (l == DEPTH - 1)
        with P.scope():
            LP = layer_consts(P, C, l)
            stage_mod(P, C, l, LP)
            with P.scope():
                hT, hT_tok = stage_norm_T(P, C, l, LP)
                stage_proj(P, C, l, LP, hT, hT_tok)
            stage_attn_a(P, C, l, LP)
            stage_attn_b(P, C, l, LP)
            stage_rglru(P, C, l, LP)
            stage_hgrn(P, C, l, LP)
            stage_merge(P, C, l, LP)
            stage_out_norm2(P, C, l, LP)
            stage_moe(P, C, l, LP, last)
    P.finalize()
    return nc, P


def col_load(P, dst_ap, src_tile, off, n, w, eng="sp"):
    k = n // 128
    src = bass.AP(src_tile.t, off, [[1, 128], [128, k]])
    P.dma(eng, dst_ap, src, w=w, allow_slow_non_contiguous=True)


def layer_consts(P, C, l):
    LP = Ctx()
    LP.n1 = P.sbuf("n1", [128, 16])
    LP.n2 = P.sbuf("n2", [128, 16])
    col_load(P, LP.n1[:], C.norm1_g, l * D, D, [LP.n1])
    col_load(P, LP.n2[:], C.norm2_g, l * D, D, [LP.n2])
    LP.qn_a = P.sbuf("qn_a", [128, 128])
    LP.kn_a = P.sbuf("kn_a", [128, 128])
    LP.qn_b = P.sbuf("qn_b", [128, 64])
    LP.kn_b = P.sbuf("kn_b", [128, 64])
    LP.onorm = P.sbuf("onorm", [128, 128])
    P.dma("sp", LP.qn_a[:], pbc(C.qn_a, l * 128, 128), w=[LP.qn_a])
    P.dma("sp", LP.kn_a[:], pbc(C.kn_a, l * 128, 128), w=[LP.kn_a])
    P.dma("sp", LP.qn_b[:], pbc(C.qn_b, l * 64, 64), w=[LP.qn_b])
    P.dma("sp", LP.kn_b[:], pbc(C.kn_b, l * 64, 64), w=[LP.kn_b])
    P.dma("sp", LP.onorm[:], pbc(C.onorm_d, l * 128, 128), w=[LP.onorm])
    LP.esink = P.sbuf("esink", [128, 8])
    P.dma("sp", LP.esink[:], pbc(C.sink_a, l * 8, 8), w=[LP.esink])
    P.op("act", lambda e: e.activation(out=LP.esink[:], in_=LP.esink[:], func=AF.Exp), r=[LP.esink], w=[LP.esink])
    lam_init = 0.8 - 0.6 * math.exp(-0.3 * l)
    LP.lam_init = lam_init
    lb_ = P.sbuf("lamb", [128, 256])
    P.dma("sp", lb_[:], pbc(C.lam_b, l * 256, 256), w=[lb_])
    pr = P.sbuf("lampr", [128, 2, 64])
    lb4 = lb_[:].rearrange("p (a b d) -> p a b d", a=2, b=2)
    P.op("dve", lambda e: e.tensor_tensor(out=pr[:], in0=lb4[:, :, 0, :], in1=lb4[:, :, 1, :], op=ALU.mult),
         r=[lb_], w=[pr])
    s2 = P.sbuf("lams2", [128, 2])
    P.op("dve", lambda e: e.tensor_reduce(out=s2[:], in_=pr[:], axis=AX.X, op=ALU.add), r=[pr], w=[s2])
    P.op("act", lambda e: e.activation(out=s2[:], in_=s2[:], func=AF.Exp), r=[s2], w=[s2])
    LP.nlam = P.sbuf("nlam", [128, 1])
    P.op("dve", lambda e: e.scalar_tensor_tensor(out=LP.nlam[:], in0=s2[:, 1:2], scalar=-lam_init, in1=s2[:, 0:1],
                                                 op0=ALU.add, op1=ALU.subtract), r=[s2], w=[LP.nlam])
    LP.subln = P.sbuf("subln", [128, 1])
    col_load(P, LP.subln[:], C.subln_b, l * 128, 128, [LP.subln])
    P.op("dve", lambda e: e.tensor_scalar(out=LP.subln[:], in0=LP.subln[:], scalar1=(1.0 - lam_init), scalar2=None,
                                          op0=ALU.mult), r=[LP.subln], w=[LP.subln])
    LP.convw = P.sbuf("convw", [128, 4, 8])
    for tap in range(4):
        col_load(P, LP.convw[:, tap, :], C.conv_w, (l * 4 + tap) * 1024, 1024, [LP.convw])
    LP.convb = P.sbuf("convb", [128, 8])
    col_load(P, LP.convb[:], C.conv_b, l * 1024, 1024, [LP.convb])
    LP.brg = P.sbuf("brg", [128, 2, 8])
    LP.big = P.sbuf("big", [128, 2, 8])
    lam = P.sbuf("lrulam", [128, 2, 8])
    for d in range(2):
        col_load(P, LP.brg[:, d, :], C.b_rg, (l * 2 + d) * 1024, 1024, [LP.brg])
        col_load(P, LP.big[:, d, :], C.b_ig, (l * 2 + d) * 1024, 1024, [LP.big])
        col_load(P, lam[:, d, :], C.lru_lambda, (l * 2 + d) * 1024, 1024, [lam])
    P.op("act", lambda e: e.activation(out=lam[:], in_=lam[:], func=AF.Exp, scale=-1.0), r=[lam], w=[lam])
    P.op("act", lambda e: e.activation(out=lam[:], in_=lam[:], func=AF.Ln, bias=1.0, scale=1.0), r=[lam], w=[lam])
    LP.sp8 = P.sbuf("sp8", [128, 2, 8])
    LP.sp16 = P.sbuf("sp16", [128, 2, 8])
    P.op("dve", lambda e: e.tensor_scalar(out=LP.sp8[:], in0=lam[:], scalar1=-8.0, scalar2=None, op0=ALU.mult),
         r=[lam], w=[LP.sp8])
    P.op("dve", lambda e: e.tensor_scalar(out=LP.sp16[:], in0=lam[:], scalar1=-16.0, scalar2=None, op0=ALU.mult),
         r=[lam], w=[LP.sp16])
    LP.lb = P.sbuf("lb", [128, 2, 8])
    LP.oml = P.sbuf("oml", [128, 2, 8])
    LP.noml = P.sbuf("noml", [128, 2, 8])
    if l == 0:
        P.op("pool", lambda e: e.memset(LP.lb[:], 0.0), w=[LP.lb])
    else:
        d0 = P.sbuf("lbd0", [128, 2, 8])
        for d in range(2):
            col_load(P, d0[:, d, :], C.lb_d, (0 * 2 + d) * 1024, 1024, [d0])
            col_load(P, LP.lb[:, d, :], C.lb_d, (1 * 2 + d) * 1024, 1024, [LP.lb])
        P.op("dve", lambda e: e.tensor_tensor(out=LP.lb[:], in0=LP.lb[:], in1=d0[:], op=ALU.subtract),
             r=[LP.lb, d0], w=[LP.lb])
        P.op("act", lambda e: e.activation(out=LP.lb[:], in_=LP.lb[:], func=AF.Sigmoid), r=[LP.lb], w=[LP.lb])
    P.op("dve", lambda e: e.tensor_scalar(out=LP.oml[:], in0=LP.lb[:], scalar1=-1.0, scalar2=1.0, op0=ALU.mult,
                                          op1=ALU.add), r=[LP.lb], w=[LP.oml])
    P.op("dve", lambda e: e.tensor_scalar(out=LP.noml[:], in0=LP.oml[:], scalar1=-1.0, scalar2=None, op0=ALU.mult),
         r=[LP.oml], w=[LP.noml])
    LP.wr = P.sbuf("wr", [128, 16, NE])
    P.dma("sp", LP.wr[:], C.w_router[:, :].rearrange("(c p) e -> p c e", p=128), w=[LP.wr])
    LP.br = P.sbuf("br", [128, NE])
    P.dma("sp", LP.br[:], pbc(C.b_router, 0, NE), w=[LP.br])
    LP.modP = P.sbuf("modP", [128, 2, 6, 16])
    LP.s1 = P.sbuf("s1", [128, 2, 16])
    LP.s2 = P.sbuf("s2", [128, 2, 16])
    return LP


def stage_mod(P, C, l, LP):
    with P.scope():
        cc = P.sbuf("cc", [128, 16, 2])
        col_load(P, cc[:, :, 0], C.c, 0, D, [cc])
        col_load(P, cc[:, :, 1], C.c_ctx, 0, D, [cc])
        scT = P.sbuf("scT", [128, 16, 2], BF16)
        P.op("act", lambda e: e.activation(out=scT[:], in_=cc[:], func=AF.Silu), r=[cc], w=[scT])
        bada = P.sbuf("bada", [2, 6 * D])
        P.dma("sp", bada[:], pbc(C.b_ada, l * 6 * D, 6 * D, parts=2), w=[bada])
        modsb = P.sbuf("modsb", [2, 6 * D])
        wsrc = C.w_ada[l, :, :].rearrange("(c p) n -> p c n", p=128)
        W = [P.sbuf(f"wada{i}", [128, 16, 512], BF16) for i in range(2)]
        ps = [P.psum(f"psmod{i}") for i in range(2)]
        for g in range(24):
            wt = W[g % 2]
            pt = ps[g % 2]
            P.dma("pool", wt[:], wsrc[:, :, g * 512:(g + 1) * 512], w=[wt])
            for c in range(16):
                P.op("pe", lambda e, c=c, wt=wt, pt=pt: e.matmul(pt[0:2, :], lhsT=scT[:, c, :], rhs=wt[:, c, :],
                                                               start=(c == 0), stop=(c == 15)),
                     r=[scT, wt], w=[pt])
            P.op("dve", lambda e, g=g, pt=pt: e.tensor_tensor(out=modsb[:, g * 512:(g + 1) * 512], in0=pt[0:2, :],
                                                              in1=bada[:, g * 512:(g + 1) * 512], op=ALU.add),
                 r=[pt, bada], w=[modsb])
        P.dma("sp", C.MOD[:, :], modsb[:], r=[modsb])
    for r in range(2):
        for k in range(6):
            col_load(P, LP.modP[:, r, k, :], C.MOD, r * 6 * D + k * D, D, [LP.modP])
    for (s, n, k) in ((LP.s1, LP.n1, 1), (LP.s2, LP.n2, 4)):
        for r in range(2):
            P.op("dve", lambda e, s=s, n=n, k=k, r=r: e.scalar_tensor_tensor(
                out=s[:, r, :], in0=LP.modP[:, r, k, :], scalar=1.0, in1=n[:], op0=ALU.add, op1=ALU.mult),
                r=[LP.modP, n], w=[s])


def norm_tile(P, xt, xn_out_dtype_tile, sq, ss, n):
    P.op("act", lambda e: e.activation(out=sq[:], in_=xt[:], func=AF.Square), r=[xt], w=[sq])
    P.op("dve", lambda e: e.tensor_reduce(out=ss[:], in_=sq[:], axis=AX.X, op=ALU.add), r=[sq], w=[ss])
    P.op("act", lambda e: e.activation(out=ss[:], in_=ss[:], func=AF.Sqrt, scale=1.0 / n, bias=EPS), r=[ss], w=[ss])
    P.op("dve", lambda e: e.reciprocal(out=ss[:], in_=ss[:]), r=[ss], w=[ss])
    P.op("dve", lambda e: e.tensor_scalar(out=xn_out_dtype_tile[:], in0=xt[:], scalar1=ss[:, 0:1], scalar2=None,
                                          op0=ALU.mult), r=[xt, ss], w=[xn_out_dtype_tile])


def stage_norm_T(P, C, l, LP):
    hT = P.sbuf("hT", [128, 16, T], BF16)
    hT_tok = [Tok(f"hT{t}") for t in range(NT)]
    with P.scope():
        xt = [P.sbuf(f"xt{i}", [128, D]) for i in range(2)]
        sq = P.sbuf("sq", [128, D])
        xn = [P.sbuf(f"xn{i}", [128, D], BF16) for i in range(2)]
        ss = [P.sbuf(f"ss{i}", [128, 1]) for i in range(2)]
        pst = [P.psum(f"pst{i}", [128, 1024], BF16) for i in range(2)]
        P.dma("sp", xt[0][:], C.XRES[0:128, :], w=[xt[0]])
        for t in range(NT):
            if t + 1 < NT:
                P.dma("sp", xt[(t + 1) % 2][:], C.XRES[(t + 1) * 128:(t + 2) * 128, :], w=[xt[(t + 1) % 2]])
            X, XN, SS = xt[t % 2], xn[t % 2], ss[t % 2]
            norm_tile(P, X, XN, sq, SS, D)
            r = 1 if t < 2 else 0
            for c4 in range(4):
                pt = pst[c4 % 2]
                for j in range(4):
                    c = c4 * 4 + j
                    P.op("pe", lambda e, c=c, j=j, pt=pt, XN=XN: e.transpose(
                        out=pt[:, j * 128:(j + 1) * 128], in_=XN[:, c * 128:(c + 1) * 128], identity=C.ident[:]),
                        r=[XN, C.ident], w=[pt])
                for j in range(4):
                    c = c4 * 4 + j
                    eng = "act" if j % 2 == 0 else "dve"
                    if eng == "act":
                        P.op("act", lambda e, c=c, j=j, pt=pt, t=t, r=r: e.activation(
                            out=hT[:, c, t * 128:(t + 1) * 128], in_=pt[:, j * 128:(j + 1) * 128], func=AF.Identity,
                            scale=LP.s1[:, r, c:c + 1], bias=LP.modP[:, r, 0, c:c + 1]),
                            r=[pt, LP.s1, LP.modP], w=[hT_tok[t]])
                    else:
                        P.op("dve", lambda e, c=c, j=j, pt=pt, t=t, r=r: e.tensor_scalar(
                            out=hT[:, c, t * 128:(t + 1) * 128], in0=pt[:, j * 128:(j + 1) * 128],
                            scalar1=LP.s1[:, r, c:c + 1], scalar2=LP.modP[:, r, 0, c:c + 1], op0=ALU.mult,
                            op1=ALU.add), r=[pt, LP.s1, LP.modP], w=[hT_tok[t]])
    return hT, hT_tok


def proj_groups():
    g = []
    g += [("aq", 0), ("aq", 1), ("akv", 0)]
    g += [("bq", 0), ("bq", 1), ("bk", 0), ("bk", 1), ("bv", 0), ("bv", 1)]
    g += [("cx", 0), ("cx", 1), ("cy", 0), ("cy", 1)]
    g += [("dq", 0), ("dq", 1), ("dff", 0), ("dff", 1), ("dfb", 0), ("dfb", 1)]
    g += [("di", 0), ("di", 1), ("dg", 0), ("dg", 1)]
    g += [("gt", i) for i in range(16)]
    return g


def stage_proj(P, C, l, LP, hT, hT_tok):
    groups = proj_groups()
    assert len(groups) * 512 == IN_W
    with P.scope():
        wsrc = C.w_in[l, :, :].rearrange("(c p) n -> p c n", p=128)
        W = [P.sbuf(f"win{i}", [128, 16, 512], BF16) for i in range(3)]
        ps = [P.psum(f"psproj{i}") for i in range(5)]
        pst = [P.psum(f"pstq{i}", [128, 1024], BF16) for i in range(2)]
        ca = [P.sbuf(f"ca{i}", [128, 128]) for i in range(2)]
        sa = [P.sbuf(f"sa{i}", [128, 128]) for i in range(2)]
        cb = [P.sbuf(f"cb{i}", [128, 64]) for i in range(2)]
        sb = [P.sbuf(f"sb{i}", [128, 64]) for i in range(2)]
        sq = P.sbuf("qsq", [128, 512])
        ss = P.sbuf("qss", [128, 8])
        qn = P.sbuf("qn", [128, 512])
        t1 = P.sbuf("qt1", [128, 512])
        t2 = P.sbuf("qt2", [128, 512])
        qb = [P.sbuf(f"qb{i}", [128, 512], BF16) for i in range(4)]
        qT = [P.sbuf(f"qT{i}", [128, 512], BF16) for i in range(2)]
        ob = [P.sbuf(f"ob{i}", [128, 512], BF16) for i in range(3)]
        of = [P.sbuf(f"of{i}", [128, 512], F32) for i in range(2)]
        gl = [P.sbuf(f"gl{i}", [128, 512], F32) for i in range(2)]
        cnt = {"ps": 0, "ob": 0, "of": 0, "qb": 0, "pst": 0, "qT": 0, "rope": 0}

        def nxt(key, lst):
            i = cnt[key]
            cnt[key] += 1
            return lst[i % len(lst)]

        pending = []

        def flush_pending(keep):
            while len(pending) > keep:
                pending.pop(0)()

        def qk_epi(pt, t, ncomp, dim, gain, cosT, sinT, dst, dst_h0, nheads_out):
            w = ncomp * dim
            P.op("act", lambda e: e.activation(out=sq[:, :w], in_=pt[:, :w], func=AF.Square), r=[pt], w=[sq])
            P.op("dve", lambda e: e.tensor_reduce(out=ss[:, :ncomp], in_=sq[:, :w].rearrange("p (h d) -> p h d", d=dim),
                                                  axis=AX.X, op=ALU.add), r=[sq], w=[ss])
            P.op("act", lambda e: e.activation(out=ss[:, :ncomp], in_=ss[:, :ncomp], func=AF.Sqrt, scale=1.0 / dim,
                                               bias=EPS), r=[ss], w=[ss])
            P.op("dve", lambda e: e.reciprocal(out=ss[:, :ncomp], in_=ss[:, :ncomp]), r=[ss], w=[ss])
            v3 = lambda tl: tl[:, :w].rearrange("p (h d) -> p h d", d=dim)
            P.op("dve", lambda e: e.tensor_tensor(out=v3(qn), in0=v3(pt), in1=bc_last(ss[:, :ncomp], dim),
                                                  op=ALU.mult), r=[pt, ss], w=[qn])
            QB = nxt("qb", qb)
            if t >= 2:
                P.op("pool", lambda e: e.tensor_tensor(out=v3(qn), in0=v3(qn), in1=bc_mid(gain[:, :dim], ncomp),
                                                       op=ALU.mult), r=[qn, gain], w=[qn])
                hd = dim // 4
                ng = w // (2 * hd)
                v4 = lambda tl: tl[:, :w].rearrange("p (g s i) -> p g s i", s=2, i=hd)
                def tb(tab, s):
                    b = tab[:, :].rearrange("p (g s i) -> p g s i", s=2, i=hd)[:, :, s, :]
                    return mk(b, [b.ap[0], [0, ncomp], b.ap[1], b.ap[2]])
                q5 = lambda tl, s: tl[:, :w].rearrange("p (h g s i) -> p h g s i", h=ncomp, s=2, i=hd)[:, :, :, s, :]
                P.op("dve", lambda e: e.tensor_tensor(out=v3(t1), in0=v3(qn), in1=bc_mid(cosT[:, :dim], ncomp),
                                                      op=ALU.mult), r=[qn, cosT], w=[t1])
                for s in range(2):
                    P.op("pool", lambda e, s=s: e.tensor_tensor(out=q5(t2, s), in0=q5(qn, 1 - s), in1=tb(sinT, s),
                                                                op=ALU.mult), r=[qn, sinT], w=[t2])
                P.op("dve", lambda e: e.tensor_tensor(out=QB[:, :w], in0=t1[:, :w], in1=t2[:, :w], op=ALU.add),
                     r=[t1, t2], w=[QB])
            else:
                P.op("pool", lambda e: e.tensor_tensor(out=v3(QB), in0=v3(qn), in1=bc_mid(gain[:, :dim], ncomp),
                                                       op=ALU.mult), r=[qn, gain], w=[QB])
            nblk = w // 128

            def part_b():
                PT = nxt("pst", pst)
                for j in range(nblk):
                    P.op("pe", lambda e, j=j: e.transpose(out=PT[:, j * 128:(j + 1) * 128],
                                                          in_=QB[:, j * 128:(j + 1) * 128], identity=C.ident[:]),
                         r=[QB, C.ident], w=[PT])
                QT = nxt("qT", qT)
                P.op("act", lambda e: e.activation(out=QT[:, :w], in_=PT[:, :w], func=AF.Copy), r=[PT], w=[QT])
                dsta = bass.AP(dst.t, dst_h0 * 128 * T + t * 128, [[T, 128], [128 * T, nblk], [1, 128]])
                P.dma("sp", dsta, QT[:, :w].rearrange("p (h q) -> p h q", q=128), r=[QT])

            pending.append(part_b)

        for gi, (kind, idx) in enumerate(groups):
            wt = W[gi % 3]
            P.dma("pool", wt[:], wsrc[:, :, gi * 512:(gi + 1) * 512], w=[wt])
            tokmajor = kind in ("aq", "akv", "bq", "bk", "bv", "di", "dg")
            if tokmajor:
                for t in range(NT):
                    pt = nxt("ps", ps)
                    for c in range(16):
                        P.op("pe", lambda e, c=c, t=t, pt=pt, wt=wt: e.matmul(
                            pt[:], lhsT=hT[:, c, t * 128:(t + 1) * 128], rhs=wt[:, c, :], start=(c == 0),
                            stop=(c == 15)), r=[hT_tok[t], wt], w=[pt])
                    flush_pending(1)
                    lat = t >= 2
                    if lat and kind in ("aq", "akv", "bq", "bk"):
                        k = cnt["rope"] % 2
                        cnt["rope"] += 1
                        r0 = (t - 2) * 128
                        if kind in ("aq", "akv"):
                            P.dma("sp", ca[k][:], C.ropeA_c[r0:r0 + 128, :], w=[ca[k]])
                            P.dma("sp", sa[k][:], C.ropeA_s[r0:r0 + 128, :], w=[sa[k]])
                            cT, sT = ca[k], sa[k]
                        else:
                            P.dma("sp", cb[k][:], C.ropeB_c[r0:r0 + 128, :], w=[cb[k]])
                            P.dma("sp", sb[k][:], C.ropeB_s[r0:r0 + 128, :], w=[sb[k]])
                            cT, sT = cb[k], sb[k]
                    else:
                        cT = sT = None
                    if kind == "aq":
                        qk_epi(pt, t, 4, 128, LP.qn_a, cT, sT, C.QAT, idx * 4, 4)
                    elif kind == "akv":
                        qk_epi(pt, t, 2, 128, LP.kn_a, cT, sT, C.KAT, 0, 2)
                        O = nxt("ob", ob)
                        P.op("act", lambda e, O=O, pt=pt: e.activation(out=O[:, :256], in_=pt[:, 256:512],
                                                                       func=AF.Copy), r=[pt], w=[O])
                        P.dma("sp", C.VA[t * 128:(t + 1) * 128, :], O[:, :256], r=[O])
                    elif kind == "bq":
                        qk_epi(pt, t, 8, 64, LP.qn_b, cT, sT, C.QBT, idx * 4, 4)
                    elif kind == "bk":
                        qk_epi(pt, t, 8, 64, LP.kn_b, cT, sT, C.KBT, idx * 4, 4)
                    else:
                        O = nxt("ob", ob)
                        dst = {"bv": C.VB, "di": C.VD, "dg": C.SGD}[kind]
                        fn = AF.Silu if kind == "dg" else AF.Copy
                        P.op("act", lambda e, O=O, pt=pt, fn=fn: e.activation(out=O[:], in_=pt[:], func=fn),
                             r=[pt], w=[O])
                        P.dma("sp", dst[t * 128:(t + 1) * 128, idx * 512:(idx + 1) * 512], O[:], r=[O])
            else:
                flush_pending(0)
                for cbk in range(4):
                    for (t0, tn) in TG:
                        pt = nxt("ps", ps)
                        toks = [hT_tok[t] for t in range(t0 // 128, (t0 + tn) // 128)]
                        for c in range(16):
                            P.op("pe", lambda e, c=c, pt=pt, wt=wt, cbk=cbk, t0=t0, tn=tn: e.matmul(
                                pt[:, :tn], lhsT=wt[:, c, cbk * 128:(cbk + 1) * 128], rhs=hT[:, c, t0:t0 + tn],
                                start=(c == 0), stop=(c == 15)), r=toks + [wt], w=[pt])
                        row = idx * 512 + cbk * 128
                        if kind in ("cx", "dff", "dfb"):
                            O = nxt("of", of)
                            P.op("act", lambda e, O=O, pt=pt, tn=tn: e.activation(out=O[:, :tn], in_=pt[:, :tn],
                                                                                  func=AF.Copy), r=[pt], w=[O])
                            if kind == "cx":
                                dst = C.CXT[row:row + 128, t0:t0 + tn]
                            else:
                                dst = C.ZF[0 if kind == "dff" else 1, row:row + 128, t0:t0 + tn]
                            P.dma("sp", dst, O[:, :tn], r=[O])
                        elif kind == "cy":
                            G = nxt("of", gl)
                            O = nxt("ob", ob)
                            P.op("act", lambda e, G=G, pt=pt, tn=tn: e.activation(out=G[:, :tn], in_=pt[:, :tn],
                                                                                  func=AF.Square), r=[pt], w=[G])
                            P.op("dve", lambda e, G=G, tn=tn: e.tensor_scalar(
                                out=G[:, :tn], in0=G[:, :tn], scalar1=0.044715 * 1.5957691216, scalar2=1.5957691216,
                                op0=ALU.mult, op1=ALU.add), r=[G], w=[G])
                            P.op("dve", lambda e, G=G, pt=pt, tn=tn: e.tensor_tensor(
                                out=G[:, :tn], in0=G[:, :tn], in1=pt[:, :tn], op=ALU.mult), r=[G, pt], w=[G])
                            P.op("act", lambda e, G=G, tn=tn: e.activation(out=G[:, :tn], in_=G[:, :tn],
                                                                           func=AF.Sigmoid), r=[G], w=[G])
                            P.op("dve", lambda e, G=G, O=O, pt=pt, tn=tn: e.tensor_tensor(
                                out=O[:, :tn], in0=G[:, :tn], in1=pt[:, :tn], op=ALU.mult), r=[G, pt], w=[O])
                            P.dma("sp", C.GCY[row:row + 128, t0:t0 + tn], O[:, :tn], r=[O])
                        else:
                            O = nxt("ob", ob)
                            fn = AF.Sigmoid if kind == "gt" else AF.Copy
                            P.op("act", lambda e, O=O, pt=pt, tn=tn, fn=fn: e.activation(
                                out=O[:, :tn], in_=pt[:, :tn], func=fn), r=[pt], w=[O])
                            dst = (C.SGT if kind == "gt" else C.DQT)[row:row + 128, t0:t0 + tn]
                            P.dma("sp", dst, O[:, :tn], r=[O])


def stage_attn_a(P, C, l, LP):
    scale = 128 ** -0.5
    with P.scope():
        QA = P.sbuf("QA", [128, 8, T], BF16)
        KA = P.sbuf("KA", [128, 2, T], BF16)
        VAs = P.sbuf("VAs", [128, NT, 256], BF16)
        OA = P.sbuf("OA", [128, 8, T], BF16)
        P.dma("sp", QA[:], C.QAT[:, :, :].rearrange("h d t -> d h t"), w=[QA])
        P.dma("sp", KA[:], C.KAT[:, :, :].rearrange("h d t -> d h t"), w=[KA])
        P.dma("sp", VAs[:], C.VA[:, :].rearrange("(n p) v -> p n v", p=128), w=[VAs])
        pss = [P.psum(f"pss{i}") for i in range(2)]
        pso = [P.psum(f"pso{i}") for i in range(2)]
        psz = [P.psum(f"psz{i}") for i in range(2)]
        pT = [P.sbuf(f"pT{i}", [128, 512], BF16) for i in range(3)]
        zs = P.sbuf("zs", [128, 512])
        ot = P.sbuf("ot", [128, 512])
        OA_tok = Tok("OA")
        steps = []
        it = 0
        for qb in range(NT):
            if qb < 2:
                kbs = [(0, None), (1, None)]
            else:
                n = qb - 2
                kbs = [(0, None), (1, None)]
                if n >= 1:
                    kbs.append((qb - 1, C.m_ge))
                kbs.append((qb, None))
                if n <= 14:
                    kbs.append((qb + 1, C.m_le))
            for hk in range(2):
                po, pz = pso[it % 2], psz[it % 2]
                it += 1
                for ki, (kb, mask) in enumerate(kbs):
                    steps.append(dict(qb=qb, hk=hk, kb=kb, mask=mask, po=po, pz=pz, first=(ki == 0),
                                      last=(ki == len(kbs) - 1)))
        for i, st in enumerate(steps):
            st["psx"] = pss[i % 2]
            st["pt"] = pT[i % 3]

        def emit_s(st):
            psx, kb, hk, qb = st["psx"], st["kb"], st["hk"], st["qb"]
            rhs_q = QA[:, 4 * hk:4 * hk + 4, qb * 128:(qb + 1) * 128]
            P.op("pe", lambda e: e.matmul(psx[:].rearrange("p (h q) -> p h q", h=4),
                                          lhsT=KA[:, hk, kb * 128:(kb + 1) * 128], rhs=rhs_q, start=True, stop=True),
                 r=[KA, QA], w=[psx])

        def emit_rest(st):
            psx, pt_, kb, hk, qb, mask, po, pz = (st["psx"], st["pt"], st["kb"], st["hk"], st["qb"], st["mask"],
                                                  st["po"], st["pz"])
            f, l_ = st["first"], st["last"]
            P.op("act", lambda e: e.activation(out=pt_[:], in_=psx[:], func=AF.Exp, scale=scale), r=[psx], w=[pt_])
            if mask is not None:
                P.op("pool", lambda e: e.tensor_tensor(
                    out=pt_[:].rearrange("p (h q) -> p h q", h=4), in0=pt_[:].rearrange("p (h q) -> p h q", h=4),
                    in1=bc_mid(mask[:, :], 4), op=ALU.mult), r=[pt_, mask], w=[pt_])
            P.op("pe", lambda e: e.matmul(po[:], lhsT=VAs[:, kb, hk * 128:(hk + 1) * 128], rhs=pt_[:], start=f,
                                          stop=l_), r=[VAs, pt_], w=[po])
            P.op("pe", lambda e: e.matmul(pz[:], lhsT=C.ones[:], rhs=pt_[:], start=f, stop=l_),
                 r=[C.ones, pt_], w=[pz])
            if l_:
                P.op("dve", lambda e: e.tensor_tensor(
                    out=zs[:].rearrange("p (h q) -> p h q", h=4), in0=pz[:].rearrange("p (h q) -> p h q", h=4),
                    in1=bc_last(LP.esink[:, 4 * hk:4 * hk + 4], 128), op=ALU.add), r=[pz, LP.esink], w=[zs])
                P.op("dve", lambda e: e.reciprocal(out=zs[:], in_=zs[:]), r=[zs], w=[zs])
                P.op("dve", lambda e: e.tensor_tensor(
                    out=OA[:, 4 * hk:4 * hk + 4, qb * 128:(qb + 1) * 128],
                    in0=po[:].rearrange("p (h q) -> p h q", h=4), in1=zs[:].rearrange("p (h q) -> p h q", h=4),
                    op=ALU.mult), r=[po, zs], w=[OA_tok])

        emit_s(steps[0])
        for i, st in enumerate(steps):
            if i + 1 < len(steps):
                emit_s(steps[i + 1])
            emit_rest(st)
        P.dma("sp", C.OT[0, :, :].rearrange("(h d) t -> d h t", d=128), OA[:], r=[OA_tok])


def stage_attn_b(P, C, l, LP):
    scale = 64 ** -0.5
    with P.scope():
        QB_ = [P.sbuf(f"QBh{i}", [128, T], BF16) for i in range(2)]
        KB_ = [P.sbuf(f"KBh{i}", [128, T], BF16) for i in range(2)]
        VB_ = [P.sbuf(f"VBh{i}", [128, NT, 128], BF16) for i in range(2)]
        OB_ = [P.sbuf(f"OBh{i}", [128, T], BF16) for i in range(2)]
        pss = [P.psum(f"bpss{i}") for i in range(2)]
        pso = [P.psum(f"bpso{i}") for i in range(2)]
        psz = [P.psum(f"bpsz{i}") for i in range(2)]
        psn = P.psum("bpsn")
        pT = [P.sbuf(f"bpT{i}", [128, 512], BF16) for i in range(3)]
        r1 = P.sbuf("br1", [128, 512])
        r2 = P.sbuf("br2", [128, 512])
        o1 = P.sbuf("bo1", [128, 512])
        o2 = P.sbuf("bo2", [128, 512])
        osq = P.sbuf("bosq", [128, 512], BF16)
        ip = 0

        def load(h):
            k = h % 2
            P.dma("sp", QB_[k][:], C.QBT[h, :, :], w=[QB_[k]])
            P.dma("sp", KB_[k][:], C.KBT[h, :, :], w=[KB_[k]])
            P.dma("sp", VB_[k][:], C.VB[:, h * 128:(h + 1) * 128].rearrange("(n p) v -> p n v", p=128), w=[VB_[k]])

        load(0)
        for h in range(8):
            if h + 1 < 8:
                load(h + 1)
            Q, K, V, O = QB_[h % 2], KB_[h % 2], VB_[h % 2], OB_[h % 2]
            steps = []
            for (q0, qn_, nkb) in [(0, 256, 2)] + [(LC + i * 512, 512, NT) for i in range(4)]:
                for c in range(2):
                    for kb in range(nkb):
                        steps.append(dict(q0=q0, qn=qn_, c=c, kb=kb, first=(kb == 0), last=(kb == nkb - 1),
                                          fin=(c == 1 and kb == nkb - 1)))
            for st in steps:
                st["psx"] = pss[ip % 2]
                st["pt"] = pT[ip % 3]
                ip += 1

            def emit_s(st, K=K, Q=Q):
                psx, c, kb, q0, qn_ = st["psx"], st["c"], st["kb"], st["q0"], st["qn"]
                P.op("pe", lambda e: e.matmul(psx[:, :qn_], lhsT=K[c * 64:(c + 1) * 64, kb * 128:(kb + 1) * 128],
                                              rhs=Q[c * 64:(c + 1) * 64, q0:q0 + qn_], start=True, stop=True),
                     r=[K, Q], w=[psx])

            def emit_rest(st, V=V, O=O):
                psx, pt_, c, kb, q0, qn_ = st["psx"], st["pt"], st["c"], st["kb"], st["q0"], st["qn"]
                f, l_ = st["first"], st["last"]
                po, pz = pso[c], psz[c]
                P.op("act", lambda e: e.activation(out=pt_[:, :qn_], in_=psx[:, :qn_], func=AF.Exp, scale=scale),
                     r=[psx], w=[pt_])
                P.op("pe", lambda e: e.matmul(po[:, :qn_], lhsT=V[:, kb, :], rhs=pt_[:, :qn_], start=f, stop=l_),
                     r=[V, pt_], w=[po])
                P.op("pe", lambda e: e.matmul(pz[:, :qn_], lhsT=C.ones[:], rhs=pt_[:, :qn_], start=f, stop=l_),
                     r=[C.ones, pt_], w=[pz])
                if not st["fin"]:
                    return
                P.op("dve", lambda e: e.reciprocal(out=r1[:, :qn_], in_=psz[0][:, :qn_]), r=[psz[0]], w=[r1])
                P.op("dve", lambda e: e.reciprocal(out=r2[:, :qn_], in_=psz[1][:, :qn_]), r=[psz[1]], w=[r2])
                P.op("dve", lambda e: e.tensor_tensor(out=o1[:, :qn_], in0=pso[0][:, :qn_], in1=r1[:, :qn_],
                                                      op=ALU.mult), r=[pso[0], r1], w=[o1])
                P.op("dve", lambda e: e.tensor_tensor(out=o2[:, :qn_], in0=pso[1][:, :qn_], in1=r2[:, :qn_],
                                                      op=ALU.mult), r=[pso[1], r2], w=[o2])
                P.op("dve", lambda e: e.scalar_tensor_tensor(
                    out=o1[:, :qn_], in0=o2[:, :qn_], scalar=LP.nlam[:, 0:1], in1=o1[:, :qn_], op0=ALU.mult,
                    op1=ALU.add), r=[o1, o2, LP.nlam], w=[o1])
                P.op("act", lambda e: e.activation(out=osq[:, :qn_], in_=o1[:, :qn_], func=AF.Square),
                     r=[o1], w=[osq])
                P.op("pe", lambda e: e.matmul(psn[:, :qn_], lhsT=C.ones[:], rhs=osq[:, :qn_], start=True, stop=True),
                     r=[C.ones, osq], w=[psn])
                P.op("act", lambda e: e.activation(out=r1[:, :qn_], in_=psn[:, :qn_], func=AF.Sqrt,
                                                   scale=1.0 / 128, bias=EPS), r=[psn], w=[r1])
                P.op("dve", lambda e: e.reciprocal(out=r1[:, :qn_], in_=r1[:, :qn_]), r=[r1], w=[r1])
                P.op("dve", lambda e: e.scalar_tensor_tensor(
                    out=O[:, q0:q0 + qn_], in0=o1[:, :qn_], scalar=LP.subln[:, 0:1], in1=r1[:, :qn_], op0=ALU.mult,
                    op1=ALU.mult), r=[o1, r1, LP.subln], w=[O])

            emit_s(steps[0])
            for i, st in enumerate(steps):
                if i + 1 < len(steps):
                    emit_s(steps[i + 1])
                emit_rest(st)
            P.dma("sp", C.OT[1, h * 128:(h + 1) * 128, :], O[:], r=[O])


def rev(ap2):
    (ps, pn), (s, n) = ap2.ap
    return bass.AP(ap2.tensor, ap2.offset + s * (n - 1), [[ps, pn], [-s, n]])


def stage_rglru(P, C, l, LP):
    with P.scope():
        wr = P.sbuf("wrg", [128, 16, 128], BF16)
        wi = P.sbuf("wig", [128, 16, 128], BF16)
        P.dma("pool", wr[:], C.w_rg[l, :, :, :, :].rearrange("d b i o -> i (d b) o"), w=[wr])
        P.dma("pool", wi[:], C.w_ig[l, :, :, :, :].rearrange("d b i o -> i (d b) o"), w=[wi])
        x = P.sbuf("cx", [128, T])
        u = P.sbuf("cu", [128, T])
        ub = P.sbuf("cub", [128, T], BF16)
        rr = P.sbuf("crr", [128, T])
        ii = P.sbuf("cii", [128, T])
        aa = P.sbuf("caa", [128, T])
        vv = P.sbuf("cvv", [128, T])
        hh = [P.sbuf(f"chh{d}", [128, T]) for d in range(2)]
        gy = P.sbuf("cgy", [128, T], BF16)
        oo = P.sbuf("coo", [128, T], BF16)
        ps = [P.psum(f"cps{i}") for i in range(4)]
        ip = 0
        for ch in range(8):
            P.dma("sp", x[:], C.CXT[ch * 128:(ch + 1) * 128, :], w=[x])
            P.dma("sp", gy[:], C.GCY[ch * 128:(ch + 1) * 128, :], w=[gy])
            P.op("dve", lambda e, ch=ch: e.tensor_scalar(out=u[:], in0=x[:], scalar1=LP.convw[:, 2, ch:ch + 1],
                                                         scalar2=LP.convb[:, ch:ch + 1], op0=ALU.mult, op1=ALU.add),
                 r=[x, LP.convw, LP.convb], w=[u])
            for (s0, s1) in ((0, LC), (LC, T)):
                for tap in (0, 1, 3):
                    off = tap - 2
                    lo = s0 + max(0, -off)
                    hi = s1 - max(0, off)
                    P.op("dve", lambda e, ch=ch, tap=tap, lo=lo, hi=hi, off=off: e.scalar_tensor_tensor(
                        out=u[:, lo:hi], in0=x[:, lo + off:hi + off], scalar=LP.convw[:, tap, ch:ch + 1],
                        in1=u[:, lo:hi], op0=ALU.mult, op1=ALU.add), r=[x, u, LP.convw], w=[u])
            P.op("act", lambda e: e.activation(out=ub[:], in_=u[:], func=AF.Copy), r=[u], w=[ub])
            for d in range(2):
                for (t0, tn) in TG:
                    pr_, pi_ = ps[ip % 4], ps[(ip + 1) % 4]
                    ip += 2
                    P.op("pe", lambda e, pr_=pr_, d=d, ch=ch, t0=t0, tn=tn: e.matmul(
                        pr_[:, :tn], lhsT=wr[:, d * 8 + ch, :], rhs=ub[:, t0:t0 + tn], start=True, stop=True),
                        r=[wr, ub], w=[pr_])
                    P.op("pe", lambda e, pi_=pi_, d=d, ch=ch, t0=t0, tn=tn: e.matmul(
                        pi_[:, :tn], lhsT=wi[:, d * 8 + ch, :], rhs=ub[:, t0:t0 + tn], start=True, stop=True),
                        r=[wi, ub], w=[pi_])
                    P.op("act", lambda e, pr_=pr_, d=d, ch=ch, t0=t0, tn=tn: e.activation(
                        out=rr[:, t0:t0 + tn], in_=pr_[:, :tn], func=AF.Sigmoid, bias=LP.brg[:, d, ch:ch + 1]),
                        r=[pr_, LP.brg], w=[rr])
                    P.op("act", lambda e, pi_=pi_, d=d, ch=ch, t0=t0, tn=tn: e.activation(
                        out=ii[:, t0:t0 + tn], in_=pi_[:, :tn], func=AF.Sigmoid, bias=LP.big[:, d, ch:ch + 1]),
                        r=[pi_, LP.big], w=[ii])
                P.op("act", lambda e, d=d, ch=ch: e.activation(out=aa[:], in_=rr[:], func=AF.Exp,
                                                               scale=LP.sp8[:, d, ch:ch + 1]), r=[rr, LP.sp8], w=[aa])
                P.op("act", lambda e, d=d, ch=ch: e.activation(out=vv[:], in_=rr[:], func=AF.Exp,
                                                               scale=LP.sp16[:, d, ch:ch + 1]), r=[rr, LP.sp16], w=[vv])
                P.op("act", lambda e: e.activation(out=vv[:], in_=vv[:], func=AF.Sqrt, scale=-1.0, bias=1.0),
                     r=[vv], w=[vv])
                P.op("dve", lambda e: e.tensor_tensor(out=vv[:], in0=vv[:], in1=ii[:], op=ALU.mult), r=[vv, ii], w=[vv])
                P.op("dve", lambda e: e.tensor_tensor(out=vv[:], in0=vv[:], in1=u[:], op=ALU.mult), r=[vv, u], w=[vv])
                H = hh[d]
                if d == 0:
                    P.op("dve", lambda e, H=H: e.tensor_tensor_scan(out=H[:], data0=aa[:], data1=vv[:], initial=0.0,
                                                                    op0=ALU.mult, op1=ALU.add), r=[aa, vv], w=[H])
                else:
                    P.op("dve", lambda e, H=H: e.tensor_tensor_scan(
                        out=rev(H[:, 0:LC]), data0=rev(aa[:, 0:LC]), data1=rev(vv[:, 0:LC]), initial=0.0,
                        op0=ALU.mult, op1=ALU.add), r=[aa, vv], w=[H])
                    P.op("dve", lambda e, H=H: e.tensor_tensor_scan(
                        out=rev(H[:, LC:T]), data0=rev(aa[:, LC:T]), data1=rev(vv[:, LC:T]), initial=H[:, 0:1],
                        op0=ALU.mult, op1=ALU.add), r=[aa, vv, H], w=[H])
            P.op("dve", lambda e: e.tensor_tensor(out=hh[0][:], in0=hh[0][:], in1=hh[1][:], op=ALU.add),
                 r=[hh[0], hh[1]], w=[hh[0]])
            P.op("dve", lambda e: e.tensor_tensor(out=oo[:], in0=hh[0][:], in1=gy[:], op=ALU.mult),
                 r=[hh[0], gy], w=[oo])
            P.dma("sp", C.OT[2, ch * 128:(ch + 1) * 128, :], oo[:], r=[oo])


def stage_hgrn(P, C, l, LP):
    NCK = T // 64
    with P.scope():
        o1 = C.ones_f[:, 0:1]
        ones_bc = mk(o1, [o1.ap[0], [0, T]])
        mf = P.sbuf("mf", [64, 64], BF16)
        mb = P.sbuf("mb", [64, 64], BF16)
        P.op("dve", lambda e: e.tensor_copy(out=mf[:], in_=C.m_le[0:64, 0:64]), r=[C.m_le], w=[mf])
        P.op("dve", lambda e: e.tensor_copy(out=mb[:], in_=C.m_ge[0:64, 0:64]), r=[C.m_ge], w=[mb])
        z = P.sbuf("dz", [128, T])
        sg = P.sbuf("dsg", [128, T])
        kk = P.sbuf("dkk", [128, T])
        Bi = P.sbuf("dBi", [128, T])
        Be_ = P.sbuf("dBe", [128, T])
        E = P.sbuf("dE", [128, T])
        E2 = z
        qT = P.sbuf("dqT", [128, T], BF16)
        vt = P.sbuf("dvt", [64, NCK, 128], BF16)
        sgd = P.sbuf("dsgd", [64, NCK, 128], BF16)
        qt_ = [P.sbuf(f"dqt{d}", [128, T], BF16) for d in range(2)]
        kt_ = [P.sbuf(f"dkt{d}", [128, T], BF16) for d in range(2)]
        qh_ = [P.sbuf(f"dqh{d}", [128, T], BF16) for d in range(2)]
        khf = P.sbuf("dkhf", [128, T], BF16)
        kh_ = [khf, khf]
        kx_ = [P.sbuf(f"dkx{d}", [128, T], BF16) for d in range(2)]
        dec = [P.sbuf(f"ddec{d}", [128, NCK]) for d in range(2)]
        Sf = [P.sbuf(f"dSf{d}", [128, 128]) for d in range(2)]
        Sb = [P.sbuf(f"dSb{d}", [128, 128], BF16) for d in range(2)]
        Od = [P.sbuf(f"dO{d}", [64, NCK, 128]) for d in range(2)]
        Od_tok = [[Tok(f"Od{d}_{c}") for c in range(NCK // 4)] for d in range(2)]
        A_all = [P.sbuf(f"dAall{d}", [64, NCK, 64], BF16) for d in range(2)]
        KH_all = [P.sbuf(f"dKHall{d}", [64, NCK, 128], BF16) for d in range(2)]
        ps_att = P.psum("dpsatt")
        P.op("dve", lambda e: e.memset(ps_att[:], 0.0), w=[ps_att])
        ps_kh = P.psum("dpskh", [128, 1024], BF16)
        ps_o1 = [P.psum(f"dpso{d}") for d in range(2)]
        ps_o = [[ps_o1[d], ps_o1[d]] for d in range(2)]
        ps_s2 = [[P.psum(f"dpss{d}{i}") for i in range(2)] for d in range(2)]
        ps_s_tok = ps_s2
        osq = Od[1]
        oss = P.sbuf("doss", [64, NCK])
        oy = KH_all[0]
        odT = P.sbuf("dodT", [128, T], BF16)

        c3 = lambda tl, off=0: tl[:, off:off + T].rearrange("p (c i) -> p c i", i=64)
        for h in range(8):
            P.dma("sp", qT[:], C.DQT[h * 128:(h + 1) * 128, :], w=[qT])
            P.dma("sp", vt[:], C.VD[:, h * 128:(h + 1) * 128].rearrange("(c p) v -> p c v", p=64), w=[vt])
            P.dma("sp", sgd[:], C.SGD[:, h * 128:(h + 1) * 128].rearrange("(c p) v -> p c v", p=64), w=[sgd])
            def gate_math(d, h):
                P.dma("sp", z[:], C.ZF[d, h * 128:(h + 1) * 128, :], w=[z])
                P.op("act", lambda e: e.activation(out=sg[:], in_=z[:], func=AF.Sigmoid), r=[z], w=[sg])
                P.op("dve", lambda e, d=d, h=h: e.tensor_scalar(
                    out=kk[:], in0=sg[:], scalar1=LP.noml[:, d, h:h + 1], scalar2=LP.oml[:, d, h:h + 1], op0=ALU.mult,
                    op1=ALU.add), r=[sg, LP.noml, LP.oml], w=[kk])
                P.op("act", lambda e, d=d, h=h: e.activation(out=sg[:], in_=sg[:], func=AF.Ln,
                                                             scale=LP.oml[:, d, h:h + 1], bias=LP.lb[:, d, h:h + 1]),
                     r=[sg, LP.oml, LP.lb], w=[sg])
                P.op("dve", lambda e: e.tensor_tensor_scan(out=Bi[:], data0=ones_bc, data1=sg[:],
                                                           initial=0.0, op0=ALU.mult, op1=ALU.add),
                     r=[C.ones_f, sg], w=[Bi])
                P.op("dve", lambda e: e.tensor_tensor(out=Be_[:], in0=Bi[:], in1=sg[:], op=ALU.subtract),
                     r=[Bi, sg], w=[Be_])
                Bx = Bi
                Bsrc = Bi if d == 0 else Be_
                Bv = c3(Bsrc)
                Bs1 = c3(Be_)[:, :, 0:1]
                Be = c3(Bi)[:, :, 63:64]
                c32 = lambda tl, off=0: tl[:, off:off + T].rearrange("p (c i) -> p c i", i=32)
                Bv32 = c32(Bsrc)
                Bref = c32(Bsrc)[:, :, 16:17]
                bcl = lambda a: mk(a, [a.ap[0], a.ap[1], [0, 64]])
                bcl32 = lambda a: mk(a, [a.ap[0], a.ap[1], [0, 32]])
                if d == 0:
                    P.op("dve", lambda e: e.tensor_tensor(out=c32(E), in0=Bv32, in1=bcl32(Bref), op=ALU.subtract),
                         r=[Bi, Be_], w=[E])
                else:
                    P.op("dve", lambda e: e.tensor_tensor(out=c32(E), in0=bcl32(Bref), in1=Bv32, op=ALU.subtract),
                         r=[Bi, Be_], w=[E])
                P.op("act", lambda e: e.activation(out=E2[:], in_=E[:], func=AF.Exp, scale=-1.0), r=[E], w=[E2])
                P.op("act", lambda e: e.activation(out=E[:], in_=E[:], func=AF.Exp), r=[E], w=[E])
                P.op("dve", lambda e, d=d: e.tensor_tensor(out=qt_[d][:], in0=qT[:], in1=E[:], op=ALU.mult),
                     r=[qT, E], w=[qt_[d]])
                P.op("pool", lambda e, d=d: e.tensor_tensor(out=kt_[d][:], in0=kk[:], in1=E2[:], op=ALU.mult),
                     r=[kk, E2], w=[kt_[d]])
                if d == 0:
                    P.op("pool", lambda e: e.tensor_tensor(out=c3(E), in0=Bv, in1=bcl(Bs1), op=ALU.subtract),
                         r=[Bi, Be_], w=[E])
                    P.op("pool", lambda e: e.tensor_tensor(out=c3(E2), in0=bcl(Be), in1=Bv, op=ALU.subtract),
                         r=[Bi, Be_], w=[E2])
                else:
                    P.op("pool", lambda e: e.tensor_tensor(out=c3(E), in0=bcl(Be), in1=Bv, op=ALU.subtract),
                         r=[Bi, Be_], w=[E])
                    P.op("pool", lambda e: e.tensor_tensor(out=c3(E2), in0=Bv, in1=bcl(Bs1), op=ALU.subtract),
                         r=[Bi, Be_], w=[E2])
                P.op("act", lambda e: e.activation(out=sg[:], in_=E[:], func=AF.Exp, scale=-1.0), r=[E], w=[sg])
                P.op("act", lambda e: e.activation(out=E[:], in_=E[:], func=AF.Exp), r=[E], w=[E])
                P.op("act", lambda e: e.activation(out=E2[:], in_=E2[:], func=AF.Exp), r=[E2], w=[E2])
                P.op("dve", lambda e, d=d: e.tensor_tensor(out=qh_[d][:], in0=qT[:], in1=E[:], op=ALU.mult),
                     r=[qT, E], w=[qh_[d]])
                P.op("dve", lambda e, d=d: e.tensor_tensor(out=kx_[d][:], in0=kk[:], in1=sg[:], op=ALU.mult),
                     r=[kk, sg], w=[kx_[d]])
                P.op("pool", lambda e, d=d: e.tensor_tensor(out=kh_[d][:], in0=kk[:], in1=E2[:], op=ALU.mult),
                     r=[kk, E2], w=[kh_[d]])
                P.op("dve", lambda e, d=d: e.tensor_tensor(out=dec[d][:].rearrange("p (c o) -> p c o", o=1), in0=Be,
                                                           in1=Bs1, op=ALU.subtract), r=[Bi, Be_], w=[dec[d]])
                P.op("act", lambda e, d=d: e.activation(out=dec[d][:], in_=dec[d][:], func=AF.Exp),
                     r=[dec[d]], w=[dec[d]])
                P.op("pool", lambda e, d=d: e.memset(Sf[d][:], 0.0), w=[Sf[d]])
                P.op("pool", lambda e, d=d: e.memset(Sb[d][:], 0.0), w=[Sb[d]])

            def pre_phase(d):
                mask = mf if d == 0 else mb
                jh, ih = (0, 1) if d == 0 else (1, 0)
                for c8 in range(0, NCK, 8):
                    n8 = min(8, NCK - c8)
                    for j in range(n8):
                        c0 = (c8 + j) * 64
                        for hb in range(2):
                            P.op("pe", lambda e, c0=c0, hb=hb, j=j: e.matmul(
                                ps_att[hb * 32:(hb + 1) * 32, j * 64 + hb * 32:j * 64 + (hb + 1) * 32],
                                lhsT=kt_[d][:, c0 + hb * 32:c0 + (hb + 1) * 32],
                                rhs=qt_[d][:, c0 + hb * 32:c0 + (hb + 1) * 32], start=True, stop=True),
                                r=[kt_[d], qt_[d]], w=[ps_att])
                        P.op("pe", lambda e, c0=c0, j=j: e.matmul(
                            ps_att[jh * 32:(jh + 1) * 32, j * 64 + ih * 32:j * 64 + (ih + 1) * 32],
                            lhsT=kx_[d][:, c0 + jh * 32:c0 + (jh + 1) * 32],
                            rhs=qh_[d][:, c0 + ih * 32:c0 + (ih + 1) * 32], start=True, stop=True),
                            r=[kx_[d], qh_[d]], w=[ps_att])
                    P.op("dve", lambda e, c8=c8, n8=n8: e.tensor_tensor(
                        out=A_all[d][:, c8:c8 + n8, :],
                        in0=ps_att[0:64, 0:n8 * 64].rearrange("p (c i) -> p c i", i=64),
                        in1=bc_mid(mask[:, :], n8), op=ALU.mult), r=[ps_att, mask], w=[A_all[d]])
                    for j in range(n8):
                        c0 = (c8 + j) * 64
                        P.op("pe", lambda e, c0=c0, j=j: e.transpose(
                            out=ps_kh[0:64, j * 128:(j + 1) * 128], in_=khf[:, c0:c0 + 64], identity=C.ident[:]),
                            r=[khf, C.ident], w=[ps_kh])
                    P.op("act", lambda e, c8=c8, n8=n8: e.activation(
                        out=KH_all[d][:, c8:c8 + n8, :],
                        in_=ps_kh[0:64, 0:n8 * 128].rearrange("p (c k) -> p c k", k=128), func=AF.Copy),
                        r=[ps_kh], w=[KH_all[d]])

            for d in range(2):
                gate_math(d, h)
                pre_phase(d)
            order = [list(range(NCK)), [3, 2, 1, 0] + list(range(NCK - 1, 3, -1))]

            def emit_smm(step, d):
                c = order[d][step]
                ss_ = step % 2
                P.op("pe", lambda e: e.matmul(ps_s2[d][ss_][:, 0:128], lhsT=KH_all[d][:, c, :],
                                              rhs=vt[:, c, :], start=True, stop=True),
                     r=[KH_all[d], vt], w=[ps_s2[d][ss_]])

            for d in range(2):
                emit_smm(0, d)
            for step in range(NCK):
                for d in range(2):
                    if step + 1 < NCK:
                        emit_smm(step + 1, d)
                    c = order[d][step]
                    sl = slice(c * 64, (c + 1) * 64)
                    b4 = step // 4
                    grp = order[d][b4 * 4:b4 * 4 + 4]
                    lo = min(grp)
                    slot = c - lo
                    po = ps_o[d][b4 % 2]
                    ot = Od_tok[d][lo // 4]
                    P.op("pe", lambda e, d=d, c=c, po=po, slot=slot: e.matmul(
                        po[0:64, slot * 128:(slot + 1) * 128], lhsT=A_all[d][:, c, :], rhs=vt[:, c, :], start=True,
                        stop=False), r=[A_all[d], vt], w=[po])
                    P.op("pe", lambda e, d=d, sl=sl, po=po, slot=slot: e.matmul(
                        po[0:64, slot * 128:(slot + 1) * 128], lhsT=qh_[d][:, sl], rhs=Sb[d][:], start=False,
                        stop=True), r=[qh_[d], Sb[d]], w=[po])
                    if step % 4 == 3:
                        P.op("act", lambda e, d=d, lo=lo, po=po: e.activation(
                            out=Od[d][:, lo:lo + 4, :], in_=po[0:64, 0:512].rearrange("p (c v) -> p c v", v=128),
                            func=AF.Copy), r=[po], w=[ot])
                    ss_ = step % 2
                    stok = ps_s_tok[d][ss_]
                    P.op("dve", lambda e, d=d, c=c, ss_=ss_: e.scalar_tensor_tensor(
                        out=Sf[d][:], in0=Sf[d][:], scalar=dec[d][:, c:c + 1], in1=ps_s2[d][ss_][:, 0:128],
                        op0=ALU.mult, op1=ALU.add), r=[Sf[d], dec[d], stok], w=[Sf[d]])
                    P.op("act", lambda e, d=d: e.activation(out=Sb[d][:], in_=Sf[d][:], func=AF.Copy),
                         r=[Sf[d]], w=[Sb[d]])
            allO = [tk for d in range(2) for tk in Od_tok[d]]
            P.op("dve", lambda e: e.tensor_tensor(out=Od[0][:], in0=Od[0][:], in1=Od[1][:], op=ALU.add),
                 r=allO, w=Od_tok[0])
            P.op("act", lambda e: e.activation(out=osq[:], in_=Od[0][:], func=AF.Square), r=Od_tok[0], w=Od_tok[1])
            P.op("dve", lambda e: e.tensor_reduce(out=oss[:], in_=osq[:], axis=AX.X, op=ALU.add), r=Od_tok[1], w=[oss])
            P.op("act", lambda e: e.activation(out=oss[:], in_=oss[:], func=AF.Sqrt, scale=1.0 / 128, bias=EPS),
                 r=[oss], w=[oss])
            P.op("dve", lambda e: e.reciprocal(out=oss[:], in_=oss[:]), r=[oss], w=[oss])
            P.op("dve", lambda e: e.tensor_tensor(out=osq[:], in0=Od[0][:], in1=bc_last(oss[:, :], 128), op=ALU.mult),
                 r=Od_tok[0] + [oss], w=Od_tok[1])
            P.op("pool", lambda e: e.tensor_tensor(out=osq[:], in0=osq[:], in1=bc_mid(LP.onorm[0:64, :], NCK),
                                                   op=ALU.mult), r=Od_tok[1] + [LP.onorm], w=Od_tok[1])
            P.op("dve", lambda e: e.tensor_tensor(out=oy[:], in0=osq[:], in1=sgd[:], op=ALU.mult),
                 r=Od_tok[1] + [sgd], w=[oy])
            for c8 in range(0, NCK, 8):
                n8 = min(8, NCK - c8)
                pt = ps_kh
                for j in range(n8):
                    P.op("pe", lambda e, pt=pt, j=j, c8=c8: e.transpose(
                        out=pt[:, j * 64:(j + 1) * 64], in_=oy[:, c8 + j, :], identity=C.ident[0:64, 0:64]),
                        r=[oy, C.ident], w=[pt])
                P.op("act", lambda e, pt=pt, c8=c8, n8=n8: e.activation(
                    out=odT[:, c8 * 64:(c8 + n8) * 64], in_=pt[:, 0:n8 * 64], func=AF.Copy), r=[pt], w=[odT])
            P.dma("sp", C.OT[3, h * 128:(h + 1) * 128, :], odT[:], r=[odT])
            if h == 7 and getattr(C, "DBG", None) is not None:
                P.dma("sp", C.DBG["O"][:, :, :], Od[0][:], r=[Od[0]])
                P.dma("sp", C.DBG["O1"][:, :, :], Od[1][:], r=[Od[1]])
                for d in range(2):
                    P.dma("sp", C.DBG["qt"][d, :, :], qt_[d][:], r=[qt_[d]])
                    P.dma("sp", C.DBG["kt"][d, :, :], kt_[d][:], r=[kt_[d]])
                    P.dma("sp", C.DBG["qh"][d, :, :], qh_[d][:], r=[qh_[d]])
                    P.dma("sp", C.DBG["dec"][d, :, :], dec[d][:], r=[dec[d]])
                P.dma("sp", C.DBG["kk"][:, :], kk[:], r=[kk])


def stage_merge(P, C, l, LP):
    SG = 1152
    SUB = [(0, 384), (384, 384), (768, 384)]
    with P.scope():
        oT = P.sbuf("moT", [128, 4, 8, SG], BF16)
        yT = P.sbuf("myT", [128, SG], BF16)
        sgt = [P.sbuf(f"msgt{i}", [128, 4, SG], BF16) for i in range(2)]
        wb = [P.sbuf(f"mwb{i}", [128, 4, 8, 512], BF16) for i in range(2)]
        acc = [P.sbuf(f"macc{i}", [128, 384]) for i in range(2)]
        tmp = [P.sbuf(f"mtmp{i}", [128, 384]) for i in range(2)]
        ps = [P.psum(f"mps{i}") for i in range(4)]
        ip = 0
        ia = 0

        def load_wb(gidx):
            g4 = gidx % 4
            for n in range(4):
                P.dma("pool", wb[g4 % 2][:, n, :, :],
                      C.w_branch[l, n, :, g4 * 512:(g4 + 1) * 512].rearrange("(c p) o -> p c o", p=128),
                      w=[wb[g4 % 2]])

        for sgi in range(2):
            g0 = sgi * SG
            for n in range(4):
                P.dma("sp", oT[:, n, :, :], C.OT[n, :, g0:g0 + SG].rearrange("(c p) t -> p c t", p=128), w=[oT])
            for dmb in range(16):
                k = dmb % 2
                kw = (dmb // 4) % 2
                jw = dmb % 4
                if jw == 0:
                    if sgi == 0 and dmb == 0:
                        load_wb(0)
                    nxt_g = sgi * 4 + dmb // 4 + 1
                    if nxt_g < 8:
                        load_wb(nxt_g)
                P.dma("sp", sgt[k][:], bass.AP(C.SGT.t, (dmb * 128) * T + g0, [[T, 128], [D * T, 4], [1, SG]]),
                      w=[sgt[k]])
                for (s0, sn) in SUB:
                    A = acc[ia % 2]
                    TM = tmp[ia % 2]
                    ia += 1
                    for n in range(4):
                        pt = ps[ip % 4]
                        ip += 1
                        for c in range(8):
                            P.op("pe", lambda e, pt=pt, kw=kw, jw=jw, n=n, c=c, s0=s0, sn=sn: e.matmul(
                                pt[:, :sn], lhsT=wb[kw][:, n, c, jw * 128:(jw + 1) * 128], rhs=oT[:, n, c, s0:s0 + sn],
                                start=(c == 0), stop=(c == 7)), r=[wb[kw], oT], w=[pt])
                        if n == 0:
                            P.op("dve", lambda e, pt=pt, k=k, A=A, s0=s0, sn=sn: e.tensor_tensor(
                                out=A[:, :sn], in0=pt[:, :sn], in1=sgt[k][:, 0, s0:s0 + sn], op=ALU.mult),
                                r=[pt, sgt[k]], w=[A])
                        else:
                            P.op("dve", lambda e, pt=pt, k=k, n=n, TM=TM, s0=s0, sn=sn: e.tensor_tensor(
                                out=TM[:, :sn], in0=pt[:, :sn], in1=sgt[k][:, n, s0:s0 + sn], op=ALU.mult),
                                r=[pt, sgt[k]], w=[TM])
                            if n < 3:
                                P.op("pool", lambda e, A=A, TM=TM, sn=sn: e.tensor_tensor(
                                    out=A[:, :sn], in0=A[:, :sn], in1=TM[:, :sn], op=ALU.add), r=[A, TM], w=[A])
                            else:
                                P.op("pool", lambda e, A=A, TM=TM, s0=s0, sn=sn: e.tensor_tensor(
                                    out=yT[:, s0:s0 + sn], in0=A[:, :sn], in1=TM[:, :sn], op=ALU.add),
                                    r=[A, TM], w=[yT])
                P.dma("sp", C.YT[dmb * 128:(dmb + 1) * 128, g0:g0 + SG], yT[:], r=[yT])


def stage_out_norm2(P, C, l, LP):
    with P.scope():
        yT = P.sbuf("oyT", [128, 16, T], BF16)
        P.dma("sp", yT[:], C.YT[:, :].rearrange("(c p) t -> p c t", p=128), w=[yT])
        wo = P.sbuf("owo", [128, 16, D], BF16)
        for c4 in range(4):
            P.dma("pool", wo[:, c4 * 4:(c4 + 1) * 4, :],
                  C.w_out[l, c4 * 512:(c4 + 1) * 512, :].rearrange("(c p) o -> p c o", p=128), w=[wo])
        g1 = P.sbuf("og1", [128, D])
        P.dma("sp", g1[:], pbc(C.MOD, 1 * 6 * D + 2 * D, D), w=[g1])
        xt = [P.sbuf(f"oxt{i}", [128, D]) for i in range(2)]
        tmp = [P.sbuf(f"otmp{i}", [128, 512]) for i in range(2)]
        sq = P.sbuf("osq", [128, D])
        xn = P.sbuf("oxn", [128, D])
        ss = P.sbuf("oss", [128, 1])
        hf = P.sbuf("ohf", [128, 16, 128])
        hb = [P.sbuf(f"ohb{i}", [128, 16, 128], BF16) for i in range(2)]
        ps = [P.psum(f"ops{i}") for i in range(4)]
        pst = [P.psum(f"opst{i}") for i in range(2)]
        psr = P.psum("opsr")
        aff = P.sbuf("raff", [128, NE])
        sel = P.sbuf("rsel", [128, NE])
        r4 = [P.sbuf(f"r4_{i}", [128, 4]) for i in range(8)]
        msk = P.sbuf("rmsk", [128, NE])
        gm = P.sbuf("rgm", [128, 1])
        gt_ = [P.sbuf(f"rgt{i}", [128, NE]) for i in range(2)]
        P.dma("sp", xt[0][:], C.XRES[0:128, :], w=[xt[0]])
        for t in range(NT):
            if t + 1 < NT:
                P.dma("sp", xt[(t + 1) % 2][:], C.XRES[(t + 1) * 128:(t + 2) * 128, :], w=[xt[(t + 1) % 2]])
            X = xt[t % 2]
            r = 1 if t < 2 else 0
            if t == 2:
                P.dma("sp", g1[:], pbc(C.MOD, 0 * 6 * D + 2 * D, D), w=[g1])
            for cg in range(4):
                pt = ps[cg]
                for c in range(16):
                    P.op("pe", lambda e, pt=pt, c=c, t=t, cg=cg: e.matmul(
                        pt[:], lhsT=yT[:, c, t * 128:(t + 1) * 128], rhs=wo[:, c, cg * 512:(cg + 1) * 512],
                        start=(c == 0), stop=(c == 15)), r=[yT, wo], w=[pt])
                TM = tmp[cg % 2]
                P.op("dve", lambda e, pt=pt, cg=cg, TM=TM: e.tensor_tensor(
                    out=TM[:], in0=pt[:], in1=g1[:, cg * 512:(cg + 1) * 512],
                    op=ALU.mult), r=[pt, g1], w=[TM])
                P.op("pool", lambda e, X=X, TM=TM, cg=cg: e.tensor_tensor(
                    out=X[:, cg * 512:(cg + 1) * 512], in0=X[:, cg * 512:(cg + 1) * 512], in1=TM[:], op=ALU.add),
                    r=[X, TM], w=[X])
            P.dma("sp", C.XRES[t * 128:(t + 1) * 128, :], X[:], r=[X])
            norm_tile(P, X, xn, sq, ss, D)
            HB = hb[t % 2]
            for c4 in range(4):
                pt = pst[c4 % 2]
                for j in range(4):
                    c = c4 * 4 + j
                    P.op("pe", lambda e, c=c, j=j, pt=pt: e.transpose(
                        out=pt[:, j * 128:(j + 1) * 128], in_=xn[:, c * 128:(c + 1) * 128], identity=C.ident_f[:]),
                        r=[xn, C.ident_f], w=[pt])
                for j in range(4):
                    c = c4 * 4 + j
                    if j % 2 == 0:
                        P.op("act", lambda e, c=c, j=j, pt=pt, r=r: e.activation(
                            out=hf[:, c, :], in_=pt[:, j * 128:(j + 1) * 128], func=AF.Identity,
                            scale=LP.s2[:, r, c:c + 1], bias=LP.modP[:, r, 3, c:c + 1]),
                            r=[pt, LP.s2, LP.modP], w=[hf])
                    else:
                        P.op("dve", lambda e, c=c, j=j, pt=pt, r=r: e.tensor_scalar(
                            out=hf[:, c, :], in0=pt[:, j * 128:(j + 1) * 128], scalar1=LP.s2[:, r, c:c + 1],
                            scalar2=LP.modP[:, r, 3, c:c + 1], op0=ALU.mult, op1=ALU.add),
                            r=[pt, LP.s2, LP.modP], w=[hf])
            P.op("act", lambda e, HB=HB: e.activation(out=HB[:], in_=hf[:], func=AF.Copy), r=[hf], w=[HB])
            P.dma("sp", C.H2T[:, t * 128:(t + 1) * 128].rearrange("(c p) t -> p c t", p=128), HB[:], r=[HB])
            for c in range(16):
                P.op("pe", lambda e, c=c: e.matmul(psr[:, 0:NE], lhsT=hf[:, c, :], rhs=LP.wr[:, c, :], start=(c == 0),
                                                   stop=(c == 15)), r=[hf, LP.wr], w=[psr])
            P.op("act", lambda e: e.activation(out=aff[:], in_=psr[:, 0:NE], func=AF.Sigmoid), r=[psr], w=[aff])
            P.op("dve", lambda e: e.tensor_tensor(out=sel[:], in0=aff[:], in1=LP.br[:], op=ALU.add),
                 r=[aff, LP.br], w=[sel])
            s3 = sel[:].rearrange("p (g k) -> p g k", k=4)
            hi1, lo1, hi2, lo2, top1, sec, gs, ing = r4
            tt = lambda o, a, b, op, rr, ww: P.op("dve", lambda e: e.tensor_tensor(out=o, in0=a, in1=b, op=op),
                                                  r=rr, w=ww)
            tt(hi1[:], s3[:, :, 0], s3[:, :, 1], ALU.max, [sel], [hi1])
            tt(lo1[:], s3[:, :, 0], s3[:, :, 1], ALU.min, [sel], [lo1])
            tt(hi2[:], s3[:, :, 2], s3[:, :, 3], ALU.max, [sel], [hi2])
            tt(lo2[:], s3[:, :, 2], s3[:, :, 3], ALU.min, [sel], [lo2])
            tt(top1[:], hi1[:], hi2[:], ALU.max, [hi1, hi2], [top1])
            tt(sec[:], hi1[:], hi2[:], ALU.min, [hi1, hi2], [sec])
            tt(lo1[:], lo1[:], lo2[:], ALU.max, [lo1, lo2], [lo1])
            tt(sec[:], sec[:], lo1[:], ALU.max, [sec, lo1], [sec])
            tt(gs[:], top1[:], sec[:], ALU.add, [top1, sec], [gs])
            P.op("dve", lambda e: e.tensor_reduce(out=gm[:], in_=gs[:], axis=AX.X, op=ALU.max), r=[gs], w=[gm])
            P.op("dve", lambda e: e.tensor_scalar(out=ing[:], in0=gs[:], scalar1=gm[:, 0:1], scalar2=None,
                                                  op0=ALU.is_equal), r=[gs, gm], w=[ing])
            m3 = msk[:].rearrange("p (g k) -> p g k", k=4)
            P.op("dve", lambda e: e.tensor_tensor(out=m3, in0=s3, in1=bc_last(sec[:, :], 4), op=ALU.is_ge),
                 r=[sel, sec], w=[msk])
            P.op("dve", lambda e: e.tensor_tensor(out=m3, in0=m3, in1=bc_last(ing[:, :], 4), op=ALU.mult),
                 r=[msk, ing], w=[msk])
            P.op("dve", lambda e: e.tensor_tensor(out=msk[:], in0=msk[:], in1=aff[:], op=ALU.mult),
                 r=[msk, aff], w=[msk])
            P.op("dve", lambda e: e.tensor_reduce(out=gm[:], in_=msk[:], axis=AX.X, op=ALU.add), r=[msk], w=[gm])
            P.op("dve", lambda e: e.reciprocal(out=gm[:], in_=gm[:]), r=[gm], w=[gm])
            G = gt_[t % 2]
            P.op("dve", lambda e, G=G: e.tensor_scalar(out=G[:], in0=msk[:], scalar1=gm[:, 0:1], scalar2=None,
                                                       op0=ALU.mult), r=[msk, gm], w=[G])
            P.dma("sp", C.GATES[t * 128:(t + 1) * 128, :], G[:], r=[G])


def stage_moe(P, C, l, LP, last=False):
    GT = 768
    with P.scope():
        g2 = P.sbuf("eg2", [128, D])
        hT = P.sbuf("ehT", [128, 16, GT], BF16)
        gts = P.sbuf("egts", [128, 6, NE])
        yacc = P.sbuf("eyacc", [128, 6, D])
        yacc_tok = [Tok(f"yacc{i}") for i in range(6)]
        wg = [P.sbuf(f"ewg{i}", [128, 16, DFF], BF16) for i in range(2)]
        wu = [P.sbuf(f"ewu{i}", [128, 16, DFF], BF16) for i in range(2)]
        wd = [P.sbuf(f"ewd{i}", [128, 4, D], BF16) for i in range(2)]
        sg = [P.sbuf(f"esg{i}", [128, DFF]) for i in range(2)]
        hid = [P.sbuf(f"ehid{i}", [128, DFF], BF16) for i in range(2)]
        hidT = [P.sbuf(f"ehidT{i}", [128, DFF], BF16) for i in range(2)]
        xt = P.sbuf("ext", [128, D])
        psg = [P.psum(f"epsg{i}") for i in range(2)]
        psu = [P.psum(f"epsu{i}") for i in range(2)]
        pst = P.psum("epst", [128, 1024], BF16)
        psy = [P.psum(f"epsy{i}") for i in range(2)]
        cnt = {"it": 0, "iy": 0}

        def issue_w(ex):
            k = ex % 2
            P.dma("pool", wg[k][:], C.w_gate[l, ex, :, :].rearrange("(c p) f -> p c f", p=128), w=[wg[k]])
            P.dma("pool", wu[k][:], C.w_up[l, ex, :, :].rearrange("(c p) f -> p c f", p=128), w=[wu[k]])
            P.dma("pool", wd[k][:], C.w_down[l, ex, :, :].rearrange("(c p) o -> p c o", p=128), w=[wd[k]])

        def emit_gu(st):
            ex, ti, pg, pu = st["ex"], st["ti"], st["pg"], st["pu"]
            k = ex % 2
            for c in range(16):
                P.op("pe", lambda e, c=c: e.matmul(pg[:], lhsT=hT[:, c, ti * 128:(ti + 1) * 128], rhs=wg[k][:, c, :],
                                                   start=(c == 0), stop=(c == 15)), r=[hT, wg[k]], w=[pg])
            for c in range(16):
                P.op("pe", lambda e, c=c: e.matmul(pu[:], lhsT=hT[:, c, ti * 128:(ti + 1) * 128], rhs=wu[k][:, c, :],
                                                   start=(c == 0), stop=(c == 15)), r=[hT, wu[k]], w=[pu])

        def emit_rest(st):
            ex, ti, pg, pu, SG_, H, HT = st["ex"], st["ti"], st["pg"], st["pu"], st["sg"], st["hid"], st["hidT"]
            k = ex % 2
            P.op("act", lambda e: e.activation(out=SG_[:], in_=pg[:], func=AF.Silu), r=[pg], w=[SG_])
            P.op("dve", lambda e: e.scalar_tensor_tensor(out=H[:], in0=SG_[:], scalar=gts[:, ti, ex:ex + 1], in1=pu[:],
                                                         op0=ALU.mult, op1=ALU.mult), r=[SG_, pu, gts], w=[H])
            for j in range(4):
                P.op("pe", lambda e, j=j: e.transpose(out=pst[:, j * 128:(j + 1) * 128],
                                                      in_=H[:, j * 128:(j + 1) * 128], identity=C.ident[:]),
                     r=[H, C.ident], w=[pst])
            P.op("act", lambda e: e.activation(out=HT[:], in_=pst[:, 0:512], func=AF.Copy), r=[pst], w=[HT])
            for cg in range(4):
                py = psy[cnt["iy"] % 2]
                cnt["iy"] += 1
                for f in range(4):
                    P.op("pe", lambda e, f=f, cg=cg, py=py: e.matmul(
                        py[:], lhsT=HT[:, f * 128:(f + 1) * 128], rhs=wd[k][:, f, cg * 512:(cg + 1) * 512],
                        start=(f == 0), stop=(f == 3)), r=[HT, wd[k]], w=[py])
                if st["firstex"]:
                    P.op("act", lambda e, py=py, cg=cg: e.activation(
                        out=yacc[:, ti, cg * 512:(cg + 1) * 512], in_=py[:], func=AF.Copy),
                        r=[py], w=[yacc_tok[ti]])
                else:
                    P.op("dve", lambda e, py=py, cg=cg: e.tensor_tensor(
                        out=yacc[:, ti, cg * 512:(cg + 1) * 512], in0=yacc[:, ti, cg * 512:(cg + 1) * 512],
                        in1=py[:], op=ALU.add), r=[py, yacc_tok[ti]], w=[yacc_tok[ti]])

        cur_g2 = None
        for t0 in range(0, T, GT):
            nt = GT // 128
            tiles = [ti for ti in range(nt) if not (last and (t0 // 128 + ti) < 2)]
            P.dma("sp", hT[:], C.H2T[:, t0:t0 + GT].rearrange("(c p) t -> p c t", p=128), w=[hT])
            P.dma("sp", gts[:], C.GATES[t0:t0 + GT, :].rearrange("(n p) e -> p n e", p=128), w=[gts])
            issue_w(0)
            steps = []
            for ex in range(NE):
                for ti in tiles:
                    i = cnt["it"]
                    cnt["it"] += 1
                    steps.append(dict(ex=ex, ti=ti, pg=psg[i % 2], pu=psu[i % 2], sg=sg[i % 2], hid=hid[i % 2],
                                      hidT=hidT[i % 2], firstex=(ex == 0), wfirst=(ti == tiles[0])))
            emit_gu(steps[0])
            for i, st in enumerate(steps):
                if st["wfirst"] and st["ex"] + 1 < NE:
                    issue_w(st["ex"] + 1)
                if i + 1 < len(steps):
                    emit_gu(steps[i + 1])
                emit_rest(st)
            for ti in tiles:
                tt_ = t0 // 128 + ti
                r = 1 if tt_ < 2 else 0
                if cur_g2 != r:
                    P.dma("sp", g2[:], pbc(C.MOD, r * 6 * D + 5 * D, D), w=[g2])
                    cur_g2 = r
                P.dma("sp", xt[:], C.XRES[tt_ * 128:(tt_ + 1) * 128, :], w=[xt])
                P.op("dve", lambda e, ti=ti: e.tensor_tensor(out=yacc[:, ti, :], in0=yacc[:, ti, :], in1=g2[:],
                                                             op=ALU.mult), r=[yacc_tok[ti], g2], w=[yacc_tok[ti]])
                P.op("pool", lambda e, ti=ti: e.tensor_tensor(out=xt[:], in0=xt[:], in1=yacc[:, ti, :], op=ALU.add),
                     r=[xt, yacc_tok[ti]], w=[xt])
                if last:
                    P.dma("sp", C.out[(tt_ - 2) * 128:(tt_ - 1) * 128, :], xt[:], r=[xt])
                else:
                    P.dma("sp", C.XRES[tt_ * 128:(tt_ + 1) * 128, :], xt[:], r=[xt])


_CACHE = {}


def make_in_maps(inputs, ncores=8):
    ca, sa, cb, sb = rope_tables()
    f = lambda a: np.ascontiguousarray(np.asarray(a, dtype=np.float32))
    shared = {
        "c_ctx": f(inputs["c_ctx"]).reshape(1, D),
        "w_ada": f(inputs["w_ada"]), "b_ada": f(inputs["b_ada"]),
        "norm1_g": f(inputs["norm1_g"]), "norm2_g": f(inputs["norm2_g"]),
        "w_in": f(inputs["w_in"]), "qn_a": f(inputs["qn_a"]), "kn_a": f(inputs["kn_a"]),
        "sink_a": f(inputs["sink_a"]), "qn_b": f(inputs["qn_b"]), "kn_b": f(inputs["kn_b"]),
        "lam_b": f(inputs["lam_b"]).reshape(DEPTH, 256), "subln_b": f(inputs["subln_b"]),
        "conv_w": f(inputs["conv_w"]), "conv_b": f(inputs["conv_b"]),
        "w_rg": f(inputs["w_rg"]), "b_rg": f(inputs["b_rg"]), "w_ig": f(inputs["w_ig"]), "b_ig": f(inputs["b_ig"]),
        "lru_lambda": f(inputs["lru_lambda"]), "lb_d": f(inputs["lb_d"]), "onorm_d": f(inputs["onorm_d"]),
        "w_branch": f(inputs["w_branch"]), "w_out": f(inputs["w_out"]),
        "w_router": f(inputs["w_router"]), "b_router": f(inputs["b_router"]).reshape(1, NE),
        "w_gate": f(inputs["w_gate"]), "w_up": f(inputs["w_up"]), "w_down": f(inputs["w_down"]),
        "ropeA_c": ca, "ropeA_s": sa, "ropeB_c": cb, "ropeB_s": sb,
    }
    x = f(inputs["x"])
    ctx = f(inputs["ctx"])
    c = f(inputs["c"])
    maps = []
    for b in range(ncores):
        m = dict(shared)
        m["x"] = x[b]
        m["ctx"] = ctx[b]
        m["c"] = c[b].reshape(1, D)
        maps.append(m)
    return maps


def kernel(**inputs):
    if "nc" not in _CACHE:
        _CACHE["nc"] = build()[0]
    nc = _CACHE["nc"]
    maps = make_in_maps(inputs, 8)
    res = run_bass_kernel_spmd(nc, maps, core_ids=list(range(8)))
    return np.stack([np.asarray(r["out"], dtype=np.float32) for r in res.results], axis=0)
```
